# Optimizing a Trainium2 kernel written in Bass

```python
import math
import jax, jax.numpy as jnp
from jax import lax
import numpy as np

D_MODEL = 1024
BATCH = 8
SEQ = 2048
DEPTH = 2

D_MIX = D_MODEL
DN_HEADS = 4
DN_HEAD_DIM = 128
DN_WIDTH = DN_HEADS * DN_HEAD_DIM
DN_CONV = 4
DN_CHUNK = 64
NSA_HEADS = 4
NSA_HEAD_DIM = 64
NSA_WIDTH = NSA_HEADS * NSA_HEAD_DIM
CMP_LEN = 32
CMP_STRIDE = 16
CMP_HIDDEN = 2 * NSA_HEAD_DIM
SLC_BLOCK = 64
SLC_TOP_N = 16
WINDOW = 512
Q_BLOCK = 128
CONV_WIDTH = D_MIX - DN_WIDTH - NSA_WIDTH
CONV_KERNEL = 31
REL_BUCKETS = 32
REL_MAX_DIST = 128
N_EXPERTS = 32
TOP_K = 4
D_FF = D_MODEL
SWIGLU_LIMIT = 7.0
SWIGLU_ALPHA = 1.702
MOE_BLOCK = 128

EPS = 1e-6
NEG_INF = -1e30
FORCE = 1e4

IN_SPLITS = (
    DN_WIDTH,
    DN_WIDTH,
    DN_WIDTH,
    DN_WIDTH,
    DN_HEADS,
    DN_HEADS,
    NSA_WIDTH,
    6 * NSA_HEAD_DIM,
    3 * NSA_HEADS,
    2 * CONV_WIDTH,
)
D_IN = sum(IN_SPLITS)

kernel_name = 'hybrid_deltanet_nsa_conformer_moe'

f32 = jnp.float32


def rms_norm(x, w):
    xf = x.astype(f32)
    y = xf * lax.rsqrt(jnp.mean(xf * xf, axis=-1, keepdims=True) + EPS)
    return (y * w.astype(f32)).astype(x.dtype)


def layer_norm(x, w, b):
    xf = x.astype(f32)
    mu = jnp.mean(xf, axis=-1, keepdims=True)
    var = jnp.mean(jnp.square(xf - mu), axis=-1, keepdims=True)
    return ((xf - mu) * lax.rsqrt(var + EPS) * w.astype(f32) + b.astype(f32)).astype(x.dtype)


def l2_normalize(x):
    return x * lax.rsqrt(jnp.sum(x * x, axis=-1, keepdims=True) + EPS)


def masked_softmax(s, mask):
    s = jnp.where(mask, s, NEG_INF)
    return jax.nn.softmax(s, axis=-1) * mask


def causal_depthwise_conv(x, w):
    k = w.shape[0]
    return lax.conv_general_dilated(
        x, w[:, None, :].astype(x.dtype), window_strides=(1,), padding=[(k - 1, 0)],
        dimension_numbers=('NHC', 'HIO', 'NHC'), feature_group_count=x.shape[-1])


def t5_bucket(dist):
    n = jnp.maximum(dist, 0)
    max_exact = REL_BUCKETS // 2
    nf = jnp.maximum(n, 1).astype(f32)
    large = max_exact + (jnp.log(nf / max_exact) / math.log(REL_MAX_DIST / max_exact)
                         * (REL_BUCKETS - max_exact)).astype(jnp.int32)
    large = jnp.minimum(large, REL_BUCKETS - 1)
    return jnp.where(n < max_exact, n, large)


def chunk_gated_delta_rule(q, k, v, g, beta):
    B, T, H, Dk = q.shape
    Dv = v.shape[-1]
    C = DN_CHUNK
    N = T // C
    to_chunks = lambda t: t.reshape(B, N, C, H, -1).transpose(1, 0, 3, 2, 4)
    q, k, v = to_chunks(q), to_chunks(k), to_chunks(v)
    g = jnp.cumsum(g.reshape(B, N, C, H).transpose(1, 0, 3, 2), axis=-1)
    beta = beta.reshape(B, N, C, H).transpose(1, 0, 3, 2)
    k_beta = k * beta[..., None]
    v_beta = v * beta[..., None]
    tril = jnp.tril(jnp.ones((C, C), bool))
    strict = jnp.tril(jnp.ones((C, C), bool), -1)
    gdiff = jnp.where(tril, g[..., :, None] - g[..., None, :], 0.0)
    decay = jnp.where(tril, jnp.exp(gdiff), 0.0)
    lower = jnp.where(strict, jnp.einsum('nbhid,nbhjd->nbhij', k_beta, k) * decay, 0.0)
    rhs = jnp.concatenate([v_beta, k_beta * jnp.exp(g)[..., None]], axis=-1)
    sol = lax.linalg.triangular_solve(lower, rhs, left_side=True, lower=True, unit_diagonal=True)
    u, w = sol[..., :Dv], sol[..., Dv:]
    attn_intra = jnp.where(tril, jnp.einsum('nbhid,nbhjd->nbhij', q, k) * decay, 0.0)

    def step(S, inp):
        q_c, k_c, u_c, w_c, g_c, a_c = inp
        v_new = u_c - jnp.einsum('bhck,bhkv->bhcv', w_c, S)
        o = (jnp.einsum('bhck,bhkv->bhcv', q_c * jnp.exp(g_c)[..., None], S)
             + jnp.einsum('bhij,bhjv->bhiv', a_c, v_new))
        g_last = g_c[..., -1:]
        S = (S * jnp.exp(g_last)[..., None]
             + jnp.einsum('bhck,bhcv->bhkv', k_c * jnp.exp(g_last - g_c)[..., None], v_new))
        return S, o

    S0 = jnp.zeros((B, H, Dk, Dv), f32)
    _, o = lax.scan(step, S0, (q, k, u, w, g, attn_intra))
    return o.transpose(1, 0, 3, 2, 4).reshape(B, T, H, Dv)


def gated_deltanet(q, k, v, z, a, b, conv_w, a_log, dt_bias, norm_w):
    B, T, _ = q.shape
    qkv = jax.nn.silu(causal_depthwise_conv(jnp.concatenate([q, k, v], axis=-1), conv_w))
    q, k, v = jnp.split(qkv, 3, axis=-1)
    heads = lambda t: t.reshape(B, T, DN_HEADS, DN_HEAD_DIM).astype(f32)
    q = l2_normalize(heads(q)) * DN_HEAD_DIM ** -0.5
    k = l2_normalize(heads(k))
    v = heads(v)
    beta = jax.nn.sigmoid(b.astype(f32))
    g = -jnp.exp(a_log.astype(f32)) * jax.nn.softplus(a.astype(f32) + dt_bias.astype(f32))
    o = chunk_gated_delta_rule(q, k, v, g, beta)
    o = rms_norm(o, norm_w) * jax.nn.silu(heads(z))
    return o.reshape(B, T, DN_WIDTH).astype(z.dtype)


def nsa_attention(q, kv, gate_logits, q_norm_w, k_norm_w, cmp_pos, cmp_w1, cmp_w2, rel_bias):
    B, T, _ = q.shape
    H, Dh = NSA_HEADS, NSA_HEAD_DIM
    dt = q.dtype
    scale = Dh ** -0.5
    rel_bias = rel_bias.astype(f32)
    q = rms_norm(q.reshape(B, T, H, Dh), q_norm_w)
    k_cmp_raw, v_cmp_raw, k_slc, v_slc, k_win, v_win = jnp.split(kv, 6, axis=-1)
    t_pos = jnp.arange(T)

    n_cmp = (T - CMP_LEN) // CMP_STRIDE + 1
    blk_idx = np.arange(n_cmp)[:, None] * CMP_STRIDE + np.arange(CMP_LEN)[None, :]

    def compress(t, pos, w1, w2):
        blocks = t[:, blk_idx] + pos.astype(t.dtype)
        return jax.nn.silu(blocks.reshape(B, n_cmp, CMP_LEN * Dh) @ w1) @ w2

    k_cmp = rms_norm(compress(k_cmp_raw, cmp_pos[0], cmp_w1[0], cmp_w2[0]), k_norm_w[0])
    v_cmp = compress(v_cmp_raw, cmp_pos[1], cmp_w1[1], cmp_w2[1])
    cmp_end = jnp.arange(n_cmp) * CMP_STRIDE + CMP_LEN - 1
    dist_cmp = t_pos[:, None] - cmp_end[None, :]
    bias_cmp = rel_bias[t5_bucket(dist_cmp)].transpose(2, 0, 1)
    s_cmp = jnp.einsum('bthd,bjd->bhtj', q, k_cmp).astype(f32) * scale + bias_cmp
    p_cmp = masked_softmax(s_cmp, dist_cmp >= 0)
    o_cmp = jnp.einsum('bhtj,bjd->bthd', p_cmp.astype(dt), v_cmp)

    n_slc = T // SLC_BLOCK
    cmp_start = np.arange(n_cmp) * CMP_STRIDE
    slc_start = np.arange(n_slc) * SLC_BLOCK
    overlap = ((cmp_start[:, None] < slc_start[None, :] + SLC_BLOCK)
               & (cmp_start[:, None] + CMP_LEN > slc_start[None, :])).astype(np.float32)
    imp = jnp.einsum('bhtj,js->bts', p_cmp, jnp.asarray(overlap))
    cur = (t_pos // SLC_BLOCK)[:, None]
    s_idx = jnp.arange(n_slc)[None, :]
    causal_blk = s_idx <= cur
    forced = (s_idx == 0) | (s_idx == cur) | (s_idx == cur - 1)
    imp = jnp.where(causal_blk & forced, FORCE, jnp.where(causal_blk, imp, -1.0))
    top_n = min(SLC_TOP_N, n_slc)
    _, sel = lax.top_k(imp, top_n)

    k_slc = rms_norm(k_slc, k_norm_w[1]).reshape(B, n_slc, SLC_BLOCK, Dh)
    v_slc = v_slc.reshape(B, n_slc, SLC_BLOCK, Dh)
    k_win_p = jnp.pad(rms_norm(k_win, k_norm_w[2]), ((0, 0), (WINDOW, 0), (0, 0)))
    v_win_p = jnp.pad(v_win, ((0, 0), (WINDOW, 0), (0, 0)))
    n_qb = T // Q_BLOCK
    q_blocks = q.reshape(B, n_qb, Q_BLOCK, H, Dh).transpose(1, 0, 2, 3, 4)
    sel_blocks = sel.reshape(B, n_qb, Q_BLOCK, top_n).transpose(1, 0, 2, 3)
    b_idx = jnp.arange(B)[:, None, None]
    n_keys = top_n * SLC_BLOCK

    def query_block(args):
        i, q_i, sel_i = args
        tq = i * Q_BLOCK + jnp.arange(Q_BLOCK)
        kg = k_slc[b_idx, sel_i]
        vg = v_slc[b_idx, sel_i].reshape(B, Q_BLOCK, n_keys, Dh)
        key_pos = sel_i[..., None] * SLC_BLOCK + jnp.arange(SLC_BLOCK)
        dist = tq[None, :, None, None] - key_pos
        bias = rel_bias[t5_bucket(dist)].transpose(0, 4, 1, 2, 3)
        s = jnp.einsum('bqhd,bqnkd->bhqnk', q_i, kg).astype(f32) * scale + bias
        p = masked_softmax(s.reshape(B, H, Q_BLOCK, n_keys),
                           (dist >= 0).reshape(B, 1, Q_BLOCK, n_keys))
        o_s = jnp.einsum('bhqm,bqmd->bqhd', p.astype(dt), vg)
        kw = lax.dynamic_slice_in_dim(k_win_p, i * Q_BLOCK, WINDOW + Q_BLOCK, axis=1)
        vw = lax.dynamic_slice_in_dim(v_win_p, i * Q_BLOCK, WINDOW + Q_BLOCK, axis=1)
        kpos = i * Q_BLOCK - WINDOW + jnp.arange(WINDOW + Q_BLOCK)
        dist_w = tq[:, None] - kpos[None, :]
        mask_w = (dist_w >= 0) & (dist_w < WINDOW) & (kpos[None, :] >= 0)
        bias_w = rel_bias[t5_bucket(dist_w)].transpose(2, 0, 1)
        s_w = jnp.einsum('bqhd,bkd->bhqk', q_i, kw).astype(f32) * scale + bias_w
        p_w = masked_softmax(s_w, mask_w)
        o_w = jnp.einsum('bhqk,bkd->bqhd', p_w.astype(dt), vw)
        return o_s, o_w

    o_slc, o_win = lax.map(query_block, (jnp.arange(n_qb), q_blocks, sel_blocks))
    o_slc = o_slc.transpose(1, 0, 2, 3, 4).reshape(B, T, H, Dh)
    o_win = o_win.transpose(1, 0, 2, 3, 4).reshape(B, T, H, Dh)
    gates = jax.nn.sigmoid(gate_logits.astype(f32)).reshape(B, T, H, 3)
    o = (gates[..., 0, None] * o_cmp.astype(f32) + gates[..., 1, None] * o_slc.astype(f32)
         + gates[..., 2, None] * o_win.astype(f32))
    return o.reshape(B, T, NSA_WIDTH).astype(dt)


def conformer_conv(u, dw_w, dw_b, ln_w, ln_b):
    a, gate = jnp.split(u, 2, axis=-1)
    h = a * jax.nn.sigmoid(gate)
    h = causal_depthwise_conv(h, dw_w) + dw_b
    h = layer_norm(h, ln_w, ln_b)
    return jax.nn.silu(h)


def moe_ffn(h, router_w, router_b, w_gate_up, b_gate_up, w_down, b_down):
    B, T, D = h.shape
    x = h.reshape(-1, D)
    n_tok = x.shape[0]
    n_assign = n_tok * TOP_K
    logits = (x @ router_w + router_b).astype(f32)
    top_logits, top_idx = lax.top_k(logits, TOP_K)
    weights = jax.nn.softmax(top_logits, axis=-1)
    flat_e = top_idx.reshape(-1)
    order = jnp.argsort(flat_e)
    sorted_e = flat_e[order]
    sorted_tok = order // TOP_K
    counts = jnp.bincount(flat_e, length=N_EXPERTS)
    padded = (counts + MOE_BLOCK - 1) // MOE_BLOCK * MOE_BLOCK
    pad_end = jnp.cumsum(padded)
    pad_start = pad_end - padded
    start = jnp.cumsum(counts) - counts
    slot = pad_start[sorted_e] + jnp.arange(n_assign) - start[sorted_e]
    n_slots = n_assign + N_EXPERTS * MOE_BLOCK
    n_blocks = n_slots // MOE_BLOCK
    tok_of_slot = jnp.zeros((n_slots,), jnp.int32).at[slot].set(sorted_tok)
    w_of_slot = jnp.zeros((n_slots,), f32).at[slot].set(weights.reshape(-1)[order])
    block_expert = jnp.minimum(
        jnp.searchsorted(pad_end, jnp.arange(n_blocks) * MOE_BLOCK, side='right'), N_EXPERTS - 1)
    xs = x[tok_of_slot].reshape(n_blocks, MOE_BLOCK, D)

    def expert_block(args):
        e, xb = args
        gu = xb @ w_gate_up[e] + b_gate_up[e]
        gate, up = jnp.split(gu, 2, axis=-1)
        gate = jnp.minimum(gate, SWIGLU_LIMIT)
        up = jnp.clip(up, -SWIGLU_LIMIT, SWIGLU_LIMIT)
        act = (up + 1.0) * gate * jax.nn.sigmoid(SWIGLU_ALPHA * gate)
        return act @ w_down[e] + b_down[e]

    ys = lax.map(expert_block, (block_expert, xs)).reshape(n_slots, D)
    out = jnp.zeros_like(x).at[tok_of_slot].add(ys * w_of_slot[:, None].astype(ys.dtype))
    return out.reshape(B, T, D)


def split_columns(p):
    offsets = [int(o) for o in np.cumsum(IN_SPLITS)[:-1]]
    return jnp.split(p, offsets, axis=-1)


def setup_inputs(seed: int = 0) -> dict:
    key = jax.random.key(seed)
    ks = iter(jax.random.split(key, 32))
    L = DEPTH

    def nrm(shape, scale):
        return scale * jax.random.normal(next(ks), shape, f32)

    dt = jnp.exp(jax.random.uniform(next(ks), (L, DN_HEADS), f32, math.log(1e-3), math.log(1e-1)))
    a_init = jax.random.uniform(next(ks), (L, DN_HEADS), f32, 1.0, 16.0)
    return {
        'x': nrm((BATCH, SEQ, D_MODEL), 1.0),
        'attn_norm_w': 1.0 + nrm((L, D_MODEL), 0.01),
        'w_in': nrm((L, D_MODEL, D_IN), D_MODEL ** -0.5),
        'dn_conv_w': nrm((L, DN_CONV, 3 * DN_WIDTH), DN_CONV ** -0.5),
        'dn_a_log': jnp.log(a_init),
        'dn_dt_bias': dt + jnp.log(-jnp.expm1(-dt)),
        'dn_norm_w': 1.0 + nrm((L, DN_HEAD_DIM), 0.01),
        'nsa_q_norm_w': 1.0 + nrm((L, NSA_HEAD_DIM), 0.01),
        'nsa_k_norm_w': 1.0 + nrm((L, 3, NSA_HEAD_DIM), 0.01),
        'nsa_cmp_pos': nrm((L, 2, CMP_LEN, NSA_HEAD_DIM), 0.1),
        'nsa_cmp_w1': nrm((L, 2, CMP_LEN * NSA_HEAD_DIM, CMP_HIDDEN), (CMP_LEN * NSA_HEAD_DIM) ** -0.5),
        'nsa_cmp_w2': nrm((L, 2, CMP_HIDDEN, NSA_HEAD_DIM), CMP_HIDDEN ** -0.5),
        'conv_dw_w': nrm((L, CONV_KERNEL, CONV_WIDTH), CONV_KERNEL ** -0.5),
        'conv_dw_b': nrm((L, CONV_WIDTH), 0.01),
        'conv_ln_w': 1.0 + nrm((L, CONV_WIDTH), 0.01),
        'conv_ln_b': nrm((L, CONV_WIDTH), 0.01),
        'w_out': nrm((L, D_MIX, D_MODEL), 0.5 * D_MIX ** -0.5),
        'ffn_norm_w': 1.0 + nrm((L, D_MODEL), 0.01),
        'router_w': nrm((L, D_MODEL, N_EXPERTS), D_MODEL ** -0.5),
        'router_b': nrm((L, N_EXPERTS), 0.01),
        'w_gate_up': nrm((L, N_EXPERTS, D_MODEL, 2 * D_FF), D_MODEL ** -0.5),
        'b_gate_up': nrm((L, N_EXPERTS, 2 * D_FF), 0.01),
        'w_down': nrm((L, N_EXPERTS, D_FF, D_MODEL), 0.5 * D_FF ** -0.5),
        'b_down': nrm((L, N_EXPERTS, D_MODEL), 0.01),
        'rel_bias': nrm((REL_BUCKETS, NSA_HEADS), 0.5),
    }


def reference(x, attn_norm_w, w_in, dn_conv_w, dn_a_log, dn_dt_bias, dn_norm_w,
              nsa_q_norm_w, nsa_k_norm_w, nsa_cmp_pos, nsa_cmp_w1, nsa_cmp_w2,
              conv_dw_w, conv_dw_b, conv_ln_w, conv_ln_b, w_out, ffn_norm_w,
              router_w, router_b, w_gate_up, b_gate_up, w_down, b_down, rel_bias):
    for l in range(DEPTH):
        h = rms_norm(x, attn_norm_w[l])
        (dn_q, dn_k, dn_v, dn_z, dn_a, dn_b,
         nsa_q, nsa_kv, nsa_g, conv_u) = split_columns(h @ w_in[l])
        y_a = gated_deltanet(dn_q, dn_k, dn_v, dn_z, dn_a, dn_b,
                             dn_conv_w[l], dn_a_log[l], dn_dt_bias[l], dn_norm_w[l])
        y_b = nsa_attention(nsa_q, nsa_kv, nsa_g, nsa_q_norm_w[l], nsa_k_norm_w[l],
                            nsa_cmp_pos[l], nsa_cmp_w1[l], nsa_cmp_w2[l], rel_bias)
        y_c = conformer_conv(conv_u, conv_dw_w[l], conv_dw_b[l], conv_ln_w[l], conv_ln_b[l])
        x = x + jnp.concatenate([y_a, y_b, y_c], axis=-1) @ w_out[l]
        x = x + moe_ffn(rms_norm(x, ffn_norm_w[l]), router_w[l], router_b[l],
                        w_gate_up[l], b_gate_up[l], w_down[l], b_down[l])
    return x
```

```python
import contextlib
import math
import numpy as np
import concourse.bass as bass
import concourse.mybir as mybir
from concourse.bass_utils import run_bass_kernel_spmd

F32 = mybir.dt.float32
BF16 = mybir.dt.bfloat16
ALU = mybir.AluOpType
AF = mybir.ActivationFunctionType
AX = mybir.AxisListType


class View:
    __slots__ = ("b", "ap")

    def __init__(self, b, ap):
        self.b = b
        self.ap = ap

    def __getitem__(self, k):
        return View(self.b, self.ap[k])

    def bc(self, shape):
        return View(self.b, self.ap.to_broadcast(list(shape)))

    def re(self, pat, **kw):
        return View(self.b, self.ap.rearrange(pat, **kw))

    def bitcast(self, dt):
        return View(self.b, self.ap.bitcast(dt))


class Buf:
    __slots__ = ("t", "w", "r", "name", "psum")

    def __init__(self, t, name, psum=False):
        self.t = t
        self.name = name
        self.w = None
        self.r = {}
        self.psum = psum

    def __getitem__(self, k):
        return View(self, self.t[k])


OUTK = ("out", "accum_out", "ap")
SAME_ENGINE_SYNC = [True]


class Prog:
    NDMA = 16

    def __init__(self, nc, stack):
        self.nc = nc
        self.stack = stack
        self.root_stack = stack
        self.engs = {"pe": nc.tensor, "dve": nc.vector, "act": nc.scalar,
                     "pool": nc.gpsimd, "sp": nc.sync}
        self.sem = {}
        self.cnt = {}
        self.seen = {}
        for k in self.engs:
            self.sem[k] = stack.enter_context(nc.semaphore("s_" + k))
            self.cnt[k] = 0
            self.seen[k] = {}
        for i in range(self.NDMA):
            k = "d%d" % i
            self.sem[k] = stack.enter_context(nc.semaphore("s_" + k))
            self.cnt[k] = 0
        self.dma_rr = 0
        self.nbuf = 0
        self.allbufs = []
        self.epoch = 0

    def new_epoch(self):
        if DEAD[0]:
            return
        self.barrier()
        self.epoch += 1
        for k in list(self.sem):
            self.sem[k] = self.root_stack.enter_context(self.nc.semaphore("s%d_%s" % (self.epoch, k)))
            self.cnt[k] = 0
        for k in self.seen:
            self.seen[k] = {}
        for b in self.allbufs:
            b.w = None
            b.r = {}

    def sb(self, shape, dtype=F32, name=None):
        self.nbuf += 1
        name = name or ("b%d" % self.nbuf)
        t = self.stack.enter_context(self.nc.sbuf_tensor(name, list(shape), dtype))
        assert self.nc.sbuf_bytes_remaining >= 16640, ("SBUF budget", name, self.nc.sbuf_bytes_remaining)
        b = Buf(t, name)
        self.allbufs.append(b)
        return b

    def ps(self, shape, dtype=F32, name=None):
        self.nbuf += 1
        name = name or ("p%d" % self.nbuf)
        t = self.stack.enter_context(self.nc.psum_tensor(name, list(shape), dtype))
        b = Buf(t, name, psum=True)
        self.allbufs.append(b)
        return b

    def _waits(self, ek, reads, writes):
        needs = {}
        for b in reads:
            if b.w is not None:
                k, c = b.w
                if needs.get(k, 0) < c:
                    needs[k] = c
            if b.psum:
                for k, c in b.r.items():
                    if k != ek and needs.get(k, 0) < c:
                        needs[k] = c
        for b in writes:
            if b.w is not None:
                k, c = b.w
                if needs.get(k, 0) < c:
                    needs[k] = c
            for k, c in b.r.items():
                if needs.get(k, 0) < c:
                    needs[k] = c
        eng = self.engs[ek]
        seen = self.seen[ek]
        for k, c in needs.items():
            if k == ek and (ek == "pe" or not SAME_ENGINE_SYNC[0]):
                continue
            if seen.get(k, 0) >= c:
                continue
            eng.wait_ge(self.sem[k], c)
            seen[k] = c

    def op(self, ek, reads, writes, fn):
        if DEAD[0]:
            return None
        self._waits(ek, reads, writes)
        ins = fn(self.engs[ek])
        self.cnt[ek] += 1
        c = self.cnt[ek]
        ins.then_inc(self.sem[ek], 1)
        for b in reads:
            b.r[ek] = c
        for b in writes:
            b.w = (ek, c)
            b.r = {}
        return ins

    def I(self, ek, meth, **kw):
        reads, writes, args = [], [], {}
        for k, v in kw.items():
            if isinstance(v, View):
                (writes if k in OUTK else reads).append(v.b)
                args[k] = v.ap
            else:
                args[k] = v
        return self.op(ek, reads, writes, lambda e: getattr(e, meth)(**args))

    def mm(self, out, lhsT, rhs, start=True, stop=True):
        return self.op("pe", [lhsT.b, rhs.b], [out.b],
                       lambda e: e.matmul(out=out.ap, lhsT=lhsT.ap, rhs=rhs.ap, start=start, stop=stop))

    def tr(self, out, in_, ident):
        return self.op("pe", [in_.b, ident.b], [out.b],
                       lambda e: e.transpose(out=out.ap, in_=in_.ap, identity=ident.ap))

    def ld(self, out, in_ap, q="sp", **kw):
        return self.dma(out.ap, in_ap, [], [out.b], q=q, **kw)

    def st(self, out_ap, in_, q="sp", **kw):
        return self.dma(out_ap, in_.ap, [in_.b], [], q=q, **kw)

    def barrier(self):
        for ek, eng in self.engs.items():
            for k, c in self.cnt.items():
                if k == ek or c == 0:
                    continue
                if self.seen[ek].get(k, 0) < c:
                    eng.wait_ge(self.sem[k], c)
                    self.seen[ek][k] = c

    @contextlib.contextmanager
    def scope(self):
        old = self.stack
        with contextlib.ExitStack() as st:
            self.stack = st
            try:
                yield
            finally:
                self.barrier()
                self.stack = old

    def sbl(self, n, shape, dtype=F32, name=None):
        self.nbuf += 1
        name = name or ("b%d" % self.nbuf)
        full = [shape[0], n] + list(shape[1:])
        t = self.stack.enter_context(self.nc.sbuf_tensor(name, full, dtype))
        assert self.nc.sbuf_bytes_remaining >= 16640, ("SBUF budget", name, self.nc.sbuf_bytes_remaining)
        out = [Buf(t[:, i], "%s_%d" % (name, i)) for i in range(n)]
        self.allbufs.extend(out)
        return out

    def psl(self, n, parts, width, name=None):
        per = 512 // width
        out = []
        while len(out) < n:
            bank = self.ps([128, 512])
            for i in range(per):
                if len(out) < n:
                    out.append(bank[0:parts, i * width:(i + 1) * width])
        return out

    def dma(self, out_ap, in_ap, reads, writes, q="sp", **kw):
        if DEAD[0]:
            return None
        self._waits(q, reads, writes)
        dk = "d%d" % self.dma_rr
        self.dma_rr = (self.dma_rr + 1) % self.NDMA
        if self.cnt[dk] > 0 and self.seen[q].get(dk, 0) < self.cnt[dk]:
            self.engs[q].wait_ge(self.sem[dk], self.cnt[dk])
            self.seen[q][dk] = self.cnt[dk]
        ins = self.engs[q].dma_start(out=out_ap, in_=in_ap, **kw)
        self.cnt[dk] += 16
        c = self.cnt[dk]
        ins.then_inc(self.sem[dk], 16)
        for b in reads:
            b.r[dk] = c
        for b in writes:
            b.w = (dk, c)
            b.r = {}
        return ins

    def finish(self, bufs):
        self._waits("sp", bufs, [])
        for k in list(self.sem):
            if k.startswith("d") and self.cnt[k] > 0:
                if self.seen["sp"].get(k, 0) < self.cnt[k]:
                    self.engs["sp"].wait_ge(self.sem[k], self.cnt[k])
                    self.seen["sp"][k] = self.cnt[k]


T = 2048
D = 1024
NT = T // 128
L_DEPTH = 2
EPS = 1e-6
C = 64
NCH = T // C
NE = 32


def make_consts():
    c = {}
    c["ident"] = np.eye(128, dtype=np.float32)
    c["ones"] = np.ones((128, 128), np.float32)
    k = np.arange(64)
    tri = (k[:, None] <= k[None, :]).astype(np.float32)
    tril = (k[None, :] <= k[:, None]).astype(np.float32)
    t64 = np.zeros((128, 128), np.float32)
    t64[:64, :64] = tri
    t64[:64, 64:] = tril
    c["tri"] = t64
    bd = np.zeros((128, 128), np.float32)
    bd[:64, :64] = 1.0
    bd[64:, 64:] = 1.0
    return np.concatenate([c["ident"], c["ones"], c["tri"], bd], axis=1)


class Ctx:
    pass


class StopBuild(Exception):
    pass


STOP = [None]


def chk(k):
    if STOP[0] == k:
        DEAD[0] = True


DEAD = [False]


def dbg_dump(P, cx, name, view, shape):
    if name not in cx.dout:
        return
    with P.scope():
        tmp = P.sb(list(shape))
        P.I("dve", "tensor_copy", out=tmp[:], in_=view)
        P.st(cx.dout[name], tmp[:])


def load_cast(P, cx, dram_ap, dst, n, parts=128):
    STG = cx.stg_n
    o = 0
    while o < n:
        m = min(STG, n - o)
        sb = cx.stg[cx.stg_i % len(cx.stg)]
        cx.stg_i += 1
        P.ld(sb[0:parts, 0:m], dram_ap[:, o:o + m])
        P.I("pool", "tensor_copy", out=dst[:, o:o + m], in_=sb[0:parts, 0:m])
        o += m


def rsqrt_inplace(P, v):
    P.I("act", "sqrt", out=v, in_=v)
    P.I("dve", "reciprocal", out=v, in_=v)


def norm_to_T(P, cx, xres, nw, hT):
    for i in range(NT):
        ss = cx.nrm_ss[i % 2]
        xn = cx.nrm_xn[i % 2]
        junk = xn
        pt = cx.nrm_pt[i % 2]
        P.I("act", "activation", out=junk[:], in_=xres[i][:], func=AF.Square, accum_out=ss[:])
        P.I("dve", "tensor_scalar", out=ss[:], in0=ss[:], scalar1=1.0 / D, scalar2=EPS,
            op0=ALU.mult, op1=ALU.add)
        rsqrt_inplace(P, ss[:])
        P.I("dve", "tensor_scalar", out=xn[:], in0=xres[i][:], scalar1=ss[:, 0:1], scalar2=None,
            op0=ALU.mult)
        for c in range(8):
            P.tr(pt[:, c, :], xn[:, c * 128:(c + 1) * 128], cx.ident_bf[:])
        P.I("dve", "tensor_tensor", out=hT[:, :, i * 128:(i + 1) * 128], in0=pt[:],
            in1=nw.bc([128, 8, 128]), op=ALU.mult)


def deltanet(P, cx, l, hT, xres_unused, yaT, d):
    ident = cx.ident_f
    tri = cx.tri
    tril = cx.tril
    ones = cx.ones_f
    with P.scope():
        gab = P.sb([64, NCH, 8])
        wtm = P.sb([128, 8, 20], BF16)
        load_cast(P, cx, d["w_tm"][l].rearrange("p c n -> p (c n)"), wtm[:].re("p c n -> p (c n)"), 160)
        with P.scope():
            pg = P.ps([64, 8, 8])
            for c0 in range(0, NCH, 8):
                for cc in range(8):
                    c = c0 + cc
                    for kc in range(8):
                        P.mm(pg[:, cc, :], hT[:, kc, c * 64:(c + 1) * 64], wtm[:, kc, 0:8],
                             start=(kc == 0), stop=(kc == 7))
                P.I("act", "copy", out=gab[:, c0:c0 + 8, :], in_=pg[:])
        chk("gab")
        alog = P.sb([64, 1, 4]); dtb = P.sb([64, 1, 4])
        P.ld(alog[:, 0, :], d["dn_alog"][l:l + 1, :].partition_broadcast(64))
        P.ld(dtb[:, 0, :], d["dn_dtb"][l:l + 1, :].partition_broadcast(64))
        nA = P.sb([64, 1, 4])
        P.I("act", "activation", out=nA[:], in_=alog[:], func=AF.Exp)
        xa = P.sb([64, NCH, 4]); t1 = P.sb([64, NCH, 4]); g = P.sb([64, NCH, 4])
        beta = P.sb([64, NCH, 4])
        P.I("dve", "tensor_tensor", out=xa[:], in0=gab[:, :, 0:4], in1=dtb[:].bc([64, NCH, 4]), op=ALU.add)
        P.I("dve", "scalar_tensor_tensor", out=t1[:], in0=xa[:], scalar=-1.0, in1=xa[:],
            op0=ALU.mult, op1=ALU.max)
        P.I("act", "activation", out=t1[:], in_=t1[:], func=AF.Exp, scale=-1.0)
        P.I("act", "activation", out=t1[:], in_=t1[:], func=AF.Ln, bias=1.0)
        P.I("dve", "scalar_tensor_tensor", out=g[:], in0=xa[:], scalar=0.0, in1=t1[:],
            op0=ALU.max, op1=ALU.add)
        P.I("dve", "tensor_tensor", out=g[:], in0=g[:], in1=nA[:].bc([64, NCH, 4]), op=ALU.mult)
        P.I("dve", "tensor_scalar", out=g[:], in0=g[:], scalar1=-1.0, scalar2=None, op0=ALU.mult)
        P.I("act", "activation", out=beta[:], in_=gab[:, :, 4:8], func=AF.Sigmoid)
        gc = P.sb([64, NCH, 4]); eg = P.sb([64, NCH, 4]); ekd = P.sb([64, NCH, 4])
        bk = P.sb([64, NCH, 4]); egl = P.sb([128, NCH, 4]); gl = P.sb([64, NCH, 4])
        with P.scope():
            pc = P.ps([128, NCH * 4])
            P.mm(pc[0:64, :], tri, g[:].re("p c h -> p (c h)"))
            P.I("dve", "tensor_copy", out=gc[:].re("p c h -> p (c h)"), in_=pc[0:64, :])
            P.I("act", "activation", out=eg[:].re("p c h -> p (c h)"), in_=pc[0:64, :], func=AF.Exp)
            P.mm(pc[:, :], ones[0:64, :], g[:].re("p c h -> p (c h)"))
            P.I("act", "activation", out=egl[:].re("p c h -> p (c h)"), in_=pc[:, :], func=AF.Exp)
            P.I("dve", "tensor_tensor", out=gl[:].re("p c h -> p (c h)"), in0=pc[0:64, :],
                in1=gc[:].re("p c h -> p (c h)"), op=ALU.subtract)
            P.I("act", "activation", out=ekd[:], in_=gl[:], func=AF.Exp)
            P.I("dve", "tensor_tensor", out=bk[:], in0=beta[:], in1=eg[:], op=ALU.mult)
        chk("gates")
        cw = P.sb([128, 12, 4])
        P.ld(cw[:], d["dn_cw"][l])
        dnw = P.sb([128, 1])
        P.ld(dnw[:], d["dn_nw"][l])

        for h in range(4):
            with P.scope():
                deltanet_head(P, cx, l, h, hT, yaT, d, dict(
                    g=g, gc=gc, eg=eg, ekd=ekd, bk=bk, egl=egl, beta=beta, cw=cw, dnw=dnw))


def deltanet_head(P, cx, l, h, hT, yaT, d, G):
    ident = cx.ident_f
    tri, tril, ones = cx.tri, cx.tril, cx.ones_f
    id64 = cx.ident_f[0:64, 0:64]
    szT = P.sb([128, T], BF16)
    qkv = [P.sb([128, T], name="qkv%d_%d_%d" % (l, h, i)) for i in range(3)]
    pp = [P.ps([128, 512]) for _ in range(2)]
    n = 0
    with P.scope():
        wdn = P.sb([128, 8, 512], BF16)
        load_cast(P, cx, d["w_dn"][l, h].rearrange("p c n -> p (c n)"), wdn[:].re("p c n -> p (c n)"), 4096)
        raw = P.sb([128, 3, T + 4], BF16)
        P.I("pool", "memset", ap=raw[:, :, 0:3], constant=0.0)
        for which in range(4):
            for tg in range(4):
                ps = pp[n % 2]; n += 1
                for kc in range(8):
                    P.mm(ps[:], wdn[:, kc, which * 128:(which + 1) * 128], hT[:, kc, tg * 512:(tg + 1) * 512],
                         start=(kc == 0), stop=(kc == 7))
                if which < 3:
                    P.I("act", "copy", out=raw[:, which, 3 + tg * 512:3 + (tg + 1) * 512], in_=ps[:])
                else:
                    P.I("act", "activation", out=szT[:, tg * 512:(tg + 1) * 512], in_=ps[:], func=AF.Silu)
        chk("proj")
        cw = G["cw"]
        for which in range(3):
            acc = qkv[which]
            ci = h * 3 + which
            P.I("dve", "tensor_scalar", out=acc[:], in0=raw[:, which, 0:T], scalar1=cw[:, ci, 0:1],
                scalar2=None, op0=ALU.mult)
            for j in range(1, 4):
                P.I("dve", "scalar_tensor_tensor", out=acc[:], in0=raw[:, which, j:j + T],
                    scalar=cw[:, ci, j:j + 1], in1=acc[:], op0=ALU.mult, op1=ALU.add)
            P.I("act", "activation", out=acc[:], in_=acc[:], func=AF.Silu)
    chk("conv")
    with P.scope():
        sq = P.sb([128, 512]); rn = P.sb([128, 512])
        for which in range(2):
            for tg in range(4):
                sl = slice(tg * 512, (tg + 1) * 512)
                ps = pp[n % 2]; n += 1
                P.I("pool", "tensor_tensor", out=sq[:], in0=qkv[which][:, sl], in1=qkv[which][:, sl], op=ALU.mult)
                P.mm(ps[:], ones, sq[:])
                P.I("dve", "tensor_scalar", out=rn[:], in0=ps[:], scalar1=EPS, scalar2=None, op0=ALU.add)
                rsqrt_inplace(P, rn[:])
                P.I("dve", "scalar_tensor_tensor", out=qkv[which][:, sl], in0=qkv[which][:, sl],
                    scalar=(128.0 ** -0.5 if which == 0 else 1.0), in1=rn[:], op0=ALU.mult, op1=ALU.mult)
    chk("l2")
    qT, kT, vT = qkv
    ktok = [P.sb([64, 4, 128]) for _ in range(2)]
    vtok = [P.sb([64, 4, 128]) for _ in range(2)]
    S = [P.sb([128, 128], name="S%d_%d_%d" % (l, h, i)) for i in range(2)]
    P.I("pool", "memset", ap=S[0][:], constant=0.0)
    oall = [P.sb([64, 4, 128]) for _ in range(2)]
    osq = P.sb([64, 4, 128]); ssq = P.sb([64, 4])
    GS = 4
    sets = []
    for s in range(GS):
        st = Ctx()
        st.pa = P.psl(3, 64, 128)
        st.dec = P.sb([64, 64]); st.decT = P.sb([64, 64]); st.AT = P.sb([64, 64])
        st.Lp = [P.sb([64, 64]) for _ in range(2)]; st.Up = [P.sb([64, 64]) for _ in range(2)]
        st.Pm = [P.sb([64, 64]) for _ in range(2)]
        st.kbg = P.sb([64, 128]); st.vb = P.sb([64, 128])
        st.wT = P.sb([128, 64]); st.u = P.sb([64, 128]); st.kd = P.sb([64, 128])
        sets.append(st)
    psc = P.psl(4, 128, 128)
    vnew = [P.sb([64, 128]) for _ in range(2)]
    o1 = [P.sb([64, 128]) for _ in range(2)]
    g, gc, eg, ekd, bk, egl, beta = (G[k] for k in ("g", "gc", "eg", "ekd", "bk", "egl", "beta"))

    def col(buf, c):
        return buf[:, c, h:h + 1]

    for c0 in range(0, NCH, GS):
        grp = list(range(c0, c0 + GS))

        def each(fn):
            for c in grp:
                fn(c, sets[c - c0])

        c4 = c0 // 4
        kt, vt, oa = ktok[c4 % 2], vtok[c4 % 2], oall[c4 % 2]
        for src, dst in ((kT, kt), (vT, vt)):
            ps = pp[n % 2]; n += 1
            for cc in range(4):
                c = c0 + cc
                P.tr(ps[0:64, cc * 128:(cc + 1) * 128], src[:, c * 64:(c + 1) * 64], ident[:])
            P.I("act", "copy", out=dst[:].re("p c d -> p (c d)"), in_=ps[0:64, :])

        chk("ktr")
        each(lambda c, s: P.mm(s.pa[0][:, 0:64], g[:, c, h:h + 1].bc([64, 64]), tri))
        each(lambda c, s: P.mm(s.pa[1][:, 0:64], kT[:, c * 64:(c + 1) * 64], kT[:, c * 64:(c + 1) * 64]))
        each(lambda c, s: P.mm(s.pa[1][:, 64:128], kT[:, c * 64:(c + 1) * 64], qT[:, c * 64:(c + 1) * 64]))
        each(lambda c, s: P.I("dve", "tensor_scalar", out=s.dec[:], in0=s.pa[0][:, 0:64], scalar1=col(gc, c),
                              scalar2=0.0, op0=ALU.subtract, op1=ALU.max))
        each(lambda c, s: P.I("dve", "tensor_scalar", out=s.decT[:], in0=s.pa[0][:, 0:64], scalar1=col(gc, c),
                              scalar2=0.0, op0=ALU.subtract, op1=ALU.min))
        each(lambda c, s: P.I("act", "activation", out=s.dec[:], in_=s.dec[:], func=AF.Exp, scale=-1.0))
        each(lambda c, s: P.I("act", "activation", out=s.decT[:], in_=s.decT[:], func=AF.Exp))
        each(lambda c, s: P.I("pool", "tensor_tensor", out=s.dec[:], in0=s.dec[:], in1=tril, op=ALU.mult))
        each(lambda c, s: P.I("pool", "tensor_tensor", out=s.decT[:], in0=s.decT[:], in1=tri, op=ALU.mult))
        each(lambda c, s: P.I("pool", "tensor_tensor", out=s.dec[:], in0=s.dec[:], in1=id64, op=ALU.subtract))
        each(lambda c, s: P.I("dve", "scalar_tensor_tensor", out=s.Lp[0][:], in0=s.pa[1][:, 0:64],
                              scalar=col(beta, c), in1=s.dec[:], op0=ALU.mult, op1=ALU.mult))
        each(lambda c, s: P.I("dve", "tensor_tensor", out=s.AT[:], in0=s.pa[1][:, 64:128], in1=s.decT[:],
                              op=ALU.mult))
        chk("dec")
        each(lambda c, s: P.mm(s.pa[2][:, 0:64], s.Lp[0][:], id64))
        chk("utr1")
        each(lambda c, s: P.I("act", "copy", out=s.Up[0][:], in_=s.pa[2][:, 0:64]))
        chk("utr2")
        each(lambda c, s: P.I("dve", "tensor_tensor", out=s.Pm[0][:], in0=id64, in1=s.Up[0][:],
                              op=ALU.subtract))
        chk("utr")
        for it in range(5):
            a, b = it % 2, (it + 1) % 2
            last = it == 4
            each(lambda c, s: P.mm(s.pa[0][:, 0:64], s.Up[a][:], s.Lp[a][:]))
            if not last:
                each(lambda c, s: P.mm(s.pa[0][:, 64:128], s.Lp[a][:], s.Up[a][:]))
            if it == 0: chk("i0a")
            each(lambda c, s: P.I("dve", "tensor_copy", out=s.Lp[b][:], in_=s.pa[0][:, 0:64]))
            if not last:
                each(lambda c, s: P.I("dve", "tensor_copy", out=s.Up[b][:], in_=s.pa[0][:, 64:128]))
            if it == 0: chk("i0b")
            each(lambda c, s: P.mm(s.pa[2][:, 0:64], s.Lp[b][:], s.Pm[a][:]))
            if it == 0: chk("i0c")
            each(lambda c, s: P.I("dve", "tensor_tensor", out=s.Pm[b][:], in0=s.pa[2][:, 0:64], in1=s.Pm[a][:],
                                  op=ALU.add))
            if it == 0: chk("i0d")
        chk("inv")
        PT = 1
        each(lambda c, s: P.I("pool", "tensor_scalar", out=s.kbg[:], in0=kt[:, c % 4, :],
                              scalar1=col(bk, c), scalar2=None, op0=ALU.mult))
        each(lambda c, s: P.I("pool", "tensor_scalar", out=s.vb[:], in0=vt[:, c % 4, :],
                              scalar1=col(beta, c), scalar2=None, op0=ALU.mult))
        each(lambda c, s: P.I("pool", "tensor_scalar", out=s.kd[:], in0=kt[:, c % 4, :],
                              scalar1=col(ekd, c), scalar2=None, op0=ALU.mult))
        each(lambda c, s: P.mm(s.pa[0][:, :], s.Pm[PT][:], s.vb[:]))
        each(lambda c, s: P.I("act", "copy", out=s.u[:], in_=s.pa[0][:, :]))
        for c in grp:
            s = sets[c - c0]
            pw = psc[3]
            P.mm(pw[:, 0:64], s.kbg[:], s.Pm[PT][:])
            P.I("dve", "tensor_copy", out=s.wT[:], in_=pw[:, 0:64])
        chk("wu")
        for c in grp:
            s = sets[c - c0]
            Sa, Sb = S[c % 2], S[(c + 1) % 2]
            vn = vnew[c % 2]; oo = o1[c % 2]
            P.mm(psc[0][0:64, :], s.wT[:], Sa[:])
            P.mm(psc[1][0:64, :], qT[:, c * 64:(c + 1) * 64], Sa[:])
            P.I("dve", "tensor_tensor", out=vn[:], in0=s.u[:], in1=psc[0][0:64, :], op=ALU.subtract)
            P.mm(psc[2][0:64, :], s.AT[:], vn[:])
            P.mm(psc[0][:, :], s.kd[:], vn[:])
            P.I("dve", "scalar_tensor_tensor", out=Sb[:], in0=Sa[:], scalar=egl[:, c, h:h + 1],
                in1=psc[0][:, :], op0=ALU.mult, op1=ALU.add)
            P.I("act", "copy", out=oo[:], in_=psc[2][0:64, :])
            P.I("dve", "scalar_tensor_tensor", out=oa[:, c % 4, :], in0=psc[1][0:64, :],
                scalar=col(eg, c), in1=oo[:], op0=ALU.mult, op1=ALU.add)
        chk("scan")
        P.I("pool", "tensor_tensor", out=osq[:], in0=oa[:], in1=oa[:], op=ALU.mult)
        P.I("dve", "tensor_reduce", out=ssq[:], in_=osq[:], axis=AX.X, op=ALU.add)
        P.I("dve", "tensor_scalar", out=ssq[:], in0=ssq[:], scalar1=1.0 / 128, scalar2=EPS,
            op0=ALU.mult, op1=ALU.add)
        rsqrt_inplace(P, ssq[:])
        P.I("dve", "tensor_tensor", out=oa[:], in0=oa[:],
            in1=ssq[:].re("p (c o) -> p c o", o=1).bc([64, 4, 128]), op=ALU.mult)
        ps = pp[n % 2]; n += 1
        for cc in range(4):
            P.tr(ps[:, cc * 64:(cc + 1) * 64], oa[:, cc, :], id64)
        P.I("dve", "scalar_tensor_tensor", out=yaT[:, h, c4 * 256:(c4 + 1) * 256], in0=ps[:, 0:256],
            scalar=G["dnw"][:, 0:1], in1=szT[:, c4 * 256:(c4 + 1) * 256], op0=ALU.mult, op1=ALU.mult)


IN_OFF = np.cumsum([0, 512, 512, 512, 512, 4, 4, 256, 384, 12, 512])


def kchunk(w):
    n = w.shape[1]
    return np.ascontiguousarray(w.reshape(8, 128, n).transpose(1, 0, 2))


def prep_shared(inp):
    f = lambda a: np.ascontiguousarray(np.asarray(a, dtype=np.float32))
    L = L_DEPTH
    sh = {}
    sh["consts"] = make_consts()
    sh["anw"] = f(np.asarray(inp["attn_norm_w"]).reshape(L, 8, 128).transpose(0, 2, 1))[..., None]
    w_in = np.asarray(inp["w_in"])
    w_dn = np.zeros((L, 4, 128, 8, 512), np.float32)
    w_tm = np.zeros((L, 128, 8, 20), np.float32)
    for l in range(L):
        for h in range(4):
            cols = np.concatenate([np.arange(o + h * 128, o + (h + 1) * 128) for o in (0, 512, 1024, 1536)])
            w_dn[l, h] = kchunk(w_in[l][:, cols])
        cols = np.concatenate([np.arange(2048, 2056), np.arange(2696, 2708)])
        w_tm[l] = kchunk(w_in[l][:, cols])
    sh["w_dn"] = w_dn
    sh["w_tm"] = w_tm
    cw = np.asarray(inp["dn_conv_w"])
    sh["dn_cw"] = f(cw.reshape(L, 4, 3, 4, 128).transpose(0, 4, 3, 2, 1).reshape(L, 128, 12, 4))
    sh["dn_alog"] = f(inp["dn_a_log"])
    sh["dn_dtb"] = f(inp["dn_dt_bias"])
    sh["dn_nw"] = f(np.asarray(inp["dn_norm_w"]).reshape(L, 128, 1))
    sh["w_out"] = f(inp["w_out"])
    prep_nsa(inp, sh)
    prep_conv(inp, sh)
    prep_moe(inp, sh)
    return sh


def build_nc(nlayers=L_DEPTH, stages=("dn", "nsa", "conv", "moe"), dbg=(), nexp=NE):
    nc = bass.Bass("TRN2", target_bir_lowering=False)
    d = {}

    def inp(name, shape):
        d[name] = nc.dram_tensor(name, list(shape), F32, kind="ExternalInput").ap()

    inp("x", [T, D]); inp("consts", [128, 512]); inp("anw", [2, 128, 8, 1])
    inp("w_out", [2, D, D])
    inp("w_nsa_fm", [2, 128, 8, 640]); inp("w_nsa_tm", [2, 128, 8, 128]); inp("nsa_nw", [2, 128, 3])
    inp("nsa_knw0", [2, 64]); inp("nsa_posT", [2, 128, 32]); inp("nsa_w1", [2, 128, 32, 128])
    inp("nsa_w2", [2, 128, 128]); inp("nsa_bias_cmp", [4, NCMP, T]); inp("nsa_bias_tile", [128, 4, 2, 128])
    inp("nsa_c31", [128, 4]); inp("nsa_sel_tab", [128, 3, NT, 32]); inp("nsa_expand", [32, T])
    inp("nsa_overlap", [NCMP, 32]); inp("nsa_mwin", [128, 8, 512])
    inp("w_conv", [2, 128, 8, 512]); inp("conv_prm", [2, 128, 2, 34])
    inp("fnw", [2, 128, 8, 1]); inp("router_w", [2, 128, 8, NE]); inp("router_b", [2, NE])
    inp("w_gu", [2, NE, 8, 128, 2048]); inp("w_dn_moe", [2, NE, D, D]); inp("b_gu", [2, NE, 128, 16])
    inp("b_dn", [2, NE, D])
    inp("w_dn", [2, 4, 128, 8, 512]); inp("w_tm", [2, 128, 8, 20]); inp("dn_cw", [2, 128, 12, 4])
    inp("dn_alog", [2, 4]); inp("dn_dtb", [2, 4]); inp("dn_nw", [2, 128, 1])
    out = nc.dram_tensor("out", [T, D], F32, kind="ExternalOutput").ap()
    dout = {}
    for name, shape in dbg:
        dout[name] = nc.dram_tensor(name, list(shape), F32, kind="ExternalOutput").ap()

    with contextlib.ExitStack() as st:
        P = Prog(nc, st)
        cx = Ctx()
        consts = P.sb([128, 512])
        P.ld(consts[:], d["consts"])
        cx.ident_f = consts[:, 0:128]
        cx.ones_f = consts[:, 128:256]
        cx.tri = consts[0:64, 256:320]
        cx.tril = consts[0:64, 320:384]
        cx.bd_ones = consts[:, 384:512]
        cx.dbg_ybT = dout.get("ybT")
        cx.dout = dout
        cx.nexp = nexp
        identb = P.sb([128, 128], BF16)
        P.I("dve", "tensor_copy", out=identb[:], in_=consts[:, 0:128])
        cx.ident_bf = identb
        cx.stg_n = 512
        cx.stg_i = 0
        cx.nrm_ss = [P.sb([128, 1]) for _ in range(2)]
        xres = P.sbl(NT, [128, D], name="xres")
        for i in range(NT):
            P.ld(xres[i][:], d["x"][i * 128:(i + 1) * 128, :])
        for l in range(nlayers):
          try:
            if l > 0:
                P.new_epoch()
            with P.scope():
                cx.stg = [P.sb([128, cx.stg_n]) for _ in range(2)]
                hT = P.sb([128, 8, T], BF16)
                nw = P.sb([128, 8, 1])
                P.ld(nw[:], d["anw"][l])
                with P.scope():
                    cx.nrm_pt = [P.ps([128, 8, 128], BF16) for _ in range(2)]
                    cx.nrm_xn = [P.sb([128, D], BF16) for _ in range(2)]
                    norm_to_T(P, cx, xres, nw[:], hT)
                chk("norm")
                if "dn" in stages:
                    with P.scope():
                        yaT = P.sb([128, 4, T], BF16)
                        deltanet(P, cx, l, hT, xres, yaT, d)
                        if "yaT" in dout:
                            with P.scope():
                                tmp = P.sb([128, 4, T])
                                P.I("dve", "tensor_copy", out=tmp[:], in_=yaT[:])
                                P.st(dout["yaT"], tmp[:])
                        apply_wout(P, cx, l, yaT, 0, 4, xres, d)
                if "nsa" in stages:
                    nsa(P, cx, l, hT, xres, d)
                if "conv" in stages:
                    conformer(P, cx, l, hT, xres, d)
            if "moe" in stages:
                P.new_epoch()
                moe(P, cx, l, xres, d, nexp=cx.nexp)
          except StopBuild:
            break
        DEAD[0] = False
        for i in range(NT):
            P.st(out[i * 128:(i + 1) * 128, :], xres[i][:])
        P.finish([])
    return nc


NEGM = -200.0
NCMP = 127


def t5_bucket_np(dist):
    n = np.maximum(dist, 0)
    nf = np.maximum(n, 1).astype(np.float32)
    large = 16 + (np.log(nf / np.float32(16)) / np.float32(math.log(8.0)) * np.float32(16)).astype(np.int32)
    large = np.minimum(large, 31)
    return np.where(n < 16, n, large)


def nsa_tables(rel_bias):
    rb = np.asarray(rel_bias, np.float32)
    tb = {}
    t = np.arange(T)
    j = np.arange(NCMP)
    dist = t[None, :] - (j[:, None] * 16 + 31)
    bk = t5_bucket_np(dist)
    bc = rb[bk]
    bc = np.where((dist >= 0)[..., None], bc, np.float32(NEGM))
    tb["bias_cmp"] = np.ascontiguousarray(bc.transpose(2, 0, 1)).astype(np.float32)
    k = np.arange(128)
    tt = np.arange(128)
    d0 = tt[None, :] - k[:, None]
    d1 = d0 + 128
    b0 = np.where((d0 >= 0)[..., None], rb[t5_bucket_np(d0)], np.float32(NEGM))
    b1 = rb[t5_bucket_np(d1)]
    tbl = np.stack([b0, b1], axis=0)
    tb["bias_tile"] = np.ascontiguousarray(tbl.transpose(1, 3, 0, 2)).astype(np.float32)
    tb["c31"] = np.ascontiguousarray(np.broadcast_to(rb[31][None, :], (128, 4))).astype(np.float32)
    tok = (np.arange(NT)[None, :] * 128 + np.arange(128)[:, None])
    cur = tok // 64
    s = np.arange(32)[None, None, :]
    causal = s <= cur[..., None]
    forced = (s == 0) | (s == cur[..., None]) | (s == cur[..., None] - 1)
    m1 = (causal & ~forced).astype(np.float32)
    cst = np.where(causal & forced, 1e4, np.where(causal, 0.0, -1.0)).astype(np.float32)
    tb["sel_tab"] = np.ascontiguousarray(np.stack([m1, cst, causal.astype(np.float32)], axis=1))
    key = np.arange(T)
    tb["expand"] = (key[None, :] // 64 == np.arange(32)[:, None]).astype(np.float32)
    cs = j * 16
    ss = np.arange(32) * 64
    ov = ((cs[:, None] < ss[None, :] + 64) & (cs[:, None] + 32 > ss[None, :])).astype(np.float32)
    tb["overlap"] = ov
    mw = np.zeros((128, 8, 512), np.float32)
    for r in range(8):
        rel = r - 4
        keyp = rel * 128 + k[:, None]
        tp = np.arange(512)[None, :]
        dd = tp - keyp
        mw[:, r, :] = ((dd >= 0) & (dd < 512)).astype(np.float32)
    tb["mwin"] = mw
    return tb


def prep_nsa(inp, sh):
    L = L_DEPTH
    w_in = np.asarray(inp["w_in"])
    w_fm = np.zeros((L, 128, 8, 640), np.float32)
    w_tmv = np.zeros((L, 128, 8, 128), np.float32)
    for l in range(L):
        q0 = 2056
        kv0 = 2312
        cols = np.concatenate([
            np.arange(q0, q0 + 256),
            np.arange(kv0 + 128, kv0 + 192), np.arange(kv0 + 128, kv0 + 192),
            np.arange(kv0 + 256, kv0 + 320), np.arange(kv0 + 256, kv0 + 320),
            np.arange(kv0, kv0 + 128),
        ])
        w_fm[l] = kchunk(w_in[l][:, cols])
        cols = np.concatenate([np.arange(kv0 + 192, kv0 + 256), np.arange(kv0 + 320, kv0 + 384)])
        w_tmv[l] = kchunk(w_in[l][:, cols])
    sh["w_nsa_fm"] = w_fm
    sh["w_nsa_tm"] = w_tmv
    qn = np.asarray(inp["nsa_q_norm_w"], np.float32)
    kn = np.asarray(inp["nsa_k_norm_w"], np.float32)
    nw = np.zeros((L, 128, 3), np.float32)
    nw[:, :, 0] = np.concatenate([qn, qn], axis=1)
    nw[:, :, 1] = np.concatenate([kn[:, 1], kn[:, 1]], axis=1)
    nw[:, :, 2] = np.concatenate([kn[:, 2], kn[:, 2]], axis=1)
    sh["nsa_nw"] = nw
    sh["nsa_knw0"] = np.ascontiguousarray(kn[:, 0, :])
    pos = np.asarray(inp["nsa_cmp_pos"], np.float32)
    sh["nsa_posT"] = np.ascontiguousarray(pos.transpose(0, 1, 3, 2).reshape(L, 128, 32))
    w1 = np.asarray(inp["nsa_cmp_w1"], np.float32)
    sh["nsa_w1"] = np.ascontiguousarray(w1.reshape(L, 2, 32, 64, 128).transpose(0, 1, 3, 2, 4).reshape(L, 128, 32, 128))
    w2 = np.asarray(inp["nsa_cmp_w2"], np.float32)
    sh["nsa_w2"] = np.ascontiguousarray(w2.transpose(0, 2, 1, 3).reshape(L, 128, 128))
    tb = nsa_tables(inp["rel_bias"])
    for k_, v_ in tb.items():
        sh["nsa_" + k_] = v_


def apply_wout(P, cx, l, yT, chunk0, nch, xres, d):
    with P.scope():
        w = P.sb([128, nch, D], BF16)
        for c in range(nch):
            load_cast(P, cx, d["w_out"][l, (chunk0 + c) * 128:(chunk0 + c + 1) * 128, :], w[:, c, :], D)
        pp = [P.ps([128, 512]) for _ in range(2)]
        n = 0
        for i in range(NT):
            for half in range(2):
                ps = pp[n % 2]; n += 1
                for c in range(nch):
                    P.mm(ps[:], yT[:, c, i * 128:(i + 1) * 128], w[:, c, half * 512:(half + 1) * 512],
                         start=(c == 0), stop=(c == nch - 1))
                P.I("dve", "tensor_tensor", out=xres[i][:, half * 512:(half + 1) * 512], in0=ps[:],
                    in1=xres[i][:, half * 512:(half + 1) * 512], op=ALU.add)


def nsa(P, cx, l, hT, xres, d):
    TINY = 1e-30
    with P.scope():
        qT = P.sb([128, 2, T], BF16)
        kslcT = P.sb([128, T], BF16)
        kwinT = P.sb([128, T], BF16)
        cmpT = P.sb([128, T], BF16)
        vaug = P.sb([128, NT, 2, 66], BF16)
        gts = P.sb([128, NT, 12])
        ynsa = P.sb([128, NT, 256])
        impacc = P.sb([128, NT, 32])
        nw = P.sb([128, 3])
        P.ld(nw[:], d["nsa_nw"][l])
        qw = P.sb([128, 1])
        P.I("dve", "tensor_scalar", out=qw[:], in0=nw[:, 0:1], scalar1=0.125, scalar2=None, op0=ALU.mult)
        P.I("pool", "memset", ap=vaug[:, :, :, 64:66], constant=1.0)
        with P.scope():
            wfm = P.sb([128, 8, 640], BF16)
            load_cast(P, cx, d["w_nsa_fm"][l].rearrange("p c n -> p (c n)"), wfm[:].re("p c n -> p (c n)"), 8 * 640)
            wtv = P.sb([128, 8, 128], BF16)
            load_cast(P, cx, d["w_nsa_tm"][l].rearrange("p c n -> p (c n)"), wtv[:].re("p c n -> p (c n)"), 8 * 128)
            wtm = P.sb([128, 8, 20], BF16)
            load_cast(P, cx, d["w_tm"][l].rearrange("p c n -> p (c n)"), wtm[:].re("p c n -> p (c n)"), 160)
            pp = [P.ps([128, 512]) for _ in range(2)]
            pq = [P.ps([128, 512]) for _ in range(2)]
            qs = [P.sb([128, 512]) for _ in range(2)]
            sq = P.sb([128, 512]); rn = P.sb([128, 512])
            n = 0
            for ch in range(5):
                for tg in range(4):
                    sl = slice(tg * 512, (tg + 1) * 512)
                    ps = pp[n % 2]; ps2 = pq[n % 2]; qsb = qs[n % 2]; n += 1
                    for kc in range(8):
                        P.mm(ps[:], wfm[:, kc, ch * 128:(ch + 1) * 128], hT[:, kc, sl], start=(kc == 0), stop=(kc == 7))
                    if ch == 4:
                        P.I("act", "copy", out=cmpT[:, sl], in_=ps[:])
                        continue
                    P.I("act", "copy", out=qsb[:], in_=ps[:])
                    P.I("pool", "tensor_tensor", out=sq[:], in0=qsb[:], in1=qsb[:], op=ALU.mult)
                    P.mm(ps2[:], cx.bd_ones, sq[:])
                    P.I("dve", "tensor_scalar", out=rn[:], in0=ps2[:], scalar1=1.0 / 64, scalar2=EPS,
                        op0=ALU.mult, op1=ALU.add)
                    rsqrt_inplace(P, rn[:])
                    if ch < 2:
                        dst, wc = qT[:, ch, sl], qw[:, 0:1]
                    elif ch == 2:
                        dst, wc = kslcT[:, sl], nw[:, 1:2]
                    else:
                        dst, wc = kwinT[:, sl], nw[:, 2:3]
                    P.I("dve", "scalar_tensor_tensor", out=dst, in0=qsb[:], scalar=wc, in1=rn[:],
                        op0=ALU.mult, op1=ALU.mult)
            for i in range(NT):
                ps = pp[n % 2]; n += 1
                for kc in range(8):
                    P.mm(ps[:, 0:128], hT[:, kc, i * 128:(i + 1) * 128], wtv[:, kc, :], start=(kc == 0), stop=(kc == 7))
                for kc in range(8):
                    P.mm(ps[:, 128:140], hT[:, kc, i * 128:(i + 1) * 128], wtm[:, kc, 8:20], start=(kc == 0), stop=(kc == 7))
                P.I("act", "copy", out=vaug[:, i, :, 0:64], in_=ps[:, 0:128].re("p (a b) -> p a b", b=64))
                P.I("act", "activation", out=gts[:, i, :], in_=ps[:, 128:140], func=AF.Sigmoid)
        chk("nsa_proj")
        with P.scope():
            w1 = P.sb([128, 32, 128], BF16)
            load_cast(P, cx, d["nsa_w1"][l].rearrange("p c n -> p (c n)"), w1[:].re("p c n -> p (c n)"), 4096)
            w2 = P.sb([128, 128], BF16)
            load_cast(P, cx, d["nsa_w2"][l], w2[:], 128)
            posT = P.sb([128, 32], BF16)
            load_cast(P, cx, d["nsa_posT"][l], posT[:], 32)
            knw0 = P.sb([128, 64])
            P.ld(knw0[0:NCMP, :], d["nsa_knw0"][l:l + 1, :].partition_broadcast(NCMP))
            ovf = P.sb([128, 32])
            P.ld(ovf[0:NCMP, :], d["nsa_overlap"])
            rhsc = P.sb([128, 98], BF16)
            hs = P.sb([128, 2, 128], BF16)
            hb = P.sb([128, 2])
            kcT = P.sb([128, 128], BF16)
            ph = P.ps([128, 512])
            pk = P.ps([128, 512])
            ptb = P.ps([128, 128], BF16)
            ph2 = P.ps([128, 512])
            phs = [ph, ph2]
            for which in range(2):
                pr = slice(which * 64, which * 64 + 64)
                for l_ in range(32):
                    P.mm(phs[which][:, 0:NCMP], w1[pr, l_, :], cmpT[pr, l_:l_ + 16 * (NCMP - 1) + 1:16],
                         start=(l_ == 0), stop=(l_ == 31))
                for l_ in range(32):
                    P.mm(phs[which][:, 256:257], w1[pr, l_, :], posT[pr, l_:l_ + 1],
                         start=(l_ == 0), stop=(l_ == 31))
            for which in range(2):
                P.I("dve", "tensor_copy", out=hb[:, which:which + 1], in_=phs[which][:, 256:257])
            for which in range(2):
                P.I("act", "activation", out=hs[:, which, 0:NCMP], in_=phs[which][:, 0:NCMP],
                    func=AF.Silu, bias=hb[:, which:which + 1])
            dbg_dump(P, cx, "phk", ph[:, 0:NCMP], [128, NCMP])
            dbg_dump(P, cx, "hb", hb[:], [128, 2])
            dbg_dump(P, cx, "cmpT", cmpT[:], [128, T])
            P.mm(pk[0:NCMP, 0:64], hs[:, 0, 0:NCMP], w2[:, 0:64])
            P.mm(pk[0:NCMP, 64:128], hs[:, 1, 0:NCMP], w2[:, 64:128])
            kc = P.sb([128, 64]); kq = P.sb([128, 64]); kss = P.sb([128, 1]); kcd = P.sb([128, 2, 64], BF16)
            P.I("dve", "tensor_copy", out=kc[0:NCMP, :], in_=pk[0:NCMP, 0:64])
            P.I("pool", "tensor_tensor", out=kq[0:NCMP, :], in0=kc[0:NCMP, :], in1=kc[0:NCMP, :], op=ALU.mult)
            P.I("dve", "tensor_reduce", out=kss[0:NCMP, :], in_=kq[0:NCMP, :], axis=AX.X, op=ALU.add)
            P.I("dve", "tensor_scalar", out=kss[0:NCMP, :], in0=kss[0:NCMP, :], scalar1=1.0 / 64, scalar2=EPS,
                op0=ALU.mult, op1=ALU.add)
            rsqrt_inplace(P, kss[0:NCMP, :])
            P.I("dve", "scalar_tensor_tensor", out=kc[0:NCMP, :], in0=kc[0:NCMP, :], scalar=kss[0:NCMP, 0:1],
                in1=knw0[0:NCMP, :], op0=ALU.mult, op1=ALU.mult)
            P.I("pool", "memset", ap=kcd[:], constant=0.0)
            P.I("dve", "tensor_copy", out=kcd[0:NCMP, 0, :], in_=kc[0:NCMP, :])
            P.I("dve", "tensor_copy", out=kcd[0:NCMP, 1, :], in_=kc[0:NCMP, :])
            P.tr(ptb[:, :], kcd[:].re("p a b -> p (a b)"), cx.ident_bf[:])
            P.I("dve", "tensor_copy", out=kcT[:], in_=ptb[:])
            P.I("pool", "memset", ap=rhsc[:], constant=1.0)
            P.I("dve", "tensor_copy", out=rhsc[0:NCMP, 0:64], in_=pk[0:NCMP, 64:128])
            P.I("pool", "tensor_copy", out=rhsc[0:NCMP, 65:97], in_=ovf[0:NCMP, :])
            dbg_dump(P, cx, "kcT", kcT[:], [128, 128])
            dbg_dump(P, cx, "rhsc", rhsc[:], [128, 98])
            dbg_dump(P, cx, "hs", hs[:, :, 0:NCMP], [128, 2, NCMP])
            dbg_dump(P, cx, "kc", kc[0:NCMP, :], [NCMP, 64])
            chk("nsa_cmpkv")
            bt = [P.sb([128, 512]) for _ in range(2)]
            ssb = [P.sb([128, 512]) for _ in range(2)]
            pTb = [P.sb([128, 512], BF16) for _ in range(2)]
            psS = [P.ps([128, 512]) for _ in range(2)]
            psO = [P.ps([128, 512]) for _ in range(2)]
            rr = P.sb([128, 4, 1]); g0 = P.sb([128, 4, 1]); itmp = P.sb([128, 4, 32])
            n = 0
            for h in range(4):
                hp = slice((h % 2) * 64, (h % 2) * 64 + 64)
                for tg in range(4):
                    sl = slice(tg * 512, (tg + 1) * 512)
                    tl = slice(tg * 4, tg * 4 + 4)
                    b_ = bt[n % 2]; s_ = ssb[n % 2]; p_ = pTb[n % 2]; pS = psS[n % 2]; pO = psO[n % 2]; n += 1
                    pOv = pO[:].re("p (a b) -> p a b", b=128)
                    P.ld(b_[0:NCMP, :], d["nsa_bias_cmp"][h, :, sl])
                    P.mm(pS[0:NCMP, :], kcT[hp, 0:NCMP], qT[hp, h // 2, sl])
                    P.I("dve", "tensor_tensor", out=s_[0:NCMP, :], in0=pS[0:NCMP, :], in1=b_[0:NCMP, :], op=ALU.add)
                    P.I("act", "activation", out=p_[0:NCMP, :], in_=s_[0:NCMP, :], func=AF.Exp)
                    for i4 in range(4):
                        P.mm(pOv[:, i4, 0:97], p_[0:NCMP, i4 * 128:(i4 + 1) * 128], rhsc[0:NCMP, 0:97])
                    P.I("dve", "tensor_scalar", out=rr[:], in0=pOv[:, :, 64:65], scalar1=TINY, scalar2=None, op0=ALU.max)
                    P.I("dve", "reciprocal", out=rr[:], in_=rr[:])
                    P.I("dve", "tensor_tensor", out=g0[:], in0=rr[:], in1=gts[:, tl, h * 3:h * 3 + 1], op=ALU.mult)
                    P.I("dve", "tensor_tensor", out=ynsa[:, tl, h * 64:(h + 1) * 64], in0=pOv[:, :, 0:64],
                        in1=g0[:].bc([128, 4, 64]), op=ALU.mult)
                    if h == 0:
                        P.I("dve", "tensor_tensor", out=impacc[:, tl, :], in0=pOv[:, :, 65:97],
                            in1=rr[:].bc([128, 4, 32]), op=ALU.mult)
                    else:
                        P.I("dve", "tensor_tensor", out=itmp[:], in0=pOv[:, :, 65:97],
                            in1=rr[:].bc([128, 4, 32]), op=ALU.mult)
                        P.I("pool", "tensor_tensor", out=impacc[:, tl, :], in0=impacc[:, tl, :], in1=itmp[:], op=ALU.add)
        chk("nsa_cmp")
        selT = P.sb([32, T], BF16)
        with P.scope():
            stab = P.sb([128, 3, NT, 32])
            P.ld(stab[:], d["nsa_sel_tab"])
            imp2 = P.sb([128, NT, 32])
            P.I("dve", "tensor_tensor", out=imp2[:], in0=impacc[:], in1=stab[:, 0], op=ALU.mult)
            P.I("dve", "tensor_tensor", out=imp2[:], in0=imp2[:], in1=stab[:, 1], op=ALU.add)
            wk = P.sb([128, 32]); m8 = P.sb([128, 8]); selb = P.sb([128, NT, 32], BF16); self32 = P.sb([128, 32])
            pt = P.ps([128, 128], BF16)
            for i in range(NT):
                P.I("dve", "max", out=m8[:], in_=imp2[:, i, :])
                P.I("dve", "match_replace", out=wk[:], in_to_replace=m8[:], in_values=imp2[:, i, :], imm_value=-1e9)
                P.I("dve", "max", out=m8[:], in_=wk[:])
                P.I("dve", "tensor_scalar", out=self32[:], in0=imp2[:, i, :], scalar1=m8[:, 7:8], scalar2=None,
                    op0=ALU.is_ge)
                P.I("dve", "tensor_tensor", out=selb[:, i, :], in0=self32[:], in1=stab[:, 2, i, :], op=ALU.mult)
                P.tr(pt[0:32, :], selb[:, i, :], cx.ident_bf[:])
                P.I("act", "copy", out=selT[:, i * 128:(i + 1) * 128], in_=pt[0:32, :])
        chk("nsa_sel")
        with P.scope():
            Tb = P.sb([128, 4, 2, 128])
            P.ld(Tb[:], d["nsa_bias_tile"])
            c31 = P.sb([128, 4])
            P.ld(c31[:], d["nsa_c31"])
            E = P.sb([32, T], BF16)
            load_cast(P, cx, d["nsa_expand"], E[:], T, parts=32)
            mskall = P.sb([128, 16, 512], BF16)
            mwin = P.sb([128, 8, 512], BF16)
            load_cast(P, cx, d["nsa_mwin"].rearrange("p a b -> p (a b)"), mwin[:].re("p a b -> p (a b)"), 4096)
            psM = P.ps([128, 512])
            psS = [P.ps([128, 512]) for _ in range(2)]
            psA = P.ps([128, 512])
            eb = [P.sb([128, 512]) for _ in range(2)]
            tmpb = [P.sb([128, 128]) for _ in range(2)]
            pmb = [P.sb([128, 512], BF16) for _ in range(2)]
            rr = P.sb([128, 4, 1]); g0 = P.sb([128, 4, 1]); otmp = P.sb([128, 4, 64])
            accv = psA[:].re("p (a b) -> p a b", b=128)
            n = 0
            for br in range(2):
                kT = kslcT if br == 0 else kwinT
                for g in range(4):
                    kt_lo = 0 if br == 0 else max(0, 4 * g - 4)
                    kts = list(range(kt_lo, 4 * g + 4))
                    tl = slice(4 * g, 4 * g + 4)
                    if br == 0:
                        for kt in kts:
                            P.mm(psM[:], E[:, kt * 128:(kt + 1) * 128], selT[:, g * 512:(g + 1) * 512])
                            P.I("act", "copy", out=mskall[:, kt, :], in_=psM[:])
                    for h in range(4):
                        hp = slice((h % 2) * 64, (h % 2) * 64 + 64)
                        for kt in kts:
                            rel = kt - 4 * g
                            c0 = max(rel, 0) * 128
                            pS = psS[n % 2]; e_ = eb[n % 2]; pm = pmb[n % 2]; n += 1
                            P.mm(pS[:, c0:512], kT[hp, kt * 128:(kt + 1) * 128], qT[hp, h // 2, g * 512 + c0:(g + 1) * 512])
                            cc = c0
                            for off in (0, 1):
                                qi = rel + off
                                if 0 <= qi < 4:
                                    tm = tmpb[(n + off) % 2]
                                    cs = slice(qi * 128, (qi + 1) * 128)
                                    P.I("dve", "tensor_tensor", out=tm[:], in0=pS[:, cs], in1=Tb[:, h, off, :], op=ALU.add)
                                    P.I("act", "activation", out=e_[:, cs], in_=tm[:], func=AF.Exp)
                                    cc = (qi + 1) * 128
                            if cc < 512:
                                P.I("act", "activation", out=e_[:, cc:512], in_=pS[:, cc:512], func=AF.Exp,
                                    bias=c31[:, h:h + 1])
                            msk = mskall[:, kt, :] if br == 0 else mwin[:, rel + 4, :]
                            P.I("pool", "tensor_tensor", out=pm[:, c0:512], in0=e_[:, c0:512], in1=msk[:, c0:512], op=ALU.mult)
                            for qi in range(c0 // 128, 4):
                                P.mm(accv[:, qi, 0:65], pm[:, qi * 128:(qi + 1) * 128], vaug[:, kt, br, 0:65],
                                     start=(kt == kts[0] and qi == 0), stop=(kt == kts[-1] and qi == 3))
                        P.I("dve", "tensor_scalar", out=rr[:], in0=accv[:, :, 64:65], scalar1=TINY, scalar2=None, op0=ALU.max)
                        P.I("dve", "reciprocal", out=rr[:], in_=rr[:])
                        P.I("dve", "tensor_tensor", out=g0[:], in0=rr[:], in1=gts[:, tl, h * 3 + 1 + br:h * 3 + 2 + br], op=ALU.mult)
                        P.I("dve", "tensor_tensor", out=otmp[:], in0=accv[:, :, 0:64], in1=g0[:].bc([128, 4, 64]), op=ALU.mult)
                        P.I("pool", "tensor_tensor", out=ynsa[:, tl, h * 64:(h + 1) * 64], in0=ynsa[:, tl, h * 64:(h + 1) * 64],
                            in1=otmp[:], op=ALU.add)
        chk("nsa_attn")
        with P.scope():
            ybT = P.sb([128, 2, T], BF16)
            yb16 = [P.sb([128, 256], BF16) for _ in range(2)]
            pt = [P.ps([128, 2, 128], BF16) for _ in range(2)]
            for i in range(NT):
                P.I("act", "copy", out=yb16[i % 2][:], in_=ynsa[:, i, :])
                for c in range(2):
                    P.tr(pt[i % 2][:, c, :], yb16[i % 2][:, c * 128:(c + 1) * 128], cx.ident_bf[:])
                P.I("dve", "tensor_copy", out=ybT[:, :, i * 128:(i + 1) * 128], in_=pt[i % 2][:])
            if cx.dbg_ybT is not None:
                with P.scope():
                    tmp = P.sb([128, 2, T])
                    P.I("dve", "tensor_copy", out=tmp[:], in_=ybT[:])
                    P.st(cx.dbg_ybT, tmp[:])
            apply_wout(P, cx, l, ybT, 4, 2, xres, d)


def prep_conv(inp, sh):
    L = L_DEPTH
    w_in = np.asarray(inp["w_in"])
    w = np.zeros((L, 128, 8, 512), np.float32)
    for l in range(L):
        w[l] = kchunk(w_in[l][:, 2708:3220])
    sh["w_conv"] = w
    dw = np.asarray(inp["conv_dw_w"], np.float32)
    prm = np.zeros((L, 128, 2, 34), np.float32)
    prm[:, :, :, 0:31] = dw.reshape(L, 31, 2, 128).transpose(0, 3, 2, 1)
    prm[:, :, :, 31] = np.asarray(inp["conv_dw_b"], np.float32).reshape(L, 2, 128).transpose(0, 2, 1)
    prm[:, :, :, 32] = np.asarray(inp["conv_ln_w"], np.float32).reshape(L, 2, 128).transpose(0, 2, 1)
    prm[:, :, :, 33] = np.asarray(inp["conv_ln_b"], np.float32).reshape(L, 2, 128).transpose(0, 2, 1)
    sh["conv_prm"] = prm


def conformer(P, cx, l, hT, xres, d):
    PAD = 32
    with P.scope():
        ycT = P.sb([128, 2, T], BF16)
        hp = P.sb([128, 2, T + PAD], BF16)
        prm = P.sb([128, 2, 34])
        P.ld(prm[:], d["conv_prm"][l])
        P.I("pool", "memset", ap=hp[:, :, 0:PAD], constant=0.0)
        dg = P.sb([128, 2, 31, 128], BF16)
        for j2 in range(2):
            for j in range(31):
                P.I("pool" if j % 2 else "dve", "tensor_scalar", out=dg[:, j2, j, :], in0=cx.ident_f,
                    scalar1=prm[:, j2, j:j + 1], scalar2=None, op0=ALU.mult)
        with P.scope():
            wcv = P.sb([128, 8, 512], BF16)
            load_cast(P, cx, d["w_conv"][l].rearrange("p c n -> p (c n)"), wcv[:].re("p c n -> p (c n)"), 4096)
            pa = [P.ps([128, 512]) for _ in range(2)]
            pg = [P.ps([128, 512]) for _ in range(2)]
            sg = [P.sb([128, 512]) for _ in range(2)]
            n = 0
            for j2 in range(2):
                for tg in range(4):
                    sl = slice(tg * 512, (tg + 1) * 512)
                    a_, g_, s_ = pa[n % 2], pg[n % 2], sg[n % 2]; n += 1
                    for kc in range(8):
                        P.mm(a_[:], wcv[:, kc, j2 * 128:(j2 + 1) * 128], hT[:, kc, sl], start=(kc == 0), stop=(kc == 7))
                    for kc in range(8):
                        P.mm(g_[:], wcv[:, kc, (2 + j2) * 128:(3 + j2) * 128], hT[:, kc, sl], start=(kc == 0), stop=(kc == 7))
                    P.I("act", "activation", out=s_[:], in_=g_[:], func=AF.Sigmoid)
                    P.I("dve", "tensor_tensor", out=hp[:, j2, PAD + tg * 512:PAD + (tg + 1) * 512], in0=a_[:], in1=s_[:],
                        op=ALU.mult)
        with P.scope():
            pc = [P.ps([128, 512]) for _ in range(2)]
            pS1 = P.ps([128, 512]); pS2 = P.ps([128, 512])
            xc = [P.sb([128, 512]) for _ in range(2)]
            sq = [P.sb([128, 512]) for _ in range(2)]
            mu = P.sb([128, 512]); var = P.sb([128, 512]); msq = P.sb([128, 512])
            tmp = [P.sb([128, 512]) for _ in range(2)]
            for tg in range(4):
                for j2 in range(2):
                    for j in range(31):
                        o = PAD - 30 + j + tg * 512
                        P.mm(pc[j2][:], dg[:, j2, j, :], hp[:, j2, o:o + 512], start=(j == 0), stop=(j == 30))
                    P.I("act", "activation", out=xc[j2][:], in_=pc[j2][:], func=AF.Identity, bias=prm[:, j2, 31:32])
                    P.I("pool", "tensor_tensor", out=sq[j2][:], in0=xc[j2][:], in1=xc[j2][:], op=ALU.mult)
                for j2 in range(2):
                    P.mm(pS1[:], cx.ones_f, xc[j2][:], start=(j2 == 0), stop=(j2 == 1))
                for j2 in range(2):
                    P.mm(pS2[:], cx.ones_f, sq[j2][:], start=(j2 == 0), stop=(j2 == 1))
                P.I("dve", "tensor_scalar", out=mu[:], in0=pS1[:], scalar1=1.0 / 256, scalar2=None, op0=ALU.mult)
                P.I("pool", "tensor_tensor", out=msq[:], in0=mu[:], in1=mu[:], op=ALU.mult)
                P.I("dve", "scalar_tensor_tensor", out=var[:], in0=pS2[:], scalar=1.0 / 256, in1=msq[:],
                    op0=ALU.mult, op1=ALU.subtract)
                P.I("dve", "tensor_scalar", out=var[:], in0=var[:], scalar1=EPS, scalar2=None, op0=ALU.add)
                rsqrt_inplace(P, var[:])
                for j2 in range(2):
                    P.I("dve", "tensor_tensor", out=tmp[j2][:], in0=xc[j2][:], in1=mu[:], op=ALU.subtract)
                    P.I("dve", "tensor_tensor", out=tmp[j2][:], in0=tmp[j2][:], in1=var[:], op=ALU.mult)
                    P.I("act", "activation", out=ycT[:, j2, tg * 512:(tg + 1) * 512], in_=tmp[j2][:], func=AF.Silu,
                        scale=prm[:, j2, 32:33], bias=prm[:, j2, 33:34])
        dbg_dump(P, cx, "ycT", ycT[:], [128, 2, T])
        apply_wout(P, cx, l, ycT, 6, 2, xres, d)


NE = 32


def prep_moe(inp, sh):
    L = L_DEPTH
    sh["fnw"] = np.ascontiguousarray(
        np.asarray(inp["ffn_norm_w"], np.float32).reshape(L, 8, 128).transpose(0, 2, 1))[..., None]
    sh["router_w"] = np.stack([kchunk(np.asarray(inp["router_w"][l], np.float32)) for l in range(L)])
    sh["router_b"] = np.ascontiguousarray(np.asarray(inp["router_b"], np.float32))
    wgu = np.asarray(inp["w_gate_up"], np.float32)
    r = wgu.reshape(L, NE, 8, 128, 2, 8, 128)
    sh["w_gu"] = np.ascontiguousarray(r.transpose(0, 1, 5, 3, 4, 2, 6)).reshape(L, NE, 8, 128, 2048)
    sh["w_dn_moe"] = np.ascontiguousarray(np.asarray(inp["w_down"], np.float32))
    bgu = np.asarray(inp["b_gate_up"], np.float32)
    sh["b_gu"] = np.ascontiguousarray(bgu.reshape(L, NE, 16, 128).transpose(0, 1, 3, 2))
    sh["b_dn"] = np.ascontiguousarray(np.asarray(inp["b_down"], np.float32))


def moe(P, cx, l, xres, d, nexp=NE):
    with P.scope():
        xnT = P.sb([128, 8, T], BF16)
        rw = P.sb([128, NT, NE])
        with P.scope():
            nw = P.sb([128, 8, 1])
            P.ld(nw[:], d["fnw"][l])
            rwt = P.sb([128, 8, NE])
            P.ld(rwt[:], d["router_w"][l])
            rb = P.sb([128, NE])
            P.ld(rb[:], d["router_b"][l:l + 1, :].partition_broadcast(128))
            xn = [P.sb([128, D]) for _ in range(2)]
            ss = [P.sb([128, 1]) for _ in range(2)]
            xT32 = [P.sb([128, 8, 128]) for _ in range(2)]
            ptr = [P.ps([128, 4, 128]) for _ in range(2)]
            pl = P.ps([128, 512])
            lg = P.sb([128, NE]); m8 = P.sb([128, 8]); nmx = P.sb([128, 1]); ex = P.sb([128, NE])
            msk = P.sb([128, NE]); sm = P.sb([128, 1])
            Bd = P.sb([NE, D])
            P.ld(Bd[:], d["b_dn"][l])
            rwT = [P.sb([NE, 128]) for _ in range(2)]
            ptw = P.ps([128, 512])
            pb = [P.ps([128, 512]) for _ in range(2)]
            for i in range(NT):
                x_, s_, xt = xn[i % 2], ss[i % 2], xT32[i % 2]
                P.I("act", "activation", out=x_[:], in_=xres[i][:], func=AF.Square, accum_out=s_[:])
                P.I("dve", "tensor_scalar", out=s_[:], in0=s_[:], scalar1=1.0 / D, scalar2=EPS, op0=ALU.mult, op1=ALU.add)
                rsqrt_inplace(P, s_[:])
                P.I("dve", "tensor_scalar", out=x_[:], in0=xres[i][:], scalar1=s_[:, 0:1], scalar2=None, op0=ALU.mult)
                for hf in range(2):
                    for c4 in range(4):
                        c = hf * 4 + c4
                        P.tr(ptr[hf][:, c4, :], x_[:, c * 128:(c + 1) * 128], cx.ident_f)
                    P.I("dve", "tensor_tensor", out=xt[:, hf * 4:hf * 4 + 4, :], in0=ptr[hf][:],
                        in1=nw[:, hf * 4:hf * 4 + 4, :].bc([128, 4, 128]), op=ALU.mult)
                P.I("act", "copy", out=xnT[:, :, i * 128:(i + 1) * 128], in_=xt[:])
                for kc in range(8):
                    P.mm(pl[:, 0:NE], xt[:, kc, :], rwt[:, kc, :], start=(kc == 0), stop=(kc == 7))
                P.I("dve", "tensor_tensor", out=lg[:], in0=pl[:, 0:NE], in1=rb[:], op=ALU.add)
                P.I("dve", "max", out=m8[:], in_=lg[:])
                P.I("dve", "tensor_scalar", out=nmx[:], in0=m8[:, 0:1], scalar1=-1.0, scalar2=None, op0=ALU.mult)
                P.I("act", "activation", out=ex[:], in_=lg[:], func=AF.Exp, bias=nmx[:, 0:1])
                P.I("dve", "tensor_scalar", out=msk[:], in0=lg[:], scalar1=m8[:, 3:4], scalar2=None, op0=ALU.is_ge)
                P.I("dve", "tensor_tensor", out=ex[:], in0=ex[:], in1=msk[:], op=ALU.mult)
                P.I("dve", "tensor_reduce", out=sm[:], in_=ex[:], axis=AX.X, op=ALU.add)
                P.I("dve", "reciprocal", out=sm[:], in_=sm[:])
                P.I("dve", "tensor_scalar", out=rw[:, i, :], in0=ex[:], scalar1=sm[:, 0:1], scalar2=None, op0=ALU.mult)
                P.tr(ptw[0:NE, 0:128], rw[:, i, :], cx.ident_f)
                P.I("act", "copy", out=rwT[i % 2][:], in_=ptw[0:NE, 0:128])
                for half in range(2):
                    hs_ = slice(half * 512, (half + 1) * 512)
                    P.mm(pb[half][:], rwT[i % 2][:], Bd[:, hs_])
                    P.I("dve", "tensor_tensor", out=xres[i][:, hs_], in0=pb[half][:], in1=xres[i][:, hs_], op=ALU.add)
        dbg_dump(P, cx, "rw", rw[:], [128, NT, NE])
        chk("moe_router")
        with P.scope():
            actT = P.sbl(8, [128, T], BF16, name="actT%d" % l)
            sgu = [P.sb([128, 2048]) for _ in range(2)]
            wgub = [P.sb([128, 2, 8, 128], BF16) for _ in range(2)]
            sdn = [P.sb([128, D])]
            wdb = P.sbl(8, [128, D], BF16, name="wdb%d" % l)
            bgu = [P.sb([128, 16]) for _ in range(2)]
            bg2 = [P.sb([128, 16]) for _ in range(2)]
            Sb = [P.sb([128, 512]) for _ in range(2)]
            ub = [P.sb([128, 512]) for _ in range(2)]
            pg = [P.ps([128, 512]) for _ in range(2)]
            pu = [P.ps([128, 512]) for _ in range(2)]
            po = [P.ps([128, 512]) for _ in range(2)]
            units = [(e, j) for e in range(nexp) for j in range(8)]
            CS = 1.702 * 7.0 / (1.0 + math.exp(-1.702 * 7.0))
            rwk = P.sb([128, NT, NE])
            P.I("dve", "tensor_scalar", out=rwk[:], in0=rw[:], scalar1=1.0 / 1.702, scalar2=None, op0=ALU.mult)

            def dma_gu(u):
                e, j = units[u]
                P.ld(sgu[u % 2][:], d["w_gu"][l, e, j])

            def cast_gu(u):
                P.I("pool", "tensor_copy", out=wgub[u % 2][:].re("p a c f -> p (a c f)"), in_=sgu[u % 2][:])

            dma_gu(0)
            cast_gu(0)
            n = 0
            no = 0
            for u, (e, j) in enumerate(units):
                if j == 0:
                    P.ld(bgu[e % 2][:], d["b_gu"][l, e])
                    P.I("dve", "tensor_scalar", out=bg2[e % 2][:, 0:8], in0=bgu[e % 2][:, 0:8], scalar1=1.702,
                        scalar2=None, op0=ALU.mult)
                    P.I("dve", "tensor_scalar", out=bg2[e % 2][:, 8:16], in0=bgu[e % 2][:, 8:16], scalar1=1.0,
                        scalar2=None, op0=ALU.add)
                if u + 1 < len(units):
                    dma_gu(u + 1)
                P.ld(sdn[0][:], d["w_dn_moe"][l, e, j * 128:(j + 1) * 128, :])
                wb = wgub[u % 2]
                for tg in range(4):
                    sl = slice(tg * 512, (tg + 1) * 512)
                    g_, u_ = pg[n % 2], pu[n % 2]
                    S_, ub_ = Sb[n % 2], ub[n % 2]
                    n += 1
                    for kc in range(8):
                        P.mm(g_[:], wb[:, 0, kc, :], xnT[:, kc, sl], start=(kc == 0), stop=(kc == 7))
                    for kc in range(8):
                        P.mm(u_[:], wb[:, 1, kc, :], xnT[:, kc, sl], start=(kc == 0), stop=(kc == 7))
                    P.I("act", "activation", out=S_[:], in_=g_[:], func=AF.Silu, scale=1.702, bias=bg2[e % 2][:, j:j + 1])
                    P.I("act", "activation", out=ub_[:], in_=u_[:], func=AF.Identity, bias=bg2[e % 2][:, 8 + j:9 + j])
                    P.I("dve", "tensor_scalar", out=ub_[:], in0=ub_[:], scalar1=-6.0, scalar2=8.0, op0=ALU.max, op1=ALU.min)
                    P.I("dve", "scalar_tensor_tensor", out=actT[j][:, sl], in0=S_[:], scalar=CS, in1=ub_[:],
                        op0=ALU.min, op1=ALU.mult)
                    if tg == 1 and u + 1 < len(units):
                        cast_gu(u + 1)
                    if tg == 3:
                        P.I("pool", "tensor_copy", out=wdb[j][:], in_=sdn[0][:])
                if j == 7:
                    for i in range(NT):
                        for half in range(2):
                            o_ = po[no % 2]; no += 1
                            hs_ = slice(half * 512, (half + 1) * 512)
                            for jj in range(8):
                                P.mm(o_[:], actT[jj][:, i * 128:(i + 1) * 128], wdb[jj][:, hs_], start=(jj == 0), stop=(jj == 7))
                            P.I("dve", "scalar_tensor_tensor", out=xres[i][:, hs_], in0=o_[:], scalar=rwk[:, i, e:e + 1],
                                in1=xres[i][:, hs_], op0=ALU.mult, op1=ALU.add)


_NC_CACHE = {}


def kernel(**inputs):
    x = np.asarray(inputs["x"], np.float32)
    sh = prep_shared(inputs)
    if "nc" not in _NC_CACHE:
        _NC_CACHE["nc"] = build_nc()
    nc = _NC_CACHE["nc"]
    in_maps = []
    for c in range(8):
        m = dict(sh)
        m["x"] = np.ascontiguousarray(x[c])
        in_maps.append(m)
    res = run_bass_kernel_spmd(nc, in_maps, core_ids=list(range(8)))
    return np.stack([np.asarray(r["out"], np.float32) for r in res.results], axis=0)
```

```python
import contextlib
import math
import numpy as np
import concourse.bass as bass
import concourse.mybir as mybir
from concourse.bass_utils import run_bass_kernel_spmd

F32 = mybir.dt.float32
BF16 = mybir.dt.bfloat16
ALU = mybir.AluOpType
AF = mybir.ActivationFunctionType
AX = mybir.AxisListType


class View:
    __slots__ = ("b", "ap")

    def __init__(self, b, ap):
        self.b = b
        self.ap = ap

    def __getitem__(self, k):
        return View(self.b, self.ap[k])

    def bc(self, shape):
        return View(self.b, self.ap.to_broadcast(list(shape)))

    def re(self, pat, **kw):
        return View(self.b, self.ap.rearrange(pat, **kw))

    def bitcast(self, dt):
        return View(self.b, self.ap.bitcast(dt))


class Buf:
    __slots__ = ("t", "w", "r", "name", "psum")

    def __init__(self, t, name, psum=False):
        self.t = t
        self.name = name
        self.w = None
        self.r = {}
        self.psum = psum

    def __getitem__(self, k):
        return View(self, self.t[k])


OUTK = ("out", "accum_out", "ap")
SAME_ENGINE_SYNC = [True]


class Prog:
    NDMA = 16

    def __init__(self, nc, stack):
        self.nc = nc
        self.stack = stack
        self.root_stack = stack
        self.engs = {"pe": nc.tensor, "dve": nc.vector, "act": nc.scalar,
                     "pool": nc.gpsimd, "sp": nc.sync}
        self.sem = {}
        self.cnt = {}
        self.seen = {}
        for k in self.engs:
            self.sem[k] = stack.enter_context(nc.semaphore("s_" + k))
            self.cnt[k] = 0
            self.seen[k] = {}
        for i in range(self.NDMA):
            k = "d%d" % i
            self.sem[k] = stack.enter_context(nc.semaphore("s_" + k))
            self.cnt[k] = 0
        self.dma_rr = 0
        self.nbuf = 0
        self.allbufs = []
        self.epoch = 0

    def new_epoch(self):
        if DEAD[0]:
            return
        self.barrier()
        self.epoch += 1
        for k in list(self.sem):
            self.sem[k] = self.root_stack.enter_context(self.nc.semaphore("s%d_%s" % (self.epoch, k)))
            self.cnt[k] = 0
        for k in self.seen:
            self.seen[k] = {}
        for b in self.allbufs:
            b.w = None
            b.r = {}

    def sb(self, shape, dtype=F32, name=None):
        self.nbuf += 1
        name = name or ("b%d" % self.nbuf)
        t = self.stack.enter_context(self.nc.sbuf_tensor(name, list(shape), dtype))
        assert self.nc.sbuf_bytes_remaining >= 16640, ("SBUF budget", name, self.nc.sbuf_bytes_remaining)
        b = Buf(t, name)
        self.allbufs.append(b)
        return b

    def ps(self, shape, dtype=F32, name=None):
        self.nbuf += 1
        name = name or ("p%d" % self.nbuf)
        t = self.stack.enter_context(self.nc.psum_tensor(name, list(shape), dtype))
        b = Buf(t, name, psum=True)
        self.allbufs.append(b)
        return b

    def _waits(self, ek, reads, writes):
        needs = {}
        for b in reads:
            if b.w is not None:
                k, c = b.w
                if needs.get(k, 0) < c:
                    needs[k] = c
            if b.psum:
                for k, c in b.r.items():
                    if k != ek and needs.get(k, 0) < c:
                        needs[k] = c
        for b in writes:
            if b.w is not None:
                k, c = b.w
                if needs.get(k, 0) < c:
                    needs[k] = c
            for k, c in b.r.items():
                if needs.get(k, 0) < c:
                    needs[k] = c
        eng = self.engs[ek]
        seen = self.seen[ek]
        for k, c in needs.items():
            if k == ek and (ek == "pe" or not SAME_ENGINE_SYNC[0]):
                continue
            if seen.get(k, 0) >= c:
                continue
            eng.wait_ge(self.sem[k], c)
            seen[k] = c

    def op(self, ek, reads, writes, fn):
        if DEAD[0]:
            return None
        self._waits(ek, reads, writes)
        ins = fn(self.engs[ek])
        self.cnt[ek] += 1
        c = self.cnt[ek]
        ins.then_inc(self.sem[ek], 1)
        for b in reads:
            b.r[ek] = c
        for b in writes:
            b.w = (ek, c)
            b.r = {}
        return ins

    def I(self, ek, meth, **kw):
        reads, writes, args = [], [], {}
        for k, v in kw.items():
            if isinstance(v, View):
                (writes if k in OUTK else reads).append(v.b)
                args[k] = v.ap
            else:
                args[k] = v
        return self.op(ek, reads, writes, lambda e: getattr(e, meth)(**args))

    def mm(self, out, lhsT, rhs, start=True, stop=True):
        return self.op("pe", [lhsT.b, rhs.b], [out.b],
                       lambda e: e.matmul(out=out.ap, lhsT=lhsT.ap, rhs=rhs.ap, start=start, stop=stop))

    def tr(self, out, in_, ident):
        return self.op("pe", [in_.b, ident.b], [out.b],
                       lambda e: e.transpose(out=out.ap, in_=in_.ap, identity=ident.ap))

    def ld(self, out, in_ap, q="sp", **kw):
        return self.dma(out.ap, in_ap, [], [out.b], q=q, **kw)

    def st(self, out_ap, in_, q="sp", **kw):
        return self.dma(out_ap, in_.ap, [in_.b], [], q=q, **kw)

    def barrier(self):
        for ek, eng in self.engs.items():
            for k, c in self.cnt.items():
                if k == ek or c == 0:
                    continue
                if self.seen[ek].get(k, 0) < c:
                    eng.wait_ge(self.sem[k], c)
                    self.seen[ek][k] = c

    @contextlib.contextmanager
    def scope(self):
        old = self.stack
        with contextlib.ExitStack() as st:
            self.stack = st
            try:
                yield
            finally:
                self.barrier()
                self.stack = old

    def sbl(self, n, shape, dtype=F32, name=None):
        self.nbuf += 1
        name = name or ("b%d" % self.nbuf)
        full = [shape[0], n] + list(shape[1:])
        t = self.stack.enter_context(self.nc.sbuf_tensor(name, full, dtype))
        assert self.nc.sbuf_bytes_remaining >= 16640, ("SBUF budget", name, self.nc.sbuf_bytes_remaining)
        out = [Buf(t[:, i], "%s_%d" % (name, i)) for i in range(n)]
        self.allbufs.extend(out)
        return out

    def psl(self, n, parts, width, name=None):
        per = 512 // width
        out = []
        while len(out) < n:
            bank = self.ps([128, 512])
            for i in range(per):
                if len(out) < n:
                    out.append(bank[0:parts, i * width:(i + 1) * width])
        return out

    def dma(self, out_ap, in_ap, reads, writes, q="sp", **kw):
        if DEAD[0]:
            return None
        self._waits(q, reads, writes)
        dk = "d%d" % self.dma_rr
        self.dma_rr = (self.dma_rr + 1) % self.NDMA
        if self.cnt[dk] > 0 and self.seen[q].get(dk, 0) < self.cnt[dk]:
            self.engs[q].wait_ge(self.sem[dk], self.cnt[dk])
            self.seen[q][dk] = self.cnt[dk]
        ins = self.engs[q].dma_start(out=out_ap, in_=in_ap, **kw)
        self.cnt[dk] += 16
        c = self.cnt[dk]
        ins.then_inc(self.sem[dk], 16)
        for b in reads:
            b.r[dk] = c
        for b in writes:
            b.w = (dk, c)
            b.r = {}
        return ins

    def finish(self, bufs):
        self._waits("sp", bufs, [])
        for k in list(self.sem):
            if k.startswith("d") and self.cnt[k] > 0:
                if self.seen["sp"].get(k, 0) < self.cnt[k]:
                    self.engs["sp"].wait_ge(self.sem[k], self.cnt[k])
                    self.seen["sp"][k] = self.cnt[k]


T = 2048
D = 1024
NT = T // 128
L_DEPTH = 2
EPS = 1e-6
C = 64
NCH = T // C
NE = 32


def make_consts():
    c = {}
    c["ident"] = np.eye(128, dtype=np.float32)
    c["ones"] = np.ones((128, 128), np.float32)
    k = np.arange(64)
    tri = (k[:, None] <= k[None, :]).astype(np.float32)
    tril = (k[None, :] <= k[:, None]).astype(np.float32)
    t64 = np.zeros((128, 128), np.float32)
    t64[:64, :64] = tri
    t64[:64, 64:] = tril
    c["tri"] = t64
    bd = np.zeros((128, 128), np.float32)
    bd[:64, :64] = 1.0
    bd[64:, 64:] = 1.0
    kk = np.arange(128)
    sut = (kk[:, None] < kk[None, :]).astype(np.float32)
    io = np.broadcast_to(np.arange(128, dtype=np.float32)[None, :], (128, 128))
    return np.concatenate([c["ident"], c["ones"], c["tri"], bd, sut, io], axis=1)


class Ctx:
    pass


class StopBuild(Exception):
    pass


STOP = [None]


def chk(k):
    if STOP[0] == k:
        DEAD[0] = True


DEAD = [False]


def dbg_dump(P, cx, name, view, shape):
    if name not in cx.dout:
        return
    with P.scope():
        tmp = P.sb(list(shape))
        P.I("dve", "tensor_copy", out=tmp[:], in_=view)
        P.st(cx.dout[name], tmp[:])


def load_cast(P, cx, dram_ap, dst, n, parts=128):
    STG = cx.stg_n
    o = 0
    while o < n:
        m = min(STG, n - o)
        sb = cx.stg[cx.stg_i % len(cx.stg)]
        cx.stg_i += 1
        P.ld(sb[0:parts, 0:m], dram_ap[:, o:o + m])
        P.I("act", "copy", out=dst[:, o:o + m], in_=sb[0:parts, 0:m])
        o += m


def rsqrt_inplace(P, v):
    P.I("act", "sqrt", out=v, in_=v)
    P.I("dve", "reciprocal", out=v, in_=v)


def norm_to_T(P, cx, xres, nw, hT):
    for i in range(NT):
        ss = cx.nrm_ss[i % 2]
        xn = cx.nrm_xn[i % 2]
        junk = xn
        pt = cx.nrm_pt[i % 2]
        P.I("act", "activation", out=junk[:], in_=xres[i][:], func=AF.Square, accum_out=ss[:])
        P.I("dve", "tensor_scalar", out=ss[:], in0=ss[:], scalar1=1.0 / D, scalar2=EPS,
            op0=ALU.mult, op1=ALU.add)
        rsqrt_inplace(P, ss[:])
        P.I("dve", "tensor_scalar", out=xn[:], in0=xres[i][:], scalar1=ss[:, 0:1], scalar2=None,
            op0=ALU.mult)
        for c in range(8):
            P.tr(pt[:, c, :], xn[:, c * 128:(c + 1) * 128], cx.ident_bf[:])
        P.I("dve", "tensor_tensor", out=hT[:, :, i * 128:(i + 1) * 128], in0=pt[:],
            in1=nw.bc([128, 8, 128]), op=ALU.mult)


def deltanet(P, cx, l, hT, xres_unused, yaT, d):
    ident = cx.ident_f
    tri = cx.tri
    tril = cx.tril
    ones = cx.ones_f
    with P.scope():
        gab = P.sb([64, NCH, 8])
        wtm = P.sb([128, 8, 20], BF16)
        load_cast(P, cx, d["w_tm"][l].rearrange("p c n -> p (c n)"), wtm[:].re("p c n -> p (c n)"), 160)
        with P.scope():
            pg = P.ps([64, 8, 8])
            for c0 in range(0, NCH, 8):
                for cc in range(8):
                    c = c0 + cc
                    for kc in range(8):
                        P.mm(pg[:, cc, :], hT[:, kc, c * 64:(c + 1) * 64], wtm[:, kc, 0:8],
                             start=(kc == 0), stop=(kc == 7))
                P.I("act", "copy", out=gab[:, c0:c0 + 8, :], in_=pg[:])
        chk("gab")
        alog = P.sb([64, 1, 4]); dtb = P.sb([64, 1, 4])
        P.ld(alog[:, 0, :], d["dn_alog"][l:l + 1, :].partition_broadcast(64))
        P.ld(dtb[:, 0, :], d["dn_dtb"][l:l + 1, :].partition_broadcast(64))
        nA = P.sb([64, 1, 4])
        P.I("act", "activation", out=nA[:], in_=alog[:], func=AF.Exp)
        xa = P.sb([64, NCH, 4]); t1 = P.sb([64, NCH, 4]); g = P.sb([64, NCH, 4])
        beta = P.sb([64, NCH, 4])
        P.I("dve", "tensor_tensor", out=xa[:], in0=gab[:, :, 0:4], in1=dtb[:].bc([64, NCH, 4]), op=ALU.add)
        P.I("dve", "scalar_tensor_tensor", out=t1[:], in0=xa[:], scalar=-1.0, in1=xa[:],
            op0=ALU.mult, op1=ALU.max)
        P.I("act", "activation", out=t1[:], in_=t1[:], func=AF.Exp, scale=-1.0)
        P.I("act", "activation", out=t1[:], in_=t1[:], func=AF.Ln, bias=1.0)
        P.I("dve", "scalar_tensor_tensor", out=g[:], in0=xa[:], scalar=0.0, in1=t1[:],
            op0=ALU.max, op1=ALU.add)
        P.I("dve", "tensor_tensor", out=g[:], in0=g[:], in1=nA[:].bc([64, NCH, 4]), op=ALU.mult)
        P.I("dve", "tensor_scalar", out=g[:], in0=g[:], scalar1=-1.0, scalar2=None, op0=ALU.mult)
        P.I("act", "activation", out=beta[:], in_=gab[:, :, 4:8], func=AF.Sigmoid)
        gc = P.sb([64, NCH, 4]); eg = P.sb([64, NCH, 4]); ekd = P.sb([64, NCH, 4])
        bk = P.sb([64, NCH, 4]); egl = P.sb([128, NCH, 4]); gl = P.sb([64, NCH, 4])
        with P.scope():
            pc = P.ps([128, NCH * 4])
            P.mm(pc[0:64, :], tri, g[:].re("p c h -> p (c h)"))
            P.I("dve", "tensor_copy", out=gc[:].re("p c h -> p (c h)"), in_=pc[0:64, :])
            P.I("act", "activation", out=eg[:].re("p c h -> p (c h)"), in_=pc[0:64, :], func=AF.Exp)
            P.mm(pc[:, :], ones[0:64, :], g[:].re("p c h -> p (c h)"))
            P.I("act", "activation", out=egl[:].re("p c h -> p (c h)"), in_=pc[:, :], func=AF.Exp)
            P.I("dve", "tensor_tensor", out=gl[:].re("p c h -> p (c h)"), in0=pc[0:64, :],
                in1=gc[:].re("p c h -> p (c h)"), op=ALU.subtract)
            P.I("act", "activation", out=ekd[:], in_=gl[:], func=AF.Exp)
            P.I("dve", "tensor_tensor", out=bk[:], in0=beta[:], in1=eg[:], op=ALU.mult)
        chk("gates")
        cw = P.sb([128, 12, 4])
        P.ld(cw[:], d["dn_cw"][l])
        dnw = P.sb([128, 1])
        P.ld(dnw[:], d["dn_nw"][l])

        for h in range(4):
            with P.scope():
                deltanet_head(P, cx, l, h, hT, yaT, d, dict(
                    g=g, gc=gc, eg=eg, ekd=ekd, bk=bk, egl=egl, beta=beta, cw=cw, dnw=dnw))


def deltanet_head(P, cx, l, h, hT, yaT, d, G):
    ident = cx.ident_f
    tri, tril, ones = cx.tri, cx.tril, cx.ones_f
    id64 = cx.ident_f[0:64, 0:64]
    szT = P.sb([128, T], BF16)
    qkv = [P.sb([128, T], name="qkv%d_%d_%d" % (l, h, i)) for i in range(3)]
    pp = [P.ps([128, 512]) for _ in range(2)]
    n = 0
    with P.scope():
        wdn = P.sb([128, 8, 512], BF16)
        load_cast(P, cx, d["w_dn"][l, h].rearrange("p c n -> p (c n)"), wdn[:].re("p c n -> p (c n)"), 4096)
        raw = P.sb([128, 3, T + 4], BF16)
        P.I("pool", "memset", ap=raw[:, :, 0:3], constant=0.0)
        for which in range(4):
            for tg in range(4):
                ps = pp[n % 2]; n += 1
                for kc in range(8):
                    P.mm(ps[:], wdn[:, kc, which * 128:(which + 1) * 128], hT[:, kc, tg * 512:(tg + 1) * 512],
                         start=(kc == 0), stop=(kc == 7))
                if which < 3:
                    P.I("act", "copy", out=raw[:, which, 3 + tg * 512:3 + (tg + 1) * 512], in_=ps[:])
                else:
                    P.I("act", "activation", out=szT[:, tg * 512:(tg + 1) * 512], in_=ps[:], func=AF.Silu)
        chk("proj")
        cw = G["cw"]
        for which in range(3):
            acc = qkv[which]
            ci = h * 3 + which
            P.I("dve", "tensor_scalar", out=acc[:], in0=raw[:, which, 0:T], scalar1=cw[:, ci, 0:1],
                scalar2=None, op0=ALU.mult)
            for j in range(1, 4):
                P.I("dve", "scalar_tensor_tensor", out=acc[:], in0=raw[:, which, j:j + T],
                    scalar=cw[:, ci, j:j + 1], in1=acc[:], op0=ALU.mult, op1=ALU.add)
            P.I("act", "activation", out=acc[:], in_=acc[:], func=AF.Silu)
    chk("conv")
    with P.scope():
        sq = P.sb([128, 512]); rn = P.sb([128, 512])
        for which in range(2):
            for tg in range(4):
                sl = slice(tg * 512, (tg + 1) * 512)
                ps = pp[n % 2]; n += 1
                P.I("act", "activation", out=sq[:], in_=qkv[which][:, sl], func=AF.Square)
                P.mm(ps[:], ones, sq[:])
                P.I("dve", "tensor_scalar", out=rn[:], in0=ps[:], scalar1=EPS, scalar2=None, op0=ALU.add)
                rsqrt_inplace(P, rn[:])
                P.I("dve", "scalar_tensor_tensor", out=qkv[which][:, sl], in0=qkv[which][:, sl],
                    scalar=(128.0 ** -0.5 if which == 0 else 1.0), in1=rn[:], op0=ALU.mult, op1=ALU.mult)
    chk("l2")
    qT, kT, vT = qkv
    ktok = [P.sb([64, 4, 128]) for _ in range(2)]
    vtok = [P.sb([64, 4, 128]) for _ in range(2)]
    S = [P.sb([128, 128], name="S%d_%d_%d" % (l, h, i)) for i in range(2)]
    P.I("pool", "memset", ap=S[0][:], constant=0.0)
    oall = [P.sb([64, 4, 128]) for _ in range(2)]
    osq = P.sb([64, 4, 128]); ssq = P.sb([64, 4])
    GS = 4
    sets = []
    for s in range(GS):
        st = Ctx()
        st.pa = P.psl(3, 64, 128)
        st.dec = P.sb([64, 64]); st.decT = P.sb([64, 64]); st.AT = P.sb([64, 64])
        st.Lp = [P.sb([64, 64]) for _ in range(2)]; st.Up = [P.sb([64, 64]) for _ in range(2)]
        st.Pm = [P.sb([64, 64]) for _ in range(2)]
        st.kbg = P.sb([64, 128]); st.vb = P.sb([64, 128])
        st.wT = P.sb([128, 64]); st.u = P.sb([64, 128]); st.kd = P.sb([64, 128])
        sets.append(st)
    psc = P.psl(4, 128, 128)
    vnew = [P.sb([64, 128]) for _ in range(2)]
    o1 = [P.sb([64, 128]) for _ in range(2)]
    g, gc, eg, ekd, bk, egl, beta = (G[k] for k in ("g", "gc", "eg", "ekd", "bk", "egl", "beta"))

    def col(buf, c):
        return buf[:, c, h:h + 1]

    for c0 in range(0, NCH, GS):
        grp = list(range(c0, c0 + GS))

        def each(fn):
            for c in grp:
                fn(c, sets[c - c0])

        c4 = c0 // 4
        kt, vt, oa = ktok[c4 % 2], vtok[c4 % 2], oall[c4 % 2]
        for src, dst in ((kT, kt), (vT, vt)):
            ps = pp[n % 2]; n += 1
            for cc in range(4):
                c = c0 + cc
                P.tr(ps[0:64, cc * 128:(cc + 1) * 128], src[:, c * 64:(c + 1) * 64], ident[:])
            P.I("act", "copy", out=dst[:].re("p c d -> p (c d)"), in_=ps[0:64, :])

        chk("ktr")
        each(lambda c, s: P.mm(s.pa[0][:, 0:64], g[:, c, h:h + 1].bc([64, 64]), tri))
        each(lambda c, s: P.mm(s.pa[1][:, 0:64], kT[:, c * 64:(c + 1) * 64], kT[:, c * 64:(c + 1) * 64]))
        each(lambda c, s: P.mm(s.pa[1][:, 64:128], kT[:, c * 64:(c + 1) * 64], qT[:, c * 64:(c + 1) * 64]))
        each(lambda c, s: P.I("dve", "tensor_scalar", out=s.dec[:], in0=s.pa[0][:, 0:64], scalar1=col(gc, c),
                              scalar2=0.0, op0=ALU.subtract, op1=ALU.max))
        each(lambda c, s: P.I("dve", "tensor_scalar", out=s.decT[:], in0=s.pa[0][:, 0:64], scalar1=col(gc, c),
                              scalar2=0.0, op0=ALU.subtract, op1=ALU.min))
        each(lambda c, s: P.I("act", "activation", out=s.dec[:], in_=s.dec[:], func=AF.Exp, scale=-1.0))
        each(lambda c, s: P.I("act", "activation", out=s.decT[:], in_=s.decT[:], func=AF.Exp))
        each(lambda c, s: P.I("dve", "tensor_tensor", out=s.dec[:], in0=s.dec[:], in1=tril, op=ALU.mult))
        each(lambda c, s: P.I("dve", "tensor_tensor", out=s.decT[:], in0=s.decT[:], in1=tri, op=ALU.mult))
        each(lambda c, s: P.I("dve", "tensor_tensor", out=s.dec[:], in0=s.dec[:], in1=id64, op=ALU.subtract))
        each(lambda c, s: P.I("dve", "scalar_tensor_tensor", out=s.Lp[0][:], in0=s.pa[1][:, 0:64],
                              scalar=col(beta, c), in1=s.dec[:], op0=ALU.mult, op1=ALU.mult))
        each(lambda c, s: P.I("dve", "tensor_tensor", out=s.AT[:], in0=s.pa[1][:, 64:128], in1=s.decT[:],
                              op=ALU.mult))
        chk("dec")
        each(lambda c, s: P.mm(s.pa[2][:, 0:64], s.Lp[0][:], id64))
        chk("utr1")
        each(lambda c, s: P.I("act", "copy", out=s.Up[0][:], in_=s.pa[2][:, 0:64]))
        chk("utr2")
        each(lambda c, s: P.I("dve", "tensor_tensor", out=s.Pm[0][:], in0=id64, in1=s.Up[0][:],
                              op=ALU.subtract))
        chk("utr")
        for it in range(5):
            a, b = it % 2, (it + 1) % 2
            last = it == 4
            each(lambda c, s: P.mm(s.pa[0][:, 0:64], s.Up[a][:], s.Lp[a][:]))
            if not last:
                each(lambda c, s: P.mm(s.pa[0][:, 64:128], s.Lp[a][:], s.Up[a][:]))
            if it == 0: chk("i0a")
            each(lambda c, s: P.I("dve", "tensor_copy", out=s.Lp[b][:], in_=s.pa[0][:, 0:64]))
            if not last:
                each(lambda c, s: P.I("dve", "tensor_copy", out=s.Up[b][:], in_=s.pa[0][:, 64:128]))
            if it == 0: chk("i0b")
            each(lambda c, s: P.mm(s.pa[2][:, 0:64], s.Lp[b][:], s.Pm[a][:]))
            if it == 0: chk("i0c")
            each(lambda c, s: P.I("dve", "tensor_tensor", out=s.Pm[b][:], in0=s.pa[2][:, 0:64], in1=s.Pm[a][:],
                                  op=ALU.add))
            if it == 0: chk("i0d")
        chk("inv")
        PT = 1
        each(lambda c, s: P.I("dve", "tensor_scalar", out=s.kbg[:], in0=kt[:, c % 4, :],
                              scalar1=col(bk, c), scalar2=None, op0=ALU.mult))
        each(lambda c, s: P.I("dve", "tensor_scalar", out=s.vb[:], in0=vt[:, c % 4, :],
                              scalar1=col(beta, c), scalar2=None, op0=ALU.mult))
        each(lambda c, s: P.I("dve", "tensor_scalar", out=s.kd[:], in0=kt[:, c % 4, :],
                              scalar1=col(ekd, c), scalar2=None, op0=ALU.mult))
        each(lambda c, s: P.mm(s.pa[0][:, :], s.Pm[PT][:], s.vb[:]))
        each(lambda c, s: P.I("act", "copy", out=s.u[:], in_=s.pa[0][:, :]))
        for c in grp:
            s = sets[c - c0]
            pw = psc[3]
            P.mm(pw[:, 0:64], s.kbg[:], s.Pm[PT][:])
            P.I("dve", "tensor_copy", out=s.wT[:], in_=pw[:, 0:64])
        chk("wu")
        for c in grp:
            s = sets[c - c0]
            Sa, Sb = S[c % 2], S[(c + 1) % 2]
            vn = vnew[c % 2]; oo = o1[c % 2]
            P.mm(psc[0][0:64, :], s.wT[:], Sa[:])
            P.mm(psc[1][0:64, :], qT[:, c * 64:(c + 1) * 64], Sa[:])
            P.I("dve", "tensor_tensor", out=vn[:], in0=s.u[:], in1=psc[0][0:64, :], op=ALU.subtract)
            P.mm(psc[2][0:64, :], s.AT[:], vn[:])
            P.mm(psc[0][:, :], s.kd[:], vn[:])
            P.I("dve", "scalar_tensor_tensor", out=Sb[:], in0=Sa[:], scalar=egl[:, c, h:h + 1],
                in1=psc[0][:, :], op0=ALU.mult, op1=ALU.add)
            P.I("act", "copy", out=oo[:], in_=psc[2][0:64, :])
            P.I("dve", "scalar_tensor_tensor", out=oa[:, c % 4, :], in0=psc[1][0:64, :],
                scalar=col(eg, c), in1=oo[:], op0=ALU.mult, op1=ALU.add)
        chk("scan")
        P.I("dve", "tensor_tensor", out=osq[:], in0=oa[:], in1=oa[:], op=ALU.mult)
        P.I("dve", "tensor_reduce", out=ssq[:], in_=osq[:], axis=AX.X, op=ALU.add)
        P.I("dve", "tensor_scalar", out=ssq[:], in0=ssq[:], scalar1=1.0 / 128, scalar2=EPS,
            op0=ALU.mult, op1=ALU.add)
        rsqrt_inplace(P, ssq[:])
        P.I("dve", "tensor_tensor", out=oa[:], in0=oa[:],
            in1=ssq[:].re("p (c o) -> p c o", o=1).bc([64, 4, 128]), op=ALU.mult)
        ps = pp[n % 2]; n += 1
        for cc in range(4):
            P.tr(ps[:, cc * 64:(cc + 1) * 64], oa[:, cc, :], id64)
        P.I("dve", "scalar_tensor_tensor", out=yaT[:, h, c4 * 256:(c4 + 1) * 256], in0=ps[:, 0:256],
            scalar=G["dnw"][:, 0:1], in1=szT[:, c4 * 256:(c4 + 1) * 256], op0=ALU.mult, op1=ALU.mult)


IN_OFF = np.cumsum([0, 512, 512, 512, 512, 4, 4, 256, 384, 12, 512])


def kchunk(w):
    n = w.shape[1]
    return np.ascontiguousarray(w.reshape(8, 128, n).transpose(1, 0, 2))


def prep_shared(inp):
    f = lambda a: np.ascontiguousarray(np.asarray(a, dtype=np.float32))
    L = L_DEPTH
    sh = {}
    sh["consts"] = make_consts()
    sh["anw"] = f(np.asarray(inp["attn_norm_w"]).reshape(L, 8, 128).transpose(0, 2, 1))[..., None]
    w_in = np.asarray(inp["w_in"])
    w_dn = np.zeros((L, 4, 128, 8, 512), np.float32)
    w_tm = np.zeros((L, 128, 8, 20), np.float32)
    for l in range(L):
        for h in range(4):
            cols = np.concatenate([np.arange(o + h * 128, o + (h + 1) * 128) for o in (0, 512, 1024, 1536)])
            w_dn[l, h] = kchunk(w_in[l][:, cols])
        cols = np.concatenate([np.arange(2048, 2056), np.arange(2696, 2708)])
        w_tm[l] = kchunk(w_in[l][:, cols])
    sh["w_dn"] = w_dn
    sh["w_tm"] = w_tm
    cw = np.asarray(inp["dn_conv_w"])
    sh["dn_cw"] = f(cw.reshape(L, 4, 3, 4, 128).transpose(0, 4, 3, 2, 1).reshape(L, 128, 12, 4))
    sh["dn_alog"] = f(inp["dn_a_log"])
    sh["dn_dtb"] = f(inp["dn_dt_bias"])
    sh["dn_nw"] = f(np.asarray(inp["dn_norm_w"]).reshape(L, 128, 1))
    sh["w_out"] = f(inp["w_out"])
    prep_nsa(inp, sh)
    prep_conv(inp, sh)
    prep_moe(inp, sh)
    return sh


def build_nc(nlayers=L_DEPTH, stages=("dn", "nsa", "conv", "moe"), dbg=(), nexp=NE):
    nc = bass.Bass("TRN2", target_bir_lowering=False)
    d = {}

    def inp(name, shape):
        d[name] = nc.dram_tensor(name, list(shape), F32, kind="ExternalInput").ap()

    inp("x", [T, D]); inp("consts", [128, 768]); inp("anw", [2, 128, 8, 1])
    inp("w_out", [2, D, D])
    inp("w_nsa_fm", [2, 128, 8, 640]); inp("w_nsa_tm", [2, 128, 8, 128]); inp("nsa_nw", [2, 128, 3])
    inp("nsa_knw0", [2, 64]); inp("nsa_posT", [2, 128, 32]); inp("nsa_w1", [2, 128, 32, 128])
    inp("nsa_w2", [2, 128, 128]); inp("nsa_bias_cmp", [4, NCMP, T]); inp("nsa_bias_tile", [128, 4, 2, 128])
    inp("nsa_c31", [128, 4]); inp("nsa_sel_tab", [128, 3, NT, 32]); inp("nsa_expand", [32, T])
    inp("nsa_overlap", [NCMP, 32]); inp("nsa_mwin", [128, 8, 512])
    inp("w_conv", [2, 128, 8, 512]); inp("conv_prm", [2, 128, 2, 34])
    inp("fnw_row", [2, D]); inp("fnw", [2, 128, 8, 1]); inp("router_w", [2, 128, 8, NE]); inp("router_b", [2, NE])
    inp("w_gu", [2, NE, 8, 128, 2048]); inp("w_dn_moe", [2, NE, D, D]); inp("b_gu", [2, NE, 128, 16])
    inp("b_dn", [2, NE, D])
    inp("w_dn", [2, 4, 128, 8, 512]); inp("w_tm", [2, 128, 8, 20]); inp("dn_cw", [2, 128, 12, 4])
    inp("dn_alog", [2, 4]); inp("dn_dtb", [2, 4]); inp("dn_nw", [2, 128, 1])
    out = nc.dram_tensor("out", [T, D], F32, kind="ExternalOutput").ap()
    dout = {}
    for name, shape in dbg:
        dout[name] = nc.dram_tensor(name, list(shape), F32, kind="ExternalOutput").ap()

    with contextlib.ExitStack() as st:
        P = Prog(nc, st)
        cx = Ctx()
        consts = P.sb([128, 768])
        P.ld(consts[:], d["consts"])
        cx.ident_f = consts[:, 0:128]
        cx.ones_f = consts[:, 128:256]
        cx.tri = consts[0:64, 256:320]
        cx.tril = consts[0:64, 320:384]
        cx.bd_ones = consts[:, 384:512]
        cx.sut = consts[:, 512:640]
        cx.iota = consts[:, 640:768]
        cx.dbg_ybT = dout.get("ybT")
        cx.dout = dout
        cx.nexp = nexp
        identb = P.sb([128, 128], BF16)
        P.I("dve", "tensor_copy", out=identb[:], in_=consts[:, 0:128])
        cx.ident_bf = identb
        cx.stg_n = 512
        cx.stg_i = 0
        cx.nrm_ss = [P.sb([128, 1]) for _ in range(2)]
        xres = P.sbl(NT, [128, D], name="xres")
        for i in range(NT):
            P.ld(xres[i][:], d["x"][i * 128:(i + 1) * 128, :])
        for l in range(nlayers):
          try:
            if l > 0:
                P.new_epoch()
            with P.scope():
                cx.stg = [P.sb([128, cx.stg_n]) for _ in range(2)]
                hT = P.sb([128, 8, T], BF16)
                nw = P.sb([128, 8, 1])
                P.ld(nw[:], d["anw"][l])
                with P.scope():
                    cx.nrm_pt = [P.ps([128, 8, 128], BF16) for _ in range(2)]
                    cx.nrm_xn = [P.sb([128, D], BF16) for _ in range(2)]
                    norm_to_T(P, cx, xres, nw[:], hT)
                chk("norm")
                if "dn" in stages:
                    with P.scope():
                        yaT = P.sb([128, 4, T], BF16)
                        deltanet(P, cx, l, hT, xres, yaT, d)
                        if "yaT" in dout:
                            with P.scope():
                                tmp = P.sb([128, 4, T])
                                P.I("dve", "tensor_copy", out=tmp[:], in_=yaT[:])
                                P.st(dout["yaT"], tmp[:])
                        apply_wout(P, cx, l, yaT, 0, 4, xres, d)
                if "nsa" in stages:
                    nsa(P, cx, l, hT, xres, d)
                if "conv" in stages:
                    conformer(P, cx, l, hT, xres, d)
            if "moe" in stages:
                P.new_epoch()
                (moe_sparse if MOE_SPARSE[0] else moe)(P, cx, l, xres, d, nexp=cx.nexp)
          except StopBuild:
            break
        DEAD[0] = False
        for i in range(NT):
            P.st(out[i * 128:(i + 1) * 128, :], xres[i][:])
        P.finish([])
    return nc


NEGM = -200.0
NCMP = 127


def t5_bucket_np(dist):
    n = np.maximum(dist, 0)
    nf = np.maximum(n, 1).astype(np.float32)
    large = 16 + (np.log(nf / np.float32(16)) / np.float32(math.log(8.0)) * np.float32(16)).astype(np.int32)
    large = np.minimum(large, 31)
    return np.where(n < 16, n, large)


def nsa_tables(rel_bias):
    rb = np.asarray(rel_bias, np.float32)
    tb = {}
    t = np.arange(T)
    j = np.arange(NCMP)
    dist = t[None, :] - (j[:, None] * 16 + 31)
    bk = t5_bucket_np(dist)
    bc = rb[bk]
    bc = np.where((dist >= 0)[..., None], bc, np.float32(NEGM))
    tb["bias_cmp"] = np.ascontiguousarray(bc.transpose(2, 0, 1)).astype(np.float32)
    k = np.arange(128)
    tt = np.arange(128)
    d0 = tt[None, :] - k[:, None]
    d1 = d0 + 128
    b0 = np.where((d0 >= 0)[..., None], rb[t5_bucket_np(d0)], np.float32(NEGM))
    b1 = rb[t5_bucket_np(d1)]
    tbl = np.stack([b0, b1], axis=0)
    tb["bias_tile"] = np.ascontiguousarray(tbl.transpose(1, 3, 0, 2)).astype(np.float32)
    tb["c31"] = np.ascontiguousarray(np.broadcast_to(rb[31][None, :], (128, 4))).astype(np.float32)
    tok = (np.arange(NT)[None, :] * 128 + np.arange(128)[:, None])
    cur = tok // 64
    s = np.arange(32)[None, None, :]
    causal = s <= cur[..., None]
    forced = (s == 0) | (s == cur[..., None]) | (s == cur[..., None] - 1)
    m1 = (causal & ~forced).astype(np.float32)
    cst = np.where(causal & forced, 1e4, np.where(causal, 0.0, -1.0)).astype(np.float32)
    tb["sel_tab"] = np.ascontiguousarray(np.stack([m1, cst, causal.astype(np.float32)], axis=1))
    key = np.arange(T)
    tb["expand"] = (key[None, :] // 64 == np.arange(32)[:, None]).astype(np.float32)
    cs = j * 16
    ss = np.arange(32) * 64
    ov = ((cs[:, None] < ss[None, :] + 64) & (cs[:, None] + 32 > ss[None, :])).astype(np.float32)
    tb["overlap"] = ov
    mw = np.zeros((128, 8, 512), np.float32)
    for r in range(8):
        rel = r - 4
        keyp = rel * 128 + k[:, None]
        tp = np.arange(512)[None, :]
        dd = tp - keyp
        mw[:, r, :] = ((dd >= 0) & (dd < 512)).astype(np.float32)
    tb["mwin"] = mw
    return tb


def prep_nsa(inp, sh):
    L = L_DEPTH
    w_in = np.asarray(inp["w_in"])
    w_fm = np.zeros((L, 128, 8, 640), np.float32)
    w_tmv = np.zeros((L, 128, 8, 128), np.float32)
    for l in range(L):
        q0 = 2056
        kv0 = 2312
        cols = np.concatenate([
            np.arange(q0, q0 + 256),
            np.arange(kv0 + 128, kv0 + 192), np.arange(kv0 + 128, kv0 + 192),
            np.arange(kv0 + 256, kv0 + 320), np.arange(kv0 + 256, kv0 + 320),
            np.arange(kv0, kv0 + 128),
        ])
        w_fm[l] = kchunk(w_in[l][:, cols])
        cols = np.concatenate([np.arange(kv0 + 192, kv0 + 256), np.arange(kv0 + 320, kv0 + 384)])
        w_tmv[l] = kchunk(w_in[l][:, cols])
    sh["w_nsa_fm"] = w_fm
    sh["w_nsa_tm"] = w_tmv
    qn = np.asarray(inp["nsa_q_norm_w"], np.float32)
    kn = np.asarray(inp["nsa_k_norm_w"], np.float32)
    nw = np.zeros((L, 128, 3), np.float32)
    nw[:, :, 0] = np.concatenate([qn, qn], axis=1)
    nw[:, :, 1] = np.concatenate([kn[:, 1], kn[:, 1]], axis=1)
    nw[:, :, 2] = np.concatenate([kn[:, 2], kn[:, 2]], axis=1)
    sh["nsa_nw"] = nw
    sh["nsa_knw0"] = np.ascontiguousarray(kn[:, 0, :])
    pos = np.asarray(inp["nsa_cmp_pos"], np.float32)
    sh["nsa_posT"] = np.ascontiguousarray(pos.transpose(0, 1, 3, 2).reshape(L, 128, 32))
    w1 = np.asarray(inp["nsa_cmp_w1"], np.float32)
    sh["nsa_w1"] = np.ascontiguousarray(w1.reshape(L, 2, 32, 64, 128).transpose(0, 1, 3, 2, 4).reshape(L, 128, 32, 128))
    w2 = np.asarray(inp["nsa_cmp_w2"], np.float32)
    sh["nsa_w2"] = np.ascontiguousarray(w2.transpose(0, 2, 1, 3).reshape(L, 128, 128))
    tb = nsa_tables(inp["rel_bias"])
    for k_, v_ in tb.items():
        sh["nsa_" + k_] = v_


def apply_wout(P, cx, l, yT, chunk0, nch, xres, d):
    with P.scope():
        w = P.sb([128, nch, D], BF16)
        for c in range(nch):
            load_cast(P, cx, d["w_out"][l, (chunk0 + c) * 128:(chunk0 + c + 1) * 128, :], w[:, c, :], D)
        pp = [P.ps([128, 512]) for _ in range(2)]
        n = 0
        for i in range(NT):
            for half in range(2):
                ps = pp[n % 2]; n += 1
                for c in range(nch):
                    P.mm(ps[:], yT[:, c, i * 128:(i + 1) * 128], w[:, c, half * 512:(half + 1) * 512],
                         start=(c == 0), stop=(c == nch - 1))
                P.I("dve", "tensor_tensor", out=xres[i][:, half * 512:(half + 1) * 512], in0=ps[:],
                    in1=xres[i][:, half * 512:(half + 1) * 512], op=ALU.add)


def nsa(P, cx, l, hT, xres, d):
    TINY = 1e-30
    with P.scope():
        qT = P.sb([128, 2, T], BF16)
        kslcT = P.sb([128, T], BF16)
        kwinT = P.sb([128, T], BF16)
        cmpT = P.sb([128, T], BF16)
        vaug = P.sb([128, NT, 2, 66], BF16)
        gts = P.sb([128, NT, 12])
        ynsa = P.sb([128, NT, 256])
        impacc = P.sb([128, NT, 32])
        nw = P.sb([128, 3])
        P.ld(nw[:], d["nsa_nw"][l])
        qw = P.sb([128, 1])
        P.I("dve", "tensor_scalar", out=qw[:], in0=nw[:, 0:1], scalar1=0.125, scalar2=None, op0=ALU.mult)
        P.I("pool", "memset", ap=vaug[:, :, :, 64:66], constant=1.0)
        with P.scope():
            wfm = P.sb([128, 8, 640], BF16)
            load_cast(P, cx, d["w_nsa_fm"][l].rearrange("p c n -> p (c n)"), wfm[:].re("p c n -> p (c n)"), 8 * 640)
            wtv = P.sb([128, 8, 128], BF16)
            load_cast(P, cx, d["w_nsa_tm"][l].rearrange("p c n -> p (c n)"), wtv[:].re("p c n -> p (c n)"), 8 * 128)
            wtm = P.sb([128, 8, 20], BF16)
            load_cast(P, cx, d["w_tm"][l].rearrange("p c n -> p (c n)"), wtm[:].re("p c n -> p (c n)"), 160)
            pp = [P.ps([128, 512]) for _ in range(2)]
            pq = [P.ps([128, 512]) for _ in range(2)]
            qs = [P.sb([128, 512]) for _ in range(2)]
            sq = P.sb([128, 512]); rn = P.sb([128, 512])
            n = 0
            for ch in range(5):
                for tg in range(4):
                    sl = slice(tg * 512, (tg + 1) * 512)
                    ps = pp[n % 2]; ps2 = pq[n % 2]; qsb = qs[n % 2]; n += 1
                    for kc in range(8):
                        P.mm(ps[:], wfm[:, kc, ch * 128:(ch + 1) * 128], hT[:, kc, sl], start=(kc == 0), stop=(kc == 7))
                    if ch == 4:
                        P.I("act", "copy", out=cmpT[:, sl], in_=ps[:])
                        continue
                    P.I("act", "copy", out=qsb[:], in_=ps[:])
                    P.I("act", "activation", out=sq[:], in_=qsb[:], func=AF.Square)
                    P.mm(ps2[:], cx.bd_ones, sq[:])
                    P.I("dve", "tensor_scalar", out=rn[:], in0=ps2[:], scalar1=1.0 / 64, scalar2=EPS,
                        op0=ALU.mult, op1=ALU.add)
                    rsqrt_inplace(P, rn[:])
                    if ch < 2:
                        dst, wc = qT[:, ch, sl], qw[:, 0:1]
                    elif ch == 2:
                        dst, wc = kslcT[:, sl], nw[:, 1:2]
                    else:
                        dst, wc = kwinT[:, sl], nw[:, 2:3]
                    P.I("dve", "scalar_tensor_tensor", out=dst, in0=qsb[:], scalar=wc, in1=rn[:],
                        op0=ALU.mult, op1=ALU.mult)
            for i in range(NT):
                ps = pp[n % 2]; n += 1
                for kc in range(8):
                    P.mm(ps[:, 0:128], hT[:, kc, i * 128:(i + 1) * 128], wtv[:, kc, :], start=(kc == 0), stop=(kc == 7))
                for kc in range(8):
                    P.mm(ps[:, 128:140], hT[:, kc, i * 128:(i + 1) * 128], wtm[:, kc, 8:20], start=(kc == 0), stop=(kc == 7))
                P.I("act", "copy", out=vaug[:, i, :, 0:64], in_=ps[:, 0:128].re("p (a b) -> p a b", b=64))
                P.I("act", "activation", out=gts[:, i, :], in_=ps[:, 128:140], func=AF.Sigmoid)
        chk("nsa_proj")
        with P.scope():
            w1 = P.sb([128, 32, 128], BF16)
            load_cast(P, cx, d["nsa_w1"][l].rearrange("p c n -> p (c n)"), w1[:].re("p c n -> p (c n)"), 4096)
            w2 = P.sb([128, 128], BF16)
            load_cast(P, cx, d["nsa_w2"][l], w2[:], 128)
            posT = P.sb([128, 32], BF16)
            load_cast(P, cx, d["nsa_posT"][l], posT[:], 32)
            knw0 = P.sb([128, 64])
            P.ld(knw0[0:NCMP, :], d["nsa_knw0"][l:l + 1, :].partition_broadcast(NCMP))
            ovf = P.sb([128, 32])
            P.ld(ovf[0:NCMP, :], d["nsa_overlap"])
            rhsc = P.sb([128, 98], BF16)
            hs = P.sb([128, 2, 128], BF16)
            hb = P.sb([128, 2])
            kcT = P.sb([128, 128], BF16)
            ph = P.ps([128, 512])
            pk = P.ps([128, 512])
            ptb = P.ps([128, 128], BF16)
            ph2 = P.ps([128, 512])
            phs = [ph, ph2]
            for which in range(2):
                pr = slice(which * 64, which * 64 + 64)
                for l_ in range(32):
                    P.mm(phs[which][:, 0:NCMP], w1[pr, l_, :], cmpT[pr, l_:l_ + 16 * (NCMP - 1) + 1:16],
                         start=(l_ == 0), stop=(l_ == 31))
                for l_ in range(32):
                    P.mm(phs[which][:, 256:257], w1[pr, l_, :], posT[pr, l_:l_ + 1],
                         start=(l_ == 0), stop=(l_ == 31))
            for which in range(2):
                P.I("dve", "tensor_copy", out=hb[:, which:which + 1], in_=phs[which][:, 256:257])
            for which in range(2):
                P.I("act", "activation", out=hs[:, which, 0:NCMP], in_=phs[which][:, 0:NCMP],
                    func=AF.Silu, bias=hb[:, which:which + 1])
            dbg_dump(P, cx, "phk", ph[:, 0:NCMP], [128, NCMP])
            dbg_dump(P, cx, "hb", hb[:], [128, 2])
            dbg_dump(P, cx, "cmpT", cmpT[:], [128, T])
            P.mm(pk[0:NCMP, 0:64], hs[:, 0, 0:NCMP], w2[:, 0:64])
            P.mm(pk[0:NCMP, 64:128], hs[:, 1, 0:NCMP], w2[:, 64:128])
            kc = P.sb([128, 64]); kq = P.sb([128, 64]); kss = P.sb([128, 1]); kcd = P.sb([128, 2, 64], BF16)
            P.I("dve", "tensor_copy", out=kc[0:NCMP, :], in_=pk[0:NCMP, 0:64])
            P.I("dve", "tensor_tensor", out=kq[0:NCMP, :], in0=kc[0:NCMP, :], in1=kc[0:NCMP, :], op=ALU.mult)
            P.I("dve", "tensor_reduce", out=kss[0:NCMP, :], in_=kq[0:NCMP, :], axis=AX.X, op=ALU.add)
            P.I("dve", "tensor_scalar", out=kss[0:NCMP, :], in0=kss[0:NCMP, :], scalar1=1.0 / 64, scalar2=EPS,
                op0=ALU.mult, op1=ALU.add)
            rsqrt_inplace(P, kss[0:NCMP, :])
            P.I("dve", "scalar_tensor_tensor", out=kc[0:NCMP, :], in0=kc[0:NCMP, :], scalar=kss[0:NCMP, 0:1],
                in1=knw0[0:NCMP, :], op0=ALU.mult, op1=ALU.mult)
            P.I("pool", "memset", ap=kcd[:], constant=0.0)
            P.I("dve", "tensor_copy", out=kcd[0:NCMP, 0, :], in_=kc[0:NCMP, :])
            P.I("dve", "tensor_copy", out=kcd[0:NCMP, 1, :], in_=kc[0:NCMP, :])
            P.tr(ptb[:, :], kcd[:].re("p a b -> p (a b)"), cx.ident_bf[:])
            P.I("dve", "tensor_copy", out=kcT[:], in_=ptb[:])
            P.I("pool", "memset", ap=rhsc[:], constant=1.0)
            P.I("dve", "tensor_copy", out=rhsc[0:NCMP, 0:64], in_=pk[0:NCMP, 64:128])
            P.I("dve", "tensor_copy", out=rhsc[0:NCMP, 65:97], in_=ovf[0:NCMP, :])
            dbg_dump(P, cx, "kcT", kcT[:], [128, 128])
            dbg_dump(P, cx, "rhsc", rhsc[:], [128, 98])
            dbg_dump(P, cx, "hs", hs[:, :, 0:NCMP], [128, 2, NCMP])
            dbg_dump(P, cx, "kc", kc[0:NCMP, :], [NCMP, 64])
            chk("nsa_cmpkv")
            bt = [P.sb([128, 512]) for _ in range(2)]
            ssb = [P.sb([128, 512]) for _ in range(2)]
            pTb = [P.sb([128, 512], BF16) for _ in range(2)]
            psS = [P.ps([128, 512]) for _ in range(2)]
            psO = [P.ps([128, 512]) for _ in range(2)]
            rr = P.sb([128, 4, 1]); g0 = P.sb([128, 4, 1]); itmp = P.sb([128, 4, 32])
            n = 0
            for h in range(4):
                hp = slice((h % 2) * 64, (h % 2) * 64 + 64)
                for tg in range(4):
                    sl = slice(tg * 512, (tg + 1) * 512)
                    tl = slice(tg * 4, tg * 4 + 4)
                    b_ = bt[n % 2]; s_ = ssb[n % 2]; p_ = pTb[n % 2]; pS = psS[n % 2]; pO = psO[n % 2]; n += 1
                    pOv = pO[:].re("p (a b) -> p a b", b=128)
                    P.ld(b_[0:NCMP, :], d["nsa_bias_cmp"][h, :, sl])
                    P.mm(pS[0:NCMP, :], kcT[hp, 0:NCMP], qT[hp, h // 2, sl])
                    P.I("dve", "tensor_tensor", out=s_[0:NCMP, :], in0=pS[0:NCMP, :], in1=b_[0:NCMP, :], op=ALU.add)
                    P.I("act", "activation", out=p_[0:NCMP, :], in_=s_[0:NCMP, :], func=AF.Exp)
                    for i4 in range(4):
                        P.mm(pOv[:, i4, 0:97], p_[0:NCMP, i4 * 128:(i4 + 1) * 128], rhsc[0:NCMP, 0:97])
                    P.I("dve", "tensor_scalar", out=rr[:], in0=pOv[:, :, 64:65], scalar1=TINY, scalar2=None, op0=ALU.max)
                    P.I("dve", "reciprocal", out=rr[:], in_=rr[:])
                    P.I("dve", "tensor_tensor", out=g0[:], in0=rr[:], in1=gts[:, tl, h * 3:h * 3 + 1], op=ALU.mult)
                    P.I("dve", "tensor_tensor", out=ynsa[:, tl, h * 64:(h + 1) * 64], in0=pOv[:, :, 0:64],
                        in1=g0[:].bc([128, 4, 64]), op=ALU.mult)
                    if h == 0:
                        P.I("dve", "tensor_tensor", out=impacc[:, tl, :], in0=pOv[:, :, 65:97],
                            in1=rr[:].bc([128, 4, 32]), op=ALU.mult)
                    else:
                        P.I("dve", "tensor_tensor", out=itmp[:], in0=pOv[:, :, 65:97],
                            in1=rr[:].bc([128, 4, 32]), op=ALU.mult)
                        P.I("dve", "tensor_tensor", out=impacc[:, tl, :], in0=impacc[:, tl, :], in1=itmp[:], op=ALU.add)
        chk("nsa_cmp")
        selT = P.sb([32, T], BF16)
        with P.scope():
            stab = P.sb([128, 3, NT, 32])
            P.ld(stab[:], d["nsa_sel_tab"])
            imp2 = P.sb([128, NT, 32])
            P.I("dve", "tensor_tensor", out=imp2[:], in0=impacc[:], in1=stab[:, 0], op=ALU.mult)
            P.I("dve", "tensor_tensor", out=imp2[:], in0=imp2[:], in1=stab[:, 1], op=ALU.add)
            wk = P.sb([128, 32]); m8 = P.sb([128, 8]); selb = P.sb([128, NT, 32], BF16); self32 = P.sb([128, 32])
            pt = P.ps([128, 128], BF16)
            for i in range(NT):
                P.I("dve", "max", out=m8[:], in_=imp2[:, i, :])
                P.I("dve", "match_replace", out=wk[:], in_to_replace=m8[:], in_values=imp2[:, i, :], imm_value=-1e9)
                P.I("dve", "max", out=m8[:], in_=wk[:])
                P.I("dve", "tensor_scalar", out=self32[:], in0=imp2[:, i, :], scalar1=m8[:, 7:8], scalar2=None,
                    op0=ALU.is_ge)
                P.I("dve", "tensor_tensor", out=selb[:, i, :], in0=self32[:], in1=stab[:, 2, i, :], op=ALU.mult)
                P.tr(pt[0:32, :], selb[:, i, :], cx.ident_bf[:])
                P.I("act", "copy", out=selT[:, i * 128:(i + 1) * 128], in_=pt[0:32, :])
        chk("nsa_sel")
        with P.scope():
            Tb = P.sb([128, 4, 2, 128])
            P.ld(Tb[:], d["nsa_bias_tile"])
            c31 = P.sb([128, 4])
            P.ld(c31[:], d["nsa_c31"])
            E = P.sb([32, T], BF16)
            load_cast(P, cx, d["nsa_expand"], E[:], T, parts=32)
            mskall = P.sb([128, 16, 512], BF16)
            mwin = P.sb([128, 8, 512], BF16)
            load_cast(P, cx, d["nsa_mwin"].rearrange("p a b -> p (a b)"), mwin[:].re("p a b -> p (a b)"), 4096)
            psM = P.ps([128, 512])
            psS = [P.ps([128, 512]) for _ in range(2)]
            psA = P.ps([128, 512])
            eb = [P.sb([128, 512], BF16) for _ in range(2)]
            tmpb = [P.sb([128, 128]) for _ in range(2)]
            pmb = [P.sb([128, 512], BF16) for _ in range(2)]
            rr = P.sb([128, 4, 1]); g0 = P.sb([128, 4, 1]); otmp = P.sb([128, 4, 64])
            accv = psA[:].re("p (a b) -> p a b", b=128)
            n = 0
            for br in range(2):
                kT = kslcT if br == 0 else kwinT
                for g in range(4):
                    kt_lo = 0 if br == 0 else max(0, 4 * g - 4)
                    kts = list(range(kt_lo, 4 * g + 4))
                    tl = slice(4 * g, 4 * g + 4)
                    if br == 0:
                        for kt in kts:
                            P.mm(psM[:], E[:, kt * 128:(kt + 1) * 128], selT[:, g * 512:(g + 1) * 512])
                            P.I("act", "copy", out=mskall[:, kt, :], in_=psM[:])
                    for h in range(4):
                        hp = slice((h % 2) * 64, (h % 2) * 64 + 64)
                        for kt in kts:
                            rel = kt - 4 * g
                            c0 = max(rel, 0) * 128
                            pS = psS[n % 2]; e_ = eb[n % 2]; pm = pmb[n % 2]; n += 1
                            P.mm(pS[:, c0:512], kT[hp, kt * 128:(kt + 1) * 128], qT[hp, h // 2, g * 512 + c0:(g + 1) * 512])
                            cc = c0
                            for off in (0, 1):
                                qi = rel + off
                                if 0 <= qi < 4:
                                    tm = tmpb[(n + off) % 2]
                                    cs = slice(qi * 128, (qi + 1) * 128)
                                    P.I("dve", "tensor_tensor", out=tm[:], in0=pS[:, cs], in1=Tb[:, h, off, :], op=ALU.add)
                                    P.I("act", "activation", out=e_[:, cs], in_=tm[:], func=AF.Exp)
                                    cc = (qi + 1) * 128
                            if cc < 512:
                                P.I("act", "activation", out=e_[:, cc:512], in_=pS[:, cc:512], func=AF.Exp,
                                    bias=c31[:, h:h + 1])
                            msk = mskall[:, kt, :] if br == 0 else mwin[:, rel + 4, :]
                            P.I("dve", "tensor_tensor", out=pm[:, c0:512], in0=e_[:, c0:512], in1=msk[:, c0:512], op=ALU.mult)
                            for qi in range(c0 // 128, 4):
                                P.mm(accv[:, qi, 0:65], pm[:, qi * 128:(qi + 1) * 128], vaug[:, kt, br, 0:65],
                                     start=(kt == kts[0] and qi == 0), stop=(kt == kts[-1] and qi == 3))
                        P.I("dve", "tensor_scalar", out=rr[:], in0=accv[:, :, 64:65], scalar1=TINY, scalar2=None, op0=ALU.max)
                        P.I("dve", "reciprocal", out=rr[:], in_=rr[:])
                        P.I("dve", "tensor_tensor", out=g0[:], in0=rr[:], in1=gts[:, tl, h * 3 + 1 + br:h * 3 + 2 + br], op=ALU.mult)
                        P.I("dve", "tensor_tensor", out=otmp[:], in0=accv[:, :, 0:64], in1=g0[:].bc([128, 4, 64]), op=ALU.mult)
                        P.I("dve", "tensor_tensor", out=ynsa[:, tl, h * 64:(h + 1) * 64], in0=ynsa[:, tl, h * 64:(h + 1) * 64],
                            in1=otmp[:], op=ALU.add)
        chk("nsa_attn")
        with P.scope():
            ybT = P.sb([128, 2, T], BF16)
            yb16 = [P.sb([128, 256], BF16) for _ in range(2)]
            pt = [P.ps([128, 2, 128], BF16) for _ in range(2)]
            for i in range(NT):
                P.I("act", "copy", out=yb16[i % 2][:], in_=ynsa[:, i, :])
                for c in range(2):
                    P.tr(pt[i % 2][:, c, :], yb16[i % 2][:, c * 128:(c + 1) * 128], cx.ident_bf[:])
                P.I("dve", "tensor_copy", out=ybT[:, :, i * 128:(i + 1) * 128], in_=pt[i % 2][:])
            if cx.dbg_ybT is not None:
                with P.scope():
                    tmp = P.sb([128, 2, T])
                    P.I("dve", "tensor_copy", out=tmp[:], in_=ybT[:])
                    P.st(cx.dbg_ybT, tmp[:])
            apply_wout(P, cx, l, ybT, 4, 2, xres, d)


def prep_conv(inp, sh):
    L = L_DEPTH
    w_in = np.asarray(inp["w_in"])
    w = np.zeros((L, 128, 8, 512), np.float32)
    for l in range(L):
        w[l] = kchunk(w_in[l][:, 2708:3220])
    sh["w_conv"] = w
    dw = np.asarray(inp["conv_dw_w"], np.float32)
    prm = np.zeros((L, 128, 2, 34), np.float32)
    prm[:, :, :, 0:31] = dw.reshape(L, 31, 2, 128).transpose(0, 3, 2, 1)
    prm[:, :, :, 31] = np.asarray(inp["conv_dw_b"], np.float32).reshape(L, 2, 128).transpose(0, 2, 1)
    prm[:, :, :, 32] = np.asarray(inp["conv_ln_w"], np.float32).reshape(L, 2, 128).transpose(0, 2, 1)
    prm[:, :, :, 33] = np.asarray(inp["conv_ln_b"], np.float32).reshape(L, 2, 128).transpose(0, 2, 1)
    sh["conv_prm"] = prm


def conformer(P, cx, l, hT, xres, d):
    PAD = 32
    with P.scope():
        ycT = P.sb([128, 2, T], BF16)
        hp = P.sb([128, 2, T + PAD], BF16)
        prm = P.sb([128, 2, 34])
        P.ld(prm[:], d["conv_prm"][l])
        P.I("pool", "memset", ap=hp[:, :, 0:PAD], constant=0.0)
        dg = P.sb([128, 2, 31, 128], BF16)
        for j2 in range(2):
            for j in range(31):
                P.I("dve", "tensor_scalar", out=dg[:, j2, j, :], in0=cx.ident_f,
                    scalar1=prm[:, j2, j:j + 1], scalar2=None, op0=ALU.mult)
        with P.scope():
            wcv = P.sb([128, 8, 512], BF16)
            load_cast(P, cx, d["w_conv"][l].rearrange("p c n -> p (c n)"), wcv[:].re("p c n -> p (c n)"), 4096)
            pa = [P.ps([128, 512]) for _ in range(2)]
            pg = [P.ps([128, 512]) for _ in range(2)]
            sg = [P.sb([128, 512]) for _ in range(2)]
            n = 0
            for j2 in range(2):
                for tg in range(4):
                    sl = slice(tg * 512, (tg + 1) * 512)
                    a_, g_, s_ = pa[n % 2], pg[n % 2], sg[n % 2]; n += 1
                    for kc in range(8):
                        P.mm(a_[:], wcv[:, kc, j2 * 128:(j2 + 1) * 128], hT[:, kc, sl], start=(kc == 0), stop=(kc == 7))
                    for kc in range(8):
                        P.mm(g_[:], wcv[:, kc, (2 + j2) * 128:(3 + j2) * 128], hT[:, kc, sl], start=(kc == 0), stop=(kc == 7))
                    P.I("act", "activation", out=s_[:], in_=g_[:], func=AF.Sigmoid)
                    P.I("dve", "tensor_tensor", out=hp[:, j2, PAD + tg * 512:PAD + (tg + 1) * 512], in0=a_[:], in1=s_[:],
                        op=ALU.mult)
        with P.scope():
            pc = [P.ps([128, 512]) for _ in range(2)]
            pS1 = P.ps([128, 512]); pS2 = P.ps([128, 512])
            xc = [P.sb([128, 512]) for _ in range(2)]
            sq = [P.sb([128, 512]) for _ in range(2)]
            mu = P.sb([128, 512]); var = P.sb([128, 512]); msq = P.sb([128, 512])
            tmp = [P.sb([128, 512]) for _ in range(2)]
            for tg in range(4):
                for j2 in range(2):
                    for j in range(31):
                        o = PAD - 30 + j + tg * 512
                        P.mm(pc[j2][:], dg[:, j2, j, :], hp[:, j2, o:o + 512], start=(j == 0), stop=(j == 30))
                    P.I("act", "activation", out=xc[j2][:], in_=pc[j2][:], func=AF.Identity, bias=prm[:, j2, 31:32])
                    P.I("act", "activation", out=sq[j2][:], in_=xc[j2][:], func=AF.Square)
                for j2 in range(2):
                    P.mm(pS1[:], cx.ones_f, xc[j2][:], start=(j2 == 0), stop=(j2 == 1))
                for j2 in range(2):
                    P.mm(pS2[:], cx.ones_f, sq[j2][:], start=(j2 == 0), stop=(j2 == 1))
                P.I("dve", "tensor_scalar", out=mu[:], in0=pS1[:], scalar1=1.0 / 256, scalar2=None, op0=ALU.mult)
                P.I("dve", "tensor_tensor", out=msq[:], in0=mu[:], in1=mu[:], op=ALU.mult)
                P.I("dve", "scalar_tensor_tensor", out=var[:], in0=pS2[:], scalar=1.0 / 256, in1=msq[:],
                    op0=ALU.mult, op1=ALU.subtract)
                P.I("dve", "tensor_scalar", out=var[:], in0=var[:], scalar1=EPS, scalar2=None, op0=ALU.add)
                rsqrt_inplace(P, var[:])
                for j2 in range(2):
                    P.I("dve", "tensor_tensor", out=tmp[j2][:], in0=xc[j2][:], in1=mu[:], op=ALU.subtract)
                    P.I("dve", "tensor_tensor", out=tmp[j2][:], in0=tmp[j2][:], in1=var[:], op=ALU.mult)
                    P.I("act", "activation", out=ycT[:, j2, tg * 512:(tg + 1) * 512], in_=tmp[j2][:], func=AF.Silu,
                        scale=prm[:, j2, 32:33], bias=prm[:, j2, 33:34])
        dbg_dump(P, cx, "ycT", ycT[:], [128, 2, T])
        apply_wout(P, cx, l, ycT, 6, 2, xres, d)


NE = 32


def prep_moe(inp, sh):
    L = L_DEPTH
    sh["fnw"] = np.ascontiguousarray(
        np.asarray(inp["ffn_norm_w"], np.float32).reshape(L, 8, 128).transpose(0, 2, 1))[..., None]
    sh["router_w"] = np.stack([kchunk(np.asarray(inp["router_w"][l], np.float32)) for l in range(L)])
    sh["fnw_row"] = np.ascontiguousarray(np.asarray(inp["ffn_norm_w"], np.float32))
    sh["router_b"] = np.ascontiguousarray(np.asarray(inp["router_b"], np.float32))
    wgu = np.asarray(inp["w_gate_up"], np.float32)
    r = wgu.reshape(L, NE, 8, 128, 2, 8, 128)
    sh["w_gu"] = np.ascontiguousarray(r.transpose(0, 1, 5, 3, 4, 2, 6)).reshape(L, NE, 8, 128, 2048)
    sh["w_dn_moe"] = np.ascontiguousarray(np.asarray(inp["w_down"], np.float32))
    bgu = np.asarray(inp["b_gate_up"], np.float32)
    sh["b_gu"] = np.ascontiguousarray(bgu.reshape(L, NE, 16, 128).transpose(0, 1, 3, 2))
    sh["b_dn"] = np.ascontiguousarray(np.asarray(inp["b_down"], np.float32))


def moe(P, cx, l, xres, d, nexp=NE):
    with P.scope():
        xnT = P.sb([128, 8, T], BF16)
        rw = P.sb([128, NT, NE])
        with P.scope():
            nw = P.sb([128, 8, 1])
            P.ld(nw[:], d["fnw"][l])
            rwt = P.sb([128, 8, NE])
            P.ld(rwt[:], d["router_w"][l])
            rb = P.sb([128, NE])
            P.ld(rb[:], d["router_b"][l:l + 1, :].partition_broadcast(128))
            xn = [P.sb([128, D]) for _ in range(2)]
            ss = [P.sb([128, 1]) for _ in range(2)]
            xT32 = [P.sb([128, 8, 128]) for _ in range(2)]
            ptr = [P.ps([128, 4, 128]) for _ in range(2)]
            pl = P.ps([128, 512])
            lg = P.sb([128, NE]); m8 = P.sb([128, 8]); nmx = P.sb([128, 1]); ex = P.sb([128, NE])
            msk = P.sb([128, NE]); sm = P.sb([128, 1])
            Bd = P.sb([NE, D])
            P.ld(Bd[:], d["b_dn"][l])
            rwT = [P.sb([NE, 128]) for _ in range(2)]
            ptw = P.ps([128, 512])
            pb = [P.ps([128, 512]) for _ in range(2)]
            for i in range(NT):
                x_, s_, xt = xn[i % 2], ss[i % 2], xT32[i % 2]
                P.I("act", "activation", out=x_[:], in_=xres[i][:], func=AF.Square, accum_out=s_[:])
                P.I("dve", "tensor_scalar", out=s_[:], in0=s_[:], scalar1=1.0 / D, scalar2=EPS, op0=ALU.mult, op1=ALU.add)
                rsqrt_inplace(P, s_[:])
                P.I("dve", "tensor_scalar", out=x_[:], in0=xres[i][:], scalar1=s_[:, 0:1], scalar2=None, op0=ALU.mult)
                for hf in range(2):
                    for c4 in range(4):
                        c = hf * 4 + c4
                        P.tr(ptr[hf][:, c4, :], x_[:, c * 128:(c + 1) * 128], cx.ident_f)
                    P.I("dve", "tensor_tensor", out=xt[:, hf * 4:hf * 4 + 4, :], in0=ptr[hf][:],
                        in1=nw[:, hf * 4:hf * 4 + 4, :].bc([128, 4, 128]), op=ALU.mult)
                P.I("act", "copy", out=xnT[:, :, i * 128:(i + 1) * 128], in_=xt[:])
                for kc in range(8):
                    P.mm(pl[:, 0:NE], xt[:, kc, :], rwt[:, kc, :], start=(kc == 0), stop=(kc == 7))
                P.I("dve", "tensor_tensor", out=lg[:], in0=pl[:, 0:NE], in1=rb[:], op=ALU.add)
                P.I("dve", "max", out=m8[:], in_=lg[:])
                P.I("dve", "tensor_scalar", out=nmx[:], in0=m8[:, 0:1], scalar1=-1.0, scalar2=None, op0=ALU.mult)
                P.I("act", "activation", out=ex[:], in_=lg[:], func=AF.Exp, bias=nmx[:, 0:1])
                P.I("dve", "tensor_scalar", out=msk[:], in0=lg[:], scalar1=m8[:, 3:4], scalar2=None, op0=ALU.is_ge)
                P.I("dve", "tensor_tensor", out=ex[:], in0=ex[:], in1=msk[:], op=ALU.mult)
                P.I("dve", "tensor_reduce", out=sm[:], in_=ex[:], axis=AX.X, op=ALU.add)
                P.I("dve", "reciprocal", out=sm[:], in_=sm[:])
                P.I("dve", "tensor_scalar", out=rw[:, i, :], in0=ex[:], scalar1=sm[:, 0:1], scalar2=None, op0=ALU.mult)
                P.tr(ptw[0:NE, 0:128], rw[:, i, :], cx.ident_f)
                P.I("act", "copy", out=rwT[i % 2][:], in_=ptw[0:NE, 0:128])
                for half in range(2):
                    hs_ = slice(half * 512, (half + 1) * 512)
                    P.mm(pb[half][:], rwT[i % 2][:], Bd[:, hs_])
                    P.I("dve", "tensor_tensor", out=xres[i][:, hs_], in0=pb[half][:], in1=xres[i][:, hs_], op=ALU.add)
        dbg_dump(P, cx, "rw", rw[:], [128, NT, NE])
        chk("moe_router")
        with P.scope():
            actT = P.sbl(8, [128, T], BF16, name="actT%d" % l)
            sgu = [P.sb([128, 2048]) for _ in range(2)]
            wgub = [P.sb([128, 2, 8, 128], BF16) for _ in range(2)]
            sdn = [P.sb([128, D])]
            wdb = P.sbl(8, [128, D], BF16, name="wdb%d" % l)
            bgu = [P.sb([128, 16]) for _ in range(2)]
            bg2 = [P.sb([128, 16]) for _ in range(2)]
            Sb = [P.sb([128, 512]) for _ in range(2)]
            ub = [P.sb([128, 512]) for _ in range(2)]
            pg = [P.ps([128, 512]) for _ in range(2)]
            pu = [P.ps([128, 512]) for _ in range(2)]
            po = [P.ps([128, 512]) for _ in range(2)]
            units = [(e, j) for e in range(nexp) for j in range(8)]
            CS = 1.702 * 7.0 / (1.0 + math.exp(-1.702 * 7.0))
            rwk = P.sb([128, NT, NE])
            P.I("dve", "tensor_scalar", out=rwk[:], in0=rw[:], scalar1=1.0 / 1.702, scalar2=None, op0=ALU.mult)

            def dma_gu(u):
                e, j = units[u]
                P.ld(sgu[u % 2][:], d["w_gu"][l, e, j])

            def cast_gu(u):
                P.I("act", "copy", out=wgub[u % 2][:].re("p a c f -> p (a c f)"), in_=sgu[u % 2][:])

            dma_gu(0)
            cast_gu(0)
            n = 0
            no = 0
            for u, (e, j) in enumerate(units):
                if j == 0:
                    P.ld(bgu[e % 2][:], d["b_gu"][l, e])
                    P.I("dve", "tensor_scalar", out=bg2[e % 2][:, 0:8], in0=bgu[e % 2][:, 0:8], scalar1=1.702,
                        scalar2=None, op0=ALU.mult)
                    P.I("dve", "tensor_scalar", out=bg2[e % 2][:, 8:16], in0=bgu[e % 2][:, 8:16], scalar1=1.0,
                        scalar2=None, op0=ALU.add)
                if u + 1 < len(units):
                    dma_gu(u + 1)
                P.ld(sdn[0][:], d["w_dn_moe"][l, e, j * 128:(j + 1) * 128, :])
                wb = wgub[u % 2]
                for tg in range(4):
                    sl = slice(tg * 512, (tg + 1) * 512)
                    g_, u_ = pg[n % 2], pu[n % 2]
                    S_, ub_ = Sb[n % 2], ub[n % 2]
                    n += 1
                    for kc in range(8):
                        P.mm(g_[:], wb[:, 0, kc, :], xnT[:, kc, sl], start=(kc == 0), stop=(kc == 7))
                    for kc in range(8):
                        P.mm(u_[:], wb[:, 1, kc, :], xnT[:, kc, sl], start=(kc == 0), stop=(kc == 7))
                    P.I("act", "activation", out=S_[:], in_=g_[:], func=AF.Silu, scale=1.702, bias=bg2[e % 2][:, j:j + 1])
                    P.I("act", "activation", out=ub_[:], in_=u_[:], func=AF.Identity, bias=bg2[e % 2][:, 8 + j:9 + j])
                    P.I("dve", "tensor_scalar", out=ub_[:], in0=ub_[:], scalar1=-6.0, scalar2=8.0, op0=ALU.max, op1=ALU.min)
                    P.I("dve", "scalar_tensor_tensor", out=actT[j][:, sl], in0=S_[:], scalar=CS, in1=ub_[:],
                        op0=ALU.min, op1=ALU.mult)
                    if tg == 1 and u + 1 < len(units):
                        cast_gu(u + 1)
                    if tg == 3:
                        P.I("act", "copy", out=wdb[j][:], in_=sdn[0][:])
                if j == 7:
                    for i in range(NT):
                        for half in range(2):
                            o_ = po[no % 2]; no += 1
                            hs_ = slice(half * 512, (half + 1) * 512)
                            for jj in range(8):
                                P.mm(o_[:], actT[jj][:, i * 128:(i + 1) * 128], wdb[jj][:, hs_], start=(jj == 0), stop=(jj == 7))
                            P.I("dve", "scalar_tensor_tensor", out=xres[i][:, hs_], in0=o_[:], scalar=rwk[:, i, e:e + 1],
                                in1=xres[i][:, hs_], op0=ALU.mult, op1=ALU.add)


MOE_SPARSE = [True]
CAP = 48
SG = 2 * CAP
NSLOT = 8 * SG


def moe_sparse(P, cx, l, xres, d, nexp=NE):
    with P.scope():
        xtok = P.sbl(NT, [128, D], BF16, name="xtok%d" % l)
        rw = P.sb([128, NT, NE])
        posm = P.sb([128, NT, NE])
        with P.scope():
            nw = P.sb([128, 8, 1])
            P.ld(nw[:], d["fnw"][l])
            nwb = P.sb([128, D])
            P.ld(nwb[:], d["fnw_row"][l:l + 1, :].partition_broadcast(128))
            rwt = P.sb([128, 8, NE])
            P.ld(rwt[:], d["router_w"][l])
            rb = P.sb([128, NE])
            P.ld(rb[:], d["router_b"][l:l + 1, :].partition_broadcast(128))
            xn = [P.sb([128, D]) for _ in range(2)]
            ss = [P.sb([128, 1]) for _ in range(2)]
            xT32 = [P.sb([128, 8, 128]) for _ in range(2)]
            ptr = [P.ps([128, 4, 128]) for _ in range(2)]
            pl = P.ps([128, 512])
            lg = P.sb([128, NE]); m8 = P.sb([128, 8]); nmx = P.sb([128, 1]); ex = P.sb([128, NE])
            msk = P.sb([128, NE]); sm = P.sb([128, 1]); okm = P.sb([128, NE])
            Bd = P.sb([NE, D])
            P.ld(Bd[:], d["b_dn"][l])
            rwT = [P.sb([NE, 128]) for _ in range(2)]
            ptw = P.ps([128, 512])
            pb = [P.ps([128, 512]) for _ in range(2)]
            for i in range(NT):
                x_, s_, xt = xn[i % 2], ss[i % 2], xT32[i % 2]
                P.I("act", "activation", out=x_[:], in_=xres[i][:], func=AF.Square, accum_out=s_[:])
                P.I("dve", "tensor_scalar", out=s_[:], in0=s_[:], scalar1=1.0 / D, scalar2=EPS, op0=ALU.mult, op1=ALU.add)
                rsqrt_inplace(P, s_[:])
                P.I("dve", "tensor_scalar", out=x_[:], in0=xres[i][:], scalar1=s_[:, 0:1], scalar2=None, op0=ALU.mult)
                P.I("dve", "tensor_tensor", out=xtok[i][:], in0=x_[:], in1=nwb[:], op=ALU.mult)
                for hf in range(2):
                    for c4 in range(4):
                        c = hf * 4 + c4
                        P.tr(ptr[hf][:, c4, :], x_[:, c * 128:(c + 1) * 128], cx.ident_f)
                    P.I("dve", "tensor_tensor", out=xt[:, hf * 4:hf * 4 + 4, :], in0=ptr[hf][:],
                        in1=nw[:, hf * 4:hf * 4 + 4, :].bc([128, 4, 128]), op=ALU.mult)
                for kc in range(8):
                    P.mm(pl[:, 0:NE], xt[:, kc, :], rwt[:, kc, :], start=(kc == 0), stop=(kc == 7))
                P.I("dve", "tensor_tensor", out=lg[:], in0=pl[:, 0:NE], in1=rb[:], op=ALU.add)
                P.I("dve", "max", out=m8[:], in_=lg[:])
                P.I("dve", "tensor_scalar", out=nmx[:], in0=m8[:, 0:1], scalar1=-1.0, scalar2=None, op0=ALU.mult)
                P.I("act", "activation", out=ex[:], in_=lg[:], func=AF.Exp, bias=nmx[:, 0:1])
                P.I("dve", "tensor_scalar", out=msk[:], in0=lg[:], scalar1=m8[:, 3:4], scalar2=None, op0=ALU.is_ge)
                P.I("dve", "tensor_tensor", out=ex[:], in0=ex[:], in1=msk[:], op=ALU.mult)
                P.I("dve", "tensor_reduce", out=sm[:], in_=ex[:], axis=AX.X, op=ALU.add)
                P.I("dve", "reciprocal", out=sm[:], in_=sm[:])
                P.I("dve", "tensor_scalar", out=rw[:, i, :], in0=ex[:], scalar1=sm[:, 0:1], scalar2=None, op0=ALU.mult)
                P.mm(pl[:, 64:64 + NE], cx.sut, msk[:])
                P.I("dve", "tensor_scalar", out=okm[:], in0=pl[:, 64:64 + NE], scalar1=CAP - 0.5, scalar2=None, op0=ALU.is_lt)
                P.I("dve", "tensor_tensor", out=okm[:], in0=okm[:], in1=msk[:], op=ALU.mult)
                P.I("dve", "scalar_tensor_tensor", out=posm[:, i, :], in0=pl[:, 64:64 + NE], scalar=1.0 + (i % 2) * CAP,
                    in1=okm[:], op0=ALU.add, op1=ALU.mult)
                P.tr(ptw[0:NE, 0:128], rw[:, i, :], cx.ident_f)
                P.I("act", "copy", out=rwT[i % 2][:], in_=ptw[0:NE, 0:128])
                for half in range(2):
                    hs_ = slice(half * 512, (half + 1) * 512)
                    P.mm(pb[half][:], rwT[i % 2][:], Bd[:, hs_])
                    P.I("dve", "tensor_tensor", out=xres[i][:, hs_], in0=pb[half][:], in1=xres[i][:, hs_], op=ALU.add)
            P.I("dve", "tensor_scalar", out=posm[:], in0=posm[:], scalar1=-1.0, scalar2=None, op0=ALU.add)
        dbg_dump(P, cx, "rw", rw[:], [128, NT, NE])
        dbg_dump(P, cx, "posm", posm[:], [128, NT, NE])
        chk("moe_router")
        with P.scope():
            H = NSLOT // 2
            actT = P.sbl(8, [128, NSLOT], BF16, name="actT%d" % l)
            xgT = P.sbl(8, [128, NSLOT], BF16, name="xgT%d" % l)
            ysb = P.sbl(2, [128, D], BF16, name="ysb%d" % l)
            psel = P.sb([128, NT, SG], BF16)
            pselT = P.sb([128, NT, 128], BF16)
            sgu = [P.sb([128, 2048]) for _ in range(2)]
            wgub = [P.sb([128, 2, 8, 128], BF16) for _ in range(2)]
            sdn = [P.sb([128, D])]
            wdb = P.sbl(8, [128, D], BF16, name="wdb%d" % l)
            bgu = [P.sb([128, 16]) for _ in range(2)]
            bg2 = [P.sb([128, 16]) for _ in range(2)]
            Sb = [P.sb([128, H]) for _ in range(2)]
            ub = [P.sb([128, H]) for _ in range(2)]
            pg = [P.ps([128, 512]) for _ in range(2)]
            pu = [P.ps([128, 512]) for _ in range(2)]
            po = [P.ps([128, 512]) for _ in range(2)]
            ptp = P.ps([128, 512], BF16)
            CS = 1.702 * 7.0 / (1.0 + math.exp(-1.702 * 7.0))
            rwk = P.sb([128, NT, NE])
            P.I("dve", "tensor_scalar", out=rwk[:], in0=rw[:], scalar1=1.0 / 1.702, scalar2=None, op0=ALU.mult)
            units = [(e, j) for e in range(nexp) for j in range(8)]

            def dma_gu(u):
                e, j = units[u]
                P.ld(sgu[u % 2][:], d["w_gu"][l, e, j])

            def cast_gu(u):
                P.I("act", "copy", out=wgub[u % 2][:].re("p a c f -> p (a c f)"), in_=sgu[u % 2][:])

            dma_gu(0)
            cast_gu(0)
            n = 0
            no = 0
            for u, (e, j) in enumerate(units):
                if j == 0:
                    P.ld(bgu[e % 2][:], d["b_gu"][l, e])
                    P.I("dve", "tensor_scalar", out=bg2[e % 2][:, 0:8], in0=bgu[e % 2][:, 0:8], scalar1=1.702,
                        scalar2=None, op0=ALU.mult)
                    P.I("dve", "tensor_scalar", out=bg2[e % 2][:, 8:16], in0=bgu[e % 2][:, 8:16], scalar1=1.0,
                        scalar2=None, op0=ALU.add)
                    P.I("dve", "tensor_tensor", out=psel[:], in0=cx.iota[:, 0:SG].re("p (o c) -> p o c", o=1).bc([128, NT, SG]),
                        in1=posm[:, :, e:e + 1].bc([128, NT, SG]), op=ALU.is_equal)
                    for kc in range(8):
                        for hf in range(2):
                            ps = (pg if hf == 0 else pu)[n % 2]
                            for s4 in range(4):
                                st = hf * 4 + s4
                                for r in range(2):
                                    i = 2 * st + r
                                    P.mm(ps[:, s4 * SG:(s4 + 1) * SG], xtok[i][:, kc * 128:(kc + 1) * 128], psel[:, i, :],
                                         start=(r == 0), stop=(r == 1))
                            P.I("act", "copy", out=xgT[kc][:, hf * H:(hf + 1) * H], in_=ps[:, 0:H])
                        n += 1
                    for i4 in range(NT // 4):
                        for ii in range(4):
                            P.tr(ptp[0:SG, ii * 128:(ii + 1) * 128], psel[:, i4 * 4 + ii, :], cx.ident_bf[:])
                        P.I("dve", "tensor_copy", out=pselT[0:SG, i4 * 4:i4 * 4 + 4, :].re("p a b -> p (a b)"), in_=ptp[0:SG, :])
                if u + 1 < len(units):
                    dma_gu(u + 1)
                P.ld(sdn[0][:], d["w_dn_moe"][l, e, j * 128:(j + 1) * 128, :])
                wb = wgub[u % 2]
                for hf in range(2):
                    sl = slice(hf * H, (hf + 1) * H)
                    g_, u_ = pg[n % 2], pu[n % 2]
                    S_, ub_ = Sb[n % 2], ub[n % 2]
                    n += 1
                    for kc in range(8):
                        P.mm(g_[:, 0:H], wb[:, 0, kc, :], xgT[kc][:, sl], start=(kc == 0), stop=(kc == 7))
                    for kc in range(8):
                        P.mm(u_[:, 0:H], wb[:, 1, kc, :], xgT[kc][:, sl], start=(kc == 0), stop=(kc == 7))
                    P.I("act", "activation", out=S_[:], in_=g_[:, 0:H], func=AF.Silu, scale=1.702, bias=bg2[e % 2][:, j:j + 1])
                    P.I("act", "activation", out=ub_[:], in_=u_[:, 0:H], func=AF.Identity, bias=bg2[e % 2][:, 8 + j:9 + j])
                    P.I("dve", "tensor_scalar", out=ub_[:], in0=ub_[:], scalar1=-6.0, scalar2=8.0, op0=ALU.max, op1=ALU.min)
                    P.I("dve", "scalar_tensor_tensor", out=actT[j][:, sl], in0=S_[:], scalar=CS, in1=ub_[:],
                        op0=ALU.min, op1=ALU.mult)
                    if hf == 0 and u + 1 < len(units):
                        cast_gu(u + 1)
                    if hf == 1:
                        P.I("act", "copy", out=wdb[j][:], in_=sdn[0][:])
                if j == 7:
                    for st in range(8):
                        for half in range(2):
                            o_ = po[no % 2]; no += 1
                            hs_ = slice(half * 512, (half + 1) * 512)
                            for jj in range(8):
                                P.mm(o_[0:SG, :], actT[jj][:, st * SG:(st + 1) * SG], wdb[jj][:, hs_], start=(jj == 0), stop=(jj == 7))
                            P.I("act", "copy", out=ysb[st % 2][0:SG, hs_], in_=o_[0:SG, :])
                        for i in (2 * st, 2 * st + 1):
                            for half in range(2):
                                o_ = po[no % 2]; no += 1
                                hs_ = slice(half * 512, (half + 1) * 512)
                                P.mm(o_[:], pselT[0:SG, i, :], ysb[st % 2][0:SG, hs_])
                                P.I("dve", "scalar_tensor_tensor", out=xres[i][:, hs_], in0=o_[:], scalar=rwk[:, i, e:e + 1],
                                    in1=xres[i][:, hs_], op0=ALU.mult, op1=ALU.add)


_NC_CACHE = {}


def kernel(**inputs):
    x = np.asarray(inputs["x"], np.float32)
    sh = prep_shared(inputs)
    if "nc" not in _NC_CACHE:
        _NC_CACHE["nc"] = build_nc()
    nc = _NC_CACHE["nc"]
    in_maps = []
    for c in range(8):
        m = dict(sh)
        m["x"] = np.ascontiguousarray(x[c])
        in_maps.append(m)
    res = run_bass_kernel_spmd(nc, in_maps, core_ids=list(range(8)))
    return np.stack([np.asarray(r["out"], np.float32) for r in res.results], axis=0)
```

```python
import contextlib
import math
import numpy as np
import concourse.bass as bass
import concourse.mybir as mybir
from concourse.bass_utils import run_bass_kernel_spmd

F32 = mybir.dt.float32
BF16 = mybir.dt.bfloat16
ALU = mybir.AluOpType
AF = mybir.ActivationFunctionType
AX = mybir.AxisListType


class View:
    __slots__ = ("b", "ap")

    def __init__(self, b, ap):
        self.b = b
        self.ap = ap

    def __getitem__(self, k):
        return View(self.b, self.ap[k])

    def bc(self, shape):
        return View(self.b, self.ap.to_broadcast(list(shape)))

    def re(self, pat, **kw):
        return View(self.b, self.ap.rearrange(pat, **kw))

    def bitcast(self, dt):
        return View(self.b, self.ap.bitcast(dt))


class Buf:
    __slots__ = ("t", "w", "r", "name", "psum")

    def __init__(self, t, name, psum=False):
        self.t = t
        self.name = name
        self.w = None
        self.r = {}
        self.psum = psum

    def __getitem__(self, k):
        return View(self, self.t[k])


OUTK = ("out", "accum_out", "ap")
SAME_ENGINE_SYNC = [True]


class Prog:
    NDMA = 16

    def __init__(self, nc, stack):
        self.nc = nc
        self.stack = stack
        self.root_stack = stack
        self.engs = {"pe": nc.tensor, "dve": nc.vector, "act": nc.scalar,
                     "pool": nc.gpsimd, "sp": nc.sync}
        self.sem = {}
        self.cnt = {}
        self.seen = {}
        for k in self.engs:
            self.sem[k] = stack.enter_context(nc.semaphore("s_" + k))
            self.cnt[k] = 0
            self.seen[k] = {}
        for i in range(self.NDMA):
            k = ("d%d" if i < self.NDMA // 2 else "g%d") % (i % (self.NDMA // 2))
            self.sem[k] = stack.enter_context(nc.semaphore("s_" + k))
            self.cnt[k] = 0
        self.dma_rr = {"sp": 0, "pool": 0}
        self.nbuf = 0
        self.allbufs = []
        self.epoch = 0

    def new_epoch(self):
        if DEAD[0]:
            return
        self.barrier()
        self.epoch += 1
        for k in list(self.sem):
            self.sem[k] = self.root_stack.enter_context(self.nc.semaphore("s%d_%s" % (self.epoch, k)))
            self.cnt[k] = 0
        for k in self.seen:
            self.seen[k] = {}
        for b in self.allbufs:
            b.w = None
            b.r = {}

    def sb(self, shape, dtype=F32, name=None):
        self.nbuf += 1
        name = name or ("b%d" % self.nbuf)
        t = self.stack.enter_context(self.nc.sbuf_tensor(name, list(shape), dtype))
        assert self.nc.sbuf_bytes_remaining >= 16640, ("SBUF budget", name, self.nc.sbuf_bytes_remaining)
        b = Buf(t, name)
        self.allbufs.append(b)
        return b

    def ps(self, shape, dtype=F32, name=None):
        self.nbuf += 1
        name = name or ("p%d" % self.nbuf)
        t = self.stack.enter_context(self.nc.psum_tensor(name, list(shape), dtype))
        b = Buf(t, name, psum=True)
        self.allbufs.append(b)
        return b

    def _waits(self, ek, reads, writes):
        needs = {}
        for b in reads:
            if b.w is not None:
                k, c = b.w
                if needs.get(k, 0) < c:
                    needs[k] = c
            if b.psum:
                for k, c in b.r.items():
                    if k != ek and needs.get(k, 0) < c:
                        needs[k] = c
        for b in writes:
            if b.w is not None:
                k, c = b.w
                if needs.get(k, 0) < c:
                    needs[k] = c
            for k, c in b.r.items():
                if needs.get(k, 0) < c:
                    needs[k] = c
        eng = self.engs[ek]
        seen = self.seen[ek]
        for k, c in needs.items():
            if k == ek and (ek == "pe" or not SAME_ENGINE_SYNC[0]):
                continue
            if seen.get(k, 0) >= c:
                continue
            eng.wait_ge(self.sem[k], c)
            seen[k] = c

    def op(self, ek, reads, writes, fn):
        if DEAD[0]:
            return None
        self._waits(ek, reads, writes)
        ins = fn(self.engs[ek])
        self.cnt[ek] += 1
        c = self.cnt[ek]
        ins.then_inc(self.sem[ek], 1)
        for b in reads:
            b.r[ek] = c
        for b in writes:
            b.w = (ek, c)
            b.r = {}
        return ins

    def I(self, ek, meth, **kw):
        reads, writes, args = [], [], {}
        for k, v in kw.items():
            if isinstance(v, View):
                (writes if k in OUTK else reads).append(v.b)
                args[k] = v.ap
            else:
                args[k] = v
        return self.op(ek, reads, writes, lambda e: getattr(e, meth)(**args))

    def mm(self, out, lhsT, rhs, start=True, stop=True):
        return self.op("pe", [lhsT.b, rhs.b], [out.b],
                       lambda e: e.matmul(out=out.ap, lhsT=lhsT.ap, rhs=rhs.ap, start=start, stop=stop))

    def tr(self, out, in_, ident):
        return self.op("pe", [in_.b, ident.b], [out.b],
                       lambda e: e.transpose(out=out.ap, in_=in_.ap, identity=ident.ap))

    def ld(self, out, in_ap, q="sp", **kw):
        return self.dma(out.ap, in_ap, [], [out.b], q=q, **kw)

    def st(self, out_ap, in_, q="sp", **kw):
        return self.dma(out_ap, in_.ap, [in_.b], [], q=q, **kw)

    def barrier(self):
        for ek, eng in self.engs.items():
            for k, c in self.cnt.items():
                if k == ek or c == 0:
                    continue
                if self.seen[ek].get(k, 0) < c:
                    eng.wait_ge(self.sem[k], c)
                    self.seen[ek][k] = c

    @contextlib.contextmanager
    def scope(self):
        old = self.stack
        with contextlib.ExitStack() as st:
            self.stack = st
            try:
                yield
            finally:
                self.barrier()
                self.stack = old

    def sbl(self, n, shape, dtype=F32, name=None):
        self.nbuf += 1
        name = name or ("b%d" % self.nbuf)
        full = [shape[0], n] + list(shape[1:])
        t = self.stack.enter_context(self.nc.sbuf_tensor(name, full, dtype))
        assert self.nc.sbuf_bytes_remaining >= 16640, ("SBUF budget", name, self.nc.sbuf_bytes_remaining)
        out = [Buf(t[:, i], "%s_%d" % (name, i)) for i in range(n)]
        self.allbufs.extend(out)
        return out

    def psl(self, n, parts, width, name=None):
        per = 512 // width
        out = []
        while len(out) < n:
            bank = self.ps([128, 512])
            for i in range(per):
                if len(out) < n:
                    out.append(bank[0:parts, i * width:(i + 1) * width])
        return out

    def dma(self, out_ap, in_ap, reads, writes, q="sp", **kw):
        if DEAD[0]:
            return None
        self._waits(q, reads, writes)
        dk = ("d%d" if q == "sp" else "g%d") % self.dma_rr[q]
        self.dma_rr[q] = (self.dma_rr[q] + 1) % (self.NDMA // 2)
        if self.cnt[dk] > 0 and self.seen[q].get(dk, 0) < self.cnt[dk]:
            self.engs[q].wait_ge(self.sem[dk], self.cnt[dk])
            self.seen[q][dk] = self.cnt[dk]
        ins = self.engs[q].dma_start(out=out_ap, in_=in_ap, **kw)
        self.cnt[dk] += 16
        c = self.cnt[dk]
        ins.then_inc(self.sem[dk], 16)
        for b in reads:
            b.r[dk] = c
        for b in writes:
            b.w = (dk, c)
            b.r = {}
        return ins

    def finish(self, bufs):
        self._waits("sp", bufs, [])
        for k in list(self.sem):
            if (k.startswith("d") or k.startswith("g")) and self.cnt[k] > 0:
                if self.seen["sp"].get(k, 0) < self.cnt[k]:
                    self.engs["sp"].wait_ge(self.sem[k], self.cnt[k])
                    self.seen["sp"][k] = self.cnt[k]


T = 2048
D = 1024
NT = T // 128
L_DEPTH = 2
EPS = 1e-6
C = 64
NCH = T // C
NE = 32


def make_consts():
    c = {}
    c["ident"] = np.eye(128, dtype=np.float32)
    c["ones"] = np.ones((128, 128), np.float32)
    k = np.arange(64)
    tri = (k[:, None] <= k[None, :]).astype(np.float32)
    tril = (k[None, :] <= k[:, None]).astype(np.float32)
    t64 = np.zeros((128, 128), np.float32)
    t64[:64, :64] = tri
    t64[:64, 64:] = tril
    c["tri"] = t64
    bd = np.zeros((128, 128), np.float32)
    bd[:64, :64] = 1.0
    bd[64:, 64:] = 1.0
    kk = np.arange(128)
    sut = (kk[:, None] < kk[None, :]).astype(np.float32)
    io = np.broadcast_to(np.arange(128, dtype=np.float32)[None, :], (128, 128))
    return np.concatenate([c["ident"], c["ones"], c["tri"], bd, sut, io], axis=1)


class Ctx:
    pass


class StopBuild(Exception):
    pass


STOP = [None]


def chk(k):
    if STOP[0] == k:
        DEAD[0] = True


DEAD = [False]


def dbg_dump(P, cx, name, view, shape):
    if name not in cx.dout:
        return
    with P.scope():
        tmp = P.sb(list(shape))
        P.I("dve", "tensor_copy", out=tmp[:], in_=view)
        P.st(cx.dout[name], tmp[:])


def load_cast(P, cx, dram_ap, dst, n, parts=128):
    P.ld(dst, dram_ap, q="pool")


def rsqrt_inplace(P, v):
    P.I("act", "activation", out=v, in_=v, func=AF.Ln)
    P.I("act", "activation", out=v, in_=v, func=AF.Exp, scale=-0.5)


def norm_to_T(P, cx, xres, nw, hT):
    for i in range(NT):
        ss = cx.nrm_ss[i % 2]
        xn = cx.nrm_xn[i % 2]
        junk = xn
        pt = cx.nrm_pt[i % 2]
        P.I("act", "activation", out=junk[:], in_=xres[i][:], func=AF.Square, accum_out=ss[:])
        P.I("dve", "tensor_scalar", out=ss[:], in0=ss[:], scalar1=1.0 / D, scalar2=EPS,
            op0=ALU.mult, op1=ALU.add)
        rsqrt_inplace(P, ss[:])
        P.I("dve", "tensor_scalar", out=xn[:], in0=xres[i][:], scalar1=ss[:, 0:1], scalar2=None,
            op0=ALU.mult)
        for c in range(8):
            P.tr(pt[:, c, :], xn[:, c * 128:(c + 1) * 128], cx.ident_bf[:])
        P.I("dve", "tensor_tensor", out=hT[:, :, i * 128:(i + 1) * 128], in0=pt[:],
            in1=nw.bc([128, 8, 128]), op=ALU.mult)


def deltanet(P, cx, l, hT, xres_unused, yaT, d):
    ident = cx.ident_f
    tri = cx.tri
    tril = cx.tril
    ones = cx.ones_f
    with P.scope():
        gab = P.sb([64, NCH, 8])
        wtm = P.sb([128, 8, 20], BF16)
        load_cast(P, cx, d["w_tm"][l].rearrange("p c n -> p (c n)"), wtm[:].re("p c n -> p (c n)"), 160)
        with P.scope():
            pg = P.ps([64, 8, 8])
            for c0 in range(0, NCH, 8):
                for cc in range(8):
                    c = c0 + cc
                    for kc in range(8):
                        P.mm(pg[:, cc, :], hT[:, kc, c * 64:(c + 1) * 64], wtm[:, kc, 0:8],
                             start=(kc == 0), stop=(kc == 7))
                P.I("act", "copy", out=gab[:, c0:c0 + 8, :], in_=pg[:])
        chk("gab")
        alog = P.sb([64, 1, 4]); dtb = P.sb([64, 1, 4])
        P.ld(alog[:, 0, :], d["dn_alog"][l:l + 1, :].partition_broadcast(64))
        P.ld(dtb[:, 0, :], d["dn_dtb"][l:l + 1, :].partition_broadcast(64))
        nA = P.sb([64, 1, 4])
        P.I("act", "activation", out=nA[:], in_=alog[:], func=AF.Exp)
        xa = P.sb([64, NCH, 4]); t1 = P.sb([64, NCH, 4]); g = P.sb([64, NCH, 4])
        beta = P.sb([64, NCH, 4])
        P.I("dve", "tensor_tensor", out=xa[:], in0=gab[:, :, 0:4], in1=dtb[:].bc([64, NCH, 4]), op=ALU.add)
        P.I("dve", "scalar_tensor_tensor", out=t1[:], in0=xa[:], scalar=-1.0, in1=xa[:],
            op0=ALU.mult, op1=ALU.max)
        P.I("act", "activation", out=t1[:], in_=t1[:], func=AF.Exp, scale=-1.0)
        P.I("act", "activation", out=t1[:], in_=t1[:], func=AF.Ln, bias=1.0)
        P.I("dve", "scalar_tensor_tensor", out=g[:], in0=xa[:], scalar=0.0, in1=t1[:],
            op0=ALU.max, op1=ALU.add)
        P.I("dve", "tensor_tensor", out=g[:], in0=g[:], in1=nA[:].bc([64, NCH, 4]), op=ALU.mult)
        P.I("dve", "tensor_scalar", out=g[:], in0=g[:], scalar1=-1.0, scalar2=None, op0=ALU.mult)
        P.I("act", "activation", out=beta[:], in_=gab[:, :, 4:8], func=AF.Sigmoid)
        gc = P.sb([64, NCH, 4]); eg = P.sb([64, NCH, 4]); ekd = P.sb([64, NCH, 4])
        bk = P.sb([64, NCH, 4]); egl = P.sb([128, NCH, 4]); gl = P.sb([64, NCH, 4])
        with P.scope():
            pc = P.ps([128, NCH * 4])
            P.mm(pc[0:64, :], tri, g[:].re("p c h -> p (c h)"))
            P.I("dve", "tensor_copy", out=gc[:].re("p c h -> p (c h)"), in_=pc[0:64, :])
            P.I("act", "activation", out=eg[:].re("p c h -> p (c h)"), in_=pc[0:64, :], func=AF.Exp)
            P.mm(pc[:, :], ones[0:64, :], g[:].re("p c h -> p (c h)"))
            P.I("act", "activation", out=egl[:].re("p c h -> p (c h)"), in_=pc[:, :], func=AF.Exp)
            P.I("dve", "tensor_tensor", out=gl[:].re("p c h -> p (c h)"), in0=pc[0:64, :],
                in1=gc[:].re("p c h -> p (c h)"), op=ALU.subtract)
            P.I("act", "activation", out=ekd[:], in_=gl[:], func=AF.Exp)
            P.I("dve", "tensor_tensor", out=bk[:], in0=beta[:], in1=eg[:], op=ALU.mult)
        chk("gates")
        cw = P.sb([128, 12, 4])
        P.ld(cw[:], d["dn_cw"][l])
        dnw = P.sb([128, 1])
        P.ld(dnw[:], d["dn_nw"][l])

        stril = P.sb([64, 64])
        P.I("dve", "tensor_tensor", out=stril[:], in0=tril, in1=cx.ident_f[0:64, 0:64], op=ALU.subtract)
        for h in range(4):
            with P.scope():
                deltanet_head(P, cx, l, h, hT, yaT, d, dict(
                    g=g, gc=gc, eg=eg, ekd=ekd, bk=bk, egl=egl, beta=beta, cw=cw, dnw=dnw, stril=stril[:]))


def deltanet_head(P, cx, l, h, hT, yaT, d, G):
    ident = cx.ident_f
    tri, tril, ones = cx.tri, cx.tril, cx.ones_f
    id64 = cx.ident_f[0:64, 0:64]
    szT = P.sb([128, T], BF16)
    qkv = [P.sb([128, T], name="qkv%d_%d_%d" % (l, h, i)) for i in range(3)]
    pp = [P.ps([128, 512]) for _ in range(2)]
    n = 0
    with P.scope():
        wdn = P.sb([128, 8, 512], BF16)
        load_cast(P, cx, d["w_dn"][l, h].rearrange("p c n -> p (c n)"), wdn[:].re("p c n -> p (c n)"), 4096)
        raw = P.sb([128, 3, T + 4], BF16)
        P.I("pool", "memset", ap=raw[:, :, 0:3], constant=0.0)
        for which in range(4):
            for tg in range(4):
                ps = pp[n % 2]; n += 1
                for kc in range(8):
                    P.mm(ps[:], wdn[:, kc, which * 128:(which + 1) * 128], hT[:, kc, tg * 512:(tg + 1) * 512],
                         start=(kc == 0), stop=(kc == 7))
                if which < 3:
                    P.I("act", "copy", out=raw[:, which, 3 + tg * 512:3 + (tg + 1) * 512], in_=ps[:])
                else:
                    P.I("act", "activation", out=szT[:, tg * 512:(tg + 1) * 512], in_=ps[:], func=AF.Silu)
        chk("proj")
        cw = G["cw"]
        for which in range(3):
            acc = qkv[which]
            ci = h * 3 + which
            P.I("dve", "tensor_scalar", out=acc[:], in0=raw[:, which, 0:T], scalar1=cw[:, ci, 0:1],
                scalar2=None, op0=ALU.mult)
            for j in range(1, 4):
                P.I("dve", "scalar_tensor_tensor", out=acc[:], in0=raw[:, which, j:j + T],
                    scalar=cw[:, ci, j:j + 1], in1=acc[:], op0=ALU.mult, op1=ALU.add)
            P.I("act", "activation", out=acc[:], in_=acc[:], func=AF.Silu)
    chk("conv")
    with P.scope():
        sq = P.sb([128, 512]); rn = P.sb([128, 512])
        for which in range(2):
            for tg in range(4):
                sl = slice(tg * 512, (tg + 1) * 512)
                ps = pp[n % 2]; n += 1
                P.I("act", "activation", out=sq[:], in_=qkv[which][:, sl], func=AF.Square)
                P.mm(ps[:], ones, sq[:])
                P.I("dve", "tensor_scalar", out=rn[:], in0=ps[:], scalar1=EPS, scalar2=None, op0=ALU.add)
                rsqrt_inplace(P, rn[:])
                P.I("dve", "scalar_tensor_tensor", out=qkv[which][:, sl], in0=qkv[which][:, sl],
                    scalar=(128.0 ** -0.5 if which == 0 else 1.0), in1=rn[:], op0=ALU.mult, op1=ALU.mult)
    chk("l2")
    qT, kT, vT = qkv
    ktok = [P.sb([64, 4, 128]) for _ in range(2)]
    vtok = [P.sb([64, 4, 128]) for _ in range(2)]
    S = [P.sb([128, 128], name="S%d_%d_%d" % (l, h, i)) for i in range(2)]
    P.I("pool", "memset", ap=S[0][:], constant=0.0)
    oall = [P.sb([64, 4, 128]) for _ in range(2)]
    ssq = P.sb([64, 4])
    GS = 4
    W = GS * 64
    bankA = P.ps([128, 512]); bankB = P.ps([128, 512]); bankC = P.ps([128, 512])
    bankD = P.ps([128, 512]); bankE = P.ps([128, 512])

    def v3(bank, half, parts=64, w=64):
        return bank[0:parts, half * W:(half + 1) * W].re("p (c d) -> p c d", d=w)

    pGr, pKK = v3(bankA, 0), v3(bankA, 1)
    pQK, pU = v3(bankB, 0), v3(bankB, 1)
    pL2, pU2 = v3(bankC, 0), v3(bankC, 1)
    pPr = v3(bankD, 0)
    pW = bankD[:, W:2 * W].re("p (c d) -> p c d", d=64)
    pUo = bankE[0:64, :].re("p (c d) -> p c d", d=128)
    dec = P.sb([64, GS, 64]); decT = P.sb([64, GS, 64]); AT = P.sb([64, GS, 64])
    dd = decT
    Lp = [P.sb([64, GS, 64]) for _ in range(2)]; Up = [P.sb([64, GS, 64]) for _ in range(2)]
    Pm = [P.sb([64, GS, 64]) for _ in range(2)]
    kbg = P.sb([64, GS, 128]); vb = P.sb([64, GS, 128]); kd = P.sb([64, GS, 128])
    wT = P.sb([128, GS, 64]); uo = P.sb([64, GS, 128])
    psc = P.psl(4, 128, 128)
    vnew = [P.sb([64, 128]) for _ in range(2)]
    o1 = [P.sb([64, 128]) for _ in range(2)]
    g, gc, eg, ekd, bk, egl, beta = (G[k] for k in ("g", "gc", "eg", "ekd", "bk", "egl", "beta"))
    stril = G["stril"]

    def col(buf, c):
        return buf[:, c, h:h + 1]

    def colg(buf, c0, w):
        return buf[:, c0:c0 + GS, h:h + 1].bc([64, GS, w])

    def m64(v):
        return v.re("p (o a) -> p o a", o=1).bc([64, GS, 64])

    AT2 = [AT, P.sb([64, GS, 64])]; uo2 = [uo, P.sb([64, GS, 128])]
    wT2 = [wT, P.sb([128, GS, 64])]; kd2 = [kd, P.sb([64, GS, 128])]
    nbox = [n]

    def intra_steps(c0):
        grp = list(range(c0, c0 + GS))
        c4 = c0 // 4
        kt, vt = ktok[c4 % 2], vtok[c4 % 2]
        AT_, uo_, wT_, kd_ = AT2[c4 % 2], uo2[c4 % 2], wT2[c4 % 2], kd2[c4 % 2]
        for src, dst in ((kT, kt), (vT, vt)):
            ps = pp[nbox[0] % 2]; nbox[0] += 1
            for cc in range(4):
                c = c0 + cc
                P.tr(ps[0:64, cc * 128:(cc + 1) * 128], src[:, c * 64:(c + 1) * 64], ident[:])
            P.I("act", "copy", out=dst[:].re("p c d -> p (c d)"), in_=ps[0:64, :])
        chk("ktr")
        yield
        for cc, c in enumerate(grp):
            cs = slice(c * 64, (c + 1) * 64)
            P.mm(pGr[:, cc, :], g[:, c, h:h + 1].bc([64, 64]), tri)
            P.mm(pKK[:, cc, :], kT[:, cs], kT[:, cs])
        for cc, c in enumerate(grp):
            cs = slice(c * 64, (c + 1) * 64)
            P.mm(pQK[:, cc, :], kT[:, cs], qT[:, cs])
        yield
        P.I("dve", "tensor_tensor", out=dd[:], in0=pGr, in1=colg(gc, c0, 64), op=ALU.subtract)
        P.I("dve", "tensor_scalar", out=dec[:], in0=dd[:], scalar1=0.0, scalar2=None, op0=ALU.max)
        P.I("dve", "tensor_scalar", out=decT[:], in0=dd[:], scalar1=0.0, scalar2=None, op0=ALU.min)
        P.I("act", "activation", out=dec[:], in_=dec[:], func=AF.Exp, scale=-1.0)
        P.I("act", "activation", out=decT[:], in_=decT[:], func=AF.Exp)
        P.I("dve", "tensor_tensor", out=dec[:], in0=dec[:], in1=m64(stril), op=ALU.mult)
        P.I("dve", "tensor_tensor", out=decT[:], in0=decT[:], in1=m64(tri), op=ALU.mult)
        yield
        P.I("dve", "tensor_tensor", out=Lp[0][:], in0=pKK, in1=dec[:], op=ALU.mult)
        P.I("dve", "tensor_tensor", out=Lp[0][:], in0=Lp[0][:], in1=colg(beta, c0, 64), op=ALU.mult)
        P.I("dve", "tensor_tensor", out=AT_[:], in0=pQK, in1=decT[:], op=ALU.mult)
        chk("dec")
        yield
        for cc in range(GS):
            P.mm(pU[:, cc, :], Lp[0][:, cc, :], id64)
        P.I("act", "copy", out=Up[0][:], in_=pU)
        P.I("dve", "tensor_tensor", out=Pm[0][:], in0=m64(id64), in1=Up[0][:], op=ALU.subtract)
        chk("utr")
        yield
        for it in range(5):
            a, b = it % 2, (it + 1) % 2
            last = it == 4
            for cc in range(GS):
                P.mm(pL2[:, cc, :], Up[a][:, cc, :], Lp[a][:, cc, :])
            if not last:
                for cc in range(GS):
                    P.mm(pU2[:, cc, :], Lp[a][:, cc, :], Up[a][:, cc, :])
            yield
            P.I("dve", "tensor_copy", out=Lp[b][:], in_=pL2)
            if not last:
                P.I("dve", "tensor_copy", out=Up[b][:], in_=pU2)
            yield
            for cc in range(GS):
                P.mm(pPr[:, cc, :], Lp[b][:, cc, :], Pm[a][:, cc, :])
            yield
            P.I("dve", "tensor_tensor", out=Pm[b][:], in0=pPr, in1=Pm[a][:], op=ALU.add)
            yield
        chk("inv")
        PT = Pm[1]
        P.I("dve", "tensor_tensor", out=kbg[:], in0=kt[:], in1=colg(bk, c0, 128), op=ALU.mult)
        P.I("dve", "tensor_tensor", out=vb[:], in0=vt[:], in1=colg(beta, c0, 128), op=ALU.mult)
        P.I("dve", "tensor_tensor", out=kd_[:], in0=kt[:], in1=colg(ekd, c0, 128), op=ALU.mult)
        yield
        for cc in range(GS):
            P.mm(pUo[:, cc, :], PT[:, cc, :], vb[:, cc, :])
        P.I("act", "copy", out=uo_[:], in_=pUo)
        for cc in range(GS):
            P.mm(pW[:, cc, :], kbg[:, cc, :], PT[:, cc, :])
        P.I("dve", "tensor_copy", out=wT_[:], in_=pW)
        chk("wu")

    def scan_steps(c0):
        grp = list(range(c0, c0 + GS))
        c4 = c0 // 4
        oa = oall[c4 % 2]
        AT_, uo_, wT_, kd_ = AT2[c4 % 2], uo2[c4 % 2], wT2[c4 % 2], kd2[c4 % 2]
        for cc, c in enumerate(grp):
            Sa, Sb = S[c % 2], S[(c + 1) % 2]
            vn = vnew[c % 2]; oo = o1[c % 2]
            P.mm(psc[0][0:64, :], wT_[:, cc, :], Sa[:])
            P.mm(psc[1][0:64, :], qT[:, c * 64:(c + 1) * 64], Sa[:])
            yield
            P.I("dve", "tensor_tensor", out=vn[:], in0=uo_[:, cc, :], in1=psc[0][0:64, :], op=ALU.subtract)
            yield
            P.mm(psc[2][0:64, :], AT_[:, cc, :], vn[:])
            P.mm(psc[0][:, :], kd_[:, cc, :], vn[:])
            yield
            P.I("dve", "scalar_tensor_tensor", out=Sb[:], in0=Sa[:], scalar=egl[:, c, h:h + 1],
                in1=psc[0][:, :], op0=ALU.mult, op1=ALU.add)
            P.I("dve", "tensor_copy", out=oo[:], in_=psc[2][0:64, :])
            P.I("dve", "scalar_tensor_tensor", out=oa[:, cc, :], in0=psc[1][0:64, :],
                scalar=col(eg, c), in1=oo[:], op0=ALU.mult, op1=ALU.add)
            yield
        chk("scan")
        osq = vb
        P.I("act", "activation", out=osq[:], in_=oa[:], func=AF.Square)
        P.I("dve", "tensor_reduce", out=ssq[:], in_=osq[:], axis=AX.X, op=ALU.add)
        P.I("dve", "tensor_scalar", out=ssq[:], in0=ssq[:], scalar1=1.0 / 128, scalar2=EPS,
            op0=ALU.mult, op1=ALU.add)
        rsqrt_inplace(P, ssq[:])
        P.I("dve", "tensor_tensor", out=oa[:], in0=oa[:],
            in1=ssq[:].re("p (c o) -> p c o", o=1).bc([64, 4, 128]), op=ALU.mult)
        ps = pp[nbox[0] % 2]; nbox[0] += 1
        for cc in range(4):
            P.tr(ps[:, cc * 64:(cc + 1) * 64], oa[:, cc, :], id64)
        P.I("dve", "scalar_tensor_tensor", out=yaT[:, h, c4 * 256:(c4 + 1) * 256], in0=ps[:, 0:256],
            scalar=G["dnw"][:, 0:1], in1=szT[:, c4 * 256:(c4 + 1) * 256], op0=ALU.mult, op1=ALU.mult)

    groups = list(range(0, NCH, GS))
    for _ in intra_steps(groups[0]):
        pass
    for gi, c0 in enumerate(groups):
        it_i = intra_steps(groups[gi + 1]) if gi + 1 < len(groups) else iter(())
        it_s = scan_steps(c0)
        done_i = done_s = False
        while not (done_i and done_s):
            if not done_i:
                try:
                    next(it_i)
                except StopIteration:
                    done_i = True
            if not done_s:
                try:
                    next(it_s)
                except StopIteration:
                    done_s = True


IN_OFF = np.cumsum([0, 512, 512, 512, 512, 4, 4, 256, 384, 12, 512])


def kchunk(w):
    n = w.shape[1]
    return np.ascontiguousarray(w.reshape(8, 128, n).transpose(1, 0, 2))


def prep_shared(inp):
    f = lambda a: np.ascontiguousarray(np.asarray(a, dtype=np.float32))
    L = L_DEPTH
    sh = {}
    sh["consts"] = make_consts()
    sh["anw"] = f(np.asarray(inp["attn_norm_w"]).reshape(L, 8, 128).transpose(0, 2, 1))[..., None]
    w_in = np.asarray(inp["w_in"])
    w_dn = np.zeros((L, 4, 128, 8, 512), np.float32)
    w_tm = np.zeros((L, 128, 8, 20), np.float32)
    for l in range(L):
        for h in range(4):
            cols = np.concatenate([np.arange(o + h * 128, o + (h + 1) * 128) for o in (0, 512, 1024, 1536)])
            w_dn[l, h] = kchunk(w_in[l][:, cols])
        cols = np.concatenate([np.arange(2048, 2056), np.arange(2696, 2708)])
        w_tm[l] = kchunk(w_in[l][:, cols])
    sh["w_dn"] = w_dn
    sh["w_tm"] = w_tm
    cw = np.asarray(inp["dn_conv_w"])
    sh["dn_cw"] = f(cw.reshape(L, 4, 3, 4, 128).transpose(0, 4, 3, 2, 1).reshape(L, 128, 12, 4))
    sh["dn_alog"] = f(inp["dn_a_log"])
    sh["dn_dtb"] = f(inp["dn_dt_bias"])
    sh["dn_nw"] = f(np.asarray(inp["dn_norm_w"]).reshape(L, 128, 1))
    sh["w_out"] = f(inp["w_out"])
    prep_nsa(inp, sh)
    prep_conv(inp, sh)
    prep_moe(inp, sh)
    return sh


def build_nc(nlayers=L_DEPTH, stages=("dn", "nsa", "conv", "moe"), dbg=(), nexp=NE):
    nc = bass.Bass("TRN2", target_bir_lowering=False)
    d = {}

    def inp(name, shape):
        d[name] = nc.dram_tensor(name, list(shape), F32, kind="ExternalInput").ap()

    inp("x", [T, D]); inp("consts", [128, 768]); inp("anw", [2, 128, 8, 1])
    inp("w_out", [2, D, D])
    inp("w_nsa_fm", [2, 128, 8, 640]); inp("w_nsa_tm", [2, 128, 8, 128]); inp("nsa_nw", [2, 128, 3])
    inp("nsa_knw0", [2, 64]); inp("nsa_posT", [2, 128, 32]); inp("nsa_w1", [2, 128, 32, 128])
    inp("nsa_w2", [2, 128, 128]); inp("nsa_bias_cmp", [4, NCMP, T]); inp("nsa_bias_tile", [128, 4, 2, 128])
    inp("nsa_c31", [128, 4]); inp("nsa_sel_tab", [128, 3, NT, 32]); inp("nsa_expand", [32, T])
    inp("nsa_overlap", [NCMP, 32]); inp("nsa_mwin", [128, 8, 512])
    inp("w_conv", [2, 128, 8, 512]); inp("conv_prm", [2, 128, 2, 34])
    inp("fnw_row", [2, D]); inp("fnw", [2, 128, 8, 1]); inp("router_w", [2, 128, 8, NE]); inp("router_b", [2, NE])
    inp("w_gu", [2, NE, 8, 128, 2048]); inp("w_dn_moe", [2, NE, D, D]); inp("b_gu", [2, NE, 128, 16])
    inp("b_dn", [2, NE, D])
    inp("w_dn", [2, 4, 128, 8, 512]); inp("w_tm", [2, 128, 8, 20]); inp("dn_cw", [2, 128, 12, 4])
    inp("dn_alog", [2, 4]); inp("dn_dtb", [2, 4]); inp("dn_nw", [2, 128, 1])
    out = nc.dram_tensor("out", [T, D], F32, kind="ExternalOutput").ap()
    dout = {}
    for name, shape in dbg:
        dout[name] = nc.dram_tensor(name, list(shape), F32, kind="ExternalOutput").ap()

    with contextlib.ExitStack() as st:
        P = Prog(nc, st)
        cx = Ctx()
        consts = P.sb([128, 768])
        P.ld(consts[:], d["consts"])
        cx.ident_f = consts[:, 0:128]
        cx.ones_f = consts[:, 128:256]
        cx.tri = consts[0:64, 256:320]
        cx.tril = consts[0:64, 320:384]
        cx.bd_ones = consts[:, 384:512]
        cx.sut = consts[:, 512:640]
        cx.iota = consts[:, 640:768]
        cx.dbg_ybT = dout.get("ybT")
        cx.dout = dout
        cx.nexp = nexp
        identb = P.sb([128, 128], BF16)
        P.I("dve", "tensor_copy", out=identb[:], in_=consts[:, 0:128])
        cx.ident_bf = identb
        cx.stg_n = 256
        cx.stg_i = 0
        cx.nrm_ss = [P.sb([128, 1]) for _ in range(2)]
        xres = P.sbl(NT, [128, D], name="xres")
        for i in range(NT):
            P.ld(xres[i][:], d["x"][i * 128:(i + 1) * 128, :])
        for l in range(nlayers):
          try:
            if l > 0:
                P.new_epoch()
            with P.scope():
                hT = P.sb([128, 8, T], BF16)
                nw = P.sb([128, 8, 1])
                P.ld(nw[:], d["anw"][l])
                with P.scope():
                    cx.nrm_pt = [P.ps([128, 8, 128], BF16) for _ in range(2)]
                    cx.nrm_xn = [P.sb([128, D], BF16) for _ in range(2)]
                    norm_to_T(P, cx, xres, nw[:], hT)
                chk("norm")
                if "dn" in stages:
                    with P.scope():
                        yaT = P.sb([128, 4, T], BF16)
                        deltanet(P, cx, l, hT, xres, yaT, d)
                        if "yaT" in dout:
                            with P.scope():
                                tmp = P.sb([128, 4, T])
                                P.I("dve", "tensor_copy", out=tmp[:], in_=yaT[:])
                                P.st(dout["yaT"], tmp[:])
                        apply_wout(P, cx, l, yaT, 0, 4, xres, d)
                if "nsa" in stages:
                    nsa(P, cx, l, hT, xres, d)
                if "conv" in stages:
                    conformer(P, cx, l, hT, xres, d)
            if "moe" in stages:
                P.new_epoch()
                (moe_sparse if MOE_SPARSE[0] else moe)(P, cx, l, xres, d, nexp=cx.nexp)
          except StopBuild:
            break
        DEAD[0] = False
        for i in range(NT):
            P.st(out[i * 128:(i + 1) * 128, :], xres[i][:])
        P.finish([])
    return nc


NEGM = -200.0
NCMP = 127


def t5_bucket_np(dist):
    n = np.maximum(dist, 0)
    nf = np.maximum(n, 1).astype(np.float32)
    large = 16 + (np.log(nf / np.float32(16)) / np.float32(math.log(8.0)) * np.float32(16)).astype(np.int32)
    large = np.minimum(large, 31)
    return np.where(n < 16, n, large)


def nsa_tables(rel_bias):
    rb = np.asarray(rel_bias, np.float32)
    tb = {}
    t = np.arange(T)
    j = np.arange(NCMP)
    dist = t[None, :] - (j[:, None] * 16 + 31)
    bk = t5_bucket_np(dist)
    bc = rb[bk]
    bc = np.where((dist >= 0)[..., None], bc, np.float32(NEGM))
    tb["bias_cmp"] = np.ascontiguousarray(bc.transpose(2, 0, 1)).astype(np.float32)
    k = np.arange(128)
    tt = np.arange(128)
    d0 = tt[None, :] - k[:, None]
    d1 = d0 + 128
    b0 = np.where((d0 >= 0)[..., None], rb[t5_bucket_np(d0)], np.float32(NEGM))
    b1 = rb[t5_bucket_np(d1)]
    tbl = np.stack([b0, b1], axis=0)
    tb["bias_tile"] = np.ascontiguousarray(tbl.transpose(1, 3, 0, 2)).astype(np.float32)
    tb["c31"] = np.ascontiguousarray(np.broadcast_to(rb[31][None, :], (128, 4))).astype(np.float32)
    tok = (np.arange(NT)[None, :] * 128 + np.arange(128)[:, None])
    cur = tok // 64
    s = np.arange(32)[None, None, :]
    causal = s <= cur[..., None]
    forced = (s == 0) | (s == cur[..., None]) | (s == cur[..., None] - 1)
    m1 = (causal & ~forced).astype(np.float32)
    cst = np.where(causal & forced, 1e4, np.where(causal, 0.0, -1.0)).astype(np.float32)
    tb["sel_tab"] = np.ascontiguousarray(np.stack([m1, cst, causal.astype(np.float32)], axis=1))
    key = np.arange(T)
    tb["expand"] = (key[None, :] // 64 == np.arange(32)[:, None]).astype(np.float32)
    cs = j * 16
    ss = np.arange(32) * 64
    ov = ((cs[:, None] < ss[None, :] + 64) & (cs[:, None] + 32 > ss[None, :])).astype(np.float32)
    tb["overlap"] = ov
    mw = np.zeros((128, 8, 512), np.float32)
    for r in range(8):
        rel = r - 4
        keyp = rel * 128 + k[:, None]
        tp = np.arange(512)[None, :]
        dd = tp - keyp
        mw[:, r, :] = ((dd >= 0) & (dd < 512)).astype(np.float32)
    tb["mwin"] = mw
    return tb


def prep_nsa(inp, sh):
    L = L_DEPTH
    w_in = np.asarray(inp["w_in"])
    w_fm = np.zeros((L, 128, 8, 640), np.float32)
    w_tmv = np.zeros((L, 128, 8, 128), np.float32)
    for l in range(L):
        q0 = 2056
        kv0 = 2312
        cols = np.concatenate([
            np.arange(q0, q0 + 256),
            np.arange(kv0 + 128, kv0 + 192), np.arange(kv0 + 128, kv0 + 192),
            np.arange(kv0 + 256, kv0 + 320), np.arange(kv0 + 256, kv0 + 320),
            np.arange(kv0, kv0 + 128),
        ])
        w_fm[l] = kchunk(w_in[l][:, cols])
        cols = np.concatenate([np.arange(kv0 + 192, kv0 + 256), np.arange(kv0 + 320, kv0 + 384)])
        w_tmv[l] = kchunk(w_in[l][:, cols])
    sh["w_nsa_fm"] = w_fm
    sh["w_nsa_tm"] = w_tmv
    qn = np.asarray(inp["nsa_q_norm_w"], np.float32)
    kn = np.asarray(inp["nsa_k_norm_w"], np.float32)
    nw = np.zeros((L, 128, 3), np.float32)
    nw[:, :, 0] = np.concatenate([qn, qn], axis=1)
    nw[:, :, 1] = np.concatenate([kn[:, 1], kn[:, 1]], axis=1)
    nw[:, :, 2] = np.concatenate([kn[:, 2], kn[:, 2]], axis=1)
    sh["nsa_nw"] = nw
    sh["nsa_knw0"] = np.ascontiguousarray(kn[:, 0, :])
    pos = np.asarray(inp["nsa_cmp_pos"], np.float32)
    sh["nsa_posT"] = np.ascontiguousarray(pos.transpose(0, 1, 3, 2).reshape(L, 128, 32))
    w1 = np.asarray(inp["nsa_cmp_w1"], np.float32)
    sh["nsa_w1"] = np.ascontiguousarray(w1.reshape(L, 2, 32, 64, 128).transpose(0, 1, 3, 2, 4).reshape(L, 128, 32, 128))
    w2 = np.asarray(inp["nsa_cmp_w2"], np.float32)
    sh["nsa_w2"] = np.ascontiguousarray(w2.transpose(0, 2, 1, 3).reshape(L, 128, 128))
    tb = nsa_tables(inp["rel_bias"])
    for k_, v_ in tb.items():
        sh["nsa_" + k_] = v_


def apply_wout(P, cx, l, yT, chunk0, nch, xres, d):
    with P.scope():
        w = P.sb([128, nch, D], BF16)
        for c in range(nch):
            load_cast(P, cx, d["w_out"][l, (chunk0 + c) * 128:(chunk0 + c + 1) * 128, :], w[:, c, :], D)
        pp = [P.ps([128, 512]) for _ in range(2)]
        n = 0
        for i in range(NT):
            for half in range(2):
                ps = pp[n % 2]; n += 1
                for c in range(nch):
                    P.mm(ps[:], yT[:, c, i * 128:(i + 1) * 128], w[:, c, half * 512:(half + 1) * 512],
                         start=(c == 0), stop=(c == nch - 1))
                P.I("dve", "tensor_tensor", out=xres[i][:, half * 512:(half + 1) * 512], in0=ps[:],
                    in1=xres[i][:, half * 512:(half + 1) * 512], op=ALU.add)


def nsa(P, cx, l, hT, xres, d):
    TINY = 1e-30
    with P.scope():
        qT = P.sb([128, 2, T], BF16)
        kslcT = P.sb([128, T], BF16)
        kwinT = P.sb([128, T], BF16)
        cmpT = P.sb([128, T], BF16)
        vaug = P.sb([128, NT, 2, 66], BF16)
        gts = P.sb([128, NT, 12])
        ynsa = P.sb([128, NT, 256])
        impacc = P.sb([128, NT, 32])
        nw = P.sb([128, 3])
        P.ld(nw[:], d["nsa_nw"][l])
        qw = P.sb([128, 1])
        P.I("dve", "tensor_scalar", out=qw[:], in0=nw[:, 0:1], scalar1=0.125, scalar2=None, op0=ALU.mult)
        P.I("pool", "memset", ap=vaug[:, :, :, 64:66], constant=1.0)
        with P.scope():
            wfm = P.sb([128, 8, 640], BF16)
            load_cast(P, cx, d["w_nsa_fm"][l].rearrange("p c n -> p (c n)"), wfm[:].re("p c n -> p (c n)"), 8 * 640)
            wtv = P.sb([128, 8, 128], BF16)
            load_cast(P, cx, d["w_nsa_tm"][l].rearrange("p c n -> p (c n)"), wtv[:].re("p c n -> p (c n)"), 8 * 128)
            wtm = P.sb([128, 8, 20], BF16)
            load_cast(P, cx, d["w_tm"][l].rearrange("p c n -> p (c n)"), wtm[:].re("p c n -> p (c n)"), 160)
            pp = [P.ps([128, 512]) for _ in range(2)]
            pq = [P.ps([128, 512]) for _ in range(2)]
            qs = [P.sb([128, 512]) for _ in range(2)]
            sq = P.sb([128, 512]); rn = P.sb([128, 512])
            n = 0
            for ch in range(5):
                for tg in range(4):
                    sl = slice(tg * 512, (tg + 1) * 512)
                    ps = pp[n % 2]; ps2 = pq[n % 2]; qsb = qs[n % 2]; n += 1
                    for kc in range(8):
                        P.mm(ps[:], wfm[:, kc, ch * 128:(ch + 1) * 128], hT[:, kc, sl], start=(kc == 0), stop=(kc == 7))
                    if ch == 4:
                        P.I("act", "copy", out=cmpT[:, sl], in_=ps[:])
                        continue
                    P.I("act", "copy", out=qsb[:], in_=ps[:])
                    P.I("act", "activation", out=sq[:], in_=qsb[:], func=AF.Square)
                    P.mm(ps2[:], cx.bd_ones, sq[:])
                    P.I("dve", "tensor_scalar", out=rn[:], in0=ps2[:], scalar1=1.0 / 64, scalar2=EPS,
                        op0=ALU.mult, op1=ALU.add)
                    rsqrt_inplace(P, rn[:])
                    if ch < 2:
                        dst, wc = qT[:, ch, sl], qw[:, 0:1]
                    elif ch == 2:
                        dst, wc = kslcT[:, sl], nw[:, 1:2]
                    else:
                        dst, wc = kwinT[:, sl], nw[:, 2:3]
                    P.I("dve", "scalar_tensor_tensor", out=dst, in0=qsb[:], scalar=wc, in1=rn[:],
                        op0=ALU.mult, op1=ALU.mult)
            for i in range(NT):
                ps = pp[n % 2]; n += 1
                for kc in range(8):
                    P.mm(ps[:, 0:128], hT[:, kc, i * 128:(i + 1) * 128], wtv[:, kc, :], start=(kc == 0), stop=(kc == 7))
                for kc in range(8):
                    P.mm(ps[:, 128:140], hT[:, kc, i * 128:(i + 1) * 128], wtm[:, kc, 8:20], start=(kc == 0), stop=(kc == 7))
                P.I("act", "copy", out=vaug[:, i, :, 0:64], in_=ps[:, 0:128].re("p (a b) -> p a b", b=64))
                P.I("act", "activation", out=gts[:, i, :], in_=ps[:, 128:140], func=AF.Sigmoid)
        chk("nsa_proj")
        with P.scope():
            w1 = P.sb([128, 32, 128], BF16)
            load_cast(P, cx, d["nsa_w1"][l].rearrange("p c n -> p (c n)"), w1[:].re("p c n -> p (c n)"), 4096)
            w2 = P.sb([128, 128], BF16)
            load_cast(P, cx, d["nsa_w2"][l], w2[:], 128)
            posT = P.sb([128, 32], BF16)
            load_cast(P, cx, d["nsa_posT"][l], posT[:], 32)
            knw0 = P.sb([128, 64])
            P.ld(knw0[0:NCMP, :], d["nsa_knw0"][l:l + 1, :].partition_broadcast(NCMP))
            ovf = P.sb([128, 32])
            P.ld(ovf[0:NCMP, :], d["nsa_overlap"])
            rhsc = P.sb([128, 98], BF16)
            hs = P.sb([128, 2, 128], BF16)
            hb = P.sb([128, 2])
            kcT = P.sb([128, 128], BF16)
            ph = P.ps([128, 512])
            pk = P.ps([128, 512])
            ptb = P.ps([128, 128], BF16)
            ph2 = P.ps([128, 512])
            phs = [ph, ph2]
            for which in range(2):
                pr = slice(which * 64, which * 64 + 64)
                for l_ in range(32):
                    P.mm(phs[which][:, 0:NCMP], w1[pr, l_, :], cmpT[pr, l_:l_ + 16 * (NCMP - 1) + 1:16],
                         start=(l_ == 0), stop=(l_ == 31))
                for l_ in range(32):
                    P.mm(phs[which][:, 256:257], w1[pr, l_, :], posT[pr, l_:l_ + 1],
                         start=(l_ == 0), stop=(l_ == 31))
            for which in range(2):
                P.I("dve", "tensor_copy", out=hb[:, which:which + 1], in_=phs[which][:, 256:257])
            for which in range(2):
                P.I("act", "activation", out=hs[:, which, 0:NCMP], in_=phs[which][:, 0:NCMP],
                    func=AF.Silu, bias=hb[:, which:which + 1])
            dbg_dump(P, cx, "phk", ph[:, 0:NCMP], [128, NCMP])
            dbg_dump(P, cx, "hb", hb[:], [128, 2])
            dbg_dump(P, cx, "cmpT", cmpT[:], [128, T])
            P.mm(pk[0:NCMP, 0:64], hs[:, 0, 0:NCMP], w2[:, 0:64])
            P.mm(pk[0:NCMP, 64:128], hs[:, 1, 0:NCMP], w2[:, 64:128])
            kc = P.sb([128, 64]); kq = P.sb([128, 64]); kss = P.sb([128, 1]); kcd = P.sb([128, 2, 64], BF16)
            P.I("dve", "tensor_copy", out=kc[0:NCMP, :], in_=pk[0:NCMP, 0:64])
            P.I("dve", "tensor_tensor", out=kq[0:NCMP, :], in0=kc[0:NCMP, :], in1=kc[0:NCMP, :], op=ALU.mult)
            P.I("dve", "tensor_reduce", out=kss[0:NCMP, :], in_=kq[0:NCMP, :], axis=AX.X, op=ALU.add)
            P.I("dve", "tensor_scalar", out=kss[0:NCMP, :], in0=kss[0:NCMP, :], scalar1=1.0 / 64, scalar2=EPS,
                op0=ALU.mult, op1=ALU.add)
            rsqrt_inplace(P, kss[0:NCMP, :])
            P.I("dve", "scalar_tensor_tensor", out=kc[0:NCMP, :], in0=kc[0:NCMP, :], scalar=kss[0:NCMP, 0:1],
                in1=knw0[0:NCMP, :], op0=ALU.mult, op1=ALU.mult)
            P.I("pool", "memset", ap=kcd[:], constant=0.0)
            P.I("dve", "tensor_copy", out=kcd[0:NCMP, 0, :], in_=kc[0:NCMP, :])
            P.I("dve", "tensor_copy", out=kcd[0:NCMP, 1, :], in_=kc[0:NCMP, :])
            P.tr(ptb[:, :], kcd[:].re("p a b -> p (a b)"), cx.ident_bf[:])
            P.I("dve", "tensor_copy", out=kcT[:], in_=ptb[:])
            P.I("pool", "memset", ap=rhsc[:], constant=1.0)
            P.I("dve", "tensor_copy", out=rhsc[0:NCMP, 0:64], in_=pk[0:NCMP, 64:128])
            P.I("dve", "tensor_copy", out=rhsc[0:NCMP, 65:97], in_=ovf[0:NCMP, :])
            dbg_dump(P, cx, "kcT", kcT[:], [128, 128])
            dbg_dump(P, cx, "rhsc", rhsc[:], [128, 98])
            dbg_dump(P, cx, "hs", hs[:, :, 0:NCMP], [128, 2, NCMP])
            dbg_dump(P, cx, "kc", kc[0:NCMP, :], [NCMP, 64])
            chk("nsa_cmpkv")
            bt = [P.sb([128, 512]) for _ in range(2)]
            ssb = [P.sb([128, 512]) for _ in range(2)]
            pTb = [P.sb([128, 512], BF16) for _ in range(2)]
            psS = [P.ps([128, 512]) for _ in range(2)]
            psO = [P.ps([128, 512]) for _ in range(2)]
            rr = P.sb([128, 4, 1]); g0 = P.sb([128, 4, 1]); itmp = P.sb([128, 4, 32])
            n = 0
            for h in range(4):
                hp = slice((h % 2) * 64, (h % 2) * 64 + 64)
                for tg in range(4):
                    sl = slice(tg * 512, (tg + 1) * 512)
                    tl = slice(tg * 4, tg * 4 + 4)
                    b_ = bt[n % 2]; s_ = ssb[n % 2]; p_ = pTb[n % 2]; pS = psS[n % 2]; pO = psO[n % 2]; n += 1
                    pOv = pO[:].re("p (a b) -> p a b", b=128)
                    P.ld(b_[0:NCMP, :], d["nsa_bias_cmp"][h, :, sl])
                    P.mm(pS[0:NCMP, :], kcT[hp, 0:NCMP], qT[hp, h // 2, sl])
                    P.I("dve", "tensor_tensor", out=s_[0:NCMP, :], in0=pS[0:NCMP, :], in1=b_[0:NCMP, :], op=ALU.add)
                    P.I("act", "activation", out=p_[0:NCMP, :], in_=s_[0:NCMP, :], func=AF.Exp)
                    for i4 in range(4):
                        P.mm(pOv[:, i4, 0:97], p_[0:NCMP, i4 * 128:(i4 + 1) * 128], rhsc[0:NCMP, 0:97])
                    P.I("dve", "tensor_scalar", out=rr[:], in0=pOv[:, :, 64:65], scalar1=TINY, scalar2=None, op0=ALU.max)
                    P.I("dve", "reciprocal", out=rr[:], in_=rr[:])
                    P.I("dve", "tensor_tensor", out=g0[:], in0=rr[:], in1=gts[:, tl, h * 3:h * 3 + 1], op=ALU.mult)
                    P.I("dve", "tensor_tensor", out=ynsa[:, tl, h * 64:(h + 1) * 64], in0=pOv[:, :, 0:64],
                        in1=g0[:].bc([128, 4, 64]), op=ALU.mult)
                    if h == 0:
                        P.I("dve", "tensor_tensor", out=impacc[:, tl, :], in0=pOv[:, :, 65:97],
                            in1=rr[:].bc([128, 4, 32]), op=ALU.mult)
                    else:
                        P.I("dve", "tensor_tensor", out=itmp[:], in0=pOv[:, :, 65:97],
                            in1=rr[:].bc([128, 4, 32]), op=ALU.mult)
                        P.I("dve", "tensor_tensor", out=impacc[:, tl, :], in0=impacc[:, tl, :], in1=itmp[:], op=ALU.add)
        chk("nsa_cmp")
        selT = P.sb([32, T], BF16)
        with P.scope():
            stab = P.sb([128, 3, NT, 32])
            P.ld(stab[:], d["nsa_sel_tab"])
            imp2 = P.sb([128, NT, 32])
            P.I("dve", "tensor_tensor", out=imp2[:], in0=impacc[:], in1=stab[:, 0], op=ALU.mult)
            P.I("dve", "tensor_tensor", out=imp2[:], in0=imp2[:], in1=stab[:, 1], op=ALU.add)
            wk = P.sb([128, 32]); m8 = P.sb([128, 8]); selb = P.sb([128, NT, 32], BF16); self32 = P.sb([128, 32])
            pt = P.ps([128, 128], BF16)
            for i in range(NT):
                P.I("dve", "max", out=m8[:], in_=imp2[:, i, :])
                P.I("dve", "match_replace", out=wk[:], in_to_replace=m8[:], in_values=imp2[:, i, :], imm_value=-1e9)
                P.I("dve", "max", out=m8[:], in_=wk[:])
                P.I("dve", "tensor_scalar", out=self32[:], in0=imp2[:, i, :], scalar1=m8[:, 7:8], scalar2=None,
                    op0=ALU.is_ge)
                P.I("dve", "tensor_tensor", out=selb[:, i, :], in0=self32[:], in1=stab[:, 2, i, :], op=ALU.mult)
                P.tr(pt[0:32, :], selb[:, i, :], cx.ident_bf[:])
                P.I("act", "copy", out=selT[:, i * 128:(i + 1) * 128], in_=pt[0:32, :])
        chk("nsa_sel")
        with P.scope():
            Tb = P.sb([128, 4, 2, 128])
            P.ld(Tb[:], d["nsa_bias_tile"])
            c31 = P.sb([128, 4])
            P.ld(c31[:], d["nsa_c31"])
            E = P.sb([32, T], BF16)
            load_cast(P, cx, d["nsa_expand"], E[:], T, parts=32)
            mskall = P.sb([128, 16, 512], BF16)
            mwin = P.sb([128, 8, 512], BF16)
            load_cast(P, cx, d["nsa_mwin"].rearrange("p a b -> p (a b)"), mwin[:].re("p a b -> p (a b)"), 4096)
            psM = P.ps([128, 512])
            psS = [P.ps([128, 512]) for _ in range(2)]
            psA = P.ps([128, 512])
            eb = [P.sb([128, 512], BF16) for _ in range(2)]
            tmpb = [P.sb([128, 128]) for _ in range(2)]
            pmb = [P.sb([128, 512], BF16) for _ in range(2)]
            rr = P.sb([128, 4, 1]); g0 = P.sb([128, 4, 1]); otmp = P.sb([128, 4, 64])
            accv = psA[:].re("p (a b) -> p a b", b=128)
            units = []
            for br in range(2):
                for g in range(4):
                    kt_lo = 0 if br == 0 else max(0, 4 * g - 4)
                    kts = list(range(kt_lo, 4 * g + 4))
                    for h in range(4):
                        for kt in kts:
                            units.append((br, g, h, kt, kts))

            def build_masks(u):
                br, g, h, kt, kts = units[u]
                if br == 0 and h == 0 and kt == kts[0]:
                    for k2 in kts:
                        P.mm(psM[:], E[:, k2 * 128:(k2 + 1) * 128], selT[:, g * 512:(g + 1) * 512])
                        P.I("act", "copy", out=mskall[:, k2, :], in_=psM[:])

            def stage_a(u):
                br, g, h, kt, kts = units[u]
                kT = kslcT if br == 0 else kwinT
                hp = slice((h % 2) * 64, (h % 2) * 64 + 64)
                rel = kt - 4 * g
                c0 = max(rel, 0) * 128
                pS = psS[u % 2]; e_ = eb[u % 2]
                P.mm(pS[:, c0:512], kT[hp, kt * 128:(kt + 1) * 128], qT[hp, h // 2, g * 512 + c0:(g + 1) * 512])
                cc = c0
                for off in (0, 1):
                    qi = rel + off
                    if 0 <= qi < 4:
                        tm = tmpb[(u + off) % 2]
                        cs = slice(qi * 128, (qi + 1) * 128)
                        P.I("dve", "tensor_tensor", out=tm[:], in0=pS[:, cs], in1=Tb[:, h, off, :], op=ALU.add)
                        P.I("act", "activation", out=e_[:, cs], in_=tm[:], func=AF.Exp)
                        cc = (qi + 1) * 128
                if cc < 512:
                    P.I("act", "activation", out=e_[:, cc:512], in_=pS[:, cc:512], func=AF.Exp, bias=c31[:, h:h + 1])

            def stage_b(u):
                br, g, h, kt, kts = units[u]
                rel = kt - 4 * g
                c0 = max(rel, 0) * 128
                tl = slice(4 * g, 4 * g + 4)
                e_ = eb[u % 2]; pm = pmb[u % 2]
                msk = mskall[:, kt, :] if br == 0 else mwin[:, rel + 4, :]
                P.I("dve", "tensor_tensor", out=pm[:, c0:512], in0=e_[:, c0:512], in1=msk[:, c0:512], op=ALU.mult)
                for qi in range(c0 // 128, 4):
                    P.mm(accv[:, qi, 0:65], pm[:, qi * 128:(qi + 1) * 128], vaug[:, kt, br, 0:65],
                         start=(kt == kts[0] and qi == 0), stop=(kt == kts[-1] and qi == 3))
                if kt == kts[-1]:
                    P.I("dve", "tensor_scalar", out=rr[:], in0=accv[:, :, 64:65], scalar1=TINY, scalar2=None, op0=ALU.max)
                    P.I("dve", "reciprocal", out=rr[:], in_=rr[:])
                    P.I("dve", "tensor_tensor", out=g0[:], in0=rr[:], in1=gts[:, tl, h * 3 + 1 + br:h * 3 + 2 + br], op=ALU.mult)
                    P.I("dve", "tensor_tensor", out=otmp[:], in0=accv[:, :, 0:64], in1=g0[:].bc([128, 4, 64]), op=ALU.mult)
                    P.I("dve", "tensor_tensor", out=ynsa[:, tl, h * 64:(h + 1) * 64], in0=ynsa[:, tl, h * 64:(h + 1) * 64],
                        in1=otmp[:], op=ALU.add)

            build_masks(0)
            stage_a(0)
            for u in range(len(units)):
                if u + 1 < len(units):
                    stage_a(u + 1)
                stage_b(u)
                if u + 1 < len(units):
                    build_masks(u + 1)
        chk("nsa_attn")
        with P.scope():
            ybT = P.sb([128, 2, T], BF16)
            yb16 = [P.sb([128, 256], BF16) for _ in range(2)]
            pt = [P.ps([128, 2, 128], BF16) for _ in range(2)]
            for i in range(NT):
                P.I("act", "copy", out=yb16[i % 2][:], in_=ynsa[:, i, :])
                for c in range(2):
                    P.tr(pt[i % 2][:, c, :], yb16[i % 2][:, c * 128:(c + 1) * 128], cx.ident_bf[:])
                P.I("dve", "tensor_copy", out=ybT[:, :, i * 128:(i + 1) * 128], in_=pt[i % 2][:])
            if cx.dbg_ybT is not None:
                with P.scope():
                    tmp = P.sb([128, 2, T])
                    P.I("dve", "tensor_copy", out=tmp[:], in_=ybT[:])
                    P.st(cx.dbg_ybT, tmp[:])
            apply_wout(P, cx, l, ybT, 4, 2, xres, d)


def prep_conv(inp, sh):
    L = L_DEPTH
    w_in = np.asarray(inp["w_in"])
    w = np.zeros((L, 128, 8, 512), np.float32)
    for l in range(L):
        w[l] = kchunk(w_in[l][:, 2708:3220])
    sh["w_conv"] = w
    dw = np.asarray(inp["conv_dw_w"], np.float32)
    prm = np.zeros((L, 128, 2, 34), np.float32)
    prm[:, :, :, 0:31] = dw.reshape(L, 31, 2, 128).transpose(0, 3, 2, 1)
    prm[:, :, :, 31] = np.asarray(inp["conv_dw_b"], np.float32).reshape(L, 2, 128).transpose(0, 2, 1)
    prm[:, :, :, 32] = np.asarray(inp["conv_ln_w"], np.float32).reshape(L, 2, 128).transpose(0, 2, 1)
    prm[:, :, :, 33] = np.asarray(inp["conv_ln_b"], np.float32).reshape(L, 2, 128).transpose(0, 2, 1)
    sh["conv_prm"] = prm


def conformer(P, cx, l, hT, xres, d):
    PAD = 32
    with P.scope():
        ycT = P.sb([128, 2, T], BF16)
        hp = P.sb([128, 2, T + PAD], BF16)
        prm = P.sb([128, 2, 34])
        P.ld(prm[:], d["conv_prm"][l])
        P.I("pool", "memset", ap=hp[:, :, 0:PAD], constant=0.0)
        dg = P.sb([128, 2, 31, 128], BF16)
        for j2 in range(2):
            for j in range(31):
                P.I("dve", "tensor_scalar", out=dg[:, j2, j, :], in0=cx.ident_f,
                    scalar1=prm[:, j2, j:j + 1], scalar2=None, op0=ALU.mult)
        with P.scope():
            wcv = P.sb([128, 8, 512], BF16)
            load_cast(P, cx, d["w_conv"][l].rearrange("p c n -> p (c n)"), wcv[:].re("p c n -> p (c n)"), 4096)
            pa = [P.ps([128, 512]) for _ in range(2)]
            pg = [P.ps([128, 512]) for _ in range(2)]
            sg = [P.sb([128, 512]) for _ in range(2)]
            n = 0
            for j2 in range(2):
                for tg in range(4):
                    sl = slice(tg * 512, (tg + 1) * 512)
                    a_, g_, s_ = pa[n % 2], pg[n % 2], sg[n % 2]; n += 1
                    for kc in range(8):
                        P.mm(a_[:], wcv[:, kc, j2 * 128:(j2 + 1) * 128], hT[:, kc, sl], start=(kc == 0), stop=(kc == 7))
                    for kc in range(8):
                        P.mm(g_[:], wcv[:, kc, (2 + j2) * 128:(3 + j2) * 128], hT[:, kc, sl], start=(kc == 0), stop=(kc == 7))
                    P.I("act", "activation", out=s_[:], in_=g_[:], func=AF.Sigmoid)
                    P.I("dve", "tensor_tensor", out=hp[:, j2, PAD + tg * 512:PAD + (tg + 1) * 512], in0=a_[:], in1=s_[:],
                        op=ALU.mult)
        with P.scope():
            pc = [P.ps([128, 512]) for _ in range(2)]
            pS1 = P.ps([128, 512]); pS2 = P.ps([128, 512])
            xc = [P.sb([128, 512]) for _ in range(2)]
            sq = [P.sb([128, 512]) for _ in range(2)]
            mu = P.sb([128, 512]); var = P.sb([128, 512]); msq = P.sb([128, 512])
            tmp = [P.sb([128, 512]) for _ in range(2)]
            for tg in range(4):
                for j2 in range(2):
                    for j in range(31):
                        o = PAD - 30 + j + tg * 512
                        P.mm(pc[j2][:], dg[:, j2, j, :], hp[:, j2, o:o + 512], start=(j == 0), stop=(j == 30))
                    P.I("act", "activation", out=xc[j2][:], in_=pc[j2][:], func=AF.Identity, bias=prm[:, j2, 31:32])
                    P.I("act", "activation", out=sq[j2][:], in_=xc[j2][:], func=AF.Square)
                for j2 in range(2):
                    P.mm(pS1[:], cx.ones_f, xc[j2][:], start=(j2 == 0), stop=(j2 == 1))
                for j2 in range(2):
                    P.mm(pS2[:], cx.ones_f, sq[j2][:], start=(j2 == 0), stop=(j2 == 1))
                P.I("dve", "tensor_scalar", out=mu[:], in0=pS1[:], scalar1=1.0 / 256, scalar2=None, op0=ALU.mult)
                P.I("dve", "tensor_tensor", out=msq[:], in0=mu[:], in1=mu[:], op=ALU.mult)
                P.I("dve", "scalar_tensor_tensor", out=var[:], in0=pS2[:], scalar=1.0 / 256, in1=msq[:],
                    op0=ALU.mult, op1=ALU.subtract)
                P.I("dve", "tensor_scalar", out=var[:], in0=var[:], scalar1=EPS, scalar2=None, op0=ALU.add)
                rsqrt_inplace(P, var[:])
                for j2 in range(2):
                    P.I("dve", "tensor_tensor", out=tmp[j2][:], in0=xc[j2][:], in1=mu[:], op=ALU.subtract)
                    P.I("dve", "tensor_tensor", out=tmp[j2][:], in0=tmp[j2][:], in1=var[:], op=ALU.mult)
                    P.I("act", "activation", out=ycT[:, j2, tg * 512:(tg + 1) * 512], in_=tmp[j2][:], func=AF.Silu,
                        scale=prm[:, j2, 32:33], bias=prm[:, j2, 33:34])
        dbg_dump(P, cx, "ycT", ycT[:], [128, 2, T])
        apply_wout(P, cx, l, ycT, 6, 2, xres, d)


NE = 32


def prep_moe(inp, sh):
    L = L_DEPTH
    sh["fnw"] = np.ascontiguousarray(
        np.asarray(inp["ffn_norm_w"], np.float32).reshape(L, 8, 128).transpose(0, 2, 1))[..., None]
    sh["router_w"] = np.stack([kchunk(np.asarray(inp["router_w"][l], np.float32)) for l in range(L)])
    sh["fnw_row"] = np.ascontiguousarray(np.asarray(inp["ffn_norm_w"], np.float32))
    sh["router_b"] = np.ascontiguousarray(np.asarray(inp["router_b"], np.float32))
    wgu = np.asarray(inp["w_gate_up"], np.float32)
    r = wgu.reshape(L, NE, 8, 128, 2, 8, 128)
    sh["w_gu"] = np.ascontiguousarray(r.transpose(0, 1, 5, 3, 4, 2, 6)).reshape(L, NE, 8, 128, 2048)
    sh["w_dn_moe"] = np.ascontiguousarray(np.asarray(inp["w_down"], np.float32))
    bgu = np.asarray(inp["b_gate_up"], np.float32)
    sh["b_gu"] = np.ascontiguousarray(bgu.reshape(L, NE, 16, 128).transpose(0, 1, 3, 2))
    sh["b_dn"] = np.ascontiguousarray(np.asarray(inp["b_down"], np.float32))


def moe(P, cx, l, xres, d, nexp=NE):
    with P.scope():
        xnT = P.sb([128, 8, T], BF16)
        rw = P.sb([128, NT, NE])
        with P.scope():
            nw = P.sb([128, 8, 1])
            P.ld(nw[:], d["fnw"][l])
            rwt = P.sb([128, 8, NE])
            P.ld(rwt[:], d["router_w"][l])
            rb = P.sb([128, NE])
            P.ld(rb[:], d["router_b"][l:l + 1, :].partition_broadcast(128))
            xn = [P.sb([128, D]) for _ in range(2)]
            ss = [P.sb([128, 1]) for _ in range(2)]
            xT32 = [P.sb([128, 8, 128]) for _ in range(2)]
            ptr = [P.ps([128, 4, 128]) for _ in range(2)]
            pl = P.ps([128, 512])
            lg = P.sb([128, NE]); m8 = P.sb([128, 8]); nmx = P.sb([128, 1]); ex = P.sb([128, NE])
            msk = P.sb([128, NE]); sm = P.sb([128, 1])
            Bd = P.sb([NE, D])
            P.ld(Bd[:], d["b_dn"][l])
            rwT = [P.sb([NE, 128]) for _ in range(2)]
            ptw = P.ps([128, 512])
            pb = [P.ps([128, 512]) for _ in range(2)]
            for i in range(NT):
                x_, s_, xt = xn[i % 2], ss[i % 2], xT32[i % 2]
                P.I("act", "activation", out=x_[:], in_=xres[i][:], func=AF.Square, accum_out=s_[:])
                P.I("dve", "tensor_scalar", out=s_[:], in0=s_[:], scalar1=1.0 / D, scalar2=EPS, op0=ALU.mult, op1=ALU.add)
                rsqrt_inplace(P, s_[:])
                P.I("dve", "tensor_scalar", out=x_[:], in0=xres[i][:], scalar1=s_[:, 0:1], scalar2=None, op0=ALU.mult)
                for hf in range(2):
                    for c4 in range(4):
                        c = hf * 4 + c4
                        P.tr(ptr[hf][:, c4, :], x_[:, c * 128:(c + 1) * 128], cx.ident_f)
                    P.I("dve", "tensor_tensor", out=xt[:, hf * 4:hf * 4 + 4, :], in0=ptr[hf][:],
                        in1=nw[:, hf * 4:hf * 4 + 4, :].bc([128, 4, 128]), op=ALU.mult)
                P.I("act", "copy", out=xnT[:, :, i * 128:(i + 1) * 128], in_=xt[:])
                for kc in range(8):
                    P.mm(pl[:, 0:NE], xt[:, kc, :], rwt[:, kc, :], start=(kc == 0), stop=(kc == 7))
                P.I("dve", "tensor_tensor", out=lg[:], in0=pl[:, 0:NE], in1=rb[:], op=ALU.add)
                P.I("dve", "max", out=m8[:], in_=lg[:])
                P.I("dve", "tensor_scalar", out=nmx[:], in0=m8[:, 0:1], scalar1=-1.0, scalar2=None, op0=ALU.mult)
                P.I("act", "activation", out=ex[:], in_=lg[:], func=AF.Exp, bias=nmx[:, 0:1])
                P.I("dve", "tensor_scalar", out=msk[:], in0=lg[:], scalar1=m8[:, 3:4], scalar2=None, op0=ALU.is_ge)
                P.I("dve", "tensor_tensor", out=ex[:], in0=ex[:], in1=msk[:], op=ALU.mult)
                P.I("dve", "tensor_reduce", out=sm[:], in_=ex[:], axis=AX.X, op=ALU.add)
                P.I("dve", "reciprocal", out=sm[:], in_=sm[:])
                P.I("dve", "tensor_scalar", out=rw[:, i, :], in0=ex[:], scalar1=sm[:, 0:1], scalar2=None, op0=ALU.mult)
                P.tr(ptw[0:NE, 0:128], rw[:, i, :], cx.ident_f)
                P.I("act", "copy", out=rwT[i % 2][:], in_=ptw[0:NE, 0:128])
                for half in range(2):
                    hs_ = slice(half * 512, (half + 1) * 512)
                    P.mm(pb[half][:], rwT[i % 2][:], Bd[:, hs_])
                    P.I("dve", "tensor_tensor", out=xres[i][:, hs_], in0=pb[half][:], in1=xres[i][:, hs_], op=ALU.add)
        dbg_dump(P, cx, "rw", rw[:], [128, NT, NE])
        chk("moe_router")
        with P.scope():
            actT = P.sbl(8, [128, T], BF16, name="actT%d" % l)
            sgu = [P.sb([128, 2048]) for _ in range(2)]
            wgub = [P.sb([128, 2, 8, 128], BF16) for _ in range(2)]
            sdn = [P.sb([128, D])]
            wdb = P.sbl(8, [128, D], BF16, name="wdb%d" % l)
            bgu = [P.sb([128, 16]) for _ in range(2)]
            bg2 = [P.sb([128, 16]) for _ in range(2)]
            Sb = [P.sb([128, 512]) for _ in range(2)]
            ub = [P.sb([128, 512]) for _ in range(2)]
            pg = [P.ps([128, 512]) for _ in range(2)]
            pu = [P.ps([128, 512]) for _ in range(2)]
            po = [P.ps([128, 512]) for _ in range(2)]
            units = [(e, j) for e in range(nexp) for j in range(8)]
            CS = 1.702 * 7.0 / (1.0 + math.exp(-1.702 * 7.0))
            rwk = P.sb([128, NT, NE])
            P.I("dve", "tensor_scalar", out=rwk[:], in0=rw[:], scalar1=1.0 / 1.702, scalar2=None, op0=ALU.mult)

            def dma_gu(u):
                e, j = units[u]
                P.ld(sgu[u % 2][:], d["w_gu"][l, e, j])

            def cast_gu(u):
                P.I("act", "copy", out=wgub[u % 2][:].re("p a c f -> p (a c f)"), in_=sgu[u % 2][:])

            dma_gu(0)
            cast_gu(0)
            n = 0
            no = 0
            for u, (e, j) in enumerate(units):
                if j == 0:
                    P.ld(bgu[e % 2][:], d["b_gu"][l, e])
                    P.I("dve", "tensor_scalar", out=bg2[e % 2][:, 0:8], in0=bgu[e % 2][:, 0:8], scalar1=1.702,
                        scalar2=None, op0=ALU.mult)
                    P.I("dve", "tensor_scalar", out=bg2[e % 2][:, 8:16], in0=bgu[e % 2][:, 8:16], scalar1=1.0,
                        scalar2=None, op0=ALU.add)
                if u + 1 < len(units):
                    dma_gu(u + 1)
                P.ld(sdn[0][:], d["w_dn_moe"][l, e, j * 128:(j + 1) * 128, :])
                wb = wgub[u % 2]
                for tg in range(4):
                    sl = slice(tg * 512, (tg + 1) * 512)
                    g_, u_ = pg[n % 2], pu[n % 2]
                    S_, ub_ = Sb[n % 2], ub[n % 2]
                    n += 1
                    for kc in range(8):
                        P.mm(g_[:], wb[:, 0, kc, :], xnT[:, kc, sl], start=(kc == 0), stop=(kc == 7))
                    for kc in range(8):
                        P.mm(u_[:], wb[:, 1, kc, :], xnT[:, kc, sl], start=(kc == 0), stop=(kc == 7))
                    P.I("act", "activation", out=S_[:], in_=g_[:], func=AF.Silu, scale=1.702, bias=bg2[e % 2][:, j:j + 1])
                    P.I("act", "activation", out=ub_[:], in_=u_[:], func=AF.Identity, bias=bg2[e % 2][:, 8 + j:9 + j])
                    P.I("dve", "tensor_scalar", out=ub_[:], in0=ub_[:], scalar1=-6.0, scalar2=8.0, op0=ALU.max, op1=ALU.min)
                    P.I("dve", "scalar_tensor_tensor", out=actT[j][:, sl], in0=S_[:], scalar=CS, in1=ub_[:],
                        op0=ALU.min, op1=ALU.mult)
                    if tg == 1 and u + 1 < len(units):
                        cast_gu(u + 1)
                    if tg == 3:
                        P.I("act", "copy", out=wdb[j][:], in_=sdn[0][:])
                if j == 7:
                    for i in range(NT):
                        for half in range(2):
                            o_ = po[no % 2]; no += 1
                            hs_ = slice(half * 512, (half + 1) * 512)
                            for jj in range(8):
                                P.mm(o_[:], actT[jj][:, i * 128:(i + 1) * 128], wdb[jj][:, hs_], start=(jj == 0), stop=(jj == 7))
                            P.I("dve", "scalar_tensor_tensor", out=xres[i][:, hs_], in0=o_[:], scalar=rwk[:, i, e:e + 1],
                                in1=xres[i][:, hs_], op0=ALU.mult, op1=ALU.add)


MOE_SPARSE = [True]
CAP = 48
SG = 2 * CAP
NSLOT = 8 * SG


def moe_sparse(P, cx, l, xres, d, nexp=NE):
    with P.scope():
        xtok = P.sbl(NT, [128, D], BF16, name="xtok%d" % l)
        rw = P.sb([128, NT, NE])
        posm = P.sb([128, NT, NE])
        with P.scope():
            nw = P.sb([128, 8, 1])
            P.ld(nw[:], d["fnw"][l])
            nwb = P.sb([128, D])
            P.ld(nwb[:], d["fnw_row"][l:l + 1, :].partition_broadcast(128))
            rwt = P.sb([128, 8, NE])
            P.ld(rwt[:], d["router_w"][l])
            rb = P.sb([128, NE])
            P.ld(rb[:], d["router_b"][l:l + 1, :].partition_broadcast(128))
            xn = [P.sb([128, D]) for _ in range(2)]
            ss = [P.sb([128, 1]) for _ in range(2)]
            xT32 = [P.sb([128, 8, 128]) for _ in range(2)]
            ptr = [P.ps([128, 4, 128]) for _ in range(2)]
            pl = P.ps([128, 512])
            lg = P.sb([128, NE]); m8 = P.sb([128, 8]); nmx = P.sb([128, 1]); ex = P.sb([128, NE])
            msk = P.sb([128, NE]); sm = P.sb([128, 1]); okm = P.sb([128, NE])
            Bd = P.sb([NE, D])
            P.ld(Bd[:], d["b_dn"][l])
            rwT = [P.sb([NE, 128]) for _ in range(2)]
            ptw = P.ps([128, 512])
            pb = [P.ps([128, 512]) for _ in range(2)]
            for i in range(NT):
                x_, s_, xt = xn[i % 2], ss[i % 2], xT32[i % 2]
                P.I("act", "activation", out=x_[:], in_=xres[i][:], func=AF.Square, accum_out=s_[:])
                P.I("dve", "tensor_scalar", out=s_[:], in0=s_[:], scalar1=1.0 / D, scalar2=EPS, op0=ALU.mult, op1=ALU.add)
                rsqrt_inplace(P, s_[:])
                P.I("dve", "tensor_scalar", out=x_[:], in0=xres[i][:], scalar1=s_[:, 0:1], scalar2=None, op0=ALU.mult)
                P.I("dve", "tensor_tensor", out=xtok[i][:], in0=x_[:], in1=nwb[:], op=ALU.mult)
                for hf in range(2):
                    for c4 in range(4):
                        c = hf * 4 + c4
                        P.tr(ptr[hf][:, c4, :], x_[:, c * 128:(c + 1) * 128], cx.ident_f)
                    P.I("dve", "tensor_tensor", out=xt[:, hf * 4:hf * 4 + 4, :], in0=ptr[hf][:],
                        in1=nw[:, hf * 4:hf * 4 + 4, :].bc([128, 4, 128]), op=ALU.mult)
                for kc in range(8):
                    P.mm(pl[:, 0:NE], xt[:, kc, :], rwt[:, kc, :], start=(kc == 0), stop=(kc == 7))
                P.I("dve", "tensor_tensor", out=lg[:], in0=pl[:, 0:NE], in1=rb[:], op=ALU.add)
                P.I("dve", "max", out=m8[:], in_=lg[:])
                P.I("dve", "tensor_scalar", out=nmx[:], in0=m8[:, 0:1], scalar1=-1.0, scalar2=None, op0=ALU.mult)
                P.I("act", "activation", out=ex[:], in_=lg[:], func=AF.Exp, bias=nmx[:, 0:1])
                P.I("dve", "tensor_scalar", out=msk[:], in0=lg[:], scalar1=m8[:, 3:4], scalar2=None, op0=ALU.is_ge)
                P.I("dve", "tensor_tensor", out=ex[:], in0=ex[:], in1=msk[:], op=ALU.mult)
                P.I("dve", "tensor_reduce", out=sm[:], in_=ex[:], axis=AX.X, op=ALU.add)
                P.I("dve", "reciprocal", out=sm[:], in_=sm[:])
                P.I("dve", "tensor_scalar", out=rw[:, i, :], in0=ex[:], scalar1=sm[:, 0:1], scalar2=None, op0=ALU.mult)
                P.mm(pl[:, 64:64 + NE], cx.sut, msk[:])
                P.I("dve", "tensor_scalar", out=okm[:], in0=pl[:, 64:64 + NE], scalar1=CAP - 0.5, scalar2=None, op0=ALU.is_lt)
                P.I("dve", "tensor_tensor", out=okm[:], in0=okm[:], in1=msk[:], op=ALU.mult)
                P.I("dve", "scalar_tensor_tensor", out=posm[:, i, :], in0=pl[:, 64:64 + NE], scalar=1.0 + (i % 2) * CAP,
                    in1=okm[:], op0=ALU.add, op1=ALU.mult)
                P.tr(ptw[0:NE, 0:128], rw[:, i, :], cx.ident_f)
                P.I("act", "copy", out=rwT[i % 2][:], in_=ptw[0:NE, 0:128])
                for half in range(2):
                    hs_ = slice(half * 512, (half + 1) * 512)
                    P.mm(pb[half][:], rwT[i % 2][:], Bd[:, hs_])
                    P.I("dve", "tensor_tensor", out=xres[i][:, hs_], in0=pb[half][:], in1=xres[i][:, hs_], op=ALU.add)
            P.I("dve", "tensor_scalar", out=posm[:], in0=posm[:], scalar1=-1.0, scalar2=None, op0=ALU.add)
        dbg_dump(P, cx, "rw", rw[:], [128, NT, NE])
        dbg_dump(P, cx, "posm", posm[:], [128, NT, NE])
        chk("moe_router")
        with P.scope():
            H = NSLOT // 2
            actT = P.sbl(8, [128, NSLOT], BF16, name="actT%d" % l)
            xgT = P.sbl(8, [128, NSLOT], BF16, name="xgT%d" % l)
            ysb = P.sbl(2, [128, D], BF16, name="ysb%d" % l)
            psel = P.sb([128, NT, SG], BF16)
            pselT = P.sb([128, NT, 128], BF16)
            NWB = 3
            wgub = [P.sb([128, 2, 8, 128], BF16) for _ in range(NWB)]
            wdb = P.sbl(8, [128, D], BF16, name="wdb%d" % l)
            bgu = [P.sb([128, 16]) for _ in range(2)]
            bg2 = [P.sb([128, 16]) for _ in range(2)]
            Sb = [P.sb([128, H]) for _ in range(2)]
            ub = [P.sb([128, H]) for _ in range(2)]
            pg = [P.ps([128, 512]) for _ in range(2)]
            pu = [P.ps([128, 512]) for _ in range(2)]
            po = [P.ps([128, 512]) for _ in range(3)]
            ptp = P.ps([128, 512], BF16)
            CS = 1.702 * 7.0 / (1.0 + math.exp(-1.702 * 7.0))
            rwk = P.sb([128, NT, NE])
            P.I("dve", "tensor_scalar", out=rwk[:], in0=rw[:], scalar1=1.0 / 1.702, scalar2=None, op0=ALU.mult)
            units = [(e, j) for e in range(nexp) for j in range(8)]

            def dma_gu(u):
                e, j = units[u]
                P.ld(wgub[u % NWB][:].re("p a c f -> p (a c f)"), d["w_gu"][l, e, j], q="pool")

            dma_gu(0)
            dma_gu(1)
            n = 0
            no = 0
            for u, (e, j) in enumerate(units):
                if j == 0:
                    P.ld(bgu[e % 2][:], d["b_gu"][l, e])
                    P.I("dve", "tensor_scalar", out=bg2[e % 2][:, 0:8], in0=bgu[e % 2][:, 0:8], scalar1=1.702,
                        scalar2=None, op0=ALU.mult)
                    P.I("dve", "tensor_scalar", out=bg2[e % 2][:, 8:16], in0=bgu[e % 2][:, 8:16], scalar1=1.0,
                        scalar2=None, op0=ALU.add)
                    P.I("dve", "tensor_tensor", out=psel[:], in0=cx.iota[:, 0:SG].re("p (o c) -> p o c", o=1).bc([128, NT, SG]),
                        in1=posm[:, :, e:e + 1].bc([128, NT, SG]), op=ALU.is_equal)
                    for kc in range(8):
                        for hf in range(2):
                            ps = (pg if hf == 0 else pu)[n % 2]
                            for s4 in range(4):
                                st = hf * 4 + s4
                                for r in range(2):
                                    i = 2 * st + r
                                    P.mm(ps[:, s4 * SG:(s4 + 1) * SG], xtok[i][:, kc * 128:(kc + 1) * 128], psel[:, i, :],
                                         start=(r == 0), stop=(r == 1))
                            P.I("act", "copy", out=xgT[kc][:, hf * H:(hf + 1) * H], in_=ps[:, 0:H])
                        n += 1
                    for i4 in range(NT // 4):
                        for ii in range(4):
                            P.tr(ptp[0:SG, ii * 128:(ii + 1) * 128], psel[:, i4 * 4 + ii, :], cx.ident_bf[:])
                        P.I("dve", "tensor_copy", out=pselT[0:SG, i4 * 4:i4 * 4 + 4, :].re("p a b -> p (a b)"), in_=ptp[0:SG, :])
                if u + 2 < len(units):
                    dma_gu(u + 2)
                P.ld(wdb[j][:], d["w_dn_moe"][l, e, j * 128:(j + 1) * 128, :], q="pool")
                wb = wgub[u % NWB]
                for hf in range(2):
                    sl = slice(hf * H, (hf + 1) * H)
                    g_, u_ = pg[n % 2], pu[n % 2]
                    S_, ub_ = Sb[n % 2], ub[n % 2]
                    n += 1
                    for kc in range(8):
                        P.mm(g_[:, 0:H], wb[:, 0, kc, :], xgT[kc][:, sl], start=(kc == 0), stop=(kc == 7))
                    for kc in range(8):
                        P.mm(u_[:, 0:H], wb[:, 1, kc, :], xgT[kc][:, sl], start=(kc == 0), stop=(kc == 7))
                    P.I("act", "activation", out=S_[:], in_=g_[:, 0:H], func=AF.Silu, scale=1.702, bias=bg2[e % 2][:, j:j + 1])
                    P.I("dve", "tensor_scalar", out=ub_[:], in0=u_[:, 0:H], scalar1=bg2[e % 2][:, 8 + j:9 + j], scalar2=-6.0,
                        op0=ALU.add, op1=ALU.max)
                    P.I("dve", "tensor_scalar", out=S_[:], in0=S_[:], scalar1=CS, scalar2=None, op0=ALU.min)
                    P.I("dve", "scalar_tensor_tensor", out=actT[j][:, sl], in0=ub_[:], scalar=8.0, in1=S_[:],
                        op0=ALU.min, op1=ALU.mult)
                if j == 7:
                    def down_st(st):
                        nonlocal no
                        for half in range(2):
                            o_ = po[no % 3]; no += 1
                            hs_ = slice(half * 512, (half + 1) * 512)
                            for jj in range(8):
                                P.mm(o_[0:SG, :], actT[jj][:, st * SG:(st + 1) * SG], wdb[jj][:, hs_], start=(jj == 0), stop=(jj == 7))
                            P.I("act", "copy", out=ysb[st % 2][0:SG, hs_], in_=o_[0:SG, :])

                    def scatter_st(st):
                        nonlocal no
                        for i in (2 * st, 2 * st + 1):
                            for half in range(2):
                                o_ = po[no % 3]; no += 1
                                hs_ = slice(half * 512, (half + 1) * 512)
                                P.mm(o_[:], pselT[0:SG, i, :], ysb[st % 2][0:SG, hs_])
                                P.I("dve", "scalar_tensor_tensor", out=xres[i][:, hs_], in0=o_[:], scalar=rwk[:, i, e:e + 1],
                                    in1=xres[i][:, hs_], op0=ALU.mult, op1=ALU.add)

                    down_st(0)
                    for st in range(8):
                        if st + 1 < 8:
                            down_st(st + 1)
                        scatter_st(st)


_NC_CACHE = {}


def kernel(**inputs):
    x = np.asarray(inputs["x"], np.float32)
    sh = prep_shared(inputs)
    if "nc" not in _NC_CACHE:
        _NC_CACHE["nc"] = build_nc()
    nc = _NC_CACHE["nc"]
    in_maps = []
    for c in range(8):
        m = dict(sh)
        m["x"] = np.ascontiguousarray(x[c])
        in_maps.append(m)
    res = run_bass_kernel_spmd(nc, in_maps, core_ids=list(range(8)))
    return np.stack([np.asarray(r["out"], np.float32) for r in res.results], axis=0)
```

```python
import contextlib
import math
import numpy as np
import concourse.bass as bass
import concourse.mybir as mybir
from concourse.bass_utils import run_bass_kernel_spmd

F32 = mybir.dt.float32
BF16 = mybir.dt.bfloat16
ALU = mybir.AluOpType
AF = mybir.ActivationFunctionType
AX = mybir.AxisListType


class View:
    __slots__ = ("b", "ap")

    def __init__(self, b, ap):
        self.b = b
        self.ap = ap

    def __getitem__(self, k):
        return View(self.b, self.ap[k])

    def bc(self, shape):
        return View(self.b, self.ap.to_broadcast(list(shape)))

    def re(self, pat, **kw):
        return View(self.b, self.ap.rearrange(pat, **kw))

    def bitcast(self, dt):
        return View(self.b, self.ap.bitcast(dt))


class Buf:
    __slots__ = ("t", "w", "r", "name", "psum")

    def __init__(self, t, name, psum=False):
        self.t = t
        self.name = name
        self.w = None
        self.r = {}
        self.psum = psum

    def __getitem__(self, k):
        return View(self, self.t[k])


OUTK = ("out", "accum_out", "ap")
SAME_ENGINE_SYNC = [True]


class Prog:
    NDMA = 16

    def __init__(self, nc, stack):
        self.nc = nc
        self.stack = stack
        self.root_stack = stack
        self.engs = {"pe": nc.tensor, "dve": nc.vector, "act": nc.scalar,
                     "pool": nc.gpsimd, "sp": nc.sync}
        self.sem = {}
        self.cnt = {}
        self.seen = {}
        for k in self.engs:
            self.sem[k] = stack.enter_context(nc.semaphore("s_" + k))
            self.cnt[k] = 0
            self.seen[k] = {}
        for i in range(self.NDMA):
            k = ("d%d" if i < self.NDMA // 2 else "g%d") % (i % (self.NDMA // 2))
            self.sem[k] = stack.enter_context(nc.semaphore("s_" + k))
            self.cnt[k] = 0
        self.dma_rr = {"sp": 0, "pool": 0}
        self.nbuf = 0
        self.allbufs = []
        self.epoch = 0

    def new_epoch(self):
        if DEAD[0]:
            return
        self.barrier()
        self.epoch += 1
        for k in list(self.sem):
            self.sem[k] = self.root_stack.enter_context(self.nc.semaphore("s%d_%s" % (self.epoch, k)))
            self.cnt[k] = 0
        for k in self.seen:
            self.seen[k] = {}
        for b in self.allbufs:
            b.w = None
            b.r = {}

    def sb(self, shape, dtype=F32, name=None):
        self.nbuf += 1
        name = name or ("b%d" % self.nbuf)
        t = self.stack.enter_context(self.nc.sbuf_tensor(name, list(shape), dtype))
        assert self.nc.sbuf_bytes_remaining >= 16640, ("SBUF budget", name, self.nc.sbuf_bytes_remaining)
        b = Buf(t, name)
        self.allbufs.append(b)
        return b

    def ps(self, shape, dtype=F32, name=None):
        self.nbuf += 1
        name = name or ("p%d" % self.nbuf)
        t = self.stack.enter_context(self.nc.psum_tensor(name, list(shape), dtype))
        b = Buf(t, name, psum=True)
        self.allbufs.append(b)
        return b

    def _waits(self, ek, reads, writes):
        needs = {}
        for b in reads:
            if b.w is not None:
                k, c = b.w
                if needs.get(k, 0) < c:
                    needs[k] = c
            if b.psum:
                for k, c in b.r.items():
                    if k != ek and needs.get(k, 0) < c:
                        needs[k] = c
        for b in writes:
            if b.w is not None:
                k, c = b.w
                if needs.get(k, 0) < c:
                    needs[k] = c
            for k, c in b.r.items():
                if needs.get(k, 0) < c:
                    needs[k] = c
        eng = self.engs[ek]
        seen = self.seen[ek]
        for k, c in needs.items():
            if k == ek and (ek == "pe" or not SAME_ENGINE_SYNC[0]):
                continue
            if seen.get(k, 0) >= c:
                continue
            eng.wait_ge(self.sem[k], c)
            seen[k] = c

    def op(self, ek, reads, writes, fn):
        if DEAD[0]:
            return None
        self._waits(ek, reads, writes)
        ins = fn(self.engs[ek])
        self.cnt[ek] += 1
        c = self.cnt[ek]
        ins.then_inc(self.sem[ek], 1)
        for b in reads:
            b.r[ek] = c
        for b in writes:
            b.w = (ek, c)
            b.r = {}
        return ins

    def I(self, ek, meth, **kw):
        reads, writes, args = [], [], {}
        for k, v in kw.items():
            if isinstance(v, View):
                (writes if k in OUTK else reads).append(v.b)
                args[k] = v.ap
            else:
                args[k] = v
        return self.op(ek, reads, writes, lambda e: getattr(e, meth)(**args))

    def mm(self, out, lhsT, rhs, start=True, stop=True):
        return self.op("pe", [lhsT.b, rhs.b], [out.b],
                       lambda e: e.matmul(out=out.ap, lhsT=lhsT.ap, rhs=rhs.ap, start=start, stop=stop))

    def tr(self, out, in_, ident):
        return self.op("pe", [in_.b, ident.b], [out.b],
                       lambda e: e.transpose(out=out.ap, in_=in_.ap, identity=ident.ap))

    def ld(self, out, in_ap, q="sp", **kw):
        return self.dma(out.ap, in_ap, [], [out.b], q=q, **kw)

    def st(self, out_ap, in_, q="sp", **kw):
        return self.dma(out_ap, in_.ap, [in_.b], [], q=q, **kw)

    def barrier(self):
        for ek, eng in self.engs.items():
            for k, c in self.cnt.items():
                if k == ek or c == 0:
                    continue
                if self.seen[ek].get(k, 0) < c:
                    eng.wait_ge(self.sem[k], c)
                    self.seen[ek][k] = c

    @contextlib.contextmanager
    def scope(self):
        old = self.stack
        with contextlib.ExitStack() as st:
            self.stack = st
            try:
                yield
            finally:
                self.barrier()
                self.stack = old

    def sbl(self, n, shape, dtype=F32, name=None):
        self.nbuf += 1
        name = name or ("b%d" % self.nbuf)
        full = [shape[0], n] + list(shape[1:])
        t = self.stack.enter_context(self.nc.sbuf_tensor(name, full, dtype))
        assert self.nc.sbuf_bytes_remaining >= 16640, ("SBUF budget", name, self.nc.sbuf_bytes_remaining)
        out = [Buf(t[:, i], "%s_%d" % (name, i)) for i in range(n)]
        self.allbufs.extend(out)
        return out

    def psl(self, n, parts, width, name=None):
        per = 512 // width
        out = []
        while len(out) < n:
            bank = self.ps([128, 512])
            for i in range(per):
                if len(out) < n:
                    out.append(bank[0:parts, i * width:(i + 1) * width])
        return out

    def dma(self, out_ap, in_ap, reads, writes, q="sp", **kw):
        if DEAD[0]:
            return None
        self._waits(q, reads, writes)
        dk = ("d%d" if q == "sp" else "g%d") % self.dma_rr[q]
        self.dma_rr[q] = (self.dma_rr[q] + 1) % (self.NDMA // 2)
        if self.cnt[dk] > 0 and self.seen[q].get(dk, 0) < self.cnt[dk]:
            self.engs[q].wait_ge(self.sem[dk], self.cnt[dk])
            self.seen[q][dk] = self.cnt[dk]
        ins = self.engs[q].dma_start(out=out_ap, in_=in_ap, **kw)
        self.cnt[dk] += 16
        c = self.cnt[dk]
        ins.then_inc(self.sem[dk], 16)
        for b in reads:
            b.r[dk] = c
        for b in writes:
            b.w = (dk, c)
            b.r = {}
        return ins

    def finish(self, bufs):
        self._waits("sp", bufs, [])
        for k in list(self.sem):
            if (k.startswith("d") or k.startswith("g")) and self.cnt[k] > 0:
                if self.seen["sp"].get(k, 0) < self.cnt[k]:
                    self.engs["sp"].wait_ge(self.sem[k], self.cnt[k])
                    self.seen["sp"][k] = self.cnt[k]


T = 2048
D = 1024
NT = T // 128
L_DEPTH = 2
EPS = 1e-6
C = 64
NCH = T // C
NE = 32


def make_consts():
    c = {}
    c["ident"] = np.eye(128, dtype=np.float32)
    c["ones"] = np.ones((128, 128), np.float32)
    k = np.arange(64)
    tri = (k[:, None] <= k[None, :]).astype(np.float32)
    tril = (k[None, :] <= k[:, None]).astype(np.float32)
    t64 = np.zeros((128, 128), np.float32)
    t64[:64, :64] = tri
    t64[:64, 64:] = tril
    c["tri"] = t64
    bd = np.zeros((128, 128), np.float32)
    bd[:64, :64] = 1.0
    bd[64:, 64:] = 1.0
    kk = np.arange(128)
    sut = (kk[:, None] < kk[None, :]).astype(np.float32)
    io = np.broadcast_to(np.arange(128, dtype=np.float32)[None, :], (128, 128))
    return np.concatenate([c["ident"], c["ones"], c["tri"], bd, sut, io], axis=1)


class Ctx:
    pass


class StopBuild(Exception):
    pass


STOP = [None]


def chk(k):
    if STOP[0] == k:
        DEAD[0] = True


DEAD = [False]


def dbg_dump(P, cx, name, view, shape):
    if name not in cx.dout:
        return
    with P.scope():
        tmp = P.sb(list(shape))
        P.I("dve", "tensor_copy", out=tmp[:], in_=view)
        P.st(cx.dout[name], tmp[:])


def load_cast(P, cx, dram_ap, dst, n, parts=128):
    P.ld(dst, dram_ap, q="pool")


def rsqrt_inplace(P, v):
    P.I("act", "activation", out=v, in_=v, func=AF.Ln)
    P.I("act", "activation", out=v, in_=v, func=AF.Exp, scale=-0.5)


def norm_to_T(P, cx, xres, nw, hT):
    for i in range(NT):
        ss = cx.nrm_ss[i % 2]
        xn = cx.nrm_xn[i % 2]
        junk = xn
        pt = cx.nrm_pt[i % 2]
        P.I("act", "activation", out=junk[:], in_=xres[i][:], func=AF.Square, accum_out=ss[:])
        P.I("dve", "tensor_scalar", out=ss[:], in0=ss[:], scalar1=1.0 / D, scalar2=EPS,
            op0=ALU.mult, op1=ALU.add)
        rsqrt_inplace(P, ss[:])
        P.I("dve", "tensor_scalar", out=xn[:], in0=xres[i][:], scalar1=ss[:, 0:1], scalar2=None,
            op0=ALU.mult)
        for c in range(8):
            P.tr(pt[:, c, :], xn[:, c * 128:(c + 1) * 128], cx.ident_bf[:])
        P.I("dve", "tensor_tensor", out=hT[:, :, i * 128:(i + 1) * 128], in0=pt[:],
            in1=nw.bc([128, 8, 128]), op=ALU.mult)


def deltanet(P, cx, l, hT, xres_unused, yaT, d):
    ident = cx.ident_f
    tri = cx.tri
    tril = cx.tril
    ones = cx.ones_f
    with P.scope():
        gab = P.sb([64, NCH, 8])
        wtm = P.sb([128, 8, 20], BF16)
        load_cast(P, cx, d["w_tm"][l].rearrange("p c n -> p (c n)"), wtm[:].re("p c n -> p (c n)"), 160)
        with P.scope():
            pg = P.ps([64, 8, 8])
            for c0 in range(0, NCH, 8):
                for cc in range(8):
                    c = c0 + cc
                    for kc in range(8):
                        P.mm(pg[:, cc, :], hT[:, kc, c * 64:(c + 1) * 64], wtm[:, kc, 0:8],
                             start=(kc == 0), stop=(kc == 7))
                P.I("act", "copy", out=gab[:, c0:c0 + 8, :], in_=pg[:])
        chk("gab")
        alog = P.sb([64, 1, 4]); dtb = P.sb([64, 1, 4])
        P.ld(alog[:, 0, :], d["dn_alog"][l:l + 1, :].partition_broadcast(64))
        P.ld(dtb[:, 0, :], d["dn_dtb"][l:l + 1, :].partition_broadcast(64))
        nA = P.sb([64, 1, 4])
        P.I("act", "activation", out=nA[:], in_=alog[:], func=AF.Exp)
        xa = P.sb([64, NCH, 4]); t1 = P.sb([64, NCH, 4]); g = P.sb([64, NCH, 4])
        beta = P.sb([64, NCH, 4])
        P.I("dve", "tensor_tensor", out=xa[:], in0=gab[:, :, 0:4], in1=dtb[:].bc([64, NCH, 4]), op=ALU.add)
        P.I("dve", "scalar_tensor_tensor", out=t1[:], in0=xa[:], scalar=-1.0, in1=xa[:],
            op0=ALU.mult, op1=ALU.max)
        P.I("act", "activation", out=t1[:], in_=t1[:], func=AF.Exp, scale=-1.0)
        P.I("act", "activation", out=t1[:], in_=t1[:], func=AF.Ln, bias=1.0)
        P.I("dve", "scalar_tensor_tensor", out=g[:], in0=xa[:], scalar=0.0, in1=t1[:],
            op0=ALU.max, op1=ALU.add)
        P.I("dve", "tensor_tensor", out=g[:], in0=g[:], in1=nA[:].bc([64, NCH, 4]), op=ALU.mult)
        P.I("dve", "tensor_scalar", out=g[:], in0=g[:], scalar1=-1.0, scalar2=None, op0=ALU.mult)
        P.I("act", "activation", out=beta[:], in_=gab[:, :, 4:8], func=AF.Sigmoid)
        gc = P.sb([64, NCH, 4]); eg = P.sb([64, NCH, 4]); ekd = P.sb([64, NCH, 4])
        bk = P.sb([64, NCH, 4]); egl = P.sb([128, NCH, 4]); gl = P.sb([64, NCH, 4])
        with P.scope():
            pc = P.ps([128, NCH * 4])
            P.mm(pc[0:64, :], tri, g[:].re("p c h -> p (c h)"))
            P.I("dve", "tensor_copy", out=gc[:].re("p c h -> p (c h)"), in_=pc[0:64, :])
            P.I("act", "activation", out=eg[:].re("p c h -> p (c h)"), in_=pc[0:64, :], func=AF.Exp)
            P.mm(pc[:, :], ones[0:64, :], g[:].re("p c h -> p (c h)"))
            P.I("act", "activation", out=egl[:].re("p c h -> p (c h)"), in_=pc[:, :], func=AF.Exp)
            P.I("dve", "tensor_tensor", out=gl[:].re("p c h -> p (c h)"), in0=pc[0:64, :],
                in1=gc[:].re("p c h -> p (c h)"), op=ALU.subtract)
            P.I("act", "activation", out=ekd[:], in_=gl[:], func=AF.Exp)
            P.I("dve", "tensor_tensor", out=bk[:], in0=beta[:], in1=eg[:], op=ALU.mult)
        chk("gates")
        cw = P.sb([128, 12, 4])
        P.ld(cw[:], d["dn_cw"][l])
        dnw = P.sb([128, 1])
        P.ld(dnw[:], d["dn_nw"][l])

        stril = P.sb([64, 64])
        P.I("dve", "tensor_tensor", out=stril[:], in0=tril, in1=cx.ident_f[0:64, 0:64], op=ALU.subtract)
        for h in range(4):
            with P.scope():
                deltanet_head(P, cx, l, h, hT, yaT, d, dict(
                    g=g, gc=gc, eg=eg, ekd=ekd, bk=bk, egl=egl, beta=beta, cw=cw, dnw=dnw, stril=stril[:]))


def deltanet_head(P, cx, l, h, hT, yaT, d, G):
    ident = cx.ident_f
    tri, tril, ones = cx.tri, cx.tril, cx.ones_f
    id64 = cx.ident_f[0:64, 0:64]
    szT = P.sb([128, T], BF16)
    qkv = [P.sb([128, T], name="qkv%d_%d_%d" % (l, h, i)) for i in range(3)]
    pp = [P.ps([128, 512]) for _ in range(2)]
    n = 0
    with P.scope():
        wdn = P.sb([128, 8, 512], BF16)
        load_cast(P, cx, d["w_dn"][l, h].rearrange("p c n -> p (c n)"), wdn[:].re("p c n -> p (c n)"), 4096)
        raw = P.sb([128, 3, T + 4], BF16)
        P.I("pool", "memset", ap=raw[:, :, 0:3], constant=0.0)
        for which in range(4):
            for tg in range(4):
                ps = pp[n % 2]; n += 1
                for kc in range(8):
                    P.mm(ps[:], wdn[:, kc, which * 128:(which + 1) * 128], hT[:, kc, tg * 512:(tg + 1) * 512],
                         start=(kc == 0), stop=(kc == 7))
                if which < 3:
                    P.I("act", "copy", out=raw[:, which, 3 + tg * 512:3 + (tg + 1) * 512], in_=ps[:])
                else:
                    P.I("act", "activation", out=szT[:, tg * 512:(tg + 1) * 512], in_=ps[:], func=AF.Silu)
        chk("proj")
        cw = G["cw"]
        for which in range(3):
            acc = qkv[which]
            ci = h * 3 + which
            P.I("dve", "tensor_scalar", out=acc[:], in0=raw[:, which, 0:T], scalar1=cw[:, ci, 0:1],
                scalar2=None, op0=ALU.mult)
            for j in range(1, 4):
                P.I("dve", "scalar_tensor_tensor", out=acc[:], in0=raw[:, which, j:j + T],
                    scalar=cw[:, ci, j:j + 1], in1=acc[:], op0=ALU.mult, op1=ALU.add)
            P.I("act", "activation", out=acc[:], in_=acc[:], func=AF.Silu)
    chk("conv")
    with P.scope():
        sq = P.sb([128, 512]); rn = P.sb([128, 512])
        for which in range(2):
            for tg in range(4):
                sl = slice(tg * 512, (tg + 1) * 512)
                ps = pp[n % 2]; n += 1
                P.I("act", "activation", out=sq[:], in_=qkv[which][:, sl], func=AF.Square)
                P.mm(ps[:], ones, sq[:])
                P.I("dve", "tensor_scalar", out=rn[:], in0=ps[:], scalar1=EPS, scalar2=None, op0=ALU.add)
                rsqrt_inplace(P, rn[:])
                P.I("dve", "scalar_tensor_tensor", out=qkv[which][:, sl], in0=qkv[which][:, sl],
                    scalar=(128.0 ** -0.5 if which == 0 else 1.0), in1=rn[:], op0=ALU.mult, op1=ALU.mult)
    chk("l2")
    qT, kT, vT = qkv
    ktok = [P.sb([64, 4, 128]) for _ in range(2)]
    vtok = [P.sb([64, 4, 128]) for _ in range(2)]
    S = [P.sb([128, 128], name="S%d_%d_%d" % (l, h, i)) for i in range(2)]
    P.I("pool", "memset", ap=S[0][:], constant=0.0)
    oall = [P.sb([64, 4, 128]) for _ in range(2)]
    ssq = P.sb([64, 4])
    GS = 4
    W = GS * 64
    bankA = P.ps([128, 512]); bankB = P.ps([128, 512]); bankC = P.ps([128, 512])
    bankD = P.ps([128, 512]); bankE = P.ps([128, 512])

    def v3(bank, half, parts=64, w=64):
        return bank[0:parts, half * W:(half + 1) * W].re("p (c d) -> p c d", d=w)

    pGr, pKK = v3(bankA, 0), v3(bankA, 1)
    pQK, pU = v3(bankB, 0), v3(bankB, 1)
    pL2, pU2 = v3(bankC, 0), v3(bankC, 1)
    pPr = v3(bankD, 0)
    pW = bankD[:, W:2 * W].re("p (c d) -> p c d", d=64)
    pUo = bankE[0:64, :].re("p (c d) -> p c d", d=128)
    dec = P.sb([64, GS, 64]); decT = P.sb([64, GS, 64]); AT = P.sb([64, GS, 64])
    dd = decT
    Lp = [P.sb([64, GS, 64]) for _ in range(2)]; Up = [P.sb([64, GS, 64]) for _ in range(2)]
    Pm = [P.sb([64, GS, 64]) for _ in range(2)]
    kbg = P.sb([64, GS, 128]); vb = P.sb([64, GS, 128]); kd = P.sb([64, GS, 128])
    wT = P.sb([128, GS, 64]); uo = P.sb([64, GS, 128])
    psc = P.psl(4, 128, 128)
    vnew = [P.sb([64, 128]) for _ in range(2)]
    o1 = [P.sb([64, 128]) for _ in range(2)]
    g, gc, eg, ekd, bk, egl, beta = (G[k] for k in ("g", "gc", "eg", "ekd", "bk", "egl", "beta"))
    stril = G["stril"]

    def col(buf, c):
        return buf[:, c, h:h + 1]

    def colg(buf, c0, w):
        return buf[:, c0:c0 + GS, h:h + 1].bc([64, GS, w])

    def m64(v):
        return v.re("p (o a) -> p o a", o=1).bc([64, GS, 64])

    AT2 = [AT, P.sb([64, GS, 64])]; uo2 = [uo, P.sb([64, GS, 128])]
    wT2 = [wT, P.sb([128, GS, 64])]; kd2 = [kd, P.sb([64, GS, 128])]
    nbox = [n]

    def intra_steps(c0):
        grp = list(range(c0, c0 + GS))
        c4 = c0 // 4
        kt, vt = ktok[c4 % 2], vtok[c4 % 2]
        AT_, uo_, wT_, kd_ = AT2[c4 % 2], uo2[c4 % 2], wT2[c4 % 2], kd2[c4 % 2]
        for src, dst in ((kT, kt), (vT, vt)):
            ps = pp[nbox[0] % 2]; nbox[0] += 1
            for cc in range(4):
                c = c0 + cc
                P.tr(ps[0:64, cc * 128:(cc + 1) * 128], src[:, c * 64:(c + 1) * 64], ident[:])
            P.I("act", "copy", out=dst[:].re("p c d -> p (c d)"), in_=ps[0:64, :])
        chk("ktr")
        yield
        for cc, c in enumerate(grp):
            cs = slice(c * 64, (c + 1) * 64)
            P.mm(pGr[:, cc, :], g[:, c, h:h + 1].bc([64, 64]), tri)
            P.mm(pKK[:, cc, :], kT[:, cs], kT[:, cs])
        for cc, c in enumerate(grp):
            cs = slice(c * 64, (c + 1) * 64)
            P.mm(pQK[:, cc, :], kT[:, cs], qT[:, cs])
        yield
        P.I("dve", "tensor_tensor", out=dd[:], in0=pGr, in1=colg(gc, c0, 64), op=ALU.subtract)
        P.I("dve", "tensor_scalar", out=dec[:], in0=dd[:], scalar1=0.0, scalar2=None, op0=ALU.max)
        P.I("dve", "tensor_scalar", out=decT[:], in0=dd[:], scalar1=0.0, scalar2=None, op0=ALU.min)
        P.I("act", "activation", out=dec[:], in_=dec[:], func=AF.Exp, scale=-1.0)
        P.I("act", "activation", out=decT[:], in_=decT[:], func=AF.Exp)
        P.I("dve", "tensor_tensor", out=dec[:], in0=dec[:], in1=m64(stril), op=ALU.mult)
        P.I("dve", "tensor_tensor", out=decT[:], in0=decT[:], in1=m64(tri), op=ALU.mult)
        yield
        P.I("dve", "tensor_tensor", out=Lp[0][:], in0=pKK, in1=dec[:], op=ALU.mult)
        P.I("dve", "tensor_tensor", out=Lp[0][:], in0=Lp[0][:], in1=colg(beta, c0, 64), op=ALU.mult)
        P.I("dve", "tensor_tensor", out=AT_[:], in0=pQK, in1=decT[:], op=ALU.mult)
        chk("dec")
        yield
        for cc in range(GS):
            P.mm(pU[:, cc, :], Lp[0][:, cc, :], id64)
        P.I("act", "copy", out=Up[0][:], in_=pU)
        P.I("dve", "tensor_tensor", out=Pm[0][:], in0=m64(id64), in1=Up[0][:], op=ALU.subtract)
        chk("utr")
        yield
        for it in range(5):
            a, b = it % 2, (it + 1) % 2
            last = it == 4
            for cc in range(GS):
                P.mm(pL2[:, cc, :], Up[a][:, cc, :], Lp[a][:, cc, :])
            if not last:
                for cc in range(GS):
                    P.mm(pU2[:, cc, :], Lp[a][:, cc, :], Up[a][:, cc, :])
            yield
            P.I("dve", "tensor_copy", out=Lp[b][:], in_=pL2)
            if not last:
                P.I("dve", "tensor_copy", out=Up[b][:], in_=pU2)
            yield
            for cc in range(GS):
                P.mm(pPr[:, cc, :], Lp[b][:, cc, :], Pm[a][:, cc, :])
            yield
            P.I("dve", "tensor_tensor", out=Pm[b][:], in0=pPr, in1=Pm[a][:], op=ALU.add)
            yield
        chk("inv")
        PT = Pm[1]
        P.I("dve", "tensor_tensor", out=kbg[:], in0=kt[:], in1=colg(bk, c0, 128), op=ALU.mult)
        P.I("dve", "tensor_tensor", out=vb[:], in0=vt[:], in1=colg(beta, c0, 128), op=ALU.mult)
        P.I("dve", "tensor_tensor", out=kd_[:], in0=kt[:], in1=colg(ekd, c0, 128), op=ALU.mult)
        yield
        for cc in range(GS):
            P.mm(pUo[:, cc, :], PT[:, cc, :], vb[:, cc, :])
        P.I("act", "copy", out=uo_[:], in_=pUo)
        for cc in range(GS):
            P.mm(pW[:, cc, :], kbg[:, cc, :], PT[:, cc, :])
        P.I("dve", "tensor_copy", out=wT_[:], in_=pW)
        chk("wu")

    def scan_steps(c0):
        grp = list(range(c0, c0 + GS))
        c4 = c0 // 4
        oa = oall[c4 % 2]
        AT_, uo_, wT_, kd_ = AT2[c4 % 2], uo2[c4 % 2], wT2[c4 % 2], kd2[c4 % 2]
        for cc, c in enumerate(grp):
            Sa, Sb = S[c % 2], S[(c + 1) % 2]
            vn = vnew[c % 2]; oo = o1[c % 2]
            P.mm(psc[0][0:64, :], wT_[:, cc, :], Sa[:])
            P.mm(psc[1][0:64, :], qT[:, c * 64:(c + 1) * 64], Sa[:])
            yield
            P.I("dve", "tensor_tensor", out=vn[:], in0=uo_[:, cc, :], in1=psc[0][0:64, :], op=ALU.subtract)
            yield
            P.mm(psc[2][0:64, :], AT_[:, cc, :], vn[:])
            P.mm(psc[0][:, :], kd_[:, cc, :], vn[:])
            yield
            P.I("dve", "scalar_tensor_tensor", out=Sb[:], in0=Sa[:], scalar=egl[:, c, h:h + 1],
                in1=psc[0][:, :], op0=ALU.mult, op1=ALU.add)
            P.I("dve", "tensor_copy", out=oo[:], in_=psc[2][0:64, :])
            P.I("dve", "scalar_tensor_tensor", out=oa[:, cc, :], in0=psc[1][0:64, :],
                scalar=col(eg, c), in1=oo[:], op0=ALU.mult, op1=ALU.add)
            yield
        chk("scan")
        osq = vb
        P.I("act", "activation", out=osq[:], in_=oa[:], func=AF.Square)
        P.I("dve", "tensor_reduce", out=ssq[:], in_=osq[:], axis=AX.X, op=ALU.add)
        P.I("dve", "tensor_scalar", out=ssq[:], in0=ssq[:], scalar1=1.0 / 128, scalar2=EPS,
            op0=ALU.mult, op1=ALU.add)
        rsqrt_inplace(P, ssq[:])
        P.I("dve", "tensor_tensor", out=oa[:], in0=oa[:],
            in1=ssq[:].re("p (c o) -> p c o", o=1).bc([64, 4, 128]), op=ALU.mult)
        ps = pp[nbox[0] % 2]; nbox[0] += 1
        for cc in range(4):
            P.tr(ps[:, cc * 64:(cc + 1) * 64], oa[:, cc, :], id64)
        P.I("dve", "scalar_tensor_tensor", out=yaT[:, h, c4 * 256:(c4 + 1) * 256], in0=ps[:, 0:256],
            scalar=G["dnw"][:, 0:1], in1=szT[:, c4 * 256:(c4 + 1) * 256], op0=ALU.mult, op1=ALU.mult)

    groups = list(range(0, NCH, GS))
    for _ in intra_steps(groups[0]):
        pass
    for gi, c0 in enumerate(groups):
        it_i = intra_steps(groups[gi + 1]) if gi + 1 < len(groups) else iter(())
        it_s = scan_steps(c0)
        done_i = done_s = False
        while not (done_i and done_s):
            if not done_i:
                try:
                    next(it_i)
                except StopIteration:
                    done_i = True
            if not done_s:
                try:
                    next(it_s)
                except StopIteration:
                    done_s = True


IN_OFF = np.cumsum([0, 512, 512, 512, 512, 4, 4, 256, 384, 12, 512])


def kchunk(w):
    n = w.shape[1]
    return np.ascontiguousarray(w.reshape(8, 128, n).transpose(1, 0, 2))


def prep_shared(inp):
    f = lambda a: np.ascontiguousarray(np.asarray(a, dtype=np.float32))
    L = L_DEPTH
    sh = {}
    sh["consts"] = make_consts()
    sh["anw"] = f(np.asarray(inp["attn_norm_w"]).reshape(L, 8, 128).transpose(0, 2, 1))[..., None]
    w_in = np.asarray(inp["w_in"])
    w_dn = np.zeros((L, 4, 128, 8, 512), np.float32)
    w_tm = np.zeros((L, 128, 8, 20), np.float32)
    for l in range(L):
        for h in range(4):
            cols = np.concatenate([np.arange(o + h * 128, o + (h + 1) * 128) for o in (0, 512, 1024, 1536)])
            w_dn[l, h] = kchunk(w_in[l][:, cols])
        cols = np.concatenate([np.arange(2048, 2056), np.arange(2696, 2708)])
        w_tm[l] = kchunk(w_in[l][:, cols])
    sh["w_dn"] = w_dn
    sh["w_tm"] = w_tm
    cw = np.asarray(inp["dn_conv_w"])
    sh["dn_cw"] = f(cw.reshape(L, 4, 3, 4, 128).transpose(0, 4, 3, 2, 1).reshape(L, 128, 12, 4))
    sh["dn_alog"] = f(inp["dn_a_log"])
    sh["dn_dtb"] = f(inp["dn_dt_bias"])
    sh["dn_nw"] = f(np.asarray(inp["dn_norm_w"]).reshape(L, 128, 1))
    sh["w_out"] = f(inp["w_out"])
    prep_nsa(inp, sh)
    prep_conv(inp, sh)
    prep_moe(inp, sh)
    return sh


def build_nc(nlayers=L_DEPTH, stages=("dn", "nsa", "conv", "moe"), dbg=(), nexp=NE):
    nc = bass.Bass("TRN2", target_bir_lowering=False)
    d = {}

    def inp(name, shape):
        d[name] = nc.dram_tensor(name, list(shape), F32, kind="ExternalInput").ap()

    inp("x", [T, D]); inp("consts", [128, 768]); inp("anw", [2, 128, 8, 1])
    inp("w_out", [2, D, D])
    inp("w_nsa_fm", [2, 128, 8, 640]); inp("w_nsa_tm", [2, 128, 8, 128]); inp("nsa_nw", [2, 128, 3])
    inp("nsa_knw0", [2, 64]); inp("nsa_posT", [2, 128, 32]); inp("nsa_w1", [2, 128, 32, 128])
    inp("nsa_w2", [2, 128, 128]); inp("nsa_bias_cmp", [4, NCMP, T]); inp("nsa_bias_tile", [128, 4, 2, 128])
    inp("nsa_c31", [128, 4]); inp("nsa_sel_tab", [128, 3, NT, 32]); inp("nsa_expand", [32, T])
    inp("nsa_overlap", [NCMP, 32]); inp("nsa_mwin", [128, 8, 512])
    inp("w_conv", [2, 128, 8, 512]); inp("conv_prm", [2, 128, 2, 34])
    inp("fnw_row", [2, D]); inp("fnw", [2, 128, 8, 1]); inp("router_w", [2, 128, 8, NE]); inp("router_b", [2, NE])
    inp("w_gu", [2, NE, 8, 128, 2048]); inp("w_dn_moe", [2, NE, D, D]); inp("b_gu", [2, NE, 128, 16])
    inp("b_dn", [2, NE, D])
    inp("w_dn", [2, 4, 128, 8, 512]); inp("w_tm", [2, 128, 8, 20]); inp("dn_cw", [2, 128, 12, 4])
    inp("dn_alog", [2, 4]); inp("dn_dtb", [2, 4]); inp("dn_nw", [2, 128, 1])
    out = nc.dram_tensor("out", [T, D], F32, kind="ExternalOutput").ap()
    dout = {}
    for name, shape in dbg:
        dout[name] = nc.dram_tensor(name, list(shape), F32, kind="ExternalOutput").ap()

    with contextlib.ExitStack() as st:
        P = Prog(nc, st)
        cx = Ctx()
        consts = P.sb([128, 768])
        P.ld(consts[:], d["consts"])
        cx.ident_f = consts[:, 0:128]
        cx.ones_f = consts[:, 128:256]
        cx.tri = consts[0:64, 256:320]
        cx.tril = consts[0:64, 320:384]
        cx.bd_ones = consts[:, 384:512]
        cx.sut = consts[:, 512:640]
        cx.iota = consts[:, 640:768]
        cx.dbg_ybT = dout.get("ybT")
        cx.dout = dout
        cx.nexp = nexp
        identb = P.sb([128, 128], BF16)
        P.I("dve", "tensor_copy", out=identb[:], in_=consts[:, 0:128])
        cx.ident_bf = identb
        cx.stg_n = 256
        cx.stg_i = 0
        cx.nrm_ss = [P.sb([128, 1]) for _ in range(2)]
        xres = P.sbl(NT, [128, D], name="xres")
        for i in range(NT):
            P.ld(xres[i][:], d["x"][i * 128:(i + 1) * 128, :])
        for l in range(nlayers):
          try:
            if l > 0:
                P.new_epoch()
            with P.scope():
                hT = P.sb([128, 8, T], BF16)
                nw = P.sb([128, 8, 1])
                P.ld(nw[:], d["anw"][l])
                with P.scope():
                    cx.nrm_pt = [P.ps([128, 8, 128], BF16) for _ in range(2)]
                    cx.nrm_xn = [P.sb([128, D], BF16) for _ in range(2)]
                    norm_to_T(P, cx, xres, nw[:], hT)
                chk("norm")
                if "dn" in stages:
                    with P.scope():
                        yaT = P.sb([128, 4, T], BF16)
                        deltanet(P, cx, l, hT, xres, yaT, d)
                        if "yaT" in dout:
                            with P.scope():
                                tmp = P.sb([128, 4, T])
                                P.I("dve", "tensor_copy", out=tmp[:], in_=yaT[:])
                                P.st(dout["yaT"], tmp[:])
                        apply_wout(P, cx, l, yaT, 0, 4, xres, d)
                if "nsa" in stages:
                    nsa(P, cx, l, hT, xres, d)
                if "conv" in stages:
                    conformer(P, cx, l, hT, xres, d)
            if "moe" in stages:
                P.new_epoch()
                (moe_sparse if MOE_SPARSE[0] else moe)(P, cx, l, xres, d, nexp=cx.nexp)
          except StopBuild:
            break
        DEAD[0] = False
        for i in range(NT):
            P.st(out[i * 128:(i + 1) * 128, :], xres[i][:])
        P.finish([])
    return nc


NEGM = -200.0
NCMP = 127


def t5_bucket_np(dist):
    n = np.maximum(dist, 0)
    nf = np.maximum(n, 1).astype(np.float32)
    large = 16 + (np.log(nf / np.float32(16)) / np.float32(math.log(8.0)) * np.float32(16)).astype(np.int32)
    large = np.minimum(large, 31)
    return np.where(n < 16, n, large)


def nsa_tables(rel_bias):
    rb = np.asarray(rel_bias, np.float32)
    tb = {}
    t = np.arange(T)
    j = np.arange(NCMP)
    dist = t[None, :] - (j[:, None] * 16 + 31)
    bk = t5_bucket_np(dist)
    bc = rb[bk]
    bc = np.where((dist >= 0)[..., None], bc, np.float32(NEGM))
    tb["bias_cmp"] = np.ascontiguousarray(bc.transpose(2, 0, 1)).astype(np.float32)
    k = np.arange(128)
    tt = np.arange(128)
    d0 = tt[None, :] - k[:, None]
    d1 = d0 + 128
    b0 = np.where((d0 >= 0)[..., None], rb[t5_bucket_np(d0)], np.float32(NEGM))
    b1 = rb[t5_bucket_np(d1)]
    tbl = np.stack([b0, b1], axis=0)
    tb["bias_tile"] = np.ascontiguousarray(tbl.transpose(1, 3, 0, 2)).astype(np.float32)
    tb["c31"] = np.ascontiguousarray(np.broadcast_to(rb[31][None, :], (128, 4))).astype(np.float32)
    tok = (np.arange(NT)[None, :] * 128 + np.arange(128)[:, None])
    cur = tok // 64
    s = np.arange(32)[None, None, :]
    causal = s <= cur[..., None]
    forced = (s == 0) | (s == cur[..., None]) | (s == cur[..., None] - 1)
    m1 = (causal & ~forced).astype(np.float32)
    cst = np.where(causal & forced, 1e4, np.where(causal, 0.0, -1.0)).astype(np.float32)
    tb["sel_tab"] = np.ascontiguousarray(np.stack([m1, cst, causal.astype(np.float32)], axis=1))
    key = np.arange(T)
    tb["expand"] = (key[None, :] // 64 == np.arange(32)[:, None]).astype(np.float32)
    cs = j * 16
    ss = np.arange(32) * 64
    ov = ((cs[:, None] < ss[None, :] + 64) & (cs[:, None] + 32 > ss[None, :])).astype(np.float32)
    tb["overlap"] = ov
    mw = np.zeros((128, 8, 512), np.float32)
    for r in range(8):
        rel = r - 4
        keyp = rel * 128 + k[:, None]
        tp = np.arange(512)[None, :]
        dd = tp - keyp
        mw[:, r, :] = ((dd >= 0) & (dd < 512)).astype(np.float32)
    tb["mwin"] = mw
    return tb


def prep_nsa(inp, sh):
    L = L_DEPTH
    w_in = np.asarray(inp["w_in"])
    w_fm = np.zeros((L, 128, 8, 640), np.float32)
    w_tmv = np.zeros((L, 128, 8, 128), np.float32)
    for l in range(L):
        q0 = 2056
        kv0 = 2312
        cols = np.concatenate([
            np.arange(q0, q0 + 256),
            np.arange(kv0 + 128, kv0 + 192), np.arange(kv0 + 128, kv0 + 192),
            np.arange(kv0 + 256, kv0 + 320), np.arange(kv0 + 256, kv0 + 320),
            np.arange(kv0, kv0 + 128),
        ])
        w_fm[l] = kchunk(w_in[l][:, cols])
        cols = np.concatenate([np.arange(kv0 + 192, kv0 + 256), np.arange(kv0 + 320, kv0 + 384)])
        w_tmv[l] = kchunk(w_in[l][:, cols])
    sh["w_nsa_fm"] = w_fm
    sh["w_nsa_tm"] = w_tmv
    qn = np.asarray(inp["nsa_q_norm_w"], np.float32)
    kn = np.asarray(inp["nsa_k_norm_w"], np.float32)
    nw = np.zeros((L, 128, 3), np.float32)
    nw[:, :, 0] = np.concatenate([qn, qn], axis=1)
    nw[:, :, 1] = np.concatenate([kn[:, 1], kn[:, 1]], axis=1)
    nw[:, :, 2] = np.concatenate([kn[:, 2], kn[:, 2]], axis=1)
    sh["nsa_nw"] = nw
    sh["nsa_knw0"] = np.ascontiguousarray(kn[:, 0, :])
    pos = np.asarray(inp["nsa_cmp_pos"], np.float32)
    sh["nsa_posT"] = np.ascontiguousarray(pos.transpose(0, 1, 3, 2).reshape(L, 128, 32))
    w1 = np.asarray(inp["nsa_cmp_w1"], np.float32)
    sh["nsa_w1"] = np.ascontiguousarray(w1.reshape(L, 2, 32, 64, 128).transpose(0, 1, 3, 2, 4).reshape(L, 128, 32, 128))
    w2 = np.asarray(inp["nsa_cmp_w2"], np.float32)
    sh["nsa_w2"] = np.ascontiguousarray(w2.transpose(0, 2, 1, 3).reshape(L, 128, 128))
    tb = nsa_tables(inp["rel_bias"])
    for k_, v_ in tb.items():
        sh["nsa_" + k_] = v_


def apply_wout(P, cx, l, yT, chunk0, nch, xres, d):
    with P.scope():
        w = P.sb([128, nch, D], BF16)
        for c in range(nch):
            load_cast(P, cx, d["w_out"][l, (chunk0 + c) * 128:(chunk0 + c + 1) * 128, :], w[:, c, :], D)
        pp = [P.ps([128, 512]) for _ in range(2)]
        n = 0
        for i in range(NT):
            for half in range(2):
                ps = pp[n % 2]; n += 1
                for c in range(nch):
                    P.mm(ps[:], yT[:, c, i * 128:(i + 1) * 128], w[:, c, half * 512:(half + 1) * 512],
                         start=(c == 0), stop=(c == nch - 1))
                P.I("dve", "tensor_tensor", out=xres[i][:, half * 512:(half + 1) * 512], in0=ps[:],
                    in1=xres[i][:, half * 512:(half + 1) * 512], op=ALU.add)


def nsa(P, cx, l, hT, xres, d):
    TINY = 1e-30
    with P.scope():
        qT = P.sb([128, 2, T], BF16)
        kslcT = P.sb([128, T], BF16)
        kwinT = P.sb([128, T], BF16)
        cmpT = P.sb([128, T], BF16)
        vaug = P.sb([128, NT, 2, 66], BF16)
        gts = P.sb([128, NT, 12])
        ynsa = P.sb([128, NT, 256])
        impacc = P.sb([128, NT, 32])
        nw = P.sb([128, 3])
        P.ld(nw[:], d["nsa_nw"][l])
        qw = P.sb([128, 1])
        P.I("dve", "tensor_scalar", out=qw[:], in0=nw[:, 0:1], scalar1=0.125, scalar2=None, op0=ALU.mult)
        P.I("pool", "memset", ap=vaug[:, :, :, 64:66], constant=1.0)
        with P.scope():
            wfm = P.sb([128, 8, 640], BF16)
            load_cast(P, cx, d["w_nsa_fm"][l].rearrange("p c n -> p (c n)"), wfm[:].re("p c n -> p (c n)"), 8 * 640)
            wtv = P.sb([128, 8, 128], BF16)
            load_cast(P, cx, d["w_nsa_tm"][l].rearrange("p c n -> p (c n)"), wtv[:].re("p c n -> p (c n)"), 8 * 128)
            wtm = P.sb([128, 8, 20], BF16)
            load_cast(P, cx, d["w_tm"][l].rearrange("p c n -> p (c n)"), wtm[:].re("p c n -> p (c n)"), 160)
            pp = [P.ps([128, 512]) for _ in range(2)]
            pq = [P.ps([128, 512]) for _ in range(2)]
            qs = [P.sb([128, 512]) for _ in range(2)]
            sq = P.sb([128, 512]); rn = P.sb([128, 512])
            n = 0
            for ch in range(5):
                for tg in range(4):
                    sl = slice(tg * 512, (tg + 1) * 512)
                    ps = pp[n % 2]; ps2 = pq[n % 2]; qsb = qs[n % 2]; n += 1
                    for kc in range(8):
                        P.mm(ps[:], wfm[:, kc, ch * 128:(ch + 1) * 128], hT[:, kc, sl], start=(kc == 0), stop=(kc == 7))
                    if ch == 4:
                        P.I("act", "copy", out=cmpT[:, sl], in_=ps[:])
                        continue
                    P.I("act", "copy", out=qsb[:], in_=ps[:])
                    P.I("act", "activation", out=sq[:], in_=qsb[:], func=AF.Square)
                    P.mm(ps2[:], cx.bd_ones, sq[:])
                    P.I("dve", "tensor_scalar", out=rn[:], in0=ps2[:], scalar1=1.0 / 64, scalar2=EPS,
                        op0=ALU.mult, op1=ALU.add)
                    rsqrt_inplace(P, rn[:])
                    if ch < 2:
                        dst, wc = qT[:, ch, sl], qw[:, 0:1]
                    elif ch == 2:
                        dst, wc = kslcT[:, sl], nw[:, 1:2]
                    else:
                        dst, wc = kwinT[:, sl], nw[:, 2:3]
                    P.I("dve", "scalar_tensor_tensor", out=dst, in0=qsb[:], scalar=wc, in1=rn[:],
                        op0=ALU.mult, op1=ALU.mult)
            for i in range(NT):
                ps = pp[n % 2]; n += 1
                for kc in range(8):
                    P.mm(ps[:, 0:128], hT[:, kc, i * 128:(i + 1) * 128], wtv[:, kc, :], start=(kc == 0), stop=(kc == 7))
                for kc in range(8):
                    P.mm(ps[:, 128:140], hT[:, kc, i * 128:(i + 1) * 128], wtm[:, kc, 8:20], start=(kc == 0), stop=(kc == 7))
                P.I("act", "copy", out=vaug[:, i, :, 0:64], in_=ps[:, 0:128].re("p (a b) -> p a b", b=64))
                P.I("act", "activation", out=gts[:, i, :], in_=ps[:, 128:140], func=AF.Sigmoid)
        chk("nsa_proj")
        with P.scope():
            w1 = P.sb([128, 32, 128], BF16)
            load_cast(P, cx, d["nsa_w1"][l].rearrange("p c n -> p (c n)"), w1[:].re("p c n -> p (c n)"), 4096)
            w2 = P.sb([128, 128], BF16)
            load_cast(P, cx, d["nsa_w2"][l], w2[:], 128)
            posT = P.sb([128, 32], BF16)
            load_cast(P, cx, d["nsa_posT"][l], posT[:], 32)
            knw0 = P.sb([128, 64])
            P.ld(knw0[0:NCMP, :], d["nsa_knw0"][l:l + 1, :].partition_broadcast(NCMP))
            ovf = P.sb([128, 32])
            P.ld(ovf[0:NCMP, :], d["nsa_overlap"])
            rhsc = P.sb([128, 98], BF16)
            hs = P.sb([128, 2, 128], BF16)
            hb = P.sb([128, 2])
            kcT = P.sb([128, 128], BF16)
            ph = P.ps([128, 512])
            pk = P.ps([128, 512])
            ptb = P.ps([128, 128], BF16)
            ph2 = P.ps([128, 512])
            phs = [ph, ph2]
            for which in range(2):
                pr = slice(which * 64, which * 64 + 64)
                for l_ in range(32):
                    P.mm(phs[which][:, 0:NCMP], w1[pr, l_, :], cmpT[pr, l_:l_ + 16 * (NCMP - 1) + 1:16],
                         start=(l_ == 0), stop=(l_ == 31))
                for l_ in range(32):
                    P.mm(phs[which][:, 256:257], w1[pr, l_, :], posT[pr, l_:l_ + 1],
                         start=(l_ == 0), stop=(l_ == 31))
            for which in range(2):
                P.I("dve", "tensor_copy", out=hb[:, which:which + 1], in_=phs[which][:, 256:257])
            for which in range(2):
                P.I("act", "activation", out=hs[:, which, 0:NCMP], in_=phs[which][:, 0:NCMP],
                    func=AF.Silu, bias=hb[:, which:which + 1])
            dbg_dump(P, cx, "phk", ph[:, 0:NCMP], [128, NCMP])
            dbg_dump(P, cx, "hb", hb[:], [128, 2])
            dbg_dump(P, cx, "cmpT", cmpT[:], [128, T])
            P.mm(pk[0:NCMP, 0:64], hs[:, 0, 0:NCMP], w2[:, 0:64])
            P.mm(pk[0:NCMP, 64:128], hs[:, 1, 0:NCMP], w2[:, 64:128])
            kc = P.sb([128, 64]); kq = P.sb([128, 64]); kss = P.sb([128, 1]); kcd = P.sb([128, 2, 64], BF16)
            P.I("dve", "tensor_copy", out=kc[0:NCMP, :], in_=pk[0:NCMP, 0:64])
            P.I("dve", "tensor_tensor", out=kq[0:NCMP, :], in0=kc[0:NCMP, :], in1=kc[0:NCMP, :], op=ALU.mult)
            P.I("dve", "tensor_reduce", out=kss[0:NCMP, :], in_=kq[0:NCMP, :], axis=AX.X, op=ALU.add)
            P.I("dve", "tensor_scalar", out=kss[0:NCMP, :], in0=kss[0:NCMP, :], scalar1=1.0 / 64, scalar2=EPS,
                op0=ALU.mult, op1=ALU.add)
            rsqrt_inplace(P, kss[0:NCMP, :])
            P.I("dve", "scalar_tensor_tensor", out=kc[0:NCMP, :], in0=kc[0:NCMP, :], scalar=kss[0:NCMP, 0:1],
                in1=knw0[0:NCMP, :], op0=ALU.mult, op1=ALU.mult)
            P.I("pool", "memset", ap=kcd[:], constant=0.0)
            P.I("dve", "tensor_copy", out=kcd[0:NCMP, 0, :], in_=kc[0:NCMP, :])
            P.I("dve", "tensor_copy", out=kcd[0:NCMP, 1, :], in_=kc[0:NCMP, :])
            P.tr(ptb[:, :], kcd[:].re("p a b -> p (a b)"), cx.ident_bf[:])
            P.I("dve", "tensor_copy", out=kcT[:], in_=ptb[:])
            P.I("pool", "memset", ap=rhsc[:], constant=1.0)
            P.I("dve", "tensor_copy", out=rhsc[0:NCMP, 0:64], in_=pk[0:NCMP, 64:128])
            P.I("dve", "tensor_copy", out=rhsc[0:NCMP, 65:97], in_=ovf[0:NCMP, :])
            dbg_dump(P, cx, "kcT", kcT[:], [128, 128])
            dbg_dump(P, cx, "rhsc", rhsc[:], [128, 98])
            dbg_dump(P, cx, "hs", hs[:, :, 0:NCMP], [128, 2, NCMP])
            dbg_dump(P, cx, "kc", kc[0:NCMP, :], [NCMP, 64])
            chk("nsa_cmpkv")
            bt = [P.sb([128, 512]) for _ in range(2)]
            ssb = [P.sb([128, 512]) for _ in range(2)]
            pTb = [P.sb([128, 512], BF16) for _ in range(2)]
            psS = [P.ps([128, 512]) for _ in range(2)]
            psO = [P.ps([128, 512]) for _ in range(2)]
            rr = P.sb([128, 4, 1]); g0 = P.sb([128, 4, 1]); itmp = P.sb([128, 4, 32])
            n = 0
            for h in range(4):
                hp = slice((h % 2) * 64, (h % 2) * 64 + 64)
                for tg in range(4):
                    sl = slice(tg * 512, (tg + 1) * 512)
                    tl = slice(tg * 4, tg * 4 + 4)
                    b_ = bt[n % 2]; s_ = ssb[n % 2]; p_ = pTb[n % 2]; pS = psS[n % 2]; pO = psO[n % 2]; n += 1
                    pOv = pO[:].re("p (a b) -> p a b", b=128)
                    P.ld(b_[0:NCMP, :], d["nsa_bias_cmp"][h, :, sl])
                    P.mm(pS[0:NCMP, :], kcT[hp, 0:NCMP], qT[hp, h // 2, sl])
                    P.I("dve", "tensor_tensor", out=s_[0:NCMP, :], in0=pS[0:NCMP, :], in1=b_[0:NCMP, :], op=ALU.add)
                    P.I("act", "activation", out=p_[0:NCMP, :], in_=s_[0:NCMP, :], func=AF.Exp)
                    for i4 in range(4):
                        P.mm(pOv[:, i4, 0:97], p_[0:NCMP, i4 * 128:(i4 + 1) * 128], rhsc[0:NCMP, 0:97])
                    P.I("dve", "tensor_scalar", out=rr[:], in0=pOv[:, :, 64:65], scalar1=TINY, scalar2=None, op0=ALU.max)
                    P.I("dve", "reciprocal", out=rr[:], in_=rr[:])
                    P.I("dve", "tensor_tensor", out=g0[:], in0=rr[:], in1=gts[:, tl, h * 3:h * 3 + 1], op=ALU.mult)
                    P.I("dve", "tensor_tensor", out=ynsa[:, tl, h * 64:(h + 1) * 64], in0=pOv[:, :, 0:64],
                        in1=g0[:].bc([128, 4, 64]), op=ALU.mult)
                    if h == 0:
                        P.I("dve", "tensor_tensor", out=impacc[:, tl, :], in0=pOv[:, :, 65:97],
                            in1=rr[:].bc([128, 4, 32]), op=ALU.mult)
                    else:
                        P.I("dve", "tensor_tensor", out=itmp[:], in0=pOv[:, :, 65:97],
                            in1=rr[:].bc([128, 4, 32]), op=ALU.mult)
                        P.I("dve", "tensor_tensor", out=impacc[:, tl, :], in0=impacc[:, tl, :], in1=itmp[:], op=ALU.add)
        chk("nsa_cmp")
        selT = P.sb([32, T], BF16)
        with P.scope():
            stab = P.sb([128, 3, NT, 32])
            P.ld(stab[:], d["nsa_sel_tab"])
            imp2 = P.sb([128, NT, 32])
            P.I("dve", "tensor_tensor", out=imp2[:], in0=impacc[:], in1=stab[:, 0], op=ALU.mult)
            P.I("dve", "tensor_tensor", out=imp2[:], in0=imp2[:], in1=stab[:, 1], op=ALU.add)
            wk = P.sb([128, NT, 32]); m8 = P.sb([128, NT, 8]); m8b = P.sb([128, NT, 8])
            selb = P.sb([128, NT, 32], BF16); self32 = P.sb([128, NT, 32])
            pt = [P.ps([128, 512], BF16) for _ in range(2)]
            for i in range(NT):
                P.I("dve", "max", out=m8[:, i, :], in_=imp2[:, i, :])
                P.I("dve", "match_replace", out=wk[:, i, :], in_to_replace=m8[:, i, :], in_values=imp2[:, i, :], imm_value=-1e9)
                P.I("dve", "max", out=m8b[:, i, :], in_=wk[:, i, :])
            P.I("dve", "tensor_tensor", out=self32[:], in0=imp2[:], in1=m8b[:, :, 7:8].bc([128, NT, 32]), op=ALU.is_ge)
            P.I("dve", "tensor_tensor", out=selb[:], in0=self32[:], in1=stab[:, 2], op=ALU.mult)
            for i4 in range(NT // 4):
                for ii in range(4):
                    P.tr(pt[i4 % 2][0:32, ii * 128:(ii + 1) * 128], selb[:, i4 * 4 + ii, :], cx.ident_bf[:])
                P.I("act", "copy", out=selT[:, i4 * 512:(i4 + 1) * 512], in_=pt[i4 % 2][0:32, :])
        chk("nsa_sel")
        with P.scope():
            Tb = P.sb([128, 4, 2, 128])
            P.ld(Tb[:], d["nsa_bias_tile"])
            c31 = P.sb([128, 4])
            P.ld(c31[:], d["nsa_c31"])
            E = P.sb([32, T], BF16)
            load_cast(P, cx, d["nsa_expand"], E[:], T, parts=32)
            mskall = P.sb([128, 16, 512], BF16)
            mwin = P.sb([128, 8, 512], BF16)
            load_cast(P, cx, d["nsa_mwin"].rearrange("p a b -> p (a b)"), mwin[:].re("p a b -> p (a b)"), 4096)
            psM = P.ps([128, 512])
            psS = [P.ps([128, 512]) for _ in range(3)]
            psA = P.ps([128, 512])
            eb = [P.sb([128, 512], BF16) for _ in range(3)]
            tmpb = [P.sb([128, 128]) for _ in range(4)]
            pmb = [P.sb([128, 512], BF16) for _ in range(3)]
            rr = P.sb([128, 4, 1]); g0 = P.sb([128, 4, 1]); otmp = P.sb([128, 4, 64])
            accv = psA[:].re("p (a b) -> p a b", b=128)
            units = []
            for br in range(2):
                for g in range(4):
                    kt_lo = 0 if br == 0 else max(0, 4 * g - 4)
                    kts = list(range(kt_lo, 4 * g + 4))
                    for h in range(4):
                        for kt in kts:
                            units.append((br, g, h, kt, kts))

            def build_masks(u):
                br, g, h, kt, kts = units[u]
                if br == 0 and h == 0 and kt == kts[0]:
                    for k2 in kts:
                        P.mm(psM[:], E[:, k2 * 128:(k2 + 1) * 128], selT[:, g * 512:(g + 1) * 512])
                        P.I("act", "copy", out=mskall[:, k2, :], in_=psM[:])

            def stage_a(u):
                br, g, h, kt, kts = units[u]
                kT = kslcT if br == 0 else kwinT
                hp = slice((h % 2) * 64, (h % 2) * 64 + 64)
                rel = kt - 4 * g
                c0 = max(rel, 0) * 128
                pS = psS[u % 3]; e_ = eb[u % 3]
                P.mm(pS[:, c0:512], kT[hp, kt * 128:(kt + 1) * 128], qT[hp, h // 2, g * 512 + c0:(g + 1) * 512])
                cc = c0
                for off in (0, 1):
                    qi = rel + off
                    if 0 <= qi < 4:
                        tm = tmpb[(2 * u + off) % 4]
                        cs = slice(qi * 128, (qi + 1) * 128)
                        P.I("dve", "tensor_tensor", out=tm[:], in0=pS[:, cs], in1=Tb[:, h, off, :], op=ALU.add)
                        P.I("act", "activation", out=e_[:, cs], in_=tm[:], func=AF.Exp)
                        cc = (qi + 1) * 128
                if cc < 512:
                    P.I("act", "activation", out=e_[:, cc:512], in_=pS[:, cc:512], func=AF.Exp, bias=c31[:, h:h + 1])

            def stage_b(u):
                br, g, h, kt, kts = units[u]
                rel = kt - 4 * g
                c0 = max(rel, 0) * 128
                tl = slice(4 * g, 4 * g + 4)
                e_ = eb[u % 3]; pm = pmb[u % 3]
                msk = mskall[:, kt, :] if br == 0 else mwin[:, rel + 4, :]
                P.I("dve", "tensor_tensor", out=pm[:, c0:512], in0=e_[:, c0:512], in1=msk[:, c0:512], op=ALU.mult)
                for qi in range(c0 // 128, 4):
                    P.mm(accv[:, qi, 0:65], pm[:, qi * 128:(qi + 1) * 128], vaug[:, kt, br, 0:65],
                         start=(kt == kts[0] and qi == 0), stop=(kt == kts[-1] and qi == 3))
                if kt == kts[-1]:
                    P.I("dve", "tensor_scalar", out=rr[:], in0=accv[:, :, 64:65], scalar1=TINY, scalar2=None, op0=ALU.max)
                    P.I("dve", "reciprocal", out=rr[:], in_=rr[:])
                    P.I("dve", "tensor_tensor", out=g0[:], in0=rr[:], in1=gts[:, tl, h * 3 + 1 + br:h * 3 + 2 + br], op=ALU.mult)
                    P.I("dve", "tensor_tensor", out=otmp[:], in0=accv[:, :, 0:64], in1=g0[:].bc([128, 4, 64]), op=ALU.mult)
                    P.I("dve", "tensor_tensor", out=ynsa[:, tl, h * 64:(h + 1) * 64], in0=ynsa[:, tl, h * 64:(h + 1) * 64],
                        in1=otmp[:], op=ALU.add)

            build_masks(0)
            stage_a(0)
            stage_a(1)
            for u in range(len(units)):
                if u + 2 < len(units):
                    stage_a(u + 2)
                stage_b(u)
                if u + 1 < len(units):
                    build_masks(u + 1)
        chk("nsa_attn")
        with P.scope():
            ybT = P.sb([128, 2, T], BF16)
            yb16 = [P.sb([128, 256], BF16) for _ in range(2)]
            pt = [P.ps([128, 2, 128], BF16) for _ in range(2)]
            for i in range(NT):
                P.I("act", "copy", out=yb16[i % 2][:], in_=ynsa[:, i, :])
                for c in range(2):
                    P.tr(pt[i % 2][:, c, :], yb16[i % 2][:, c * 128:(c + 1) * 128], cx.ident_bf[:])
                P.I("dve", "tensor_copy", out=ybT[:, :, i * 128:(i + 1) * 128], in_=pt[i % 2][:])
            if cx.dbg_ybT is not None:
                with P.scope():
                    tmp = P.sb([128, 2, T])
                    P.I("dve", "tensor_copy", out=tmp[:], in_=ybT[:])
                    P.st(cx.dbg_ybT, tmp[:])
            apply_wout(P, cx, l, ybT, 4, 2, xres, d)


def prep_conv(inp, sh):
    L = L_DEPTH
    w_in = np.asarray(inp["w_in"])
    w = np.zeros((L, 128, 8, 512), np.float32)
    for l in range(L):
        w[l] = kchunk(w_in[l][:, 2708:3220])
    sh["w_conv"] = w
    dw = np.asarray(inp["conv_dw_w"], np.float32)
    prm = np.zeros((L, 128, 2, 34), np.float32)
    prm[:, :, :, 0:31] = dw.reshape(L, 31, 2, 128).transpose(0, 3, 2, 1)
    prm[:, :, :, 31] = np.asarray(inp["conv_dw_b"], np.float32).reshape(L, 2, 128).transpose(0, 2, 1)
    prm[:, :, :, 32] = np.asarray(inp["conv_ln_w"], np.float32).reshape(L, 2, 128).transpose(0, 2, 1)
    prm[:, :, :, 33] = np.asarray(inp["conv_ln_b"], np.float32).reshape(L, 2, 128).transpose(0, 2, 1)
    sh["conv_prm"] = prm


def conformer(P, cx, l, hT, xres, d):
    PAD = 32
    with P.scope():
        ycT = P.sb([128, 2, T], BF16)
        hp = P.sb([128, 2, T + PAD], BF16)
        prm = P.sb([128, 2, 34])
        P.ld(prm[:], d["conv_prm"][l])
        P.I("pool", "memset", ap=hp[:, :, 0:PAD], constant=0.0)
        dg = P.sb([128, 2, 31, 128], BF16)
        for j2 in range(2):
            for j in range(31):
                P.I("dve", "tensor_scalar", out=dg[:, j2, j, :], in0=cx.ident_f,
                    scalar1=prm[:, j2, j:j + 1], scalar2=None, op0=ALU.mult)
        with P.scope():
            wcv = P.sb([128, 8, 512], BF16)
            load_cast(P, cx, d["w_conv"][l].rearrange("p c n -> p (c n)"), wcv[:].re("p c n -> p (c n)"), 4096)
            pa = [P.ps([128, 512]) for _ in range(2)]
            pg = [P.ps([128, 512]) for _ in range(2)]
            sg = [P.sb([128, 512]) for _ in range(2)]
            n = 0
            for j2 in range(2):
                for tg in range(4):
                    sl = slice(tg * 512, (tg + 1) * 512)
                    a_, g_, s_ = pa[n % 2], pg[n % 2], sg[n % 2]; n += 1
                    for kc in range(8):
                        P.mm(a_[:], wcv[:, kc, j2 * 128:(j2 + 1) * 128], hT[:, kc, sl], start=(kc == 0), stop=(kc == 7))
                    for kc in range(8):
                        P.mm(g_[:], wcv[:, kc, (2 + j2) * 128:(3 + j2) * 128], hT[:, kc, sl], start=(kc == 0), stop=(kc == 7))
                    P.I("act", "activation", out=s_[:], in_=g_[:], func=AF.Sigmoid)
                    P.I("dve", "tensor_tensor", out=hp[:, j2, PAD + tg * 512:PAD + (tg + 1) * 512], in0=a_[:], in1=s_[:],
                        op=ALU.mult)
        with P.scope():
            pc = [P.ps([128, 512]) for _ in range(2)]
            pS1 = P.ps([128, 512]); pS2 = P.ps([128, 512])
            xc = [P.sb([128, 512]) for _ in range(2)]
            sq = [P.sb([128, 512]) for _ in range(2)]
            mu = P.sb([128, 512]); var = P.sb([128, 512]); msq = P.sb([128, 512])
            tmp = [P.sb([128, 512]) for _ in range(2)]
            for tg in range(4):
                for j2 in range(2):
                    for j in range(31):
                        o = PAD - 30 + j + tg * 512
                        P.mm(pc[j2][:], dg[:, j2, j, :], hp[:, j2, o:o + 512], start=(j == 0), stop=(j == 30))
                    P.I("act", "activation", out=xc[j2][:], in_=pc[j2][:], func=AF.Identity, bias=prm[:, j2, 31:32])
                    P.I("act", "activation", out=sq[j2][:], in_=xc[j2][:], func=AF.Square)
                for j2 in range(2):
                    P.mm(pS1[:], cx.ones_f, xc[j2][:], start=(j2 == 0), stop=(j2 == 1))
                for j2 in range(2):
                    P.mm(pS2[:], cx.ones_f, sq[j2][:], start=(j2 == 0), stop=(j2 == 1))
                P.I("dve", "tensor_scalar", out=mu[:], in0=pS1[:], scalar1=1.0 / 256, scalar2=None, op0=ALU.mult)
                P.I("dve", "tensor_tensor", out=msq[:], in0=mu[:], in1=mu[:], op=ALU.mult)
                P.I("dve", "scalar_tensor_tensor", out=var[:], in0=pS2[:], scalar=1.0 / 256, in1=msq[:],
                    op0=ALU.mult, op1=ALU.subtract)
                P.I("dve", "tensor_scalar", out=var[:], in0=var[:], scalar1=EPS, scalar2=None, op0=ALU.add)
                rsqrt_inplace(P, var[:])
                for j2 in range(2):
                    P.I("dve", "tensor_tensor", out=tmp[j2][:], in0=xc[j2][:], in1=mu[:], op=ALU.subtract)
                    P.I("dve", "tensor_tensor", out=tmp[j2][:], in0=tmp[j2][:], in1=var[:], op=ALU.mult)
                    P.I("act", "activation", out=ycT[:, j2, tg * 512:(tg + 1) * 512], in_=tmp[j2][:], func=AF.Silu,
                        scale=prm[:, j2, 32:33], bias=prm[:, j2, 33:34])
        dbg_dump(P, cx, "ycT", ycT[:], [128, 2, T])
        apply_wout(P, cx, l, ycT, 6, 2, xres, d)


NE = 32


def prep_moe(inp, sh):
    L = L_DEPTH
    sh["fnw"] = np.ascontiguousarray(
        np.asarray(inp["ffn_norm_w"], np.float32).reshape(L, 8, 128).transpose(0, 2, 1))[..., None]
    sh["router_w"] = np.stack([kchunk(np.asarray(inp["router_w"][l], np.float32)) for l in range(L)])
    sh["fnw_row"] = np.ascontiguousarray(np.asarray(inp["ffn_norm_w"], np.float32))
    sh["router_b"] = np.ascontiguousarray(np.asarray(inp["router_b"], np.float32))
    wgu = np.asarray(inp["w_gate_up"], np.float32)
    r = wgu.reshape(L, NE, 8, 128, 2, 8, 128)
    sh["w_gu"] = np.ascontiguousarray(r.transpose(0, 1, 5, 3, 4, 2, 6)).reshape(L, NE, 8, 128, 2048)
    sh["w_dn_moe"] = np.ascontiguousarray(np.asarray(inp["w_down"], np.float32))
    bgu = np.asarray(inp["b_gate_up"], np.float32)
    sh["b_gu"] = np.ascontiguousarray(bgu.reshape(L, NE, 16, 128).transpose(0, 1, 3, 2))
    sh["b_dn"] = np.ascontiguousarray(np.asarray(inp["b_down"], np.float32))


def moe(P, cx, l, xres, d, nexp=NE):
    with P.scope():
        xnT = P.sb([128, 8, T], BF16)
        rw = P.sb([128, NT, NE])
        with P.scope():
            nw = P.sb([128, 8, 1])
            P.ld(nw[:], d["fnw"][l])
            rwt = P.sb([128, 8, NE])
            P.ld(rwt[:], d["router_w"][l])
            rb = P.sb([128, NE])
            P.ld(rb[:], d["router_b"][l:l + 1, :].partition_broadcast(128))
            xn = [P.sb([128, D]) for _ in range(2)]
            ss = [P.sb([128, 1]) for _ in range(2)]
            xT32 = [P.sb([128, 8, 128]) for _ in range(2)]
            ptr = [P.ps([128, 4, 128]) for _ in range(2)]
            pl = P.ps([128, 512])
            lg = P.sb([128, NE]); m8 = P.sb([128, 8]); nmx = P.sb([128, 1]); ex = P.sb([128, NE])
            msk = P.sb([128, NE]); sm = P.sb([128, 1])
            Bd = P.sb([NE, D])
            P.ld(Bd[:], d["b_dn"][l])
            rwT = [P.sb([NE, 128]) for _ in range(2)]
            ptw = P.ps([128, 512])
            pb = [P.ps([128, 512]) for _ in range(2)]
            for i in range(NT):
                x_, s_, xt = xn[i % 2], ss[i % 2], xT32[i % 2]
                P.I("act", "activation", out=x_[:], in_=xres[i][:], func=AF.Square, accum_out=s_[:])
                P.I("dve", "tensor_scalar", out=s_[:], in0=s_[:], scalar1=1.0 / D, scalar2=EPS, op0=ALU.mult, op1=ALU.add)
                rsqrt_inplace(P, s_[:])
                P.I("dve", "tensor_scalar", out=x_[:], in0=xres[i][:], scalar1=s_[:, 0:1], scalar2=None, op0=ALU.mult)
                for hf in range(2):
                    for c4 in range(4):
                        c = hf * 4 + c4
                        P.tr(ptr[hf][:, c4, :], x_[:, c * 128:(c + 1) * 128], cx.ident_f)
                    P.I("dve", "tensor_tensor", out=xt[:, hf * 4:hf * 4 + 4, :], in0=ptr[hf][:],
                        in1=nw[:, hf * 4:hf * 4 + 4, :].bc([128, 4, 128]), op=ALU.mult)
                P.I("act", "copy", out=xnT[:, :, i * 128:(i + 1) * 128], in_=xt[:])
                for kc in range(8):
                    P.mm(pl[:, 0:NE], xt[:, kc, :], rwt[:, kc, :], start=(kc == 0), stop=(kc == 7))
                P.I("dve", "tensor_tensor", out=lg[:], in0=pl[:, 0:NE], in1=rb[:], op=ALU.add)
                P.I("dve", "max", out=m8[:], in_=lg[:])
                P.I("dve", "tensor_scalar", out=nmx[:], in0=m8[:, 0:1], scalar1=-1.0, scalar2=None, op0=ALU.mult)
                P.I("act", "activation", out=ex[:], in_=lg[:], func=AF.Exp, bias=nmx[:, 0:1])
                P.I("dve", "tensor_scalar", out=msk[:], in0=lg[:], scalar1=m8[:, 3:4], scalar2=None, op0=ALU.is_ge)
                P.I("dve", "tensor_tensor", out=ex[:], in0=ex[:], in1=msk[:], op=ALU.mult)
                P.I("dve", "tensor_reduce", out=sm[:], in_=ex[:], axis=AX.X, op=ALU.add)
                P.I("dve", "reciprocal", out=sm[:], in_=sm[:])
                P.I("dve", "tensor_scalar", out=rw[:, i, :], in0=ex[:], scalar1=sm[:, 0:1], scalar2=None, op0=ALU.mult)
                P.tr(ptw[0:NE, 0:128], rw[:, i, :], cx.ident_f)
                P.I("act", "copy", out=rwT[i % 2][:], in_=ptw[0:NE, 0:128])
                for half in range(2):
                    hs_ = slice(half * 512, (half + 1) * 512)
                    P.mm(pb[half][:], rwT[i % 2][:], Bd[:, hs_])
                    P.I("dve", "tensor_tensor", out=xres[i][:, hs_], in0=pb[half][:], in1=xres[i][:, hs_], op=ALU.add)
        dbg_dump(P, cx, "rw", rw[:], [128, NT, NE])
        chk("moe_router")
        with P.scope():
            actT = P.sbl(8, [128, T], BF16, name="actT%d" % l)
            sgu = [P.sb([128, 2048]) for _ in range(2)]
            wgub = [P.sb([128, 2, 8, 128], BF16) for _ in range(2)]
            sdn = [P.sb([128, D])]
            wdb = P.sbl(8, [128, D], BF16, name="wdb%d" % l)
            bgu = [P.sb([128, 16]) for _ in range(2)]
            bg2 = [P.sb([128, 16]) for _ in range(2)]
            Sb = [P.sb([128, 512]) for _ in range(2)]
            ub = [P.sb([128, 512]) for _ in range(2)]
            pg = [P.ps([128, 512]) for _ in range(2)]
            pu = [P.ps([128, 512]) for _ in range(2)]
            po = [P.ps([128, 512]) for _ in range(2)]
            units = [(e, j) for e in range(nexp) for j in range(8)]
            CS = 1.702 * 7.0 / (1.0 + math.exp(-1.702 * 7.0))
            rwk = P.sb([128, NT, NE])
            P.I("dve", "tensor_scalar", out=rwk[:], in0=rw[:], scalar1=1.0 / 1.702, scalar2=None, op0=ALU.mult)

            def dma_gu(u):
                e, j = units[u]
                P.ld(sgu[u % 2][:], d["w_gu"][l, e, j])

            def cast_gu(u):
                P.I("act", "copy", out=wgub[u % 2][:].re("p a c f -> p (a c f)"), in_=sgu[u % 2][:])

            dma_gu(0)
            cast_gu(0)
            n = 0
            no = 0
            for u, (e, j) in enumerate(units):
                if j == 0:
                    P.ld(bgu[e % 2][:], d["b_gu"][l, e])
                    P.I("dve", "tensor_scalar", out=bg2[e % 2][:, 0:8], in0=bgu[e % 2][:, 0:8], scalar1=1.702,
                        scalar2=None, op0=ALU.mult)
                    P.I("dve", "tensor_scalar", out=bg2[e % 2][:, 8:16], in0=bgu[e % 2][:, 8:16], scalar1=1.0,
                        scalar2=None, op0=ALU.add)
                if u + 1 < len(units):
                    dma_gu(u + 1)
                P.ld(sdn[0][:], d["w_dn_moe"][l, e, j * 128:(j + 1) * 128, :])
                wb = wgub[u % 2]
                for tg in range(4):
                    sl = slice(tg * 512, (tg + 1) * 512)
                    g_, u_ = pg[n % 2], pu[n % 2]
                    S_, ub_ = Sb[n % 2], ub[n % 2]
                    n += 1
                    for kc in range(8):
                        P.mm(g_[:], wb[:, 0, kc, :], xnT[:, kc, sl], start=(kc == 0), stop=(kc == 7))
                    for kc in range(8):
                        P.mm(u_[:], wb[:, 1, kc, :], xnT[:, kc, sl], start=(kc == 0), stop=(kc == 7))
                    P.I("act", "activation", out=S_[:], in_=g_[:], func=AF.Silu, scale=1.702, bias=bg2[e % 2][:, j:j + 1])
                    P.I("act", "activation", out=ub_[:], in_=u_[:], func=AF.Identity, bias=bg2[e % 2][:, 8 + j:9 + j])
                    P.I("dve", "tensor_scalar", out=ub_[:], in0=ub_[:], scalar1=-6.0, scalar2=8.0, op0=ALU.max, op1=ALU.min)
                    P.I("dve", "scalar_tensor_tensor", out=actT[j][:, sl], in0=S_[:], scalar=CS, in1=ub_[:],
                        op0=ALU.min, op1=ALU.mult)
                    if tg == 1 and u + 1 < len(units):
                        cast_gu(u + 1)
                    if tg == 3:
                        P.I("act", "copy", out=wdb[j][:], in_=sdn[0][:])
                if j == 7:
                    for i in range(NT):
                        for half in range(2):
                            o_ = po[no % 2]; no += 1
                            hs_ = slice(half * 512, (half + 1) * 512)
                            for jj in range(8):
                                P.mm(o_[:], actT[jj][:, i * 128:(i + 1) * 128], wdb[jj][:, hs_], start=(jj == 0), stop=(jj == 7))
                            P.I("dve", "scalar_tensor_tensor", out=xres[i][:, hs_], in0=o_[:], scalar=rwk[:, i, e:e + 1],
                                in1=xres[i][:, hs_], op0=ALU.mult, op1=ALU.add)


MOE_SPARSE = [True]
CAP = 48
SG = 2 * CAP
NSLOT = 8 * SG


def moe_sparse(P, cx, l, xres, d, nexp=NE):
    with P.scope():
        xtok = P.sbl(NT, [128, D], BF16, name="xtok%d" % l)
        rw = P.sb([128, NT, NE])
        posm = P.sb([128, NT, NE])
        with P.scope():
            nw = P.sb([128, 8, 1])
            P.ld(nw[:], d["fnw"][l])
            nwb = P.sb([128, D])
            P.ld(nwb[:], d["fnw_row"][l:l + 1, :].partition_broadcast(128))
            rwt = P.sb([128, 8, NE])
            P.ld(rwt[:], d["router_w"][l])
            rb = P.sb([128, NE])
            P.ld(rb[:], d["router_b"][l:l + 1, :].partition_broadcast(128))
            xn = [P.sb([128, D]) for _ in range(2)]
            ssa = P.sb([128, NT])
            xT32 = [P.sb([128, 8, 128]) for _ in range(2)]
            ptr = [P.ps([128, 4, 128]) for _ in range(2)]
            pl = P.ps([128, 512])
            ppos = P.ps([128, 512])
            lga = P.sb([128, NT, NE]); m8a = P.sb([128, NT, 8]); exa = P.sb([128, NT, NE])
            mska = P.sb([128, NT, NE]); sma = P.sb([128, NT]); okm = P.sb([128, NT, NE])
            Bd = P.sb([NE, D], BF16)
            P.ld(Bd[:], d["b_dn"][l], q="pool")
            rwT = [P.sb([NE, 128], BF16) for _ in range(2)]
            rwb = P.sb([128, NT, NE], BF16)
            ptw = P.ps([128, 512], BF16)
            pb = [P.ps([128, 512]) for _ in range(2)]
            for i in range(NT):
                P.I("act", "activation", out=xn[i % 2][:], in_=xres[i][:], func=AF.Square, accum_out=ssa[:, i:i + 1])
            P.I("dve", "tensor_scalar", out=ssa[:], in0=ssa[:], scalar1=1.0 / D, scalar2=EPS, op0=ALU.mult, op1=ALU.add)
            rsqrt_inplace(P, ssa[:])
            for i in range(NT):
                x_, xt = xn[i % 2], xT32[i % 2]
                P.I("dve", "tensor_scalar", out=x_[:], in0=xres[i][:], scalar1=ssa[:, i:i + 1], scalar2=None, op0=ALU.mult)
                P.I("dve", "tensor_tensor", out=xtok[i][:], in0=x_[:], in1=nwb[:], op=ALU.mult)
                for hf in range(2):
                    for c4 in range(4):
                        c = hf * 4 + c4
                        P.tr(ptr[hf][:, c4, :], x_[:, c * 128:(c + 1) * 128], cx.ident_f)
                    P.I("act" if hf else "dve", "tensor_tensor" if not hf else "activation",
                        **(dict(out=xt[:, 0:4, :], in0=ptr[0][:], in1=nw[:, 0:4, :].bc([128, 4, 128]), op=ALU.mult)
                           if not hf else dict(out=xt[:, 4:8, :], in_=ptr[1][:], func=AF.Copy)))
                if True:
                    P.I("dve", "tensor_tensor", out=xt[:, 4:8, :], in0=xt[:, 4:8, :],
                        in1=nw[:, 4:8, :].bc([128, 4, 128]), op=ALU.mult)
                for kc in range(8):
                    P.mm(pl[:, 0:NE], xt[:, kc, :], rwt[:, kc, :], start=(kc == 0), stop=(kc == 7))
                P.I("dve", "tensor_tensor", out=lga[:, i, :], in0=pl[:, 0:NE], in1=rb[:], op=ALU.add)
                P.I("dve", "max", out=m8a[:, i, :], in_=lga[:, i, :])
            P.I("dve", "tensor_tensor", out=exa[:], in0=lga[:], in1=m8a[:, :, 0:1].bc([128, NT, NE]), op=ALU.subtract)
            P.I("act", "activation", out=exa[:], in_=exa[:], func=AF.Exp)
            P.I("dve", "tensor_tensor", out=mska[:], in0=lga[:], in1=m8a[:, :, 3:4].bc([128, NT, NE]), op=ALU.is_ge)
            P.I("dve", "tensor_tensor", out=exa[:], in0=exa[:], in1=mska[:], op=ALU.mult)
            P.I("dve", "tensor_reduce", out=sma[:], in_=exa[:], axis=AX.X, op=ALU.add)
            P.I("dve", "reciprocal", out=sma[:], in_=sma[:])
            P.I("dve", "tensor_tensor", out=rw[:], in0=exa[:], in1=sma[:].re("p (a o) -> p a o", o=1).bc([128, NT, NE]),
                op=ALU.mult)
            P.I("dve", "tensor_copy", out=rwb[:], in_=rw[:])
            for i in range(NT):
                P.mm(ppos[:, i * NE:(i + 1) * NE], cx.sut, mska[:, i, :])
            pv = ppos[:].re("p (a b) -> p a b", b=NE)
            P.I("dve", "tensor_scalar", out=okm[:], in0=pv, scalar1=CAP - 0.5, scalar2=None, op0=ALU.is_lt)
            P.I("dve", "tensor_tensor", out=okm[:], in0=okm[:], in1=mska[:], op=ALU.mult)
            for par in range(2):
                P.I("dve", "scalar_tensor_tensor", out=posm[:, par::2, :], in0=pv[:, par::2, :], scalar=1.0 + par * CAP,
                    in1=okm[:, par::2, :], op0=ALU.add, op1=ALU.mult)
            P.I("dve", "tensor_scalar", out=posm[:], in0=posm[:], scalar1=-1.0, scalar2=None, op0=ALU.add)
            for i in range(NT):
                P.tr(ptw[0:NE, 0:128], rwb[:, i, :], cx.ident_bf[:])
                P.I("act", "copy", out=rwT[i % 2][:], in_=ptw[0:NE, 0:128])
                for half in range(2):
                    hs_ = slice(half * 512, (half + 1) * 512)
                    P.mm(pb[half][:], rwT[i % 2][:], Bd[:, hs_])
                    P.I("dve", "tensor_tensor", out=xres[i][:, hs_], in0=pb[half][:], in1=xres[i][:, hs_], op=ALU.add)
        dbg_dump(P, cx, "rw", rw[:], [128, NT, NE])
        dbg_dump(P, cx, "posm", posm[:], [128, NT, NE])
        chk("moe_router")
        with P.scope():
            H = NSLOT // 2
            actT = P.sbl(8, [128, NSLOT], BF16, name="actT%d" % l)
            xgT = P.sbl(8, [128, NSLOT], BF16, name="xgT%d" % l)
            ysb = P.sbl(2, [128, D], BF16, name="ysb%d" % l)
            psel = P.sb([128, NT, SG], BF16)
            pselT = P.sb([128, NT, 128], BF16)
            NWB = 3
            wgub = [P.sb([128, 2, 8, 128], BF16) for _ in range(NWB)]
            wdb = P.sbl(8, [128, D], BF16, name="wdb%d" % l)
            bgu = [P.sb([128, 16]) for _ in range(2)]
            bg2 = [P.sb([128, 16]) for _ in range(2)]
            Sb = [P.sb([128, H]) for _ in range(2)]
            ub = [P.sb([128, H]) for _ in range(2)]
            pg = [P.ps([128, 512]) for _ in range(2)]
            pu = [P.ps([128, 512]) for _ in range(2)]
            po = [P.ps([128, 512]) for _ in range(3)]
            ptp = P.ps([128, 512], BF16)
            CS = 1.702 * 7.0 / (1.0 + math.exp(-1.702 * 7.0))
            rwk = P.sb([128, NT, NE])
            P.I("dve", "tensor_scalar", out=rwk[:], in0=rw[:], scalar1=1.0 / 1.702, scalar2=None, op0=ALU.mult)
            units = [(e, j) for e in range(nexp) for j in range(8)]

            def dma_gu(u):
                e, j = units[u]
                P.ld(wgub[u % NWB][:].re("p a c f -> p (a c f)"), d["w_gu"][l, e, j], q="pool")

            dma_gu(0)
            dma_gu(1)
            n = 0
            no = 0
            for u, (e, j) in enumerate(units):
                if j == 0:
                    P.ld(bgu[e % 2][:], d["b_gu"][l, e])
                    P.I("dve", "tensor_scalar", out=bg2[e % 2][:, 0:8], in0=bgu[e % 2][:, 0:8], scalar1=1.702,
                        scalar2=None, op0=ALU.mult)
                    P.I("dve", "tensor_scalar", out=bg2[e % 2][:, 8:16], in0=bgu[e % 2][:, 8:16], scalar1=1.0,
                        scalar2=None, op0=ALU.add)
                    P.I("dve", "tensor_tensor", out=psel[:], in0=cx.iota[:, 0:SG].re("p (o c) -> p o c", o=1).bc([128, NT, SG]),
                        in1=posm[:, :, e:e + 1].bc([128, NT, SG]), op=ALU.is_equal)
                    for kc in range(8):
                        for hf in range(2):
                            ps = (pg if hf == 0 else pu)[n % 2]
                            for s4 in range(4):
                                st = hf * 4 + s4
                                for r in range(2):
                                    i = 2 * st + r
                                    P.mm(ps[:, s4 * SG:(s4 + 1) * SG], xtok[i][:, kc * 128:(kc + 1) * 128], psel[:, i, :],
                                         start=(r == 0), stop=(r == 1))
                            P.I("act", "copy", out=xgT[kc][:, hf * H:(hf + 1) * H], in_=ps[:, 0:H])
                        n += 1
                    for i4 in range(NT // 4):
                        for ii in range(4):
                            P.tr(ptp[0:SG, ii * 128:(ii + 1) * 128], psel[:, i4 * 4 + ii, :], cx.ident_bf[:])
                        P.I("dve", "tensor_copy", out=pselT[0:SG, i4 * 4:i4 * 4 + 4, :].re("p a b -> p (a b)"), in_=ptp[0:SG, :])
                if u + 2 < len(units):
                    dma_gu(u + 2)
                P.ld(wdb[j][:], d["w_dn_moe"][l, e, j * 128:(j + 1) * 128, :], q="pool")
                wb = wgub[u % NWB]
                for hf in range(2):
                    sl = slice(hf * H, (hf + 1) * H)
                    g_, u_ = pg[n % 2], pu[n % 2]
                    S_, ub_ = Sb[n % 2], ub[n % 2]
                    n += 1
                    for kc in range(8):
                        P.mm(g_[:, 0:H], wb[:, 0, kc, :], xgT[kc][:, sl], start=(kc == 0), stop=(kc == 7))
                    for kc in range(8):
                        P.mm(u_[:, 0:H], wb[:, 1, kc, :], xgT[kc][:, sl], start=(kc == 0), stop=(kc == 7))
                    P.I("act", "activation", out=S_[:], in_=g_[:, 0:H], func=AF.Silu, scale=1.702, bias=bg2[e % 2][:, j:j + 1])
                    P.I("dve", "tensor_scalar", out=ub_[:], in0=u_[:, 0:H], scalar1=bg2[e % 2][:, 8 + j:9 + j], scalar2=-6.0,
                        op0=ALU.add, op1=ALU.max)
                    P.I("dve", "tensor_scalar", out=S_[:], in0=S_[:], scalar1=CS, scalar2=None, op0=ALU.min)
                    P.I("dve", "scalar_tensor_tensor", out=actT[j][:, sl], in0=ub_[:], scalar=8.0, in1=S_[:],
                        op0=ALU.min, op1=ALU.mult)
                if j == 7:
                    def down_st(st):
                        nonlocal no
                        for half in range(2):
                            o_ = po[no % 3]; no += 1
                            hs_ = slice(half * 512, (half + 1) * 512)
                            for jj in range(8):
                                P.mm(o_[0:SG, :], actT[jj][:, st * SG:(st + 1) * SG], wdb[jj][:, hs_], start=(jj == 0), stop=(jj == 7))
                            P.I("act", "copy", out=ysb[st % 2][0:SG, hs_], in_=o_[0:SG, :])

                    def scatter_st(st):
                        nonlocal no
                        for i in (2 * st, 2 * st + 1):
                            for half in range(2):
                                o_ = po[no % 3]; no += 1
                                hs_ = slice(half * 512, (half + 1) * 512)
                                P.mm(o_[:], pselT[0:SG, i, :], ysb[st % 2][0:SG, hs_])
                                P.I("dve", "scalar_tensor_tensor", out=xres[i][:, hs_], in0=o_[:], scalar=rwk[:, i, e:e + 1],
                                    in1=xres[i][:, hs_], op0=ALU.mult, op1=ALU.add)

                    down_st(0)
                    for st in range(8):
                        if st + 1 < 8:
                            down_st(st + 1)
                        scatter_st(st)


_NC_CACHE = {}


def kernel(**inputs):
    x = np.asarray(inputs["x"], np.float32)
    sh = prep_shared(inputs)
    if "nc" not in _NC_CACHE:
        _NC_CACHE["nc"] = build_nc()
    nc = _NC_CACHE["nc"]
    in_maps = []
    for c in range(8):
        m = dict(sh)
        m["x"] = np.ascontiguousarray(x[c])
        in_maps.append(m)
    res = run_bass_kernel_spmd(nc, in_maps, core_ids=list(range(8)))
    return np.stack([np.asarray(r["out"], np.float32) for r in res.results], axis=0)
```

```python
import contextlib
import math
import numpy as np
import concourse.bass as bass
import concourse.mybir as mybir
from concourse.bass_utils import run_bass_kernel_spmd

F32 = mybir.dt.float32
BF16 = mybir.dt.bfloat16
ALU = mybir.AluOpType
AF = mybir.ActivationFunctionType
AX = mybir.AxisListType


class View:
    __slots__ = ("b", "ap")

    def __init__(self, b, ap):
        self.b = b
        self.ap = ap

    def __getitem__(self, k):
        return View(self.b, self.ap[k])

    def bc(self, shape):
        return View(self.b, self.ap.to_broadcast(list(shape)))

    def re(self, pat, **kw):
        return View(self.b, self.ap.rearrange(pat, **kw))

    def bitcast(self, dt):
        return View(self.b, self.ap.bitcast(dt))


class Buf:
    __slots__ = ("t", "w", "r", "name", "psum")

    def __init__(self, t, name, psum=False):
        self.t = t
        self.name = name
        self.w = None
        self.r = {}
        self.psum = psum

    def __getitem__(self, k):
        return View(self, self.t[k])


OUTK = ("out", "accum_out", "ap")
SAME_ENGINE_SYNC = [True]


class Prog:
    NDMA = 16

    def __init__(self, nc, stack):
        self.nc = nc
        self.stack = stack
        self.root_stack = stack
        self.engs = {"pe": nc.tensor, "dve": nc.vector, "act": nc.scalar,
                     "pool": nc.gpsimd, "sp": nc.sync}
        self.sem = {}
        self.cnt = {}
        self.seen = {}
        for k in self.engs:
            self.sem[k] = stack.enter_context(nc.semaphore("s_" + k))
            self.cnt[k] = 0
            self.seen[k] = {}
        for i in range(self.NDMA):
            k = ("d%d" if i < self.NDMA // 2 else "g%d") % (i % (self.NDMA // 2))
            self.sem[k] = stack.enter_context(nc.semaphore("s_" + k))
            self.cnt[k] = 0
        self.dma_rr = {"sp": 0, "pool": 0}
        self.nbuf = 0
        self.allbufs = []
        self.epoch = 0

    def new_epoch(self):
        if DEAD[0]:
            return
        self.barrier()
        self.epoch += 1
        for k in list(self.sem):
            self.sem[k] = self.root_stack.enter_context(self.nc.semaphore("s%d_%s" % (self.epoch, k)))
            self.cnt[k] = 0
        for k in self.seen:
            self.seen[k] = {}
        for b in self.allbufs:
            b.w = None
            b.r = {}

    def sb(self, shape, dtype=F32, name=None):
        self.nbuf += 1
        name = name or ("b%d" % self.nbuf)
        t = self.stack.enter_context(self.nc.sbuf_tensor(name, list(shape), dtype))
        assert self.nc.sbuf_bytes_remaining >= 16640, ("SBUF budget", name, self.nc.sbuf_bytes_remaining)
        b = Buf(t, name)
        self.allbufs.append(b)
        return b

    def ps(self, shape, dtype=F32, name=None):
        self.nbuf += 1
        name = name or ("p%d" % self.nbuf)
        t = self.stack.enter_context(self.nc.psum_tensor(name, list(shape), dtype))
        b = Buf(t, name, psum=True)
        self.allbufs.append(b)
        return b

    def _waits(self, ek, reads, writes):
        needs = {}
        for b in reads:
            if b.w is not None:
                k, c = b.w
                if needs.get(k, 0) < c:
                    needs[k] = c
            if b.psum:
                for k, c in b.r.items():
                    if k != ek and needs.get(k, 0) < c:
                        needs[k] = c
        for b in writes:
            if b.w is not None:
                k, c = b.w
                if k != ek and needs.get(k, 0) < c:
                    needs[k] = c
            for k, c in b.r.items():
                if k != ek and needs.get(k, 0) < c:
                    needs[k] = c
        eng = self.engs[ek]
        seen = self.seen[ek]
        for k, c in needs.items():
            if k == ek and (ek == "pe" or not SAME_ENGINE_SYNC[0]):
                continue
            if seen.get(k, 0) >= c:
                continue
            eng.wait_ge(self.sem[k], c)
            seen[k] = c

    def op(self, ek, reads, writes, fn):
        if DEAD[0]:
            return None
        self._waits(ek, reads, writes)
        ins = fn(self.engs[ek])
        self.cnt[ek] += 1
        c = self.cnt[ek]
        ins.then_inc(self.sem[ek], 1)
        for b in reads:
            b.r[ek] = c
        for b in writes:
            b.w = (ek, c)
            b.r = {}
        return ins

    def I(self, ek, meth, **kw):
        reads, writes, args = [], [], {}
        for k, v in kw.items():
            if isinstance(v, View):
                (writes if k in OUTK else reads).append(v.b)
                args[k] = v.ap
            else:
                args[k] = v
        return self.op(ek, reads, writes, lambda e: getattr(e, meth)(**args))

    def mm(self, out, lhsT, rhs, start=True, stop=True):
        return self.op("pe", [lhsT.b, rhs.b], [out.b],
                       lambda e: e.matmul(out=out.ap, lhsT=lhsT.ap, rhs=rhs.ap, start=start, stop=stop))

    def tr(self, out, in_, ident):
        return self.op("pe", [in_.b, ident.b], [out.b],
                       lambda e: e.transpose(out=out.ap, in_=in_.ap, identity=ident.ap))

    def ld(self, out, in_ap, q="sp", **kw):
        return self.dma(out.ap, in_ap, [], [out.b], q=q, **kw)

    def st(self, out_ap, in_, q="sp", **kw):
        return self.dma(out_ap, in_.ap, [in_.b], [], q=q, **kw)

    def barrier(self):
        for ek, eng in self.engs.items():
            for k, c in self.cnt.items():
                if k == ek or c == 0:
                    continue
                if self.seen[ek].get(k, 0) < c:
                    eng.wait_ge(self.sem[k], c)
                    self.seen[ek][k] = c

    @contextlib.contextmanager
    def scope(self):
        old = self.stack
        with contextlib.ExitStack() as st:
            self.stack = st
            try:
                yield
            finally:
                self.barrier()
                self.stack = old

    def sbl(self, n, shape, dtype=F32, name=None):
        self.nbuf += 1
        name = name or ("b%d" % self.nbuf)
        full = [shape[0], n] + list(shape[1:])
        t = self.stack.enter_context(self.nc.sbuf_tensor(name, full, dtype))
        assert self.nc.sbuf_bytes_remaining >= 16640, ("SBUF budget", name, self.nc.sbuf_bytes_remaining)
        out = [Buf(t[:, i], "%s_%d" % (name, i)) for i in range(n)]
        self.allbufs.extend(out)
        return out

    def psl(self, n, parts, width, name=None):
        per = 512 // width
        out = []
        while len(out) < n:
            bank = self.ps([128, 512])
            for i in range(per):
                if len(out) < n:
                    out.append(bank[0:parts, i * width:(i + 1) * width])
        return out

    def dma(self, out_ap, in_ap, reads, writes, q="sp", **kw):
        if DEAD[0]:
            return None
        self._waits(q, reads, writes)
        dk = ("d%d" if q == "sp" else "g%d") % self.dma_rr[q]
        self.dma_rr[q] = (self.dma_rr[q] + 1) % (self.NDMA // 2)
        if self.cnt[dk] > 0 and self.seen[q].get(dk, 0) < self.cnt[dk]:
            self.engs[q].wait_ge(self.sem[dk], self.cnt[dk])
            self.seen[q][dk] = self.cnt[dk]
        ins = self.engs[q].dma_start(out=out_ap, in_=in_ap, **kw)
        self.cnt[dk] += 16
        c = self.cnt[dk]
        ins.then_inc(self.sem[dk], 16)
        for b in reads:
            b.r[dk] = c
        for b in writes:
            b.w = (dk, c)
            b.r = {}
        return ins

    def finish(self, bufs):
        self._waits("sp", bufs, [])
        for k in list(self.sem):
            if (k.startswith("d") or k.startswith("g")) and self.cnt[k] > 0:
                if self.seen["sp"].get(k, 0) < self.cnt[k]:
                    self.engs["sp"].wait_ge(self.sem[k], self.cnt[k])
                    self.seen["sp"][k] = self.cnt[k]


T = 2048
D = 1024
NT = T // 128
L_DEPTH = 2
EPS = 1e-6
C = 64
NCH = T // C
NE = 32


def make_consts():
    c = {}
    c["ident"] = np.eye(128, dtype=np.float32)
    c["ones"] = np.ones((128, 128), np.float32)
    k = np.arange(64)
    tri = (k[:, None] <= k[None, :]).astype(np.float32)
    tril = (k[None, :] <= k[:, None]).astype(np.float32)
    t64 = np.zeros((128, 128), np.float32)
    t64[:64, :64] = tri
    t64[:64, 64:] = tril
    c["tri"] = t64
    bd = np.zeros((128, 128), np.float32)
    bd[:64, :64] = 1.0
    bd[64:, 64:] = 1.0
    kk = np.arange(128)
    sut = (kk[:, None] < kk[None, :]).astype(np.float32)
    io = np.broadcast_to(np.arange(128, dtype=np.float32)[None, :], (128, 128))
    return np.concatenate([c["ident"], c["ones"], c["tri"], bd, sut, io], axis=1)


class Ctx:
    pass


class StopBuild(Exception):
    pass


STOP = [None]


def chk(k):
    if STOP[0] == k:
        DEAD[0] = True


DEAD = [False]


def dbg_dump(P, cx, name, view, shape):
    if name not in cx.dout:
        return
    with P.scope():
        tmp = P.sb(list(shape))
        P.I("dve", "tensor_copy", out=tmp[:], in_=view)
        P.st(cx.dout[name], tmp[:])


def load_cast(P, cx, dram_ap, dst, n, parts=128):
    P.ld(dst, dram_ap, q="pool")


def rsqrt_inplace(P, v):
    P.I("act", "activation", out=v, in_=v, func=AF.Ln)
    P.I("act", "activation", out=v, in_=v, func=AF.Exp, scale=-0.5)


def norm_to_T(P, cx, xres, nw, hT):
    for i in range(NT):
        ss = cx.nrm_ss[i % 2]
        xn = cx.nrm_xn[i % 2]
        junk = xn
        pt = cx.nrm_pt[i % 2]
        P.I("act", "activation", out=junk[:], in_=xres[i][:], func=AF.Square, accum_out=ss[:])
        P.I("dve", "tensor_scalar", out=ss[:], in0=ss[:], scalar1=1.0 / D, scalar2=EPS,
            op0=ALU.mult, op1=ALU.add)
        rsqrt_inplace(P, ss[:])
        P.I("dve", "tensor_scalar", out=xn[:], in0=xres[i][:], scalar1=ss[:, 0:1], scalar2=None,
            op0=ALU.mult)
        for c in range(8):
            P.tr(pt[:, c, :], xn[:, c * 128:(c + 1) * 128], cx.ident_bf[:])
        P.I("dve", "tensor_tensor", out=hT[:, :, i * 128:(i + 1) * 128], in0=pt[:],
            in1=nw.bc([128, 8, 128]), op=ALU.mult)


def deltanet(P, cx, l, hT, xres_unused, yaT, d):
    ident = cx.ident_f
    tri = cx.tri
    tril = cx.tril
    ones = cx.ones_f
    with P.scope():
        gab = P.sb([64, NCH, 8])
        wtm = P.sb([128, 8, 20], BF16)
        load_cast(P, cx, d["w_tm"][l].rearrange("p c n -> p (c n)"), wtm[:].re("p c n -> p (c n)"), 160)
        with P.scope():
            pg = P.ps([64, 8, 8])
            for c0 in range(0, NCH, 8):
                for cc in range(8):
                    c = c0 + cc
                    for kc in range(8):
                        P.mm(pg[:, cc, :], hT[:, kc, c * 64:(c + 1) * 64], wtm[:, kc, 0:8],
                             start=(kc == 0), stop=(kc == 7))
                P.I("act", "copy", out=gab[:, c0:c0 + 8, :], in_=pg[:])
        chk("gab")
        alog = P.sb([64, 1, 4]); dtb = P.sb([64, 1, 4])
        P.ld(alog[:, 0, :], d["dn_alog"][l:l + 1, :].partition_broadcast(64))
        P.ld(dtb[:, 0, :], d["dn_dtb"][l:l + 1, :].partition_broadcast(64))
        nA = P.sb([64, 1, 4])
        P.I("act", "activation", out=nA[:], in_=alog[:], func=AF.Exp)
        xa = P.sb([64, NCH, 4]); t1 = P.sb([64, NCH, 4]); g = P.sb([64, NCH, 4])
        beta = P.sb([64, NCH, 4])
        P.I("dve", "tensor_tensor", out=xa[:], in0=gab[:, :, 0:4], in1=dtb[:].bc([64, NCH, 4]), op=ALU.add)
        P.I("dve", "scalar_tensor_tensor", out=t1[:], in0=xa[:], scalar=-1.0, in1=xa[:],
            op0=ALU.mult, op1=ALU.max)
        P.I("act", "activation", out=t1[:], in_=t1[:], func=AF.Exp, scale=-1.0)
        P.I("act", "activation", out=t1[:], in_=t1[:], func=AF.Ln, bias=1.0)
        P.I("dve", "scalar_tensor_tensor", out=g[:], in0=xa[:], scalar=0.0, in1=t1[:],
            op0=ALU.max, op1=ALU.add)
        P.I("dve", "tensor_tensor", out=g[:], in0=g[:], in1=nA[:].bc([64, NCH, 4]), op=ALU.mult)
        P.I("dve", "tensor_scalar", out=g[:], in0=g[:], scalar1=-1.0, scalar2=None, op0=ALU.mult)
        P.I("act", "activation", out=beta[:], in_=gab[:, :, 4:8], func=AF.Sigmoid)
        gc = P.sb([64, NCH, 4]); eg = P.sb([64, NCH, 4]); ekd = P.sb([64, NCH, 4])
        bk = P.sb([64, NCH, 4]); egl = P.sb([128, NCH, 4]); gl = P.sb([64, NCH, 4])
        with P.scope():
            pc = P.ps([128, NCH * 4])
            P.mm(pc[0:64, :], tri, g[:].re("p c h -> p (c h)"))
            P.I("dve", "tensor_copy", out=gc[:].re("p c h -> p (c h)"), in_=pc[0:64, :])
            P.I("act", "activation", out=eg[:].re("p c h -> p (c h)"), in_=pc[0:64, :], func=AF.Exp)
            P.mm(pc[:, :], ones[0:64, :], g[:].re("p c h -> p (c h)"))
            P.I("act", "activation", out=egl[:].re("p c h -> p (c h)"), in_=pc[:, :], func=AF.Exp)
            P.I("dve", "tensor_tensor", out=gl[:].re("p c h -> p (c h)"), in0=pc[0:64, :],
                in1=gc[:].re("p c h -> p (c h)"), op=ALU.subtract)
            P.I("act", "activation", out=ekd[:], in_=gl[:], func=AF.Exp)
            P.I("dve", "tensor_tensor", out=bk[:], in0=beta[:], in1=eg[:], op=ALU.mult)
        chk("gates")
        cw = P.sb([128, 12, 4])
        P.ld(cw[:], d["dn_cw"][l])
        dnw = P.sb([128, 1])
        P.ld(dnw[:], d["dn_nw"][l])

        stril = P.sb([64, 64])
        P.I("dve", "tensor_tensor", out=stril[:], in0=tril, in1=cx.ident_f[0:64, 0:64], op=ALU.subtract)
        for h in range(4):
            with P.scope():
                deltanet_head(P, cx, l, h, hT, yaT, d, dict(
                    g=g, gc=gc, eg=eg, ekd=ekd, bk=bk, egl=egl, beta=beta, cw=cw, dnw=dnw, stril=stril[:]))


def deltanet_head(P, cx, l, h, hT, yaT, d, G):
    ident = cx.ident_f
    tri, tril, ones = cx.tri, cx.tril, cx.ones_f
    id64 = cx.ident_f[0:64, 0:64]
    szT = P.sb([128, T], BF16)
    qkv = [P.sb([128, T], name="qkv%d_%d_%d" % (l, h, i)) for i in range(3)]
    pp = [P.ps([128, 512]) for _ in range(2)]
    n = 0
    with P.scope():
        wdn = P.sb([128, 8, 512], BF16)
        load_cast(P, cx, d["w_dn"][l, h].rearrange("p c n -> p (c n)"), wdn[:].re("p c n -> p (c n)"), 4096)
        raw = P.sb([128, 3, T + 4], BF16)
        P.I("pool", "memset", ap=raw[:, :, 0:3], constant=0.0)
        for which in range(4):
            for tg in range(4):
                ps = pp[n % 2]; n += 1
                for kc in range(8):
                    P.mm(ps[:], wdn[:, kc, which * 128:(which + 1) * 128], hT[:, kc, tg * 512:(tg + 1) * 512],
                         start=(kc == 0), stop=(kc == 7))
                if which < 3:
                    P.I("act", "copy", out=raw[:, which, 3 + tg * 512:3 + (tg + 1) * 512], in_=ps[:])
                else:
                    P.I("act", "activation", out=szT[:, tg * 512:(tg + 1) * 512], in_=ps[:], func=AF.Silu)
        chk("proj")
        cw = G["cw"]
        for which in range(3):
            acc = qkv[which]
            ci = h * 3 + which
            P.I("dve", "tensor_scalar", out=acc[:], in0=raw[:, which, 0:T], scalar1=cw[:, ci, 0:1],
                scalar2=None, op0=ALU.mult)
            for j in range(1, 4):
                P.I("dve", "scalar_tensor_tensor", out=acc[:], in0=raw[:, which, j:j + T],
                    scalar=cw[:, ci, j:j + 1], in1=acc[:], op0=ALU.mult, op1=ALU.add)
            P.I("act", "activation", out=acc[:], in_=acc[:], func=AF.Silu)
    chk("conv")
    with P.scope():
        sq = P.sb([128, 512]); rn = P.sb([128, 512])
        for which in range(2):
            for tg in range(4):
                sl = slice(tg * 512, (tg + 1) * 512)
                ps = pp[n % 2]; n += 1
                P.I("act", "activation", out=sq[:], in_=qkv[which][:, sl], func=AF.Square)
                P.mm(ps[:], ones, sq[:])
                P.I("dve", "tensor_scalar", out=rn[:], in0=ps[:], scalar1=EPS, scalar2=None, op0=ALU.add)
                rsqrt_inplace(P, rn[:])
                P.I("dve", "scalar_tensor_tensor", out=qkv[which][:, sl], in0=qkv[which][:, sl],
                    scalar=(128.0 ** -0.5 if which == 0 else 1.0), in1=rn[:], op0=ALU.mult, op1=ALU.mult)
    chk("l2")
    qT, kT, vT = qkv
    ktok = [P.sb([64, 4, 128]) for _ in range(2)]
    vtok = [P.sb([64, 4, 128]) for _ in range(2)]
    S = [P.sb([128, 128], name="S%d_%d_%d" % (l, h, i)) for i in range(2)]
    P.I("pool", "memset", ap=S[0][:], constant=0.0)
    oall = [P.sb([64, 4, 128]) for _ in range(2)]
    ssq = P.sb([64, 4])
    GS = 4
    W = GS * 64
    bankA = P.ps([128, 512]); bankB = P.ps([128, 512]); bankC = P.ps([128, 512])
    bankD = P.ps([128, 512]); bankE = P.ps([128, 512])

    def v3(bank, half, parts=64, w=64):
        return bank[0:parts, half * W:(half + 1) * W].re("p (c d) -> p c d", d=w)

    pGr, pKK = v3(bankA, 0), v3(bankA, 1)
    pQK, pU = v3(bankB, 0), v3(bankB, 1)
    pL2, pU2 = v3(bankC, 0), v3(bankC, 1)
    pPr = v3(bankD, 0)
    pW = bankD[:, W:2 * W].re("p (c d) -> p c d", d=64)
    pUo = bankE[0:64, :].re("p (c d) -> p c d", d=128)
    dec = P.sb([64, GS, 64]); decT = P.sb([64, GS, 64]); AT = P.sb([64, GS, 64])
    dd = decT
    Lp = [P.sb([64, GS, 64]) for _ in range(2)]; Up = [P.sb([64, GS, 64]) for _ in range(2)]
    Pm = [P.sb([64, GS, 64]) for _ in range(2)]
    kbg = P.sb([64, GS, 128]); vb = P.sb([64, GS, 128]); kd = P.sb([64, GS, 128])
    wT = P.sb([128, GS, 64]); uo = P.sb([64, GS, 128])
    psc = P.psl(4, 128, 128)
    vnew = [P.sb([64, 128]) for _ in range(2)]
    o1 = [P.sb([64, 128]) for _ in range(2)]
    g, gc, eg, ekd, bk, egl, beta = (G[k] for k in ("g", "gc", "eg", "ekd", "bk", "egl", "beta"))
    stril = G["stril"]

    def col(buf, c):
        return buf[:, c, h:h + 1]

    def colg(buf, c0, w):
        return buf[:, c0:c0 + GS, h:h + 1].bc([64, GS, w])

    def m64(v):
        return v.re("p (o a) -> p o a", o=1).bc([64, GS, 64])

    AT2 = [AT, P.sb([64, GS, 64])]; uo2 = [uo, P.sb([64, GS, 128])]
    wT2 = [wT, P.sb([128, GS, 64])]; kd2 = [kd, P.sb([64, GS, 128])]
    nbox = [n]

    def intra_steps(c0):
        grp = list(range(c0, c0 + GS))
        c4 = c0 // 4
        kt, vt = ktok[c4 % 2], vtok[c4 % 2]
        AT_, uo_, wT_, kd_ = AT2[c4 % 2], uo2[c4 % 2], wT2[c4 % 2], kd2[c4 % 2]
        for src, dst in ((kT, kt), (vT, vt)):
            ps = pp[nbox[0] % 2]; nbox[0] += 1
            for cc in range(4):
                c = c0 + cc
                P.tr(ps[0:64, cc * 128:(cc + 1) * 128], src[:, c * 64:(c + 1) * 64], ident[:])
            P.I("act", "copy", out=dst[:].re("p c d -> p (c d)"), in_=ps[0:64, :])
        chk("ktr")
        yield
        for cc, c in enumerate(grp):
            cs = slice(c * 64, (c + 1) * 64)
            P.mm(pGr[:, cc, :], g[:, c, h:h + 1].bc([64, 64]), tri)
            P.mm(pKK[:, cc, :], kT[:, cs], kT[:, cs])
        for cc, c in enumerate(grp):
            cs = slice(c * 64, (c + 1) * 64)
            P.mm(pQK[:, cc, :], kT[:, cs], qT[:, cs])
        yield
        P.I("dve", "tensor_tensor", out=dd[:], in0=pGr, in1=colg(gc, c0, 64), op=ALU.subtract)
        P.I("dve", "tensor_scalar", out=dec[:], in0=dd[:], scalar1=0.0, scalar2=None, op0=ALU.max)
        P.I("dve", "tensor_scalar", out=decT[:], in0=dd[:], scalar1=0.0, scalar2=None, op0=ALU.min)
        P.I("act", "activation", out=dec[:], in_=dec[:], func=AF.Exp, scale=-1.0)
        P.I("act", "activation", out=decT[:], in_=decT[:], func=AF.Exp)
        P.I("dve", "tensor_tensor", out=dec[:], in0=dec[:], in1=m64(stril), op=ALU.mult)
        P.I("dve", "tensor_tensor", out=decT[:], in0=decT[:], in1=m64(tri), op=ALU.mult)
        yield
        P.I("dve", "tensor_tensor", out=Lp[0][:], in0=pKK, in1=dec[:], op=ALU.mult)
        P.I("dve", "tensor_tensor", out=Lp[0][:], in0=Lp[0][:], in1=colg(beta, c0, 64), op=ALU.mult)
        P.I("dve", "tensor_tensor", out=AT_[:], in0=pQK, in1=decT[:], op=ALU.mult)
        chk("dec")
        yield
        for cc in range(GS):
            P.mm(pU[:, cc, :], Lp[0][:, cc, :], id64)
        P.I("act", "copy", out=Up[0][:], in_=pU)
        P.I("dve", "tensor_tensor", out=Pm[0][:], in0=m64(id64), in1=Up[0][:], op=ALU.subtract)
        chk("utr")
        yield
        for it in range(5):
            a, b = it % 2, (it + 1) % 2
            last = it == 4
            for cc in range(GS):
                P.mm(pL2[:, cc, :], Up[a][:, cc, :], Lp[a][:, cc, :])
            if not last:
                for cc in range(GS):
                    P.mm(pU2[:, cc, :], Lp[a][:, cc, :], Up[a][:, cc, :])
            yield
            P.I("dve", "tensor_copy", out=Lp[b][:], in_=pL2)
            if not last:
                P.I("dve", "tensor_copy", out=Up[b][:], in_=pU2)
            yield
            for cc in range(GS):
                P.mm(pPr[:, cc, :], Lp[b][:, cc, :], Pm[a][:, cc, :])
            yield
            P.I("dve", "tensor_tensor", out=Pm[b][:], in0=pPr, in1=Pm[a][:], op=ALU.add)
            yield
        chk("inv")
        PT = Pm[1]
        P.I("dve", "tensor_tensor", out=kbg[:], in0=kt[:], in1=colg(bk, c0, 128), op=ALU.mult)
        P.I("dve", "tensor_tensor", out=vb[:], in0=vt[:], in1=colg(beta, c0, 128), op=ALU.mult)
        P.I("dve", "tensor_tensor", out=kd_[:], in0=kt[:], in1=colg(ekd, c0, 128), op=ALU.mult)
        yield
        for cc in range(GS):
            P.mm(pUo[:, cc, :], PT[:, cc, :], vb[:, cc, :])
        P.I("act", "copy", out=uo_[:], in_=pUo)
        for cc in range(GS):
            P.mm(pW[:, cc, :], kbg[:, cc, :], PT[:, cc, :])
        P.I("dve", "tensor_copy", out=wT_[:], in_=pW)
        chk("wu")

    def scan_steps(c0):
        grp = list(range(c0, c0 + GS))
        c4 = c0 // 4
        oa = oall[c4 % 2]
        AT_, uo_, wT_, kd_ = AT2[c4 % 2], uo2[c4 % 2], wT2[c4 % 2], kd2[c4 % 2]
        for cc, c in enumerate(grp):
            Sa, Sb = S[c % 2], S[(c + 1) % 2]
            vn = vnew[c % 2]; oo = o1[c % 2]
            P.mm(psc[0][0:64, :], wT_[:, cc, :], Sa[:])
            P.mm(psc[1][0:64, :], qT[:, c * 64:(c + 1) * 64], Sa[:])
            yield
            P.I("dve", "tensor_tensor", out=vn[:], in0=uo_[:, cc, :], in1=psc[0][0:64, :], op=ALU.subtract)
            yield
            P.mm(psc[2][0:64, :], AT_[:, cc, :], vn[:])
            P.mm(psc[0][:, :], kd_[:, cc, :], vn[:])
            yield
            P.I("dve", "scalar_tensor_tensor", out=Sb[:], in0=Sa[:], scalar=egl[:, c, h:h + 1],
                in1=psc[0][:, :], op0=ALU.mult, op1=ALU.add)
            P.I("dve", "tensor_copy", out=oo[:], in_=psc[2][0:64, :])
            P.I("dve", "scalar_tensor_tensor", out=oa[:, cc, :], in0=psc[1][0:64, :],
                scalar=col(eg, c), in1=oo[:], op0=ALU.mult, op1=ALU.add)
            yield
        chk("scan")
        osq = vb
        P.I("act", "activation", out=osq[:], in_=oa[:], func=AF.Square)
        P.I("dve", "tensor_reduce", out=ssq[:], in_=osq[:], axis=AX.X, op=ALU.add)
        P.I("dve", "tensor_scalar", out=ssq[:], in0=ssq[:], scalar1=1.0 / 128, scalar2=EPS,
            op0=ALU.mult, op1=ALU.add)
        rsqrt_inplace(P, ssq[:])
        P.I("dve", "tensor_tensor", out=oa[:], in0=oa[:],
            in1=ssq[:].re("p (c o) -> p c o", o=1).bc([64, 4, 128]), op=ALU.mult)
        ps = pp[nbox[0] % 2]; nbox[0] += 1
        for cc in range(4):
            P.tr(ps[:, cc * 64:(cc + 1) * 64], oa[:, cc, :], id64)
        P.I("dve", "scalar_tensor_tensor", out=yaT[:, h, c4 * 256:(c4 + 1) * 256], in0=ps[:, 0:256],
            scalar=G["dnw"][:, 0:1], in1=szT[:, c4 * 256:(c4 + 1) * 256], op0=ALU.mult, op1=ALU.mult)

    groups = list(range(0, NCH, GS))
    for _ in intra_steps(groups[0]):
        pass
    for gi, c0 in enumerate(groups):
        it_i = intra_steps(groups[gi + 1]) if gi + 1 < len(groups) else iter(())
        it_s = scan_steps(c0)
        done_i = done_s = False
        while not (done_i and done_s):
            if not done_i:
                try:
                    next(it_i)
                except StopIteration:
                    done_i = True
            if not done_s:
                try:
                    next(it_s)
                except StopIteration:
                    done_s = True


IN_OFF = np.cumsum([0, 512, 512, 512, 512, 4, 4, 256, 384, 12, 512])


def kchunk(w):
    n = w.shape[1]
    return np.ascontiguousarray(w.reshape(8, 128, n).transpose(1, 0, 2))


def prep_shared(inp):
    f = lambda a: np.ascontiguousarray(np.asarray(a, dtype=np.float32))
    L = L_DEPTH
    sh = {}
    sh["consts"] = make_consts()
    sh["anw"] = f(np.asarray(inp["attn_norm_w"]).reshape(L, 8, 128).transpose(0, 2, 1))[..., None]
    w_in = np.asarray(inp["w_in"])
    w_dn = np.zeros((L, 4, 128, 8, 512), np.float32)
    w_tm = np.zeros((L, 128, 8, 20), np.float32)
    for l in range(L):
        for h in range(4):
            cols = np.concatenate([np.arange(o + h * 128, o + (h + 1) * 128) for o in (0, 512, 1024, 1536)])
            w_dn[l, h] = kchunk(w_in[l][:, cols])
        cols = np.concatenate([np.arange(2048, 2056), np.arange(2696, 2708)])
        w_tm[l] = kchunk(w_in[l][:, cols])
    sh["w_dn"] = w_dn
    sh["w_tm"] = w_tm
    cw = np.asarray(inp["dn_conv_w"])
    sh["dn_cw"] = f(cw.reshape(L, 4, 3, 4, 128).transpose(0, 4, 3, 2, 1).reshape(L, 128, 12, 4))
    sh["dn_alog"] = f(inp["dn_a_log"])
    sh["dn_dtb"] = f(inp["dn_dt_bias"])
    sh["dn_nw"] = f(np.asarray(inp["dn_norm_w"]).reshape(L, 128, 1))
    sh["w_out"] = f(inp["w_out"])
    prep_nsa(inp, sh)
    prep_conv(inp, sh)
    prep_moe(inp, sh)
    return sh


def build_nc(nlayers=L_DEPTH, stages=("dn", "nsa", "conv", "moe"), dbg=(), nexp=NE):
    nc = bass.Bass("TRN2", target_bir_lowering=False)
    d = {}

    def inp(name, shape):
        d[name] = nc.dram_tensor(name, list(shape), F32, kind="ExternalInput").ap()

    inp("x", [T, D]); inp("consts", [128, 768]); inp("anw", [2, 128, 8, 1])
    inp("w_out", [2, D, D])
    inp("w_nsa_fm", [2, 128, 8, 640]); inp("w_nsa_tm", [2, 128, 8, 128]); inp("nsa_nw", [2, 128, 3])
    inp("nsa_knw0", [2, 64]); inp("nsa_posT", [2, 128, 32]); inp("nsa_w1", [2, 128, 32, 128])
    inp("nsa_w2", [2, 128, 128]); inp("nsa_bias_cmp", [4, NCMP, T]); inp("nsa_bias_tile", [128, 4, 2, 128])
    inp("nsa_c31", [128, 4]); inp("nsa_sel_tab", [128, 3, NT, 32]); inp("nsa_expand", [32, T])
    inp("nsa_overlap", [NCMP, 32]); inp("nsa_mwin", [128, 8, 512])
    inp("w_conv", [2, 128, 8, 512]); inp("conv_prm", [2, 128, 2, 34])
    inp("fnw_row", [2, D]); inp("fnw", [2, 128, 8, 1]); inp("router_w", [2, 128, 8, NE]); inp("router_b", [2, NE])
    inp("w_gu", [2, NE, 8, 128, 2048]); inp("w_dn_moe", [2, NE, D, D]); inp("b_gu", [2, NE, 128, 16])
    inp("b_dn", [2, NE, D])
    inp("w_dn", [2, 4, 128, 8, 512]); inp("w_tm", [2, 128, 8, 20]); inp("dn_cw", [2, 128, 12, 4])
    inp("dn_alog", [2, 4]); inp("dn_dtb", [2, 4]); inp("dn_nw", [2, 128, 1])
    out = nc.dram_tensor("out", [T, D], F32, kind="ExternalOutput").ap()
    dout = {}
    for name, shape in dbg:
        dout[name] = nc.dram_tensor(name, list(shape), F32, kind="ExternalOutput").ap()

    with contextlib.ExitStack() as st:
        P = Prog(nc, st)
        cx = Ctx()
        consts = P.sb([128, 768])
        P.ld(consts[:], d["consts"])
        cx.ident_f = consts[:, 0:128]
        cx.ones_f = consts[:, 128:256]
        cx.tri = consts[0:64, 256:320]
        cx.tril = consts[0:64, 320:384]
        cx.bd_ones = consts[:, 384:512]
        cx.sut = consts[:, 512:640]
        cx.iota = consts[:, 640:768]
        cx.dbg_ybT = dout.get("ybT")
        cx.dout = dout
        cx.nexp = nexp
        identb = P.sb([128, 128], BF16)
        P.I("dve", "tensor_copy", out=identb[:], in_=consts[:, 0:128])
        cx.ident_bf = identb
        cx.stg_n = 256
        cx.stg_i = 0
        cx.nrm_ss = [P.sb([128, 1]) for _ in range(2)]
        xres = P.sbl(NT, [128, D], name="xres")
        for i in range(NT):
            P.ld(xres[i][:], d["x"][i * 128:(i + 1) * 128, :])
        for l in range(nlayers):
          try:
            if l > 0:
                P.new_epoch()
            with P.scope():
                hT = P.sb([128, 8, T], BF16)
                nw = P.sb([128, 8, 1])
                P.ld(nw[:], d["anw"][l])
                with P.scope():
                    cx.nrm_pt = [P.ps([128, 8, 128], BF16) for _ in range(2)]
                    cx.nrm_xn = [P.sb([128, D], BF16) for _ in range(2)]
                    norm_to_T(P, cx, xres, nw[:], hT)
                chk("norm")
                if "dn" in stages:
                    with P.scope():
                        yaT = P.sb([128, 4, T], BF16)
                        deltanet(P, cx, l, hT, xres, yaT, d)
                        if "yaT" in dout:
                            with P.scope():
                                tmp = P.sb([128, 4, T])
                                P.I("dve", "tensor_copy", out=tmp[:], in_=yaT[:])
                                P.st(dout["yaT"], tmp[:])
                        apply_wout(P, cx, l, yaT, 0, 4, xres, d)
                if "nsa" in stages:
                    nsa(P, cx, l, hT, xres, d)
                if "conv" in stages:
                    conformer(P, cx, l, hT, xres, d)
            if "moe" in stages:
                P.new_epoch()
                (moe_sparse if MOE_SPARSE[0] else moe)(P, cx, l, xres, d, nexp=cx.nexp)
          except StopBuild:
            break
        DEAD[0] = False
        for i in range(NT):
            P.st(out[i * 128:(i + 1) * 128, :], xres[i][:])
        P.finish([])
    return nc


NEGM = -200.0
NCMP = 127


def t5_bucket_np(dist):
    n = np.maximum(dist, 0)
    nf = np.maximum(n, 1).astype(np.float32)
    large = 16 + (np.log(nf / np.float32(16)) / np.float32(math.log(8.0)) * np.float32(16)).astype(np.int32)
    large = np.minimum(large, 31)
    return np.where(n < 16, n, large)


def nsa_tables(rel_bias):
    rb = np.asarray(rel_bias, np.float32)
    tb = {}
    t = np.arange(T)
    j = np.arange(NCMP)
    dist = t[None, :] - (j[:, None] * 16 + 31)
    bk = t5_bucket_np(dist)
    bc = rb[bk]
    bc = np.where((dist >= 0)[..., None], bc, np.float32(NEGM))
    tb["bias_cmp"] = np.ascontiguousarray(bc.transpose(2, 0, 1)).astype(np.float32)
    k = np.arange(128)
    tt = np.arange(128)
    d0 = tt[None, :] - k[:, None]
    d1 = d0 + 128
    b0 = np.where((d0 >= 0)[..., None], rb[t5_bucket_np(d0)], np.float32(NEGM))
    b1 = rb[t5_bucket_np(d1)]
    tbl = np.stack([b0, b1], axis=0)
    tb["bias_tile"] = np.ascontiguousarray(tbl.transpose(1, 3, 0, 2)).astype(np.float32)
    tb["c31"] = np.ascontiguousarray(np.broadcast_to(rb[31][None, :], (128, 4))).astype(np.float32)
    tok = (np.arange(NT)[None, :] * 128 + np.arange(128)[:, None])
    cur = tok // 64
    s = np.arange(32)[None, None, :]
    causal = s <= cur[..., None]
    forced = (s == 0) | (s == cur[..., None]) | (s == cur[..., None] - 1)
    m1 = (causal & ~forced).astype(np.float32)
    cst = np.where(causal & forced, 1e4, np.where(causal, 0.0, -1.0)).astype(np.float32)
    tb["sel_tab"] = np.ascontiguousarray(np.stack([m1, cst, causal.astype(np.float32)], axis=1))
    key = np.arange(T)
    tb["expand"] = (key[None, :] // 64 == np.arange(32)[:, None]).astype(np.float32)
    cs = j * 16
    ss = np.arange(32) * 64
    ov = ((cs[:, None] < ss[None, :] + 64) & (cs[:, None] + 32 > ss[None, :])).astype(np.float32)
    tb["overlap"] = ov
    mw = np.zeros((128, 8, 512), np.float32)
    for r in range(8):
        rel = r - 4
        keyp = rel * 128 + k[:, None]
        tp = np.arange(512)[None, :]
        dd = tp - keyp
        mw[:, r, :] = ((dd >= 0) & (dd < 512)).astype(np.float32)
    tb["mwin"] = mw
    return tb


def prep_nsa(inp, sh):
    L = L_DEPTH
    w_in = np.asarray(inp["w_in"])
    w_fm = np.zeros((L, 128, 8, 640), np.float32)
    w_tmv = np.zeros((L, 128, 8, 128), np.float32)
    for l in range(L):
        q0 = 2056
        kv0 = 2312
        cols = np.concatenate([
            np.arange(q0, q0 + 256),
            np.arange(kv0 + 128, kv0 + 192), np.arange(kv0 + 128, kv0 + 192),
            np.arange(kv0 + 256, kv0 + 320), np.arange(kv0 + 256, kv0 + 320),
            np.arange(kv0, kv0 + 128),
        ])
        w_fm[l] = kchunk(w_in[l][:, cols])
        cols = np.concatenate([np.arange(kv0 + 192, kv0 + 256), np.arange(kv0 + 320, kv0 + 384)])
        w_tmv[l] = kchunk(w_in[l][:, cols])
    sh["w_nsa_fm"] = w_fm
    sh["w_nsa_tm"] = w_tmv
    qn = np.asarray(inp["nsa_q_norm_w"], np.float32)
    kn = np.asarray(inp["nsa_k_norm_w"], np.float32)
    nw = np.zeros((L, 128, 3), np.float32)
    nw[:, :, 0] = np.concatenate([qn, qn], axis=1)
    nw[:, :, 1] = np.concatenate([kn[:, 1], kn[:, 1]], axis=1)
    nw[:, :, 2] = np.concatenate([kn[:, 2], kn[:, 2]], axis=1)
    sh["nsa_nw"] = nw
    sh["nsa_knw0"] = np.ascontiguousarray(kn[:, 0, :])
    pos = np.asarray(inp["nsa_cmp_pos"], np.float32)
    sh["nsa_posT"] = np.ascontiguousarray(pos.transpose(0, 1, 3, 2).reshape(L, 128, 32))
    w1 = np.asarray(inp["nsa_cmp_w1"], np.float32)
    sh["nsa_w1"] = np.ascontiguousarray(w1.reshape(L, 2, 32, 64, 128).transpose(0, 1, 3, 2, 4).reshape(L, 128, 32, 128))
    w2 = np.asarray(inp["nsa_cmp_w2"], np.float32)
    sh["nsa_w2"] = np.ascontiguousarray(w2.transpose(0, 2, 1, 3).reshape(L, 128, 128))
    tb = nsa_tables(inp["rel_bias"])
    for k_, v_ in tb.items():
        sh["nsa_" + k_] = v_


def apply_wout(P, cx, l, yT, chunk0, nch, xres, d):
    with P.scope():
        w = P.sb([128, nch, D], BF16)
        for c in range(nch):
            load_cast(P, cx, d["w_out"][l, (chunk0 + c) * 128:(chunk0 + c + 1) * 128, :], w[:, c, :], D)
        pp = [P.ps([128, 512]) for _ in range(2)]
        n = 0
        for i in range(NT):
            for half in range(2):
                ps = pp[n % 2]; n += 1
                for c in range(nch):
                    P.mm(ps[:], yT[:, c, i * 128:(i + 1) * 128], w[:, c, half * 512:(half + 1) * 512],
                         start=(c == 0), stop=(c == nch - 1))
                P.I("dve", "tensor_tensor", out=xres[i][:, half * 512:(half + 1) * 512], in0=ps[:],
                    in1=xres[i][:, half * 512:(half + 1) * 512], op=ALU.add)


def nsa(P, cx, l, hT, xres, d):
    TINY = 1e-30
    with P.scope():
        qT = P.sb([128, 2, T], BF16)
        kslcT = P.sb([128, T], BF16)
        kwinT = P.sb([128, T], BF16)
        cmpT = P.sb([128, T], BF16)
        vaug = P.sb([128, NT, 2, 66], BF16)
        gts = P.sb([128, NT, 12])
        ynsa = P.sb([128, NT, 256])
        impacc = P.sb([128, NT, 32])
        nw = P.sb([128, 3])
        P.ld(nw[:], d["nsa_nw"][l])
        qw = P.sb([128, 1])
        P.I("dve", "tensor_scalar", out=qw[:], in0=nw[:, 0:1], scalar1=0.125, scalar2=None, op0=ALU.mult)
        P.I("pool", "memset", ap=vaug[:, :, :, 64:66], constant=1.0)
        with P.scope():
            wfm = P.sb([128, 8, 640], BF16)
            load_cast(P, cx, d["w_nsa_fm"][l].rearrange("p c n -> p (c n)"), wfm[:].re("p c n -> p (c n)"), 8 * 640)
            wtv = P.sb([128, 8, 128], BF16)
            load_cast(P, cx, d["w_nsa_tm"][l].rearrange("p c n -> p (c n)"), wtv[:].re("p c n -> p (c n)"), 8 * 128)
            wtm = P.sb([128, 8, 20], BF16)
            load_cast(P, cx, d["w_tm"][l].rearrange("p c n -> p (c n)"), wtm[:].re("p c n -> p (c n)"), 160)
            pp = [P.ps([128, 512]) for _ in range(2)]
            pq = [P.ps([128, 512]) for _ in range(2)]
            qs = [P.sb([128, 512]) for _ in range(2)]
            sq = P.sb([128, 512]); rn = P.sb([128, 512])
            n = 0
            for ch in range(5):
                for tg in range(4):
                    sl = slice(tg * 512, (tg + 1) * 512)
                    ps = pp[n % 2]; ps2 = pq[n % 2]; qsb = qs[n % 2]; n += 1
                    for kc in range(8):
                        P.mm(ps[:], wfm[:, kc, ch * 128:(ch + 1) * 128], hT[:, kc, sl], start=(kc == 0), stop=(kc == 7))
                    if ch == 4:
                        P.I("act", "copy", out=cmpT[:, sl], in_=ps[:])
                        continue
                    P.I("act", "copy", out=qsb[:], in_=ps[:])
                    P.I("act", "activation", out=sq[:], in_=qsb[:], func=AF.Square)
                    P.mm(ps2[:], cx.bd_ones, sq[:])
                    P.I("dve", "tensor_scalar", out=rn[:], in0=ps2[:], scalar1=1.0 / 64, scalar2=EPS,
                        op0=ALU.mult, op1=ALU.add)
                    rsqrt_inplace(P, rn[:])
                    if ch < 2:
                        dst, wc = qT[:, ch, sl], qw[:, 0:1]
                    elif ch == 2:
                        dst, wc = kslcT[:, sl], nw[:, 1:2]
                    else:
                        dst, wc = kwinT[:, sl], nw[:, 2:3]
                    P.I("dve", "scalar_tensor_tensor", out=dst, in0=qsb[:], scalar=wc, in1=rn[:],
                        op0=ALU.mult, op1=ALU.mult)
            for i in range(NT):
                ps = pp[n % 2]; n += 1
                for kc in range(8):
                    P.mm(ps[:, 0:128], hT[:, kc, i * 128:(i + 1) * 128], wtv[:, kc, :], start=(kc == 0), stop=(kc == 7))
                for kc in range(8):
                    P.mm(ps[:, 128:140], hT[:, kc, i * 128:(i + 1) * 128], wtm[:, kc, 8:20], start=(kc == 0), stop=(kc == 7))
                P.I("act", "copy", out=vaug[:, i, :, 0:64], in_=ps[:, 0:128].re("p (a b) -> p a b", b=64))
                P.I("act", "activation", out=gts[:, i, :], in_=ps[:, 128:140], func=AF.Sigmoid)
        chk("nsa_proj")
        with P.scope():
            w1 = P.sb([128, 32, 128], BF16)
            load_cast(P, cx, d["nsa_w1"][l].rearrange("p c n -> p (c n)"), w1[:].re("p c n -> p (c n)"), 4096)
            w2 = P.sb([128, 128], BF16)
            load_cast(P, cx, d["nsa_w2"][l], w2[:], 128)
            posT = P.sb([128, 32], BF16)
            load_cast(P, cx, d["nsa_posT"][l], posT[:], 32)
            knw0 = P.sb([128, 64])
            P.ld(knw0[0:NCMP, :], d["nsa_knw0"][l:l + 1, :].partition_broadcast(NCMP))
            ovf = P.sb([128, 32])
            P.ld(ovf[0:NCMP, :], d["nsa_overlap"])
            rhsc = P.sb([128, 98], BF16)
            hs = P.sb([128, 2, 128], BF16)
            hb = P.sb([128, 2])
            kcT = P.sb([128, 128], BF16)
            ph = P.ps([128, 512])
            pk = P.ps([128, 512])
            ptb = P.ps([128, 128], BF16)
            ph2 = P.ps([128, 512])
            phs = [ph, ph2]
            for which in range(2):
                pr = slice(which * 64, which * 64 + 64)
                for l_ in range(32):
                    P.mm(phs[which][:, 0:NCMP], w1[pr, l_, :], cmpT[pr, l_:l_ + 16 * (NCMP - 1) + 1:16],
                         start=(l_ == 0), stop=(l_ == 31))
                for l_ in range(32):
                    P.mm(phs[which][:, 256:257], w1[pr, l_, :], posT[pr, l_:l_ + 1],
                         start=(l_ == 0), stop=(l_ == 31))
            for which in range(2):
                P.I("dve", "tensor_copy", out=hb[:, which:which + 1], in_=phs[which][:, 256:257])
            for which in range(2):
                P.I("act", "activation", out=hs[:, which, 0:NCMP], in_=phs[which][:, 0:NCMP],
                    func=AF.Silu, bias=hb[:, which:which + 1])
            dbg_dump(P, cx, "phk", ph[:, 0:NCMP], [128, NCMP])
            dbg_dump(P, cx, "hb", hb[:], [128, 2])
            dbg_dump(P, cx, "cmpT", cmpT[:], [128, T])
            P.mm(pk[0:NCMP, 0:64], hs[:, 0, 0:NCMP], w2[:, 0:64])
            P.mm(pk[0:NCMP, 64:128], hs[:, 1, 0:NCMP], w2[:, 64:128])
            kc = P.sb([128, 64]); kq = P.sb([128, 64]); kss = P.sb([128, 1]); kcd = P.sb([128, 2, 64], BF16)
            P.I("dve", "tensor_copy", out=kc[0:NCMP, :], in_=pk[0:NCMP, 0:64])
            P.I("dve", "tensor_tensor", out=kq[0:NCMP, :], in0=kc[0:NCMP, :], in1=kc[0:NCMP, :], op=ALU.mult)
            P.I("dve", "tensor_reduce", out=kss[0:NCMP, :], in_=kq[0:NCMP, :], axis=AX.X, op=ALU.add)
            P.I("dve", "tensor_scalar", out=kss[0:NCMP, :], in0=kss[0:NCMP, :], scalar1=1.0 / 64, scalar2=EPS,
                op0=ALU.mult, op1=ALU.add)
            rsqrt_inplace(P, kss[0:NCMP, :])
            P.I("dve", "scalar_tensor_tensor", out=kc[0:NCMP, :], in0=kc[0:NCMP, :], scalar=kss[0:NCMP, 0:1],
                in1=knw0[0:NCMP, :], op0=ALU.mult, op1=ALU.mult)
            P.I("pool", "memset", ap=kcd[:], constant=0.0)
            P.I("dve", "tensor_copy", out=kcd[0:NCMP, 0, :], in_=kc[0:NCMP, :])
            P.I("dve", "tensor_copy", out=kcd[0:NCMP, 1, :], in_=kc[0:NCMP, :])
            P.tr(ptb[:, :], kcd[:].re("p a b -> p (a b)"), cx.ident_bf[:])
            P.I("dve", "tensor_copy", out=kcT[:], in_=ptb[:])
            P.I("pool", "memset", ap=rhsc[:], constant=1.0)
            P.I("dve", "tensor_copy", out=rhsc[0:NCMP, 0:64], in_=pk[0:NCMP, 64:128])
            P.I("dve", "tensor_copy", out=rhsc[0:NCMP, 65:97], in_=ovf[0:NCMP, :])
            dbg_dump(P, cx, "kcT", kcT[:], [128, 128])
            dbg_dump(P, cx, "rhsc", rhsc[:], [128, 98])
            dbg_dump(P, cx, "hs", hs[:, :, 0:NCMP], [128, 2, NCMP])
            dbg_dump(P, cx, "kc", kc[0:NCMP, :], [NCMP, 64])
            chk("nsa_cmpkv")
            bt = [P.sb([128, 512]) for _ in range(2)]
            ssb = [P.sb([128, 512]) for _ in range(2)]
            pTb = [P.sb([128, 512], BF16) for _ in range(2)]
            psS = [P.ps([128, 512]) for _ in range(2)]
            psO = [P.ps([128, 512]) for _ in range(2)]
            rr = P.sb([128, 4, 1]); g0 = P.sb([128, 4, 1]); itmp = P.sb([128, 4, 32])
            n = 0
            for h in range(4):
                hp = slice((h % 2) * 64, (h % 2) * 64 + 64)
                for tg in range(4):
                    sl = slice(tg * 512, (tg + 1) * 512)
                    tl = slice(tg * 4, tg * 4 + 4)
                    b_ = bt[n % 2]; s_ = ssb[n % 2]; p_ = pTb[n % 2]; pS = psS[n % 2]; pO = psO[n % 2]; n += 1
                    pOv = pO[:].re("p (a b) -> p a b", b=128)
                    P.ld(b_[0:NCMP, :], d["nsa_bias_cmp"][h, :, sl])
                    P.mm(pS[0:NCMP, :], kcT[hp, 0:NCMP], qT[hp, h // 2, sl])
                    P.I("dve", "tensor_tensor", out=s_[0:NCMP, :], in0=pS[0:NCMP, :], in1=b_[0:NCMP, :], op=ALU.add)
                    P.I("act", "activation", out=p_[0:NCMP, :], in_=s_[0:NCMP, :], func=AF.Exp)
                    for i4 in range(4):
                        P.mm(pOv[:, i4, 0:97], p_[0:NCMP, i4 * 128:(i4 + 1) * 128], rhsc[0:NCMP, 0:97])
                    P.I("dve", "tensor_scalar", out=rr[:], in0=pOv[:, :, 64:65], scalar1=TINY, scalar2=None, op0=ALU.max)
                    P.I("dve", "reciprocal", out=rr[:], in_=rr[:])
                    P.I("dve", "tensor_tensor", out=g0[:], in0=rr[:], in1=gts[:, tl, h * 3:h * 3 + 1], op=ALU.mult)
                    P.I("dve", "tensor_tensor", out=ynsa[:, tl, h * 64:(h + 1) * 64], in0=pOv[:, :, 0:64],
                        in1=g0[:].bc([128, 4, 64]), op=ALU.mult)
                    if h == 0:
                        P.I("dve", "tensor_tensor", out=impacc[:, tl, :], in0=pOv[:, :, 65:97],
                            in1=rr[:].bc([128, 4, 32]), op=ALU.mult)
                    else:
                        P.I("dve", "tensor_tensor", out=itmp[:], in0=pOv[:, :, 65:97],
                            in1=rr[:].bc([128, 4, 32]), op=ALU.mult)
                        P.I("dve", "tensor_tensor", out=impacc[:, tl, :], in0=impacc[:, tl, :], in1=itmp[:], op=ALU.add)
        chk("nsa_cmp")
        selT = P.sb([32, T], BF16)
        with P.scope():
            stab = P.sb([128, 3, NT, 32])
            P.ld(stab[:], d["nsa_sel_tab"])
            imp2 = P.sb([128, NT, 32])
            P.I("dve", "tensor_tensor", out=imp2[:], in0=impacc[:], in1=stab[:, 0], op=ALU.mult)
            P.I("dve", "tensor_tensor", out=imp2[:], in0=imp2[:], in1=stab[:, 1], op=ALU.add)
            wk = P.sb([128, NT, 32]); m8 = P.sb([128, NT, 8]); m8b = P.sb([128, NT, 8])
            selb = P.sb([128, NT, 32], BF16); self32 = P.sb([128, NT, 32])
            pt = [P.ps([128, 512], BF16) for _ in range(2)]
            for i in range(NT):
                P.I("dve", "max", out=m8[:, i, :], in_=imp2[:, i, :])
                P.I("dve", "match_replace", out=wk[:, i, :], in_to_replace=m8[:, i, :], in_values=imp2[:, i, :], imm_value=-1e9)
                P.I("dve", "max", out=m8b[:, i, :], in_=wk[:, i, :])
            P.I("dve", "tensor_tensor", out=self32[:], in0=imp2[:], in1=m8b[:, :, 7:8].bc([128, NT, 32]), op=ALU.is_ge)
            P.I("dve", "tensor_tensor", out=selb[:], in0=self32[:], in1=stab[:, 2], op=ALU.mult)
            for i4 in range(NT // 4):
                for ii in range(4):
                    P.tr(pt[i4 % 2][0:32, ii * 128:(ii + 1) * 128], selb[:, i4 * 4 + ii, :], cx.ident_bf[:])
                P.I("act", "copy", out=selT[:, i4 * 512:(i4 + 1) * 512], in_=pt[i4 % 2][0:32, :])
        chk("nsa_sel")
        with P.scope():
            Tb = P.sb([128, 4, 2, 128])
            P.ld(Tb[:], d["nsa_bias_tile"])
            c31 = P.sb([128, 4])
            P.ld(c31[:], d["nsa_c31"])
            E = P.sb([32, T], BF16)
            load_cast(P, cx, d["nsa_expand"], E[:], T, parts=32)
            mskall = P.sb([128, 16, 512], BF16)
            mwin = P.sb([128, 8, 512], BF16)
            load_cast(P, cx, d["nsa_mwin"].rearrange("p a b -> p (a b)"), mwin[:].re("p a b -> p (a b)"), 4096)
            psM = P.ps([128, 512])
            psS = [P.ps([128, 512]) for _ in range(3)]
            psA = P.ps([128, 512])
            eb = [P.sb([128, 512], BF16) for _ in range(3)]
            tmpb = [P.sb([128, 128]) for _ in range(4)]
            pmb = [P.sb([128, 512], BF16) for _ in range(3)]
            rr = P.sb([128, 4, 1]); g0 = P.sb([128, 4, 1]); otmp = P.sb([128, 4, 64])
            accv = psA[:].re("p (a b) -> p a b", b=128)
            units = []
            for br in range(2):
                for g in range(4):
                    kt_lo = 0 if br == 0 else max(0, 4 * g - 4)
                    kts = list(range(kt_lo, 4 * g + 4))
                    for h in range(4):
                        for kt in kts:
                            units.append((br, g, h, kt, kts))

            def build_masks(u):
                br, g, h, kt, kts = units[u]
                if br == 0 and h == 0 and kt == kts[0]:
                    for k2 in kts:
                        P.mm(psM[:], E[:, k2 * 128:(k2 + 1) * 128], selT[:, g * 512:(g + 1) * 512])
                        P.I("act", "copy", out=mskall[:, k2, :], in_=psM[:])

            def stage_a(u):
                br, g, h, kt, kts = units[u]
                kT = kslcT if br == 0 else kwinT
                hp = slice((h % 2) * 64, (h % 2) * 64 + 64)
                rel = kt - 4 * g
                c0 = max(rel, 0) * 128
                pS = psS[u % 3]; e_ = eb[u % 3]
                P.mm(pS[:, c0:512], kT[hp, kt * 128:(kt + 1) * 128], qT[hp, h // 2, g * 512 + c0:(g + 1) * 512])
                cc = c0
                for off in (0, 1):
                    qi = rel + off
                    if 0 <= qi < 4:
                        tm = tmpb[(2 * u + off) % 4]
                        cs = slice(qi * 128, (qi + 1) * 128)
                        P.I("dve", "tensor_tensor", out=tm[:], in0=pS[:, cs], in1=Tb[:, h, off, :], op=ALU.add)
                        P.I("act", "activation", out=e_[:, cs], in_=tm[:], func=AF.Exp)
                        cc = (qi + 1) * 128
                if cc < 512:
                    P.I("act", "activation", out=e_[:, cc:512], in_=pS[:, cc:512], func=AF.Exp, bias=c31[:, h:h + 1])

            def stage_b(u):
                br, g, h, kt, kts = units[u]
                rel = kt - 4 * g
                c0 = max(rel, 0) * 128
                tl = slice(4 * g, 4 * g + 4)
                e_ = eb[u % 3]; pm = pmb[u % 3]
                msk = mskall[:, kt, :] if br == 0 else mwin[:, rel + 4, :]
                P.I("dve", "tensor_tensor", out=pm[:, c0:512], in0=e_[:, c0:512], in1=msk[:, c0:512], op=ALU.mult)
                for qi in range(c0 // 128, 4):
                    P.mm(accv[:, qi, 0:65], pm[:, qi * 128:(qi + 1) * 128], vaug[:, kt, br, 0:65],
                         start=(kt == kts[0] and qi == 0), stop=(kt == kts[-1] and qi == 3))
                if kt == kts[-1]:
                    P.I("dve", "tensor_scalar", out=rr[:], in0=accv[:, :, 64:65], scalar1=TINY, scalar2=None, op0=ALU.max)
                    P.I("dve", "reciprocal", out=rr[:], in_=rr[:])
                    P.I("dve", "tensor_tensor", out=g0[:], in0=rr[:], in1=gts[:, tl, h * 3 + 1 + br:h * 3 + 2 + br], op=ALU.mult)
                    P.I("dve", "tensor_tensor", out=otmp[:], in0=accv[:, :, 0:64], in1=g0[:].bc([128, 4, 64]), op=ALU.mult)
                    P.I("dve", "tensor_tensor", out=ynsa[:, tl, h * 64:(h + 1) * 64], in0=ynsa[:, tl, h * 64:(h + 1) * 64],
                        in1=otmp[:], op=ALU.add)

            build_masks(0)
            stage_a(0)
            stage_a(1)
            for u in range(len(units)):
                if u + 2 < len(units):
                    stage_a(u + 2)
                stage_b(u)
                if u + 1 < len(units):
                    build_masks(u + 1)
        chk("nsa_attn")
        with P.scope():
            ybT = P.sb([128, 2, T], BF16)
            yb16 = [P.sb([128, 256], BF16) for _ in range(2)]
            pt = [P.ps([128, 2, 128], BF16) for _ in range(2)]
            for i in range(NT):
                P.I("act", "copy", out=yb16[i % 2][:], in_=ynsa[:, i, :])
                for c in range(2):
                    P.tr(pt[i % 2][:, c, :], yb16[i % 2][:, c * 128:(c + 1) * 128], cx.ident_bf[:])
                P.I("dve", "tensor_copy", out=ybT[:, :, i * 128:(i + 1) * 128], in_=pt[i % 2][:])
            if cx.dbg_ybT is not None:
                with P.scope():
                    tmp = P.sb([128, 2, T])
                    P.I("dve", "tensor_copy", out=tmp[:], in_=ybT[:])
                    P.st(cx.dbg_ybT, tmp[:])
            apply_wout(P, cx, l, ybT, 4, 2, xres, d)


def prep_conv(inp, sh):
    L = L_DEPTH
    w_in = np.asarray(inp["w_in"])
    w = np.zeros((L, 128, 8, 512), np.float32)
    for l in range(L):
        w[l] = kchunk(w_in[l][:, 2708:3220])
    sh["w_conv"] = w
    dw = np.asarray(inp["conv_dw_w"], np.float32)
    prm = np.zeros((L, 128, 2, 34), np.float32)
    prm[:, :, :, 0:31] = dw.reshape(L, 31, 2, 128).transpose(0, 3, 2, 1)
    prm[:, :, :, 31] = np.asarray(inp["conv_dw_b"], np.float32).reshape(L, 2, 128).transpose(0, 2, 1)
    prm[:, :, :, 32] = np.asarray(inp["conv_ln_w"], np.float32).reshape(L, 2, 128).transpose(0, 2, 1)
    prm[:, :, :, 33] = np.asarray(inp["conv_ln_b"], np.float32).reshape(L, 2, 128).transpose(0, 2, 1)
    sh["conv_prm"] = prm


def conformer(P, cx, l, hT, xres, d):
    PAD = 32
    with P.scope():
        ycT = P.sb([128, 2, T], BF16)
        hp = P.sb([128, 2, T + PAD], BF16)
        prm = P.sb([128, 2, 34])
        P.ld(prm[:], d["conv_prm"][l])
        P.I("pool", "memset", ap=hp[:, :, 0:PAD], constant=0.0)
        dg = P.sb([128, 2, 31, 128], BF16)
        for j2 in range(2):
            for j in range(31):
                P.I("dve", "tensor_scalar", out=dg[:, j2, j, :], in0=cx.ident_f,
                    scalar1=prm[:, j2, j:j + 1], scalar2=None, op0=ALU.mult)
        with P.scope():
            wcv = P.sb([128, 8, 512], BF16)
            load_cast(P, cx, d["w_conv"][l].rearrange("p c n -> p (c n)"), wcv[:].re("p c n -> p (c n)"), 4096)
            pa = [P.ps([128, 512]) for _ in range(2)]
            pg = [P.ps([128, 512]) for _ in range(2)]
            sg = [P.sb([128, 512]) for _ in range(2)]
            n = 0
            for j2 in range(2):
                for tg in range(4):
                    sl = slice(tg * 512, (tg + 1) * 512)
                    a_, g_, s_ = pa[n % 2], pg[n % 2], sg[n % 2]; n += 1
                    for kc in range(8):
                        P.mm(a_[:], wcv[:, kc, j2 * 128:(j2 + 1) * 128], hT[:, kc, sl], start=(kc == 0), stop=(kc == 7))
                    for kc in range(8):
                        P.mm(g_[:], wcv[:, kc, (2 + j2) * 128:(3 + j2) * 128], hT[:, kc, sl], start=(kc == 0), stop=(kc == 7))
                    P.I("act", "activation", out=s_[:], in_=g_[:], func=AF.Sigmoid)
                    P.I("dve", "tensor_tensor", out=hp[:, j2, PAD + tg * 512:PAD + (tg + 1) * 512], in0=a_[:], in1=s_[:],
                        op=ALU.mult)
        with P.scope():
            pc = [P.ps([128, 512]) for _ in range(2)]
            pS1 = P.ps([128, 512]); pS2 = P.ps([128, 512])
            xc = [P.sb([128, 512]) for _ in range(2)]
            sq = [P.sb([128, 512]) for _ in range(2)]
            mu = P.sb([128, 512]); var = P.sb([128, 512]); msq = P.sb([128, 512])
            tmp = [P.sb([128, 512]) for _ in range(2)]
            for tg in range(4):
                for j2 in range(2):
                    for j in range(31):
                        o = PAD - 30 + j + tg * 512
                        P.mm(pc[j2][:], dg[:, j2, j, :], hp[:, j2, o:o + 512], start=(j == 0), stop=(j == 30))
                    P.I("act", "activation", out=xc[j2][:], in_=pc[j2][:], func=AF.Identity, bias=prm[:, j2, 31:32])
                    P.I("act", "activation", out=sq[j2][:], in_=xc[j2][:], func=AF.Square)
                for j2 in range(2):
                    P.mm(pS1[:], cx.ones_f, xc[j2][:], start=(j2 == 0), stop=(j2 == 1))
                for j2 in range(2):
                    P.mm(pS2[:], cx.ones_f, sq[j2][:], start=(j2 == 0), stop=(j2 == 1))
                P.I("dve", "tensor_scalar", out=mu[:], in0=pS1[:], scalar1=1.0 / 256, scalar2=None, op0=ALU.mult)
                P.I("dve", "tensor_tensor", out=msq[:], in0=mu[:], in1=mu[:], op=ALU.mult)
                P.I("dve", "scalar_tensor_tensor", out=var[:], in0=pS2[:], scalar=1.0 / 256, in1=msq[:],
                    op0=ALU.mult, op1=ALU.subtract)
                P.I("dve", "tensor_scalar", out=var[:], in0=var[:], scalar1=EPS, scalar2=None, op0=ALU.add)
                rsqrt_inplace(P, var[:])
                for j2 in range(2):
                    P.I("dve", "tensor_tensor", out=tmp[j2][:], in0=xc[j2][:], in1=mu[:], op=ALU.subtract)
                    P.I("dve", "tensor_tensor", out=tmp[j2][:], in0=tmp[j2][:], in1=var[:], op=ALU.mult)
                    P.I("act", "activation", out=ycT[:, j2, tg * 512:(tg + 1) * 512], in_=tmp[j2][:], func=AF.Silu,
                        scale=prm[:, j2, 32:33], bias=prm[:, j2, 33:34])
        dbg_dump(P, cx, "ycT", ycT[:], [128, 2, T])
        apply_wout(P, cx, l, ycT, 6, 2, xres, d)


NE = 32


def prep_moe(inp, sh):
    L = L_DEPTH
    sh["fnw"] = np.ascontiguousarray(
        np.asarray(inp["ffn_norm_w"], np.float32).reshape(L, 8, 128).transpose(0, 2, 1))[..., None]
    sh["router_w"] = np.stack([kchunk(np.asarray(inp["router_w"][l], np.float32)) for l in range(L)])
    sh["fnw_row"] = np.ascontiguousarray(np.asarray(inp["ffn_norm_w"], np.float32))
    sh["router_b"] = np.ascontiguousarray(np.asarray(inp["router_b"], np.float32))
    wgu = np.asarray(inp["w_gate_up"], np.float32)
    r = wgu.reshape(L, NE, 8, 128, 2, 8, 128)
    sh["w_gu"] = np.ascontiguousarray(r.transpose(0, 1, 5, 3, 4, 2, 6)).reshape(L, NE, 8, 128, 2048)
    sh["w_dn_moe"] = np.ascontiguousarray(np.asarray(inp["w_down"], np.float32))
    bgu = np.asarray(inp["b_gate_up"], np.float32)
    sh["b_gu"] = np.ascontiguousarray(bgu.reshape(L, NE, 16, 128).transpose(0, 1, 3, 2))
    sh["b_dn"] = np.ascontiguousarray(np.asarray(inp["b_down"], np.float32))


def moe(P, cx, l, xres, d, nexp=NE):
    with P.scope():
        xnT = P.sb([128, 8, T], BF16)
        rw = P.sb([128, NT, NE])
        with P.scope():
            nw = P.sb([128, 8, 1])
            P.ld(nw[:], d["fnw"][l])
            rwt = P.sb([128, 8, NE])
            P.ld(rwt[:], d["router_w"][l])
            rb = P.sb([128, NE])
            P.ld(rb[:], d["router_b"][l:l + 1, :].partition_broadcast(128))
            xn = [P.sb([128, D]) for _ in range(2)]
            ss = [P.sb([128, 1]) for _ in range(2)]
            xT32 = [P.sb([128, 8, 128]) for _ in range(2)]
            ptr = [P.ps([128, 4, 128]) for _ in range(2)]
            pl = P.ps([128, 512])
            lg = P.sb([128, NE]); m8 = P.sb([128, 8]); nmx = P.sb([128, 1]); ex = P.sb([128, NE])
            msk = P.sb([128, NE]); sm = P.sb([128, 1])
            Bd = P.sb([NE, D])
            P.ld(Bd[:], d["b_dn"][l])
            rwT = [P.sb([NE, 128]) for _ in range(2)]
            ptw = P.ps([128, 512])
            pb = [P.ps([128, 512]) for _ in range(2)]
            for i in range(NT):
                x_, s_, xt = xn[i % 2], ss[i % 2], xT32[i % 2]
                P.I("act", "activation", out=x_[:], in_=xres[i][:], func=AF.Square, accum_out=s_[:])
                P.I("dve", "tensor_scalar", out=s_[:], in0=s_[:], scalar1=1.0 / D, scalar2=EPS, op0=ALU.mult, op1=ALU.add)
                rsqrt_inplace(P, s_[:])
                P.I("dve", "tensor_scalar", out=x_[:], in0=xres[i][:], scalar1=s_[:, 0:1], scalar2=None, op0=ALU.mult)
                for hf in range(2):
                    for c4 in range(4):
                        c = hf * 4 + c4
                        P.tr(ptr[hf][:, c4, :], x_[:, c * 128:(c + 1) * 128], cx.ident_f)
                    P.I("dve", "tensor_tensor", out=xt[:, hf * 4:hf * 4 + 4, :], in0=ptr[hf][:],
                        in1=nw[:, hf * 4:hf * 4 + 4, :].bc([128, 4, 128]), op=ALU.mult)
                P.I("act", "copy", out=xnT[:, :, i * 128:(i + 1) * 128], in_=xt[:])
                for kc in range(8):
                    P.mm(pl[:, 0:NE], xt[:, kc, :], rwt[:, kc, :], start=(kc == 0), stop=(kc == 7))
                P.I("dve", "tensor_tensor", out=lg[:], in0=pl[:, 0:NE], in1=rb[:], op=ALU.add)
                P.I("dve", "max", out=m8[:], in_=lg[:])
                P.I("dve", "tensor_scalar", out=nmx[:], in0=m8[:, 0:1], scalar1=-1.0, scalar2=None, op0=ALU.mult)
                P.I("act", "activation", out=ex[:], in_=lg[:], func=AF.Exp, bias=nmx[:, 0:1])
                P.I("dve", "tensor_scalar", out=msk[:], in0=lg[:], scalar1=m8[:, 3:4], scalar2=None, op0=ALU.is_ge)
                P.I("dve", "tensor_tensor", out=ex[:], in0=ex[:], in1=msk[:], op=ALU.mult)
                P.I("dve", "tensor_reduce", out=sm[:], in_=ex[:], axis=AX.X, op=ALU.add)
                P.I("dve", "reciprocal", out=sm[:], in_=sm[:])
                P.I("dve", "tensor_scalar", out=rw[:, i, :], in0=ex[:], scalar1=sm[:, 0:1], scalar2=None, op0=ALU.mult)
                P.tr(ptw[0:NE, 0:128], rw[:, i, :], cx.ident_f)
                P.I("act", "copy", out=rwT[i % 2][:], in_=ptw[0:NE, 0:128])
                for half in range(2):
                    hs_ = slice(half * 512, (half + 1) * 512)
                    P.mm(pb[half][:], rwT[i % 2][:], Bd[:, hs_])
                    P.I("dve", "tensor_tensor", out=xres[i][:, hs_], in0=pb[half][:], in1=xres[i][:, hs_], op=ALU.add)
        dbg_dump(P, cx, "rw", rw[:], [128, NT, NE])
        chk("moe_router")
        with P.scope():
            actT = P.sbl(8, [128, T], BF16, name="actT%d" % l)
            sgu = [P.sb([128, 2048]) for _ in range(2)]
            wgub = [P.sb([128, 2, 8, 128], BF16) for _ in range(2)]
            sdn = [P.sb([128, D])]
            wdb = P.sbl(8, [128, D], BF16, name="wdb%d" % l)
            bgu = [P.sb([128, 16]) for _ in range(2)]
            bg2 = [P.sb([128, 16]) for _ in range(2)]
            Sb = [P.sb([128, 512]) for _ in range(2)]
            ub = [P.sb([128, 512]) for _ in range(2)]
            pg = [P.ps([128, 512]) for _ in range(2)]
            pu = [P.ps([128, 512]) for _ in range(2)]
            po = [P.ps([128, 512]) for _ in range(2)]
            units = [(e, j) for e in range(nexp) for j in range(8)]
            CS = 1.702 * 7.0 / (1.0 + math.exp(-1.702 * 7.0))
            rwk = P.sb([128, NT, NE])
            P.I("dve", "tensor_scalar", out=rwk[:], in0=rw[:], scalar1=1.0 / 1.702, scalar2=None, op0=ALU.mult)

            def dma_gu(u):
                e, j = units[u]
                P.ld(sgu[u % 2][:], d["w_gu"][l, e, j])

            def cast_gu(u):
                P.I("act", "copy", out=wgub[u % 2][:].re("p a c f -> p (a c f)"), in_=sgu[u % 2][:])

            dma_gu(0)
            cast_gu(0)
            n = 0
            no = 0
            for u, (e, j) in enumerate(units):
                if j == 0:
                    P.ld(bgu[e % 2][:], d["b_gu"][l, e])
                    P.I("dve", "tensor_scalar", out=bg2[e % 2][:, 0:8], in0=bgu[e % 2][:, 0:8], scalar1=1.702,
                        scalar2=None, op0=ALU.mult)
                    P.I("dve", "tensor_scalar", out=bg2[e % 2][:, 8:16], in0=bgu[e % 2][:, 8:16], scalar1=1.0,
                        scalar2=None, op0=ALU.add)
                if u + 1 < len(units):
                    dma_gu(u + 1)
                P.ld(sdn[0][:], d["w_dn_moe"][l, e, j * 128:(j + 1) * 128, :])
                wb = wgub[u % 2]
                for tg in range(4):
                    sl = slice(tg * 512, (tg + 1) * 512)
                    g_, u_ = pg[n % 2], pu[n % 2]
                    S_, ub_ = Sb[n % 2], ub[n % 2]
                    n += 1
                    for kc in range(8):
                        P.mm(g_[:], wb[:, 0, kc, :], xnT[:, kc, sl], start=(kc == 0), stop=(kc == 7))
                    for kc in range(8):
                        P.mm(u_[:], wb[:, 1, kc, :], xnT[:, kc, sl], start=(kc == 0), stop=(kc == 7))
                    P.I("act", "activation", out=S_[:], in_=g_[:], func=AF.Silu, scale=1.702, bias=bg2[e % 2][:, j:j + 1])
                    P.I("act", "activation", out=ub_[:], in_=u_[:], func=AF.Identity, bias=bg2[e % 2][:, 8 + j:9 + j])
                    P.I("dve", "tensor_scalar", out=ub_[:], in0=ub_[:], scalar1=-6.0, scalar2=8.0, op0=ALU.max, op1=ALU.min)
                    P.I("dve", "scalar_tensor_tensor", out=actT[j][:, sl], in0=S_[:], scalar=CS, in1=ub_[:],
                        op0=ALU.min, op1=ALU.mult)
                    if tg == 1 and u + 1 < len(units):
                        cast_gu(u + 1)
                    if tg == 3:
                        P.I("act", "copy", out=wdb[j][:], in_=sdn[0][:])
                if j == 7:
                    for i in range(NT):
                        for half in range(2):
                            o_ = po[no % 2]; no += 1
                            hs_ = slice(half * 512, (half + 1) * 512)
                            for jj in range(8):
                                P.mm(o_[:], actT[jj][:, i * 128:(i + 1) * 128], wdb[jj][:, hs_], start=(jj == 0), stop=(jj == 7))
                            P.I("dve", "scalar_tensor_tensor", out=xres[i][:, hs_], in0=o_[:], scalar=rwk[:, i, e:e + 1],
                                in1=xres[i][:, hs_], op0=ALU.mult, op1=ALU.add)


MOE_SPARSE = [True]
CAP = 48
SG = 2 * CAP
NSLOT = 8 * SG


def moe_sparse(P, cx, l, xres, d, nexp=NE):
    with P.scope():
        xtok = P.sbl(NT, [128, D], BF16, name="xtok%d" % l)
        rw = P.sb([128, NT, NE])
        posm = P.sb([128, NT, NE])
        with P.scope():
            nw = P.sb([128, 8, 1])
            P.ld(nw[:], d["fnw"][l])
            nwb = P.sb([128, D])
            P.ld(nwb[:], d["fnw_row"][l:l + 1, :].partition_broadcast(128))
            rwt = P.sb([128, 8, NE])
            P.ld(rwt[:], d["router_w"][l])
            rb = P.sb([128, NE])
            P.ld(rb[:], d["router_b"][l:l + 1, :].partition_broadcast(128))
            xn = [P.sb([128, D]) for _ in range(2)]
            ssa = P.sb([128, NT])
            xT32 = [P.sb([128, 8, 128]) for _ in range(2)]
            ptr = [P.ps([128, 4, 128]) for _ in range(2)]
            pl = P.ps([128, 512])
            ppos = P.ps([128, 512])
            lga = P.sb([128, NT, NE]); m8a = P.sb([128, NT, 8]); exa = P.sb([128, NT, NE])
            mska = P.sb([128, NT, NE]); sma = P.sb([128, NT]); okm = P.sb([128, NT, NE])
            Bd = P.sb([NE, D], BF16)
            P.ld(Bd[:], d["b_dn"][l], q="pool")
            rwT = [P.sb([NE, 128], BF16) for _ in range(2)]
            rwb = P.sb([128, NT, NE], BF16)
            ptw = P.ps([128, 512], BF16)
            pb = [P.ps([128, 512]) for _ in range(2)]
            for i in range(NT):
                P.I("act", "activation", out=xn[i % 2][:], in_=xres[i][:], func=AF.Square, accum_out=ssa[:, i:i + 1])
            P.I("dve", "tensor_scalar", out=ssa[:], in0=ssa[:], scalar1=1.0 / D, scalar2=EPS, op0=ALU.mult, op1=ALU.add)
            rsqrt_inplace(P, ssa[:])
            for i in range(NT):
                x_, xt = xn[i % 2], xT32[i % 2]
                P.I("dve", "tensor_scalar", out=x_[:], in0=xres[i][:], scalar1=ssa[:, i:i + 1], scalar2=None, op0=ALU.mult)
                P.I("dve", "tensor_tensor", out=xtok[i][:], in0=x_[:], in1=nwb[:], op=ALU.mult)
                for hf in range(2):
                    for c4 in range(4):
                        c = hf * 4 + c4
                        P.tr(ptr[hf][:, c4, :], x_[:, c * 128:(c + 1) * 128], cx.ident_f)
                    P.I("act" if hf else "dve", "tensor_tensor" if not hf else "activation",
                        **(dict(out=xt[:, 0:4, :], in0=ptr[0][:], in1=nw[:, 0:4, :].bc([128, 4, 128]), op=ALU.mult)
                           if not hf else dict(out=xt[:, 4:8, :], in_=ptr[1][:], func=AF.Copy)))
                if True:
                    P.I("dve", "tensor_tensor", out=xt[:, 4:8, :], in0=xt[:, 4:8, :],
                        in1=nw[:, 4:8, :].bc([128, 4, 128]), op=ALU.mult)
                for kc in range(8):
                    P.mm(pl[:, 0:NE], xt[:, kc, :], rwt[:, kc, :], start=(kc == 0), stop=(kc == 7))
                P.I("dve", "tensor_tensor", out=lga[:, i, :], in0=pl[:, 0:NE], in1=rb[:], op=ALU.add)
                P.I("dve", "max", out=m8a[:, i, :], in_=lga[:, i, :])
            P.I("dve", "tensor_tensor", out=exa[:], in0=lga[:], in1=m8a[:, :, 0:1].bc([128, NT, NE]), op=ALU.subtract)
            P.I("act", "activation", out=exa[:], in_=exa[:], func=AF.Exp)
            P.I("dve", "tensor_tensor", out=mska[:], in0=lga[:], in1=m8a[:, :, 3:4].bc([128, NT, NE]), op=ALU.is_ge)
            P.I("dve", "tensor_tensor", out=exa[:], in0=exa[:], in1=mska[:], op=ALU.mult)
            P.I("dve", "tensor_reduce", out=sma[:], in_=exa[:], axis=AX.X, op=ALU.add)
            P.I("dve", "reciprocal", out=sma[:], in_=sma[:])
            P.I("dve", "tensor_tensor", out=rw[:], in0=exa[:], in1=sma[:].re("p (a o) -> p a o", o=1).bc([128, NT, NE]),
                op=ALU.mult)
            P.I("dve", "tensor_copy", out=rwb[:], in_=rw[:])
            for i in range(NT):
                P.mm(ppos[:, i * NE:(i + 1) * NE], cx.sut, mska[:, i, :])
            pv = ppos[:].re("p (a b) -> p a b", b=NE)
            P.I("dve", "tensor_scalar", out=okm[:], in0=pv, scalar1=CAP - 0.5, scalar2=None, op0=ALU.is_lt)
            P.I("dve", "tensor_tensor", out=okm[:], in0=okm[:], in1=mska[:], op=ALU.mult)
            for par in range(2):
                P.I("dve", "scalar_tensor_tensor", out=posm[:, par::2, :], in0=pv[:, par::2, :], scalar=1.0 + par * CAP,
                    in1=okm[:, par::2, :], op0=ALU.add, op1=ALU.mult)
            P.I("dve", "tensor_scalar", out=posm[:], in0=posm[:], scalar1=-1.0, scalar2=None, op0=ALU.add)
            for i in range(NT):
                P.tr(ptw[0:NE, 0:128], rwb[:, i, :], cx.ident_bf[:])
                P.I("act", "copy", out=rwT[i % 2][:], in_=ptw[0:NE, 0:128])
                for half in range(2):
                    hs_ = slice(half * 512, (half + 1) * 512)
                    P.mm(pb[half][:], rwT[i % 2][:], Bd[:, hs_])
                    P.I("dve", "tensor_tensor", out=xres[i][:, hs_], in0=pb[half][:], in1=xres[i][:, hs_], op=ALU.add)
        dbg_dump(P, cx, "rw", rw[:], [128, NT, NE])
        dbg_dump(P, cx, "posm", posm[:], [128, NT, NE])
        chk("moe_router")
        with P.scope():
            H = NSLOT // 2
            actT = P.sbl(8, [128, NSLOT], BF16, name="actT%d" % l)
            xgT = P.sbl(8, [128, NSLOT], BF16, name="xgT%d" % l)
            ysb = P.sbl(2, [128, D], BF16, name="ysb%d" % l)
            psel = P.sb([128, NT, SG], BF16)
            pselT = P.sb([128, NT, 128], BF16)
            NWB = 3
            wgub = [P.sb([128, 2, 8, 128], BF16) for _ in range(NWB)]
            wdb = P.sbl(8, [128, D], BF16, name="wdb%d" % l)
            bgu = [P.sb([128, 16]) for _ in range(2)]
            bg2 = [P.sb([128, 16]) for _ in range(2)]
            Sb = [P.sb([128, H]) for _ in range(2)]
            ub = [P.sb([128, H]) for _ in range(2)]
            pg = [P.ps([128, 512]) for _ in range(2)]
            pu = [P.ps([128, 512]) for _ in range(2)]
            po = [P.ps([128, 512]) for _ in range(3)]
            ptp = P.ps([128, 512], BF16)
            CS = 1.702 * 7.0 / (1.0 + math.exp(-1.702 * 7.0))
            rwk = P.sb([128, NT, NE])
            P.I("dve", "tensor_scalar", out=rwk[:], in0=rw[:], scalar1=1.0 / 1.702, scalar2=None, op0=ALU.mult)
            units = [(e, j) for e in range(nexp) for j in range(8)]

            def dma_gu(u):
                e, j = units[u]
                P.ld(wgub[u % NWB][:].re("p a c f -> p (a c f)"), d["w_gu"][l, e, j], q="pool")

            dma_gu(0)
            dma_gu(1)
            n = 0
            no = 0
            for u, (e, j) in enumerate(units):
                if j == 0:
                    P.ld(bgu[e % 2][:], d["b_gu"][l, e])
                    P.I("dve", "tensor_scalar", out=bg2[e % 2][:, 0:8], in0=bgu[e % 2][:, 0:8], scalar1=1.702,
                        scalar2=None, op0=ALU.mult)
                    P.I("dve", "tensor_scalar", out=bg2[e % 2][:, 8:16], in0=bgu[e % 2][:, 8:16], scalar1=1.0,
                        scalar2=None, op0=ALU.add)
                    P.I("dve", "tensor_tensor", out=psel[:], in0=cx.iota[:, 0:SG].re("p (o c) -> p o c", o=1).bc([128, NT, SG]),
                        in1=posm[:, :, e:e + 1].bc([128, NT, SG]), op=ALU.is_equal)
                    for kc in range(8):
                        for hf in range(2):
                            ps = (pg if hf == 0 else pu)[n % 2]
                            for s4 in range(4):
                                st = hf * 4 + s4
                                for r in range(2):
                                    i = 2 * st + r
                                    P.mm(ps[:, s4 * SG:(s4 + 1) * SG], xtok[i][:, kc * 128:(kc + 1) * 128], psel[:, i, :],
                                         start=(r == 0), stop=(r == 1))
                            P.I("act", "copy", out=xgT[kc][:, hf * H:(hf + 1) * H], in_=ps[:, 0:H])
                        n += 1
                    for i4 in range(NT // 4):
                        for ii in range(4):
                            P.tr(ptp[0:SG, ii * 128:(ii + 1) * 128], psel[:, i4 * 4 + ii, :], cx.ident_bf[:])
                        P.I("dve", "tensor_copy", out=pselT[0:SG, i4 * 4:i4 * 4 + 4, :].re("p a b -> p (a b)"), in_=ptp[0:SG, :])
                if u + 2 < len(units):
                    dma_gu(u + 2)
                P.ld(wdb[j][:], d["w_dn_moe"][l, e, j * 128:(j + 1) * 128, :], q="pool")
                wb = wgub[u % NWB]
                for hf in range(2):
                    sl = slice(hf * H, (hf + 1) * H)
                    g_, u_ = pg[n % 2], pu[n % 2]
                    S_, ub_ = Sb[n % 2], ub[n % 2]
                    n += 1
                    for kc in range(8):
                        P.mm(g_[:, 0:H], wb[:, 0, kc, :], xgT[kc][:, sl], start=(kc == 0), stop=(kc == 7))
                    for kc in range(8):
                        P.mm(u_[:, 0:H], wb[:, 1, kc, :], xgT[kc][:, sl], start=(kc == 0), stop=(kc == 7))
                    P.I("act", "activation", out=S_[:], in_=g_[:, 0:H], func=AF.Silu, scale=1.702, bias=bg2[e % 2][:, j:j + 1])
                    P.I("dve", "tensor_scalar", out=ub_[:], in0=u_[:, 0:H], scalar1=bg2[e % 2][:, 8 + j:9 + j], scalar2=-6.0,
                        op0=ALU.add, op1=ALU.max)
                    P.I("dve", "tensor_scalar", out=S_[:], in0=S_[:], scalar1=CS, scalar2=None, op0=ALU.min)
                    P.I("dve", "scalar_tensor_tensor", out=actT[j][:, sl], in0=ub_[:], scalar=8.0, in1=S_[:],
                        op0=ALU.min, op1=ALU.mult)
                if j == 7:
                    def down_st(st):
                        nonlocal no
                        for half in range(2):
                            o_ = po[no % 3]; no += 1
                            hs_ = slice(half * 512, (half + 1) * 512)
                            for jj in range(8):
                                P.mm(o_[0:SG, :], actT[jj][:, st * SG:(st + 1) * SG], wdb[jj][:, hs_], start=(jj == 0), stop=(jj == 7))
                            P.I("act", "copy", out=ysb[st % 2][0:SG, hs_], in_=o_[0:SG, :])

                    def scatter_st(st):
                        nonlocal no
                        for i in (2 * st, 2 * st + 1):
                            for half in range(2):
                                o_ = po[no % 3]; no += 1
                                hs_ = slice(half * 512, (half + 1) * 512)
                                P.mm(o_[:], pselT[0:SG, i, :], ysb[st % 2][0:SG, hs_])
                                P.I("dve", "scalar_tensor_tensor", out=xres[i][:, hs_], in0=o_[:], scalar=rwk[:, i, e:e + 1],
                                    in1=xres[i][:, hs_], op0=ALU.mult, op1=ALU.add)

                    down_st(0)
                    for st in range(8):
                        if st + 1 < 8:
                            down_st(st + 1)
                        scatter_st(st)


_NC_CACHE = {}


def kernel(**inputs):
    x = np.asarray(inputs["x"], np.float32)
    sh = prep_shared(inputs)
    if "nc" not in _NC_CACHE:
        _NC_CACHE["nc"] = build_nc()
    nc = _NC_CACHE["nc"]
    in_maps = []
    for c in range(8):
        m = dict(sh)
        m["x"] = np.ascontiguousarray(x[c])
        in_maps.append(m)
    res = run_bass_kernel_spmd(nc, in_maps, core_ids=list(range(8)))
    return np.stack([np.asarray(r["out"], np.float32) for r in res.results], axis=0)
```
